# Optimizing a Trainium2 kernel written in Bass

```python
import math
import jax, jax.numpy as jnp
from jax import lax
import numpy as np

D_MODEL = 1024
BATCH = 8
SEQ = 4096
DEPTH = 2

CHUNK = 64
N_A_LAYERS = DEPTH // 2
N_B_LAYERS = DEPTH - N_A_LAYERS
N_DENSE = (DEPTH + 1) // 2
N_MOE = DEPTH // 2
SSM_GROUP = 16
SSM_GROUPS = D_MODEL // SSM_GROUP
SSM_STATE = 64
DT_MIN = 1e-3
DT_MAX = 1e-1
N_HEADS = 16
QK_NOPE = 64
QK_ROPE = 32
V_HEAD = 64
Q_LORA = 512
KV_LORA = 256
ROPE_BASE = 10000.0
Q_BLOCK = 128
D_FF = 2688
N_EXPERTS = 8
TOP_K = 2
MOE_FF = 3584
EPS = 1e-6

kernel_name = "yoco_s5_mla_moe_hybrid"

F32 = jnp.float32


def rmsnorm(x, g):
    xf = x.astype(F32)
    y = xf * lax.rsqrt(jnp.mean(xf * xf, axis=-1, keepdims=True) + EPS)
    return (y * g.astype(F32)).astype(x.dtype)


def apply_rope(x, cos, sin):
    x1, x2 = jnp.split(x.astype(F32), 2, axis=-1)
    return jnp.concatenate([x1 * cos - x2 * sin, x2 * cos + x1 * sin], axis=-1).astype(x.dtype)


def swiglu(h, w_gate, w_up, w_down):
    return (jax.nn.silu(h @ w_gate) * (h @ w_up)) @ w_down


def s5_mixer(h, w_in, lam_re, lam_im, log_dt, b_re, b_im, c_re, c_im, d_skip, w_glu, w_out):
    bsz, seq, _ = h.shape
    u = (h @ w_in).astype(F32).reshape(bsz, seq, SSM_GROUPS, SSM_GROUP)
    dt = jnp.exp(log_dt.astype(F32))[:, None]
    lr, li = lam_re.astype(F32), lam_im.astype(F32)
    mag = jnp.exp(lr * dt)
    ab_re, ab_im = mag * jnp.cos(li * dt), mag * jnp.sin(li * dt)
    den = lr * lr + li * li
    nr, ni = ab_re - 1.0, ab_im
    coef_re = (nr * lr + ni * li) / den
    coef_im = (ni * lr - nr * li) / den
    br, bi = b_re.astype(F32), b_im.astype(F32)
    bb_re = coef_re[..., None] * br - coef_im[..., None] * bi
    bb_im = coef_re[..., None] * bi + coef_im[..., None] * br
    bu_re = jnp.einsum('blgn,gpn->blgp', u, bb_re)
    bu_im = jnp.einsum('blgn,gpn->blgp', u, bb_im)
    a_re = jnp.broadcast_to(ab_re[None, None], (1, seq, SSM_GROUPS, SSM_STATE))
    a_im = jnp.broadcast_to(ab_im[None, None], (1, seq, SSM_GROUPS, SSM_STATE))

    def combine(e1, e2):
        a1r, a1i, b1r, b1i = e1
        a2r, a2i, b2r, b2i = e2
        return (a2r * a1r - a2i * a1i,
                a2r * a1i + a2i * a1r,
                a2r * b1r - a2i * b1i + b2r,
                a2r * b1i + a2i * b1r + b2i)

    _, _, s_re, s_im = lax.associative_scan(combine, (a_re, a_im, bu_re, bu_im), axis=1)
    y = (jnp.einsum('blgp,gnp->blgn', s_re, c_re.astype(F32))
         - jnp.einsum('blgp,gnp->blgn', s_im, c_im.astype(F32)))
    y = y.reshape(bsz, seq, D_MODEL) + d_skip.astype(F32) * u.reshape(bsz, seq, D_MODEL)
    y = jax.nn.gelu(y).astype(h.dtype)
    z = y * jax.nn.sigmoid(y @ w_glu)
    return z @ w_out


def shared_kv(h, w_dkv, kv_latent_norm, w_ukv, cos, sin):
    bsz, seq, _ = h.shape
    ckr = h @ w_dkv
    c_kv = rmsnorm(ckr[..., :KV_LORA], kv_latent_norm)
    k_rope = apply_rope(ckr[..., KV_LORA:], cos, sin)
    kv = (c_kv @ w_ukv).reshape(bsz, seq, N_HEADS, QK_NOPE + V_HEAD)
    return kv[..., :QK_NOPE], k_rope, kv[..., QK_NOPE:]


def mla_mixer(h, k_nope, k_rope, v, w_dq, q_latent_norm, w_uq, w_o, cos, sin):
    bsz, seq, _ = h.shape
    cq = rmsnorm(h @ w_dq, q_latent_norm)
    q = (cq @ w_uq).reshape(bsz, seq, N_HEADS, QK_NOPE + QK_ROPE)
    scale = (QK_NOPE + QK_ROPE) ** -0.5
    q_nope = q[..., :QK_NOPE] * scale
    q_rope = apply_rope(q[..., QK_NOPE:], cos[:, :, None], sin[:, :, None]) * scale
    n_blk = seq // Q_BLOCK
    qn_b = q_nope.reshape(bsz, n_blk, Q_BLOCK, N_HEADS, QK_NOPE).transpose(1, 0, 2, 3, 4)
    qr_b = q_rope.reshape(bsz, n_blk, Q_BLOCK, N_HEADS, QK_ROPE).transpose(1, 0, 2, 3, 4)
    k_chunk = jnp.arange(seq) // CHUNK

    def block(args):
        i, qn, qr = args
        s = (jnp.einsum('bqhd,bkhd->bhqk', qn, k_nope, preferred_element_type=F32)
             + jnp.einsum('bqhr,bkr->bhqk', qr, k_rope, preferred_element_type=F32))
        q_chunk = (i * Q_BLOCK + jnp.arange(Q_BLOCK)) // CHUNK
        mask = k_chunk[None, :] <= q_chunk[:, None]
        p = jax.nn.softmax(jnp.where(mask, s, -jnp.inf), axis=-1)
        return jnp.einsum('bhqk,bkhd->bqhd', p.astype(v.dtype), v)

    o = lax.map(block, (jnp.arange(n_blk), qn_b, qr_b))
    o = o.transpose(1, 0, 2, 3, 4).reshape(bsz, seq, N_HEADS * V_HEAD)
    return o @ w_o


def moe_swiglu(h, router_w, w_gate, w_up, w_down):
    bsz, seq, d = h.shape
    t = h.reshape(-1, d)
    logits = (t @ router_w).astype(F32)
    top_logit, top_idx = lax.top_k(logits, TOP_K)
    top_w = jax.nn.softmax(top_logit, axis=-1)
    gates = jnp.einsum('nk,nke->ne', top_w, jax.nn.one_hot(top_idx, N_EXPERTS, dtype=F32))
    out = jnp.zeros_like(t)
    for e in range(N_EXPERTS):
        out = out + gates[:, e:e + 1].astype(t.dtype) * swiglu(t, w_gate[e], w_up[e], w_down[e])
    return out.reshape(bsz, seq, d)


def setup_inputs(seed: int = 0) -> dict:
    key = jax.random.key(seed)
    ks = iter(jax.random.split(key, 40))

    def nrm(shape, fan_in):
        return jax.random.normal(next(ks), shape, F32) * (fan_in ** -0.5)

    def gain(shape):
        return 1.0 + 0.02 * jax.random.normal(next(ks), shape, F32)

    x = jax.random.normal(next(ks), (BATCH, SEQ, D_MODEL), F32)
    offsets = jax.random.randint(next(ks), (BATCH, 1), 0, 1024, dtype=jnp.int32)
    positions = offsets + jnp.arange(SEQ, dtype=jnp.int32)[None, :]
    n_idx = jnp.arange(SSM_STATE, dtype=F32)
    gp = (N_A_LAYERS, SSM_GROUPS, SSM_STATE)
    s5_lambda_re = -0.5 + 0.01 * jax.random.normal(next(ks), gp, F32)
    s5_lambda_im = math.pi * n_idx + 0.01 * jax.random.normal(next(ks), gp, F32)
    s5_log_dt = jax.random.uniform(next(ks), (N_A_LAYERS, SSM_GROUPS), F32,
                                   math.log(DT_MIN), math.log(DT_MAX))
    bshape = (N_A_LAYERS, SSM_GROUPS, SSM_STATE, SSM_GROUP)
    cshape = (N_A_LAYERS, SSM_GROUPS, SSM_GROUP, SSM_STATE)
    return {
        "x": x,
        "positions": positions,
        "norm_mix": gain((DEPTH, D_MODEL)),
        "norm_ffn": gain((DEPTH, D_MODEL)),
        "final_norm": gain((D_MODEL,)),
        "s5_w_in": nrm((N_A_LAYERS, D_MODEL, D_MODEL), D_MODEL),
        "s5_lambda_re": s5_lambda_re,
        "s5_lambda_im": s5_lambda_im,
        "s5_log_dt": s5_log_dt,
        "s5_b_re": nrm(bshape, 2 * SSM_GROUP),
        "s5_b_im": nrm(bshape, 2 * SSM_GROUP),
        "s5_c_re": nrm(cshape, SSM_STATE),
        "s5_c_im": nrm(cshape, SSM_STATE),
        "s5_d": gain((N_A_LAYERS, D_MODEL)),
        "s5_w_glu": nrm((N_A_LAYERS, D_MODEL, D_MODEL), D_MODEL),
        "s5_w_out": nrm((N_A_LAYERS, D_MODEL, D_MODEL), D_MODEL),
        "kv_norm": gain((D_MODEL,)),
        "w_dkv": nrm((D_MODEL, KV_LORA + QK_ROPE), D_MODEL),
        "kv_latent_norm": gain((KV_LORA,)),
        "w_ukv": nrm((KV_LORA, N_HEADS * (QK_NOPE + V_HEAD)), KV_LORA),
        "w_dq": nrm((N_B_LAYERS, D_MODEL, Q_LORA), D_MODEL),
        "q_latent_norm": gain((N_B_LAYERS, Q_LORA)),
        "w_uq": nrm((N_B_LAYERS, Q_LORA, N_HEADS * (QK_NOPE + QK_ROPE)), Q_LORA),
        "w_o": nrm((N_B_LAYERS, N_HEADS * V_HEAD, D_MODEL), N_HEADS * V_HEAD),
        "ffn_w_gate": nrm((N_DENSE, D_MODEL, D_FF), D_MODEL),
        "ffn_w_up": nrm((N_DENSE, D_MODEL, D_FF), D_MODEL),
        "ffn_w_down": nrm((N_DENSE, D_FF, D_MODEL), D_FF),
        "router_w": nrm((N_MOE, D_MODEL, N_EXPERTS), D_MODEL),
        "moe_w_gate": nrm((N_MOE, N_EXPERTS, D_MODEL, MOE_FF), D_MODEL),
        "moe_w_up": nrm((N_MOE, N_EXPERTS, D_MODEL, MOE_FF), D_MODEL),
        "moe_w_down": nrm((N_MOE, N_EXPERTS, MOE_FF, D_MODEL), MOE_FF),
    }


def reference(x, positions, norm_mix, norm_ffn, final_norm, s5_w_in, s5_lambda_re, s5_lambda_im,
              s5_log_dt, s5_b_re, s5_b_im, s5_c_re, s5_c_im, s5_d, s5_w_glu, s5_w_out,
              kv_norm, w_dkv, kv_latent_norm, w_ukv, w_dq, q_latent_norm, w_uq, w_o,
              ffn_w_gate, ffn_w_up, ffn_w_down, router_w, moe_w_gate, moe_w_up, moe_w_down):
    half = QK_ROPE // 2
    inv_freq = ROPE_BASE ** (-jnp.arange(half, dtype=F32) * (2.0 / QK_ROPE))
    ang = positions.astype(F32)[..., None] * inv_freq
    cos, sin = jnp.cos(ang), jnp.sin(ang)
    kv = None
    for i in range(DEPTH):
        if i < N_A_LAYERS:
            a = i
            h = rmsnorm(x, norm_mix[i])
            x = x + s5_mixer(h, s5_w_in[a], s5_lambda_re[a], s5_lambda_im[a], s5_log_dt[a],
                             s5_b_re[a], s5_b_im[a], s5_c_re[a], s5_c_im[a], s5_d[a],
                             s5_w_glu[a], s5_w_out[a])
        else:
            if kv is None:
                kv = shared_kv(rmsnorm(x, kv_norm), w_dkv, kv_latent_norm, w_ukv, cos, sin)
            b = i - N_A_LAYERS
            k_nope, k_rope, v = kv
            h = rmsnorm(x, norm_mix[i])
            x = x + mla_mixer(h, k_nope, k_rope, v, w_dq[b], q_latent_norm[b], w_uq[b],
                              w_o[b], cos, sin)
        h = rmsnorm(x, norm_ffn[i])
        if i % 2 == 0:
            j = i // 2
            x = x + swiglu(h, ffn_w_gate[j], ffn_w_up[j], ffn_w_down[j])
        else:
            j = i // 2
            x = x + moe_swiglu(h, router_w[j], moe_w_gate[j], moe_w_up[j], moe_w_down[j])
    return rmsnorm(x, final_norm)
```

```python
import math
import numpy as np
import ml_dtypes
import concourse.bass as bass
import concourse.mybir as mybir
from concourse.bass_utils import run_bass_kernel_spmd

F32 = mybir.dt.float32
BF16 = mybir.dt.bfloat16
I32 = mybir.dt.int32
AF = mybir.ActivationFunctionType
ALU = mybir.AluOpType
AX = mybir.AxisListType

L = 4096
D = 1024
NB = L // 128
G = 64
GS = 16
PS = 64
TCH = 8
D_FF = 2688
NE = 8
MOE_FF = 3584
NH = 16
QK_NOPE = 64
QK_ROPE = 32
V_HEAD = 64
Q_LORA = 512
KV_LORA = 256
EPS = 1e-6
DT_MIN = 1e-3
DT_MAX = 1e-1
SLAB = 1024
NSLAB = (2 * L) // SLAB + NE
NGRP = MOE_FF // 512


class Buf:
    __slots__ = ("name", "w", "r")

    def __init__(self, name):
        self.name = name
        self.w = {}
        self.r = {}


class KB:
    def __init__(self, nc):
        self.nc = nc
        self.eng = {"pe": nc.tensor, "act": nc.scalar, "dve": nc.vector,
                    "pool": nc.gpsimd, "sp": nc.sync}
        self.sem = {k: nc.alloc_semaphore(name="sem_" + k) for k in self.eng}
        self.cnt = {k: 0 for k in self.eng}
        self.known = {k: {} for k in self.eng}
        self.lanes = {}
        self.bufs = []
        self.n_ins = 0

    def _lane(self, lane):
        if lane not in self.lanes:
            pool = self.__dict__.setdefault("_lane_pool", [])
            if pool:
                self.lanes[lane] = pool.pop()
            else:
                self._nl = getattr(self, "_nl", 0) + 1
                self.lanes[lane] = [self.nc.alloc_semaphore(name=f"ln{self._nl}"), 0]

    def buf(self, name):
        b = Buf(name)
        self.bufs.append(b)
        return b

    def bufs_n(self, name, n):
        return [self.buf(f"{name}{i}") for i in range(n)]

    def _semof(self, key):
        if key[0] == "e":
            return self.sem[key[1]]
        return self.lanes[key[1]][0]

    def _need(self, reads, writes):
        need = {}
        for b in reads:
            for k, v in b.w.items():
                if need.get(k, 0) < v:
                    need[k] = v
        for b in writes:
            for k, v in b.w.items():
                if need.get(k, 0) < v:
                    need[k] = v
            for k, v in b.r.items():
                if need.get(k, 0) < v:
                    need[k] = v
        return need

    def _wait(self, e, need):
        kn = self.known[e]
        for k, v in need.items():
            if e == "pe" and k == ("e", "pe"):
                continue
            if kn.get(k, 0) >= v:
                continue
            self.eng[e].wait_ge(self._semof(k), v)
            kn[k] = v

    def op(self, e, fn, reads=(), writes=()):
        self._wait(e, self._need(reads, writes))
        ins = fn(self.eng[e])
        self.cnt[e] += 1
        c = self.cnt[e]
        ins.then_inc(self.sem[e], 1)
        key = ("e", e)
        for b in reads:
            b.r[key] = c
        for b in writes:
            b.w = {key: c}
            b.r = {}
        self.n_ins += 1
        return ins

    def dma(self, q, out, in_, reads=(), writes=(), lane=None, disjoint=False, **kw):
        if lane is None:
            lane = writes[0].name
        self._lane(lane)
        need = self._need(reads, writes)
        if disjoint:
            need.pop(("l", lane), None)
        self._wait(q, need)
        ins = self.eng[q].dma_start(out=out, in_=in_, **kw)
        ln = self.lanes[lane]
        ln[1] += 16
        ins.then_inc(ln[0], 16)
        key = ("l", lane)
        for b in reads:
            b.r[key] = ln[1]
        for b in writes:
            neww = {k: v for k, v in b.w.items() if k[0] == "l" and k != key}
            neww[key] = ln[1]
            b.w = neww
            b.r = {}
        self.n_ins += 1
        return ins

    def idma(self, out, in_, out_idx=None, in_idx=None, bound=None, reads=(), writes=(), lane=None, disjoint=False):
        if lane is None:
            lane = writes[0].name
        self._lane(lane)
        need = self._need(reads, writes)
        if disjoint:
            need.pop(("l", lane), None)
        self._wait("pool", need)
        oo = bass.IndirectOffsetOnAxis(ap=out_idx, axis=0) if out_idx is not None else None
        io = bass.IndirectOffsetOnAxis(ap=in_idx, axis=0) if in_idx is not None else None
        ins = self.nc.gpsimd.indirect_dma_start(out=out, out_offset=oo, in_=in_, in_offset=io)
        ln = self.lanes[lane]
        ln[1] += 16
        ins.then_inc(ln[0], 16)
        key = ("l", lane)
        for b in reads:
            b.r[key] = ln[1]
        for b in writes:
            neww = {k: v for k, v in b.w.items() if k[0] == "l" and k != key}
            neww[key] = ln[1]
            b.w = neww
            b.r = {}
        self.n_ins += 1
        return ins

    def finish(self, bufs):
        need = {}
        for b in bufs:
            for k, v in b.w.items():
                need[k] = max(need.get(k, 0), v)
        self._wait("sp", need)
        allneed = {("l", ln): v[1] for ln, v in self.lanes.items() if v[1] > 0}
        for e in self.eng:
            if self.cnt[e] > 0:
                allneed[("e", e)] = self.cnt[e]
        allneed.pop(("e", "sp"), None)
        self._wait("sp", allneed)

    def barrier(self):
        need = {("l", ln): v[1] for ln, v in self.lanes.items() if v[1] > 0}
        for e in self.eng:
            if self.cnt[e] > 0:
                need[("e", e)] = self.cnt[e]
        for e in self.eng:
            n2 = dict(need)
            n2.pop(("e", e), None)
            self._wait(e, n2)
        for b in self.bufs:
            b.w = {}
            b.r = {}
        pool = self.__dict__.setdefault("_lane_pool", [])
        for ln, v in self.lanes.items():
            pool.append(v)
        self.lanes = {}
        for e in self.eng:
            self.known[e] = {k: v for k, v in self.known[e].items() if k[0] == "e"}


class Ctx:
    def __init__(self, nc):
        self.nc = nc
        self.kb = KB(nc)
        self.ps = []
        self.psb = []
        for i in range(8):
            self.ps.append(nc.alloc_psum_tensor(f"ps{i}", [128, 512], F32).ap())
            self.psb.append(self.kb.buf(f"ps{i}"))
        self._mark = None

    def sb(self, name, shape, dtype=F32):
        self._n = getattr(self, "_n", 0) + 1
        t = self.nc.alloc_sbuf_tensor(f"sb{self._n}_{name}", list(shape), dtype).ap()
        return t, self.kb.buf(f"sb{self._n}_{name}")

    def mark(self):
        return (self.nc.sbuf_base, self.nc.sbuf_top)

    def release(self, m):
        self.kb.barrier()
        self.nc.sbuf_base, self.nc.sbuf_top = m

    def tt(self, e, out, in0, in1, op, r, w):
        return self.kb.op(e, lambda g: g.tensor_tensor(out=out, in0=in0, in1=in1, op=op), r, w)

    def ts(self, e, out, in0, s1, s2, op0, op1, r, w):
        if s2 is None:
            return self.kb.op(e, lambda g: g.tensor_scalar(out=out, in0=in0, scalar1=s1, scalar2=None, op0=op0), r, w)
        return self.kb.op(e, lambda g: g.tensor_scalar(out=out, in0=in0, scalar1=s1, scalar2=s2, op0=op0, op1=op1), r, w)

    def stt(self, e, out, in0, scalar, in1, op0, op1, r, w):
        return self.kb.op(e, lambda g: g.scalar_tensor_tensor(out=out, in0=in0, scalar=scalar, in1=in1, op0=op0, op1=op1), r, w)

    def cp(self, e, out, in_, r, w):
        if e == "act":
            return self.kb.op(e, lambda g: g.activation(out=out, in_=in_, func=AF.Copy), r, w)
        return self.kb.op(e, lambda g: g.tensor_copy(out=out, in_=in_), r, w)

    def act(self, out, in_, func, r, w, scale=1.0, bias=None, accum=None):
        kw = {}
        if bias is not None:
            kw["bias"] = bias
        if accum is not None:
            kw["accum_out"] = accum
        return self.kb.op("act", lambda g: g.activation(out=out, in_=in_, func=func, scale=scale, **kw), r, w)

    def mm(self, out, lhsT, rhs, start, stop, r, w, **kw):
        return self.kb.op("pe", lambda g: g.matmul(out, lhsT=lhsT, rhs=rhs, start=start, stop=stop, **kw), r, w)

    def tr(self, out, in_, ident, r, w):
        return self.kb.op("pe", lambda g: g.transpose(out=out, in_=in_, identity=ident), r, w)

    def memset(self, e, out, val, w):
        return self.kb.op(e, lambda g: g.memset(out, val), (), w)


PI = math.pi


def setup_ident(cx, din):
    P = {"bufs": {}}
    P["identf"], bf = cx.sb("identf", [128, 128], F32)
    P["identb"], bb = cx.sb("identb", [128, 128], BF16)
    cx.kb.dma("sp", P["identf"], din["ident"], writes=[bf])
    cx.cp("dve", P["identb"], P["identf"], [bf], [bb])
    P["bufs"]["identf"], P["bufs"]["identb"] = bf, bb
    return P


def phase0(cx, P, din, pre_hook=None):
    nc, kb = cx.nc, cx.kb
    b_identf, b_identb = P["bufs"]["identf"], P["bufs"]["identb"]
    P["WBre"], b_WBre = cx.sb("WBre", [128, 8, 8, 128], BF16)
    P["WBim"], b_WBim = cx.sb("WBim", [128, 8, 8, 128], BF16)
    P["WCre"], b_WCre = cx.sb("WCre", [128, 32, 8, 2, 16], BF16)
    P["WCim"], b_WCim = cx.sb("WCim", [128, 32, 8, 2, 16], BF16)
    P["FIRW"], b_FIRW = cx.sb("FIRW", [128, 8, 8, 128], BF16)
    P["A0c"], b_A0c = cx.sb("A0c", [128, 32, 8], F32)
    P["A0s"], b_A0s = cx.sb("A0s", [128, 32, 8], F32)
    P["A1c"], b_A1c = cx.sb("A1c", [128, 32, 8], F32)
    P["A1s"], b_A1s = cx.sb("A1s", [128, 32, 8], F32)
    P["RM8"], b_RM8 = cx.sb("RM8", [128, 32], F32)
    P["dT"], b_dT = cx.sb("dT", [128, 8], F32)
    P["bufs"].update(dict(WBre=b_WBre, WBim=b_WBim, WCre=b_WCre,
                          WCim=b_WCim, FIRW=b_FIRW, A0c=b_A0c, A0s=b_A0s, A1c=b_A1c, A1s=b_A1s, RM8=b_RM8, dT=b_dT))
    m = cx.mark()
    if pre_hook is not None:
        pre_hook()
    LR, b_LR = cx.sb("LR", [128, 32]); LI, b_LI = cx.sb("LI", [128, 32]); LDT, b_LDT = cx.sb("LDT", [128, 32])
    TH, b_TH = cx.sb("TH", [128, 32]); LM, b_LM = cx.sb("LM", [128, 32])
    EV, b_EV = cx.sb("EV", [128, 9, 32]); ANG, b_ANG = cx.sb("ANG", [128, 9, 32]); MAG, b_MAG = cx.sb("MAG", [128, 9, 32])
    SN, b_SN = cx.sb("SN", [128, 9, 32]); CS, b_CS = cx.sb("CS", [128, 9, 32]); IT, b_IT = cx.sb("IT", [128, 9, 32], I32)
    ARE, b_ARE = cx.sb("ARE", [128, 9, 32]); AIM, b_AIM = cx.sb("AIM", [128, 9, 32])
    NR, b_NR = cx.sb("NR", [128, 32]); DEN, b_DEN = cx.sb("DEN", [128, 32]); TMPa, b_TMPa = cx.sb("TMPa", [128, 32])
    TMPb, b_TMPb = cx.sb("TMPb", [128, 32])
    CRE, b_CRE = cx.sb("CRE", [128, 32]); CIM, b_CIM = cx.sb("CIM", [128, 32])
    WRE, b_WRE = cx.sb("WRE", [128, 8, 32]); WIM, b_WIM = cx.sb("WIM", [128, 8, 32])
    W8a, b_W8a = cx.sb("W8a", [128, 8, 32]); W8b, b_W8b = cx.sb("W8b", [128, 8, 32])
    BTre, b_BTre = cx.sb("BTre", [128, 32, 16]); BTim, b_BTim = cx.sb("BTim", [128, 32, 16])
    CTre, b_CTre = cx.sb("CTre", [128, 32, 16]); CTim, b_CTim = cx.sb("CTim", [128, 32, 16])
    T1, b_T1 = cx.sb("T1", [128, 32, 16]); T2, b_T2 = cx.sb("T2", [128, 32, 16])
    T3, b_T3 = cx.sb("T3", [128, 32, 16]); T4, b_T4 = cx.sb("T4", [128, 32, 16])
    XPre, b_XPre = cx.sb("XPre", [128, 8, 32, 2, 16], BF16); XPim, b_XPim = cx.sb("XPim", [128, 8, 32, 2, 16], BF16)
    CPre, b_CPre = cx.sb("CPre", [128, 32, 2, 16], BF16); CPnim, b_CPnim = cx.sb("CPnim", [128, 32, 2, 16], BF16)
    BM, b_BM = cx.sb("BM", [128, 128], F32)
    TK, b_TK = cx.sb("TK", [128, 128], F32)

    kb.dma("sp", BM, din["bmask"], writes=[b_BM])
    kb.dma("sp", EV, din["ev"].rearrange("p (e q) -> p e q", e=9), writes=[b_EV])
    kb.dma("sp", LR, din["lamT_re"], writes=[b_LR])
    kb.dma("sp", LI, din["lamT_im"], writes=[b_LI])
    kb.dma("sp", LDT, din["ldtT"], writes=[b_LDT])
    kb.dma("sp", P["dT"], din["dT"], writes=[b_dT])
    kb.dma("sp", BTre, din["bT_re"].rearrange("p (q n) -> p q n", n=16), writes=[b_BTre])
    kb.dma("sp", BTim, din["bT_im"].rearrange("p (q n) -> p q n", n=16), writes=[b_BTim])
    kb.dma("sp", CTre, din["cT_re"].rearrange("p (q n) -> p q n", n=16), writes=[b_CTre])
    kb.dma("sp", CTim, din["cT_im"].rearrange("p (q n) -> p q n", n=16), writes=[b_CTim])

    cx.act(LDT, LDT, AF.Exp, [b_LDT], [b_LDT])
    cx.tt("dve", TH, LI, LDT, ALU.mult, [b_LI, b_LDT], [b_TH])
    cx.tt("dve", LM, LR, LDT, ALU.mult, [b_LR, b_LDT], [b_LM])
    bc9 = lambda t: t.unsqueeze(1).to_broadcast([128, 9, 32])
    bc8 = lambda t: t.unsqueeze(1).to_broadcast([128, 8, 32])
    cx.tt("dve", ANG, EV, bc9(TH), ALU.mult, [b_EV, b_TH], [b_ANG])
    cx.tt("dve", MAG, EV, bc9(LM), ALU.mult, [b_EV, b_LM], [b_MAG])
    cx.act(MAG, MAG, AF.Exp, [b_MAG], [b_MAG])
    cx.ts("dve", SN, ANG, 1.0 / (2.0 * PI), None, ALU.mult, None, [b_ANG], [b_SN])
    cx.cp("dve", IT, SN, [b_SN], [b_IT])
    cx.cp("dve", SN, IT, [b_IT], [b_SN])
    cx.stt("dve", SN, SN, -2.0 * PI, ANG, ALU.mult, ALU.add, [b_SN, b_ANG], [b_SN])
    cx.ts("dve", CS, ANG, 1.0 / (2.0 * PI), 0.25, ALU.mult, ALU.add, [b_ANG], [b_CS])
    cx.cp("dve", IT, CS, [b_CS], [b_IT])
    cx.cp("dve", CS, IT, [b_IT], [b_CS])
    cx.stt("dve", CS, CS, -2.0 * PI, ANG, ALU.mult, ALU.add, [b_CS, b_ANG], [b_CS])
    cx.ts("dve", CS, CS, 0.5 * PI, None, ALU.add, None, [b_CS], [b_CS])
    cx.ts("dve", SN, SN, -PI, PI, ALU.max, ALU.min, [b_SN], [b_SN])
    cx.ts("dve", CS, CS, -PI, PI, ALU.max, ALU.min, [b_CS], [b_CS])
    cx.act(SN, SN, AF.Sin, [b_SN], [b_SN])
    cx.act(CS, CS, AF.Sin, [b_CS], [b_CS])
    cx.tt("dve", ARE, MAG, CS, ALU.mult, [b_MAG, b_CS], [b_ARE])
    cx.tt("dve", AIM, MAG, SN, ALU.mult, [b_MAG, b_SN], [b_AIM])
    cx.ts("dve", NR, ARE[:, 1, :], -1.0, None, ALU.add, None, [b_ARE], [b_NR])
    NI = AIM[:, 1, :]
    cx.tt("dve", DEN, LR, LR, ALU.mult, [b_LR], [b_DEN])
    cx.tt("dve", TMPa, LI, LI, ALU.mult, [b_LI], [b_TMPa])
    cx.tt("dve", DEN, DEN, TMPa, ALU.add, [b_DEN, b_TMPa], [b_DEN])
    kb.op("dve", lambda g: g.reciprocal(out=DEN, in_=DEN), [b_DEN], [b_DEN])
    cx.tt("dve", TMPa, NR, LR, ALU.mult, [b_NR, b_LR], [b_TMPa])
    cx.tt("dve", TMPb, NI, LI, ALU.mult, [b_AIM, b_LI], [b_TMPb])
    cx.tt("dve", TMPa, TMPa, TMPb, ALU.add, [b_TMPa, b_TMPb], [b_TMPa])
    cx.tt("dve", CRE, TMPa, DEN, ALU.mult, [b_TMPa, b_DEN], [b_CRE])
    cx.tt("dve", TMPa, NI, LR, ALU.mult, [b_AIM, b_LR], [b_TMPa])
    cx.tt("dve", TMPb, NR, LI, ALU.mult, [b_NR, b_LI], [b_TMPb])
    cx.tt("dve", TMPa, TMPa, TMPb, ALU.subtract, [b_TMPa, b_TMPb], [b_TMPa])
    cx.tt("dve", CIM, TMPa, DEN, ALU.mult, [b_TMPa, b_DEN], [b_CIM])
    cx.tt("dve", W8a, ARE[:, 0:8, :], bc8(CRE), ALU.mult, [b_ARE, b_CRE], [b_W8a])
    cx.tt("dve", W8b, AIM[:, 0:8, :], bc8(CIM), ALU.mult, [b_AIM, b_CIM], [b_W8b])
    cx.tt("dve", WRE, W8a, W8b, ALU.subtract, [b_W8a, b_W8b], [b_WRE])
    cx.tt("dve", W8a, ARE[:, 0:8, :], bc8(CIM), ALU.mult, [b_ARE, b_CIM], [b_W8a])
    cx.tt("dve", W8b, AIM[:, 0:8, :], bc8(CRE), ALU.mult, [b_AIM, b_CRE], [b_W8b])
    cx.tt("dve", WIM, W8a, W8b, ALU.add, [b_W8a, b_W8b], [b_WIM])
    cx.cp("dve", P["RM8"], MAG[:, 8, :], [b_MAG], [b_RM8])
    A0c, A0s, A1c, A1s = P["A0c"], P["A0s"], P["A1c"], P["A1s"]
    cx.cp("dve", A0c[:, :, 0], CS[:, 8, :], [b_CS], [b_A0c])
    cx.cp("dve", A0s[:, :, 0], SN[:, 8, :], [b_SN], [b_A0s])
    for i in range(1, 8):
        cx.tt("dve", TMPa, A0c[:, :, i - 1], A0c[:, :, 0], ALU.mult, [b_A0c], [b_TMPa])
        cx.tt("dve", TMPb, A0s[:, :, i - 1], A0s[:, :, 0], ALU.mult, [b_A0s], [b_TMPb])
        cx.tt("dve", A0c[:, :, i], TMPa, TMPb, ALU.subtract, [b_TMPa, b_TMPb], [b_A0c])
        cx.tt("dve", TMPa, A0c[:, :, i - 1], A0s[:, :, 0], ALU.mult, [b_A0c, b_A0s], [b_TMPa])
        cx.tt("dve", TMPb, A0s[:, :, i - 1], A0c[:, :, 0], ALU.mult, [b_A0s, b_A0c], [b_TMPb])
        cx.tt("dve", A0s[:, :, i], TMPa, TMPb, ALU.add, [b_TMPa, b_TMPb], [b_A0s])
    cx.memset("dve", A1c[:, :, 0], 1.0, [b_A1c])
    cx.memset("dve", A1s[:, :, 0], 0.0, [b_A1s])
    for i in range(1, 8):
        cx.tt("dve", TMPa, A1c[:, :, i - 1], A0c[:, :, 7], ALU.mult, [b_A1c, b_A0c], [b_TMPa])
        cx.tt("dve", TMPb, A1s[:, :, i - 1], A0s[:, :, 7], ALU.mult, [b_A1s, b_A0s], [b_TMPb])
        cx.tt("dve", A1c[:, :, i], TMPa, TMPb, ALU.subtract, [b_TMPa, b_TMPb], [b_A1c])
        cx.tt("dve", TMPa, A1c[:, :, i - 1], A0s[:, :, 7], ALU.mult, [b_A1c, b_A0s], [b_TMPa])
        cx.tt("dve", TMPb, A1s[:, :, i - 1], A0c[:, :, 7], ALU.mult, [b_A1s, b_A0c], [b_TMPb])
        cx.tt("dve", A1s[:, :, i], TMPa, TMPb, ALU.add, [b_TMPa, b_TMPb], [b_A1s])

    cx.memset("pool", XPre, 0.0, [b_XPre])
    cx.memset("pool", XPim, 0.0, [b_XPim])
    cx.memset("pool", CPre, 0.0, [b_CPre])
    cx.memset("pool", CPnim, 0.0, [b_CPnim])
    cx.memset("pool", P["WCre"], 0.0, [b_WCre])
    cx.memset("pool", P["WCim"], 0.0, [b_WCim])
    bcn = lambda t: t.unsqueeze(2).to_broadcast([128, 32, 16])
    for s in range(8):
        e = 7 - s
        cx.tt("dve", T1, BTre, bcn(WRE[:, e, :]), ALU.mult, [b_BTre, b_WRE], [b_T1])
        cx.tt("dve", T2, BTim, bcn(WIM[:, e, :]), ALU.mult, [b_BTim, b_WIM], [b_T2])
        cx.tt("dve", T3, BTim, bcn(WRE[:, e, :]), ALU.mult, [b_BTim, b_WRE], [b_T3])
        cx.tt("dve", T4, BTre, bcn(WIM[:, e, :]), ALU.mult, [b_BTre, b_WIM], [b_T4])
        for par in range(2):
            sl = slice(64 * par, 64 * par + 64)
            cx.tt("dve", XPre[sl, s, :, par, :], T1[sl], T2[sl], ALU.subtract, [b_T1, b_T2], [b_XPre])
            cx.tt("dve", XPim[sl, s, :, par, :], T3[sl], T4[sl], ALU.add, [b_T3, b_T4], [b_XPim])
    for par in range(2):
        sl = slice(64 * par, 64 * par + 64)
        cx.cp("dve", CPre[sl, :, par, :], CTre[sl], [b_CTre], [b_CPre])
        cx.ts("dve", CPnim[sl, :, par, :], CTim[sl], -1.0, None, ALU.mult, None, [b_CTim], [b_CPnim])
    for j in range(8):
        e = j + 1
        cx.tt("dve", T1, CTre, bcn(ARE[:, e, :]), ALU.mult, [b_CTre, b_ARE], [b_T1])
        cx.tt("dve", T2, CTim, bcn(AIM[:, e, :]), ALU.mult, [b_CTim, b_AIM], [b_T2])
        cx.tt("dve", T3, CTre, bcn(AIM[:, e, :]), ALU.mult, [b_CTre, b_AIM], [b_T3])
        cx.tt("dve", T4, CTim, bcn(ARE[:, e, :]), ALU.mult, [b_CTim, b_ARE], [b_T4])
        for par in range(2):
            sl = slice(64 * par, 64 * par + 64)
            cx.tt("dve", P["WCre"][sl, :, j, par, :], T1[sl], T2[sl], ALU.subtract, [b_T1, b_T2], [b_WCre])
            cx.stt("dve", P["WCim"][sl, :, j, par, :], T3[sl], -1.0, T4[sl], ALU.mult, ALU.subtract,
                   [b_T3, b_T4], [b_WCim])
    for fc in range(8):
        pq = slice(4 * fc, 4 * fc + 4)
        for k in range(8):
            pi = (fc * 8 + k) % 4
            ps, bps = cx.ps[pi], cx.psb[pi]
            cx.mm(ps[:, 0:128], XPre[:, 7 - k, pq, :, :], CPre[:, pq, :, :], True, False, [b_XPre, b_CPre], [bps])
            cx.mm(ps[:, 0:128], XPim[:, 7 - k, pq, :, :], CPnim[:, pq, :, :], False, True, [b_XPim, b_CPnim], [bps])
            if k == 0:
                cx.tt("dve", TK, ps[:, 0:128], BM, ALU.mult, [bps, b_BM], [b_TK])
                cx.stt("dve", P["FIRW"][:, fc, 0, :], P["identf"], P["dT"][:, fc:fc + 1], TK, ALU.mult, ALU.add,
                       [b_identf, b_dT, b_TK], [b_FIRW])
            else:
                cx.tt("dve", P["FIRW"][:, fc, k, :], ps[:, 0:128], BM, ALU.mult, [bps, b_BM], [b_FIRW])
    for (XP, b_XP, WB, b_WB) in ((XPre, b_XPre, P["WBre"], b_WBre), (XPim, b_XPim, P["WBim"], b_WBim)):
        for fc in range(8):
            pq = slice(4 * fc, 4 * fc + 4)
            pi = 4 + (fc % 2)
            psb16 = cx.ps[pi].bitcast(BF16).rearrange("p (s c) -> p s c", s=8)
            for s in range(8):
                cx.tr(psb16[:, s, :], XP[:, s, pq, :, :], P["identb"], [b_XP, b_identb], [cx.psb[pi]])
            cx.cp("act" if fc % 2 else "dve", WB[:, fc, :, :], psb16, [cx.psb[pi]], [b_WB])
    cx.release(m)
    return P


class PsRot:
    def __init__(self, cx, banks):
        self.cx = cx
        self.banks = list(banks)
        self.i = 0

    def next(self):
        b = self.banks[self.i % len(self.banks)]
        self.i += 1
        return self.cx.ps[b], self.cx.psb[b]


class WStream:
    def __init__(self, cx, nslots, slot_elems, name="ws", direct=None, ahead=None):
        self.cx = cx
        self.slots = []
        for i in range(nslots):
            t, b = cx.sb(f"{name}{i}", [128, slot_elems], BF16)
            self.slots.append((t, b))
        self.jobs = []
        self.issued = 0
        self.used = 0
        self.res = {}
        self.direct = direct
        self.ahead = ahead

    def plan(self, jobs):
        self.jobs.extend(jobs)

    def _issue(self, i):
        t, b = self.slots[i % len(self.slots)]
        off = 0
        views = []
        if self.direct is not None:
            src, n = self.jobs[i]
            self.cx.kb.dma("sp", t[:, 0:n], src, reads=[self.direct], writes=[b])
            self.res[i] = (t, b)
            return
        for (src, a, n) in self.jobs[i]:
            v = t[:, off:off + a * n].rearrange("p (a n) -> p a n", n=n)
            self.cx.kb.dma("pool", v, src, writes=[b])
            views.append(v)
            off += a * n
        self.res[i] = (views, b)

    def get(self):
        i = self.used
        ahead = self.ahead if self.ahead is not None else max(1, len(self.slots) - 2)
        while self.issued < min(len(self.jobs), i + 1 + ahead):
            self._issue(self.issued)
            self.issued += 1
        self.used += 1
        return self.res.pop(i)


def rms_to_hT(cx, P, xt, b_xt, nblk, gT, b_gT, htok, b_htok, hT, b_hT, ss, b_ss, trbanks, d=D, extra=None):
    nkc = d // 128
    bx = b_xt if isinstance(b_xt, list) else [b_xt]
    for b in range(nblk):
        cx.act(htok[:, b, :], xt[:, b, :], AF.Square, bx, [b_htok, b_ss], accum=ss[:, b:b + 1])
    cx.ts("dve", ss[:, 0:nblk], ss[:, 0:nblk], 1.0 / d, EPS, ALU.mult, ALU.add, [b_ss], [b_ss])
    cx.act(ss[:, 0:nblk], ss[:, 0:nblk], AF.Sqrt, [b_ss], [b_ss])
    cx.kb.op("dve", lambda g: g.reciprocal(out=ss[:, 0:nblk], in_=ss[:, 0:nblk]), [b_ss], [b_ss])
    for b in range(nblk):
        cx.ts("dve", htok[:, b, :], xt[:, b, :], ss[:, b:b + 1], None, ALU.mult, None, bx + [b_ss], [b_htok])
    for b in range(nblk):
        pi = trbanks[b % len(trbanks)]
        p16 = cx.ps[pi].bitcast(BF16).rearrange("p (k c) -> p k c", c=128)
        for kc in range(nkc):
            cx.tr(p16[:, kc, :], htok[:, b, kc * 128:(kc + 1) * 128], P["identb"], [b_htok, P["bufs"]["identb"]], [cx.psb[pi]])
        cx.tt("dve", hT[:, 0:nkc, b * 128:(b + 1) * 128], p16[:, 0:nkc, :],
              gT[:, 0:nkc].unsqueeze(2).to_broadcast([128, nkc, 128]), ALU.mult, [cx.psb[pi], b_gT], [b_hT])
        if extra is not None:
            gT2, hT2, b_hT2 = extra
            cx.tt("dve", hT2[:, 0:nkc, b * 128:(b + 1) * 128], p16[:, 0:nkc, :],
                  gT2[:, 0:nkc].unsqueeze(2).to_broadcast([128, nkc, 128]), ALU.mult, [cx.psb[pi], b_gT], [b_hT2])


N_L0_JOBS = 6 + D_FF // 128


def layer0_convert(cx, din, W0, b_W0, nstage=4):
    kb = cx.kb
    stage = [cx.sb(f"l0st{i}", [128, 4096], BF16) for i in range(nstage)]
    j = 0
    for w in (din["s5_w_in"], din["s5_w_glu"], din["s5_w_out"]):
        for half in range(2):
            st, b_st = stage[j % nstage]
            kb.dma("pool", st.rearrange("p (k n) -> p k n", n=512),
                   w[:, half * 512:(half + 1) * 512].rearrange("(k p) n -> p k n", p=128), writes=[b_st])
            kb.dma("sp", W0[j * 128:(j + 1) * 128, :], st, reads=[b_st], writes=[b_W0], disjoint=True)
            j += 1
    for f in range(D_FF // 128):
        st, b_st = stage[j % nstage]
        cs = slice(f * 128, (f + 1) * 128)
        kb.dma("pool", st[:, 0:1024].rearrange("p (k n) -> p k n", n=128),
               din["ffn_w_gate"][:, cs].rearrange("(k p) n -> p k n", p=128), writes=[b_st])
        kb.dma("pool", st[:, 1024:2048].rearrange("p (k n) -> p k n", n=128),
               din["ffn_w_up"][:, cs].rearrange("(k p) n -> p k n", p=128), writes=[b_st])
        kb.dma("pool", st[:, 2048:3072], din["ffn_w_down"][cs, :], writes=[b_st])
        kb.dma("sp", W0[j * 128:(j + 1) * 128, 0:3072], st[:, 0:3072], reads=[b_st], writes=[b_W0], disjoint=True)
        j += 1


GELU_C = 0.044715
GELU_S = 2.0 * math.sqrt(2.0 / math.pi)


def phase1(cx, P, din, xs, b_xs, W0, b_W0, ntiles=8):
    nc, kb = cx.nc, cx.kb
    B = P["bufs"]
    m = cx.mark()
    xt, _ = cx.sb("xt", [128, 4, 1024], F32)
    b_xt = kb.bufs_n("xt8_", 8)
    regA, b_A = cx.sb("regA", [128, 4096], F32)
    regB, b_B = cx.sb("regB", [128, 2048], F32)
    regC, b_C = cx.sb("regC", [128, 2048], F32)
    uT, _ = cx.sb("uT", [128, 8, 512], BF16)
    b_u = kb.bufs_n("uT", 8)
    yT, _ = cx.sb("yT", [128, 8, 512], BF16)
    b_y = kb.bufs_n("yT", 8)
    SR, b_SR = cx.sb("SR", [128, 32, 65], F32)
    SI, b_SI = cx.sb("SI", [128, 32, 65], F32)
    SB, b_SB = cx.sb("SB", [128, 32, 2, 64], BF16)
    ss, b_ss = cx.sb("ss", [128, 8], F32)
    gT, b_gT = cx.sb("gT", [128, 2, 8], F32)
    CAR, b_CAR = cx.sb("CAR", [128, 2, 32], F32)
    ws = WStream(cx, 3, 4096, direct=b_W0, ahead=2)
    y32 = regA.rearrange("p (f t) -> p f t", t=512)
    t3 = regA[:, 0:2048].rearrange("p (q c) -> p q c", c=64)
    t4 = regA[:, 2048:4096].rearrange("p (q c) -> p q c", c=64)
    htok = regB.bitcast(BF16).rearrange("p (b d) -> p b d", d=1024)
    t1 = regB.rearrange("p (q c) -> p q c", c=64)
    hT = regC.bitcast(BF16).rearrange("p (k t) -> p k t", t=512)
    t2 = regC.rearrange("p (q c) -> p q c", c=64)
    zT, b_z = uT, b_u
    sgs = [regB[:, i * 512:(i + 1) * 512] for i in range(2)]
    acts = [yT.rearrange("p f t -> p (f t)")[:, i * 512:(i + 1) * 512] for i in range(4)]
    rot = PsRot(cx, [2, 3, 4, 5, 6, 7])
    ytmp = [(regA[:, i * 512:(i + 1) * 512], kb.buf(f"ytmp{i}")) for i in range(4)]
    nt = 0

    kb.dma("sp", gT[:, 0, :], din["gT_mix0"], writes=[b_gT])
    kb.dma("sp", gT[:, 1, :], din["gT_ffn0"], writes=[b_gT])
    cx.memset("dve", CAR, 0.0, [b_CAR])
    w_in, w_glu, w_out = din["s5_w_in"], din["s5_w_glu"], din["s5_w_out"]
    wg, wu, wd = din["ffn_w_gate"], din["ffn_w_up"], din["ffn_w_down"]

    def wcols(w, c0, n):
        return (w[:, c0:c0 + n].rearrange("(k p) n -> p k n", p=128), 8, n)

    jobs = []
    for T in range(ntiles):
        for j in range(N_L0_JOBS):
            jobs.append((W0[j * 128:(j + 1) * 128, 0:(4096 if j < 6 else 3072)], 4096 if j < 6 else 3072))
    ws.plan(jobs)

    for T in range(ntiles):
        t0 = T * 512
        kb.dma("sp", xt, din["x"][t0:t0 + 512, :].rearrange("(b p) d -> p b d", p=128), writes=b_xt, lane="xt")
        rms_to_hT(cx, P, xt, b_xt, 4, gT[:, 0, :], b_gT, htok, b_B, hT, b_C, ss, b_ss, [0, 1])
        for half in range(2):
            wt_, b_w = ws.get()
            wv = wt_.rearrange("p (k n) -> p k n", n=512)
            for f4 in range(4):
                fc = half * 4 + f4
                ps, bps = rot.next()
                for kc in range(8):
                    cx.mm(ps, wv[:, kc, f4 * 128:(f4 + 1) * 128], hT[:, kc, :], kc == 0, kc == 7, [b_w, b_C], [bps])
                cx.cp("act", uT[:, fc, :], ps, [bps], [b_u[fc]])
        a0c = lambda: P["A0c"].unsqueeze(2).to_broadcast([128, 32, 8, 8])
        a0s = lambda: P["A0s"].unsqueeze(2).to_broadcast([128, 32, 8, 8])
        a1c = lambda: P["A1c"].unsqueeze(3).to_broadcast([128, 32, 8, 8])
        a1s = lambda: P["A1s"].unsqueeze(3).to_broadcast([128, 32, 8, 8])
        v4 = lambda t: t.rearrange("p q (a b) -> p q a b", b=8)
        SRv, SIv = SR[:, :, 0:64], SI[:, :, 0:64]
        for qb in range(4):
            psr, bpsr = rot.next()
            psi, bpsi = rot.next()
            for q8 in range(8):
                q = qb * 8 + q8
                fc, q4 = q // 4, q % 4
                rows = slice(32 * q4, 32 * q4 + 32)
                uv = uT[rows, fc, :].rearrange("p (c s) -> p c s", s=8)
                for (pp, bpp, WB, bWB) in ((psr, bpsr, P["WBre"], B["WBre"]), (psi, bpsi, P["WBim"], B["WBim"])):
                    for s in range(8):
                        cx.mm(pp[:, q8 * 64:(q8 + 1) * 64], WB[rows, fc, s, :], uv[:, :, s], s == 0, s == 7,
                              [bWB, b_u[fc]], [bpp], tile_position=(32 * q4, 0))
            qs = slice(qb * 8, (qb + 1) * 8)
            pr4 = psr.rearrange("p (q a b) -> p q a b", a=8, b=8)
            pi4 = psi.rearrange("p (q a b) -> p q a b", a=8, b=8)
            c4 = P["A0c"][:, qs, :].unsqueeze(2).to_broadcast([128, 8, 8, 8])
            s4 = P["A0s"][:, qs, :].unsqueeze(2).to_broadcast([128, 8, 8, 8])
            cx.tt("dve", v4(t3)[:, qs], pr4, c4, ALU.mult, [bpsr, B["A0c"]], [b_A])
            cx.tt("dve", v4(t4)[:, qs], pi4, s4, ALU.mult, [bpsi, B["A0s"]], [b_A])
            cx.tt("dve", v4(SRv)[:, qs], v4(t3)[:, qs], v4(t4)[:, qs], ALU.add, [b_A], [b_SR])
            cx.tt("dve", v4(t3)[:, qs], pi4, c4, ALU.mult, [bpsi, B["A0c"]], [b_A])
            cx.tt("dve", v4(t4)[:, qs], pr4, s4, ALU.mult, [bpsr, B["A0s"]], [b_A])
            cx.tt("dve", v4(SIv)[:, qs], v4(t3)[:, qs], v4(t4)[:, qs], ALU.subtract, [b_A], [b_SI])
        cx.tt("dve", v4(t3), v4(SRv), a1c(), ALU.mult, [b_SR, B["A1c"]], [b_A])
        cx.tt("dve", v4(t4), v4(SIv), a1s(), ALU.mult, [b_SI, B["A1s"]], [b_A])
        cx.tt("dve", v4(t1), v4(t3), v4(t4), ALU.add, [b_A], [b_B])
        cx.tt("dve", v4(t3), v4(SIv), a1c(), ALU.mult, [b_SI, B["A1c"]], [b_A])
        cx.tt("dve", v4(t4), v4(SRv), a1s(), ALU.mult, [b_SR, B["A1s"]], [b_A])
        cx.tt("dve", v4(t2), v4(t3), v4(t4), ALU.subtract, [b_A], [b_C])
        for q in range(32):
            rm = P["RM8"][:, q:q + 1].to_broadcast([128, 64])
            kb.op("dve", lambda g_, q=q, rm=rm: g_.tensor_tensor_scan(out=SRv[:, q, :], data0=rm, data1=t1[:, q, :],
                  initial=CAR[:, 0, q:q + 1], op0=ALU.mult, op1=ALU.add), [b_B, B["RM8"], b_CAR], [b_SR])
            kb.op("dve", lambda g_, q=q, rm=rm: g_.tensor_tensor_scan(out=SIv[:, q, :], data0=rm, data1=t2[:, q, :],
                  initial=CAR[:, 1, q:q + 1], op0=ALU.mult, op1=ALU.add), [b_C, B["RM8"], b_CAR], [b_SI])
        cx.tt("dve", v4(t3), v4(SRv), a1c(), ALU.mult, [b_SR, B["A1c"]], [b_A])
        cx.tt("dve", v4(t4), v4(SIv), a1s(), ALU.mult, [b_SI, B["A1s"]], [b_A])
        cx.tt("dve", v4(t1), v4(t3), v4(t4), ALU.subtract, [b_A], [b_B])
        cx.tt("dve", v4(t3), v4(SIv), a1c(), ALU.mult, [b_SI, B["A1c"]], [b_A])
        cx.tt("dve", v4(t4), v4(SRv), a1s(), ALU.mult, [b_SR, B["A1s"]], [b_A])
        cx.tt("dve", v4(t2), v4(t3), v4(t4), ALU.add, [b_A], [b_C])
        cx.cp("dve", SB[:, :, 0, 0:1], CAR[:, 0, :].unsqueeze(2), [b_CAR], [b_SB])
        cx.cp("dve", SB[:, :, 1, 0:1], CAR[:, 1, :].unsqueeze(2), [b_CAR], [b_SB])
        cx.tt("dve", v4(t3), v4(t1), a0c(), ALU.mult, [b_B, B["A0c"]], [b_A])
        cx.tt("dve", v4(t4), v4(t2), a0s(), ALU.mult, [b_C, B["A0s"]], [b_A])
        cx.tt("dve", SB[:, :, 0, 1:64], t3[:, :, 0:63], t4[:, :, 0:63], ALU.subtract, [b_A], [b_SB])
        cx.tt("dve", CAR[:, 0, :].unsqueeze(2), t3[:, :, 63:64], t4[:, :, 63:64], ALU.subtract, [b_A], [b_CAR])
        cx.tt("dve", v4(t3), v4(t2), a0c(), ALU.mult, [b_C, B["A0c"]], [b_A])
        cx.tt("dve", v4(t4), v4(t1), a0s(), ALU.mult, [b_B, B["A0s"]], [b_A])
        cx.tt("dve", SB[:, :, 1, 1:64], t3[:, :, 0:63], t4[:, :, 0:63], ALU.add, [b_A], [b_SB])
        cx.tt("dve", CAR[:, 1, :].unsqueeze(2), t3[:, :, 63:64], t4[:, :, 63:64], ALU.add, [b_A], [b_CAR])
        for fc in range(8):
            ps, bps = rot.next()
            uv = uT[:, fc, :].rearrange("p (c s) -> p c s", s=8)
            pv = ps.rearrange("p (c s) -> p c s", s=8)
            for k in range(8):
                cx.mm(pv[:, :, k:8], P["FIRW"][:, fc, k, :], uv[:, :, 0:8 - k], k == 0, False,
                      [B["FIRW"], b_u[fc]], [bps])
            for q4 in range(4):
                q = fc * 4 + q4
                rows = slice(32 * q4, 32 * q4 + 32)
                for j in range(8):
                    last = (q4 == 3 and j == 7)
                    cx.mm(pv[rows, :, j], P["WCre"][:, q, j, :, :], SB[:, q, 0, :], False, False,
                          [B["WCre"], b_SB], [bps], tile_position=(0, 32 * q4))
                    cx.mm(pv[rows, :, j], P["WCim"][:, q, j, :, :], SB[:, q, 1, :], False, last,
                          [B["WCim"], b_SB], [bps], tile_position=(0, 32 * q4))
            yv = y32[:, fc, :]
            g1 = sgs[fc % 2]
            cx.act(g1, ps, AF.Square, [bps], [b_B])
            cx.ts("dve", g1, g1, GELU_C, 1.0, ALU.mult, ALU.add, [b_B], [b_B])
            cx.tt("dve", g1, g1, ps, ALU.mult, [b_B, bps], [b_B])
            cx.act(g1, g1, AF.Sigmoid, [b_B], [b_B], scale=GELU_S)
            cx.tt("dve", yv, g1, ps, ALU.mult, [b_B, bps], [b_A])
            cx.cp("act", yT[:, fc, :], yv, [b_A], [b_y[fc]])
        for half in range(2):
            wt_, b_w = ws.get()
            wv = wt_.rearrange("p (k n) -> p k n", n=512)
            for f4 in range(4):
                fc = half * 4 + f4
                ps, bps = rot.next()
                for kc in range(8):
                    cx.mm(ps, wv[:, kc, f4 * 128:(f4 + 1) * 128], yT[:, kc, :], kc == 0, kc == 7, [b_w, b_y[kc]], [bps])
                g1 = sgs[fc % 2]
                cx.act(g1, ps, AF.Sigmoid, [bps], [b_B])
                cx.tt("dve", zT[:, fc, :], y32[:, fc, :], g1, ALU.mult, [b_A, b_B], [b_z[fc]])
        for half in range(2):
            wt_, b_w = ws.get()
            wv = wt_.rearrange("p (k n) -> p k n", n=512)
            for b in range(4):
                ps, bps = rot.next()
                for kc in range(8):
                    cx.mm(ps, zT[:, kc, b * 128:(b + 1) * 128], wv[:, kc, :], kc == 0, kc == 7, [b_z[kc], b_w], [bps])
                xv = xt[:, b, half * 512:(half + 1) * 512]
                cx.tt("dve", xv, xv, ps, ALU.add, [b_xt[b * 2 + half], bps], [b_xt[b * 2 + half]])
        rms_to_hT(cx, P, xt, b_xt, 4, gT[:, 1, :], b_gT, htok, b_B, hT, b_C, ss, b_ss, [0, 1])
        for f in range(D_FF // 128):
            wt_, b_w = ws.get()
            gv = wt_[:, 0:1024].rearrange("p (k n) -> p k n", n=128)
            uvw = wt_[:, 1024:2048].rearrange("p (k n) -> p k n", n=128)
            dv = wt_[:, 2048:3072].rearrange("p (a d) -> p a d", d=1024)
            pg, bpg = rot.next()
            pu, bpu = rot.next()
            for kc in range(8):
                cx.mm(pg, gv[:, kc, :], hT[:, kc, :], kc == 0, kc == 7, [b_w, b_C], [bpg])
            for kc in range(8):
                cx.mm(pu, uvw[:, kc, :], hT[:, kc, :], kc == 0, kc == 7, [b_w, b_C], [bpu])
            g1 = sgs[f % 2]
            a1 = acts[f % 4]
            b_a = b_y[(f % 4)]
            cx.act(g1, pg, AF.Silu, [bpg], [b_B])
            cx.tt("dve", a1, g1, pu, ALU.mult, [b_B, bpu], [b_a])
            for b in range(4):
                for half in range(2):
                    ps, bps = rot.next()
                    cx.mm(ps, a1[:, b * 128:(b + 1) * 128], dv[:, 0, half * 512:(half + 1) * 512], True, True,
                          [b_a, b_w], [bps])
                    xv = xt[:, b, half * 512:(half + 1) * 512]
                    bx = b_xt[b * 2 + half]
                    if (b * 2 + half) % 2 == 0:
                        cx.tt("dve", xv, xv, ps, ALU.add, [bx, bps], [bx])
                    else:
                        tb, b_tb = ytmp[nt % 4]
                        nt += 1
                        cx.cp("act", tb, ps, [bps], [b_tb])
                        cx.tt("pool", xv, xv, tb, ALU.add, [bx, b_tb], [bx])
        kb.dma("sp", xs[t0:t0 + 512, :].rearrange("(b p) d -> p b d", p=128), xt, reads=b_xt, writes=[b_xs], disjoint=True)
    cx.release(m)


IN_SPECS = {
    "x": ([L, D], F32), "pos": ([1, L], I32),
    "ident": ([128, 128], F32), "bmask": ([128, 128], F32), "ev": ([128, 9 * 32], F32),
    "ltri": ([128, 128], F32), "wtab": ([128, NSLAB * NE], F32), "ctab": ([128, 7], F32),
    "invf_t": ([128, 16], F32), "invf_f": ([128, 1], F32), "esel": ([128, 31], F32), "dmask": ([128, 128], F32),
    "lamT_re": ([128, 32], F32), "lamT_im": ([128, 32], F32), "ldtT": ([128, 32], F32),
    "bT_re": ([128, 512], F32), "bT_im": ([128, 512], F32),
    "cT_re": ([128, 512], F32), "cT_im": ([128, 512], F32), "dT": ([128, 8], F32),
    "gT_mix0": ([128, 8], F32), "gT_ffn0": ([128, 8], F32), "gT_kv": ([128, 8], F32),
    "gT_mix1": ([128, 8], F32), "gT_ffn1": ([128, 8], F32), "g_final": ([D], F32),
    "g_kvlat": ([KV_LORA], F32), "g_qlat": ([Q_LORA], F32),
    "s5_w_in": ([D, D], F32), "s5_w_glu": ([D, D], F32), "s5_w_out": ([D, D], F32),
    "ffn_w_gate": ([D, D_FF], F32), "ffn_w_up": ([D, D_FF], F32), "ffn_w_down": ([D_FF, D], F32),
    "w_dkv": ([D, KV_LORA + QK_ROPE], F32), "w_ukv": ([KV_LORA, NH * 128], F32),
    "w_dq": ([D, Q_LORA], F32), "w_uq": ([Q_LORA, NH * 96], F32), "w_o": ([NH * V_HEAD, D], F32),
    "router_w": ([D, NE], F32), "router_wT": ([NE, D], F32), "g_ffn1": ([D], F32), "sel": ([8, NE * 128], F32),
    "moe_w_gate": ([NE, D, MOE_FF], F32), "moe_w_up": ([NE, D, MOE_FF], F32),
    "moe_w_down": ([NE, MOE_FF, D], F32),
}


def host_consts():
    c = {}
    c["ident"] = np.eye(128, dtype=np.float32)
    blk = np.arange(128) // 16
    c["bmask"] = (blk[:, None] == blk[None, :]).astype(np.float32)
    c["ev"] = np.broadcast_to(np.arange(9, dtype=np.float32)[None, :, None], (128, 9, 32)).reshape(128, 288).copy()
    invf = (np.float32(10000.0) ** (-np.arange(16, dtype=np.float32) * np.float32(2.0 / QK_ROPE))).astype(np.float32)
    c["invf_t"] = np.broadcast_to(invf[None, :], (128, 16)).copy()
    ff = np.zeros((128, 1), np.float32)
    ff[64:80, 0] = invf
    ff[80:96, 0] = invf
    c["invf_f"] = ff
    es = np.zeros((128, 31), np.float32)
    es[:, 15] = 1.0
    c["esel"] = es
    kk = np.arange(128)[:, None]
    qq = np.arange(128)[None, :]
    c["dmask"] = ((kk < 64) | (qq >= 64)).astype(np.float32)
    se = np.zeros((8, NE, 128), np.float32)
    for e in range(NE):
        se[e, e, :] = 1.0
    c["sel"] = se.reshape(8, NE * 128)
    c["ltri"] = (np.arange(128)[:, None] < np.arange(128)[None, :]).astype(np.float32)
    c["wtab"] = np.broadcast_to(np.arange(NSLAB, dtype=np.float32)[None, :, None], (128, NSLAB, NE)).reshape(128, NSLAB * NE).copy()
    c["ctab"] = (np.arange(7, dtype=np.float32)[None, :] * 128.0 + np.arange(128, dtype=np.float32)[:, None]).copy()
    return c


def _gT(g):
    return np.ascontiguousarray(g.reshape(8, 128).T)


def host_shared(inp):
    f = lambda a: np.ascontiguousarray(a, dtype=np.float32)
    pair = lambda a: f(a.reshape(32, 2, 64).transpose(1, 2, 0).reshape(128, 32))
    s = dict(host_consts())
    s["lamT_re"] = pair(inp["s5_lambda_re"][0])
    s["lamT_im"] = pair(inp["s5_lambda_im"][0])
    s["ldtT"] = pair(np.broadcast_to(inp["s5_log_dt"][0][:, None], (64, 64)))
    bt = lambda b: f(b.reshape(32, 2, 64, 16).transpose(1, 2, 0, 3).reshape(128, 512))
    ct = lambda c: f(c.reshape(32, 2, 16, 64).transpose(1, 3, 0, 2).reshape(128, 512))
    s["bT_re"], s["bT_im"] = bt(inp["s5_b_re"][0]), bt(inp["s5_b_im"][0])
    s["cT_re"], s["cT_im"] = ct(inp["s5_c_re"][0]), ct(inp["s5_c_im"][0])
    s["dT"] = _gT(f(inp["s5_d"][0]))
    s["gT_mix0"], s["gT_mix1"] = _gT(f(inp["norm_mix"][0])), _gT(f(inp["norm_mix"][1]))
    s["gT_ffn0"], s["gT_ffn1"] = _gT(f(inp["norm_ffn"][0])), _gT(f(inp["norm_ffn"][1]))
    s["gT_kv"] = _gT(f(inp["kv_norm"]))
    s["g_final"] = f(inp["final_norm"])
    s["g_kvlat"] = f(inp["kv_latent_norm"])
    s["g_qlat"] = f(inp["q_latent_norm"][0])
    for k in ("s5_w_in", "s5_w_glu", "s5_w_out", "ffn_w_gate", "ffn_w_up", "ffn_w_down",
              "w_dq", "w_uq", "w_o", "moe_w_gate", "moe_w_up", "moe_w_down"):
        s[k] = f(inp[k][0])
    s["router_wT"] = f(inp["router_w"][0].T)
    s["router_w"] = f(inp["router_w"][0])
    s["g_ffn1"] = f(inp["norm_ffn"][1])
    s["w_dkv"] = f(inp["w_dkv"])
    s["w_ukv"] = f(inp["w_ukv"])
    return s


def declare_inputs(nc, names):
    din = {}
    for k in names:
        shape, dt = IN_SPECS[k]
        din[k] = nc.dram_tensor(k, list(shape), dt, kind="ExternalInput").ap()
    return din


ATT_SCALE = (QK_NOPE + QK_ROPE) ** -0.5
KMAX_MARGIN = 1.02


def range_reduce_sin(cx, out, ang, it, b_out, b_ang, b_it, shift):
    cx.ts("dve", out, ang, 1.0 / (2.0 * PI), shift / (2.0 * PI), ALU.mult, ALU.add, [b_ang], [b_out])
    cx.cp("dve", it, out, [b_out], [b_it])
    cx.cp("dve", out, it, [b_it], [b_out])
    cx.stt("dve", out, out, -2.0 * PI, ang, ALU.mult, ALU.add, [b_out, b_ang], [b_out])
    cx.ts("dve", out, out, shift, -PI, ALU.add, ALU.max, [b_out], [b_out])
    cx.ts("dve", out, out, PI, None, ALU.min, None, [b_out], [b_out])
    cx.act(out, out, AF.Sin, [b_out], [b_out])


def phase15(cx, P, din, xs, b_xs, KT, b_KT, QT, b_QT, VS, b_VS, ntiles=8, hook=None):
    nc, kb = cx.nc, cx.kb
    B = P["bufs"]
    identb, b_identb = P["identb"], B["identb"]
    m = cx.mark()
    xt, b_xt = cx.sb("xt", [128, 4, 1024], F32)
    htok, b_htok = cx.sb("htok", [128, 4, 1024], BF16)
    hT, b_hT = cx.sb("hT", [128, 8, 512], BF16)
    ss, b_ss = cx.sb("ss", [128, 8], F32)
    gT, b_gT = cx.sb("gT", [128, 2, 8], F32)
    wdkv, b_wdkv = cx.sb("wdkv", [128, 8, 288], BF16)
    wukv, b_wukv = cx.sb("wukv", [128, 2, 2048], BF16)
    wdq, b_wdq = cx.sb("wdq", [128, 8, 512], BF16)
    wuq, b_wuq = cx.sb("wuq", [128, 4, 1536], BF16)
    wrot, b_wrot = cx.sb("wrot", [128, 4, 16, 32], BF16)
    gkv, b_gkv = cx.sb("gkv", [128, 256], F32)
    gq, b_gq = cx.sb("gq", [128, 512], F32)
    invf_t, b_invf_t = cx.sb("invf_t", [128, 16], F32)
    invf_f, b_invf_f = cx.sb("invf_f", [128, 1], F32)
    esel, b_esel = cx.sb("esel", [128, 31], BF16)
    eself, b_eself = cx.sb("eself", [128, 31], F32)
    posi, b_posi = cx.sb("posi", [128, 512], I32)
    posf, b_posf = cx.sb("posf", [128, 512], F32)
    ptok_i, b_ptok_i = cx.sb("ptok_i", [128, 4], I32)
    ptok, b_ptok = cx.sb("ptok", [128, 4], F32)
    angf, b_angf = cx.sb("angf", [128, 512], F32)
    cosf, b_cosf = cx.sb("cosf", [128, 512], F32)
    sinf, b_sinf = cx.sb("sinf", [128, 512], F32)
    itf, b_itf = cx.sb("itf", [128, 512], I32)
    angt, b_angt = cx.sb("angt", [128, 16], F32)
    cost, b_cost = cx.sb("cost", [128, 16], F32)
    sint, b_sint = cx.sb("sint", [128, 16], F32)
    itt, b_itt = cx.sb("itt", [128, 16], I32)
    ckvn, b_ckvn = cx.sb("ckvn", [128, 256], BF16)
    kro, b_kro = cx.sb("kro", [128, 32], BF16)
    krt, b_krt = cx.sb("krt", [128, 4, 16], F32)
    ckvT, b_ckvT = cx.sb("ckvT", [128, 2, 512], BF16)
    krT, b_krT = cx.sb("krT", [128, 512], BF16)
    cqn, b_cqn = cx.sb("cqn", [128, 512], BF16)
    cqnT, b_cqnT = cx.sb("cqnT", [128, 4, 512], BF16)
    KTt, b_KTt = cx.sb("KTt", [128, 16, 512], BF16)
    QTt, b_QTt = cx.sb("QTt", [128, 16, 512], BF16)
    SQ, b_SQ = cx.sb("SQ", [128, 16, 512], BF16)
    VA, b_VA = cx.sb("VA", [128, 16, 4, 65], BF16)
    nrm, b_nrm = cx.sb("nrm", [16, 512], F32)
    nrmb, b_nrmb = cx.sb("nrmb", [16, 512], BF16)
    rmax, b_rmax = cx.sb("rmax", [16, 2], F32)
    KM, b_KM = cx.sb("KM", [16, 4096], BF16)
    t96a, b_t96a = cx.sb("t96a", [128, 512], F32)
    t96b, b_t96b = cx.sb("t96b", [128, 512], F32)
    rot = PsRot(cx, [2, 3, 4, 5, 6, 7])

    kb.dma("sp", gT[:, 0, :], din["gT_kv"], writes=[b_gT])
    kb.dma("sp", gT[:, 1, :], din["gT_mix1"], writes=[b_gT])
    kb.dma("sp", gkv, din["g_kvlat"].partition_broadcast(128), writes=[b_gkv])
    kb.dma("sp", gq, din["g_qlat"].partition_broadcast(128), writes=[b_gq])
    kb.dma("sp", invf_t, din["invf_t"], writes=[b_invf_t])
    kb.dma("sp", invf_f, din["invf_f"], writes=[b_invf_f])
    kb.dma("sp", eself, din["esel"], writes=[b_eself])
    cx.cp("dve", esel, eself, [b_eself], [b_esel])
    kb.dma("pool", wdkv, din["w_dkv"].rearrange("(k p) n -> p k n", p=128), writes=[b_wdkv])
    kb.dma("pool", wukv, din["w_ukv"].rearrange("(k p) n -> p k n", p=128), writes=[b_wukv])
    kb.dma("pool", wdq, din["w_dq"].rearrange("(k p) n -> p k n", p=128), writes=[b_wdq])
    kb.dma("pool", wuq, din["w_uq"].rearrange("(k p) n -> p k n", p=128), writes=[b_wuq])
    wuq4 = wuq.rearrange("p k (h c) -> p k h c", c=96)
    for kc in range(4):
        cx.ts("dve", wrot[:, kc, :, 0:16], wuq4[:, kc, :, 80:96], -1.0, None, ALU.mult, None, [b_wuq], [b_wrot])
        cx.cp("dve", wrot[:, kc, :, 16:32], wuq4[:, kc, :, 64:80], [b_wuq], [b_wrot])
    cx.memset("dve", rmax, 0.0, [b_rmax])
    cx.memset("dve", VA, 1.0, [b_VA])
    cx.memset("pool", kro, 0.0, [b_kro])

    def head_norms(Tt, b_Tt, dst_row96, b_dst, t0, is_k):
        cx.act(SQ[0:96], Tt[0:96], AF.Square, [b_Tt], [b_SQ])
        ps, bps = rot.next()
        for h in range(16):
            cx.mm(ps[0:16, :], esel[0:96, 15 - h:31 - h], SQ[0:96, h, :], h == 0, h == 15, [b_esel, b_SQ], [bps])
        if is_k:
            cx.kb.op("dve", lambda g: g.reduce_max(out=rmax[:, 1:2], in_=ps[0:16, :], axis=AX.X), [bps], [b_rmax])
            cx.tt("dve", rmax[:, 0:1], rmax[:, 0:1], rmax[:, 1:2], ALU.max, [b_rmax], [b_rmax])
        else:
            cx.act(nrm, ps[0:16, :], AF.Sqrt, [bps], [b_nrm])
            cx.ts("dve", nrmb, nrm, -1.0, None, ALU.mult, None, [b_nrm], [b_nrmb])
            kb.dma("sp", dst_row96[:, t0:t0 + 512], nrmb, reads=[b_nrmb], writes=[b_dst])

    for T in range(ntiles):
        t0 = T * 512
        kb.dma("sp", xt, xs[t0:t0 + 512, :].rearrange("(b p) d -> p b d", p=128), reads=[b_xs], writes=[b_xt])
        kb.dma("sp", posi, din["pos"][0, t0:t0 + 512].partition_broadcast(128), writes=[b_posi])
        kb.dma("sp", ptok_i, din["pos"][0, t0:t0 + 512].rearrange("(b p) -> p b", p=128), writes=[b_ptok_i],
               allow_slow_non_contiguous=True)
        cx.cp("dve", posf, posi, [b_posi], [b_posf])
        cx.cp("dve", ptok, ptok_i, [b_ptok_i], [b_ptok])
        cx.ts("dve", angf, posf, invf_f[:, 0:1], None, ALU.mult, None, [b_posf, b_invf_f], [b_angf])
        range_reduce_sin(cx, sinf, angf, itf, b_sinf, b_angf, b_itf, 0.0)
        range_reduce_sin(cx, cosf, angf, itf, b_cosf, b_angf, b_itf, 0.5 * PI)
        cx.ts("dve", sinf, sinf, ATT_SCALE, None, ALU.mult, None, [b_sinf], [b_sinf])
        cx.ts("dve", cosf, cosf, ATT_SCALE, None, ALU.mult, None, [b_cosf], [b_cosf])

        rms_to_hT(cx, P, xt, b_xt, 4, gT[:, 0, :], b_gT, htok, b_htok, hT, b_hT, ss, b_ss, [0, 1])
        for b in range(4):
            ps, bps = rot.next()
            for kc in range(8):
                cx.mm(ps[:, 0:288], hT[:, kc, b * 128:(b + 1) * 128], wdkv[:, kc, :], kc == 0, kc == 7, [b_hT, b_wdkv], [bps])
            cx.act(ckvn, ps[:, 0:256], AF.Square, [bps], [b_ckvn, b_ss], accum=ss[:, 4:5])
            cx.ts("dve", ss[:, 4:5], ss[:, 4:5], 1.0 / KV_LORA, EPS, ALU.mult, ALU.add, [b_ss], [b_ss])
            cx.act(ss[:, 4:5], ss[:, 4:5], AF.Sqrt, [b_ss], [b_ss])
            cx.kb.op("dve", lambda g: g.reciprocal(out=ss[:, 4:5], in_=ss[:, 4:5]), [b_ss], [b_ss])
            cx.stt("dve", ckvn, ps[:, 0:256], ss[:, 4:5], gkv, ALU.mult, ALU.mult, [bps, b_ss, b_gkv], [b_ckvn])
            cx.ts("dve", angt, invf_t, ptok[:, b:b + 1], None, ALU.mult, None, [b_invf_t, b_ptok], [b_angt])
            range_reduce_sin(cx, sint, angt, itt, b_sint, b_angt, b_itt, 0.0)
            range_reduce_sin(cx, cost, angt, itt, b_cost, b_angt, b_itt, 0.5 * PI)
            x1, x2 = ps[:, 256:272], ps[:, 272:288]
            cx.tt("dve", krt[:, 0, :], x1, cost, ALU.mult, [bps, b_cost], [b_krt])
            cx.tt("dve", krt[:, 1, :], x2, sint, ALU.mult, [bps, b_sint], [b_krt])
            cx.tt("dve", krt[:, 2, :], x2, cost, ALU.mult, [bps, b_cost], [b_krt])
            cx.tt("dve", krt[:, 3, :], x1, sint, ALU.mult, [bps, b_sint], [b_krt])
            cx.tt("dve", kro[:, 0:16], krt[:, 0, :], krt[:, 1, :], ALU.subtract, [b_krt], [b_kro])
            cx.tt("dve", kro[:, 16:32], krt[:, 2, :], krt[:, 3, :], ALU.add, [b_krt], [b_kro])
            p16 = cx.ps[b % 2].bitcast(BF16).rearrange("p (k c) -> p k c", c=128)
            bp16 = cx.psb[b % 2]
            for k2 in range(2):
                cx.tr(p16[:, k2, :], ckvn[:, k2 * 128:(k2 + 1) * 128], identb, [b_ckvn, b_identb], [bp16])
            cx.tr(p16[0:32, 2, :], kro, identb, [b_kro, b_identb], [bp16])
            cx.cp("act", ckvT[:, :, b * 128:(b + 1) * 128], p16[:, 0:2, :], [bp16], [b_ckvT])
            cx.cp("dve", krT[64:96, b * 128:(b + 1) * 128], p16[0:32, 2, :], [bp16], [b_krT])
            w4 = wukv.rearrange("p k (h c) -> p k h c", c=128)
            for hh in range(2):
                pv_, bpv = rot.next()
                for k2 in range(2):
                    cx.mm(pv_, ckvT[:, k2, b * 128:(b + 1) * 128], w4[:, k2, hh * 8:(hh + 1) * 8, 64:128],
                          k2 == 0, k2 == 1, [b_ckvT, b_wukv], [bpv])
                cx.cp("act" if hh else "dve", VA[:, hh * 8:(hh + 1) * 8, b, 0:64],
                      pv_.rearrange("p (h c) -> p h c", c=64), [bpv], [b_VA])
        for h in range(16):
            ps, bps = rot.next()
            for k2 in range(2):
                cx.mm(ps[0:64, :], wukv[:, k2, h * 128:h * 128 + 64], ckvT[:, k2, :], k2 == 0, k2 == 1, [b_wukv, b_ckvT], [bps])
            cx.cp("act" if h % 2 else "dve", KTt[0:64, h, :], ps[0:64, :], [bps], [b_KTt])
        cx.cp("dve", KTt[64:96, :, :], krT[64:96, :].unsqueeze(1).to_broadcast([32, 16, 512]), [b_krT], [b_KTt])
        kb.dma("sp", KT[:, 0:96, t0:t0 + 512].rearrange("h r t -> r h t"), KTt[0:96], reads=[b_KTt], writes=[b_KT])
        head_norms(KTt, b_KTt, None, None, t0, True)
        kb.dma("sp", VS[:, :, 4 * T:4 * T + 4, :].rearrange("h p b e -> p h b e"), VA, reads=[b_VA], writes=[b_VS])

        if hook is not None:
            hook()
        rms_to_hT(cx, P, xt, b_xt, 4, gT[:, 1, :], b_gT, htok, b_htok, hT, b_hT, ss, b_ss, [0, 1])
        for b in range(4):
            ps, bps = rot.next()
            for kc in range(8):
                cx.mm(ps, hT[:, kc, b * 128:(b + 1) * 128], wdq[:, kc, :], kc == 0, kc == 7, [b_hT, b_wdq], [bps])
            cx.act(cqn, ps, AF.Square, [bps], [b_cqn, b_ss], accum=ss[:, 5:6])
            cx.ts("dve", ss[:, 5:6], ss[:, 5:6], 1.0 / Q_LORA, EPS, ALU.mult, ALU.add, [b_ss], [b_ss])
            cx.act(ss[:, 5:6], ss[:, 5:6], AF.Sqrt, [b_ss], [b_ss])
            cx.kb.op("dve", lambda g: g.reciprocal(out=ss[:, 5:6], in_=ss[:, 5:6]), [b_ss], [b_ss])
            cx.stt("dve", cqn, ps, ss[:, 5:6], gq, ALU.mult, ALU.mult, [bps, b_ss, b_gq], [b_cqn])
            p16 = cx.ps[b % 2].bitcast(BF16).rearrange("p (k c) -> p k c", c=128)
            bp16 = cx.psb[b % 2]
            for k4 in range(4):
                cx.tr(p16[:, k4, :], cqn[:, k4 * 128:(k4 + 1) * 128], identb, [b_cqn, b_identb], [bp16])
            cx.cp("act", cqnT[:, :, b * 128:(b + 1) * 128], p16[:, 0:4, :], [bp16], [b_cqnT])
        for h in range(16):
            pa, bpa = rot.next()
            pb_, bpb = rot.next()
            for k4 in range(4):
                cx.mm(pa[0:96, :], wuq[:, k4, h * 96:(h + 1) * 96], cqnT[:, k4, :], k4 == 0, k4 == 3, [b_wuq, b_cqnT], [bpa])
            for k4 in range(4):
                cx.mm(pb_[64:96, :], wrot[:, k4, h, :], cqnT[:, k4, :], k4 == 0, k4 == 3, [b_wrot, b_cqnT], [bpb],
                      tile_position=(0, 64))
            cx.act(QTt[0:64, h, :], pa[0:64, :], AF.Copy, [bpa], [b_QTt], scale=ATT_SCALE)
            cx.tt("dve", t96a[64:96], pa[64:96, :], cosf[64:96], ALU.mult, [bpa, b_cosf], [b_t96a])
            cx.tt("dve", t96b[64:96], pb_[64:96, :], sinf[64:96], ALU.mult, [bpb, b_sinf], [b_t96b])
            cx.tt("pool", QTt[64:96, h, :], t96a[64:96], t96b[64:96], ALU.add, [b_t96a, b_t96b], [b_QTt])
        kb.dma("sp", QT[:, 0:96, t0:t0 + 512].rearrange("h r t -> r h t"), QTt[0:96], reads=[b_QTt], writes=[b_QT])
        head_norms(QTt, b_QTt, QT[:, 96, :], b_QT, t0, False)
    cx.act(rmax[:, 1:2], rmax[:, 0:1], AF.Sqrt, [b_rmax], [b_rmax])
    cx.memset("pool", KM, 0.0, [b_KM])
    cx.ts("dve", KM, KM, rmax[:, 1:2], KMAX_MARGIN, ALU.add, ALU.mult, [b_KM, b_rmax], [b_KM])
    kb.dma("sp", KT[:, 96, :], KM, reads=[b_KM], writes=[b_KT])
    cx.release(m)


def phase2(cx, P, din, KT, b_KT, QT, b_QT, VS, b_VS, OT, b_OT, nheads=16, ngroups=8, hook=None):
    nc, kb = cx.nc, cx.kb
    m = cx.mark()
    KTh, QTh, VAh, b_KTh, b_QTh, b_VAh = [], [], [], [], [], []
    for i in range(2):
        t, b = cx.sb(f"KTh{i}", [128, 4096], BF16); KTh.append(t); b_KTh.append(b)
        t, b = cx.sb(f"QTh{i}", [128, 4096], BF16); QTh.append(t); b_QTh.append(b)
        t, b = cx.sb(f"VAh{i}", [128, 32, 65], BF16); VAh.append(t); b_VAh.append(b)
    PT, b_PT = [], []
    for i in range(2):
        t, _ = cx.sb(f"PT{i}", [128, 32, 512], BF16)
        PT.append(t)
        b_PT.append(kb.bufs_n(f"PT{i}_", 32))
    dmask, b_dmask = cx.sb("dmask", [128, 128], F32)
    onesr, b_onesr = cx.sb("onesr", [128, 64], F32)
    R, b_R = cx.sb("R", [128, 512], F32)
    RB, b_RB = cx.sb("RB", [128, 512], F32)
    OTs, b_OTs = [], []
    for i in range(2):
        t, b = cx.sb(f"OTs{i}", [128, 4096], BF16); OTs.append(t); b_OTs.append(b)
    kb.dma("sp", dmask, din["dmask"], writes=[b_dmask])
    cx.memset("dve", onesr, 1.0, [b_onesr])
    rot = PsRot(cx, [0, 1, 2, 3, 4])
    rot_o = PsRot(cx, [5, 6])

    def emit_pv(st, j):
        nkt = st["nkt"]
        c0 = max(0, j - 4 * st["G"]) * 128
        s_ = st["s"]
        cx.mm(st["po"][0:65, c0:512], VAh[s_][:, j, :], st["pt"][:, j, c0:512], j == 0, j == nkt - 1,
              [b_VAh[s_], st["b_pt"][j]], [st["bpo"]])

    def emit_fin(st):
        po, bpo, s_, G = st["po"], st["bpo"], st["s"], st["G"]
        ot, b_ot = st["ot"], st["b_ot"]
        cx.kb.op("dve", lambda g: g.reciprocal(out=R[64:65, :], in_=po[64:65, :]), [bpo], [b_R])
        pb_, bpb = cx.ps[7], cx.psb[7]
        cx.mm(pb_[0:64, :], onesr[64:65, :], R[64:65, :], True, True, [b_onesr, b_R], [bpb])
        cx.cp("act", RB[0:64, :], pb_[0:64, :], [bpb], [b_RB])
        dst = ot[64 * s_:64 * s_ + 64, G * 512:(G + 1) * 512]
        cx.tt("dve", dst, po[0:64, :], RB[0:64, :], ALU.mult, [bpo, b_RB], [b_ot])
        if st["store"] is not None:
            kb.dma("sp", OT[st["store"]], ot, reads=[b_ot], writes=[b_OT])

    prev = None
    fin_q = None
    gi = 0
    def load_head(hh):
        ss_ = hh % 2
        kb.dma("sp", KTh[ss_][0:97, :], KT[hh], reads=[b_KT], writes=[b_KTh[ss_]])
        kb.dma("sp", QTh[ss_][0:97, :], QT[hh], reads=[b_QT], writes=[b_QTh[ss_]])
        kb.dma("sp", VAh[ss_], VS[hh], reads=[b_VS], writes=[b_VAh[ss_]])

    load_head(0)
    for h in range(nheads):
        s = h % 2
        pr = h // 2
        ot, b_ot = OTs[pr % 2], b_OTs[pr % 2]
        for G in range(ngroups):
            if G == 1 and h + 1 < nheads:
                load_head(h + 1)
            if hook is not None:
                hook()
            pt, b_pt = PT[gi % 2], b_PT[gi % 2]
            gi += 1
            nkt = 4 * G + 4
            if prev is not None:
                prev["po"], prev["bpo"] = rot_o.next()
            npv = prev["nkt"] if prev is not None else 0
            for j in range(max(nkt, npv)):
                if j < nkt:
                    c0 = max(0, j - 4 * G) * 128
                    ps, bps = rot.next()
                    cx.mm(ps[:, c0:512], KTh[s][0:97, j * 128:(j + 1) * 128], QTh[s][0:97, G * 512 + c0:(G + 1) * 512],
                          True, True, [b_KTh[s], b_QTh[s]], [bps])
                    cx.act(pt[:, j, c0:512], ps[:, c0:512], AF.Exp, [bps], [b_pt[j]])
                    if j >= 4 * G:
                        cx.tt("pool", pt[:, j, c0:c0 + 128], pt[:, j, c0:c0 + 128], dmask, ALU.mult, [b_pt[j], b_dmask], [b_pt[j]])
                if j < npv:
                    emit_pv(prev, j)
                if j == 1 and fin_q is not None:
                    emit_fin(fin_q)
                    fin_q = None
            if fin_q is not None:
                emit_fin(fin_q)
                fin_q = None
            fin_q = prev
            last_of_pair = (G == ngroups - 1) and (s == 1 or h == nheads - 1)
            prev = dict(pt=pt, b_pt=b_pt, nkt=nkt, G=G, s=s, ot=ot, b_ot=b_ot, store=(pr if last_of_pair else None))
    if prev is not None:
        prev["po"], prev["bpo"] = rot_o.next()
        for j in range(prev["nkt"]):
            emit_pv(prev, j)
    if fin_q is not None:
        emit_fin(fin_q)
    if prev is not None:
        emit_fin(prev)
    cx.release(m)


def phase3a(cx, P, din, xs, b_xs, OT, b_OT, out, b_out, ntiles=8):
    nc, kb = cx.nc, cx.kb
    m = cx.mark()
    xt, b_xt = cx.sb("xt", [128, 4, 1024], F32)
    ot, b_ot = cx.sb("ot", [128, 8, 512], BF16)
    wo, b_wo = cx.sb("wo", [128, 8, 1024], BF16)
    kb.dma("pool", wo, din["w_o"].rearrange("(k p) n -> p k n", p=128), writes=[b_wo])
    rot = PsRot(cx, [0, 1, 2, 3])
    for T in range(ntiles):
        t0 = T * 512
        kb.dma("sp", xt, xs[t0:t0 + 512, :].rearrange("(b p) d -> p b d", p=128), reads=[b_xs], writes=[b_xt])
        kb.dma("sp", ot, OT[:, :, t0:t0 + 512].rearrange("k p t -> p k t"), reads=[b_OT], writes=[b_ot])
        for b in range(4):
            for half in range(2):
                ps, bps = rot.next()
                for k in range(8):
                    cx.mm(ps, ot[:, k, b * 128:(b + 1) * 128], wo[:, k, half * 512:(half + 1) * 512], k == 0, k == 7, [b_ot, b_wo], [bps])
                xv = xt[:, b, half * 512:(half + 1) * 512]
                cx.tt("dve", xv, xv, ps, ALU.add, [b_xt, bps], [b_xt])
        kb.dma("sp", out[t0:t0 + 512, :].rearrange("(b p) d -> p b d", p=128), xt, reads=[b_xt], writes=[b_out])
    cx.release(m)


def phase3(cx, P, din, xs, b_xs, OT, b_OT, out, b_out, ntiles=4, nexp=NE):
    nc, kb = cx.nc, cx.kb
    B = P["bufs"]
    identf, b_identf = P["identf"], B["identf"]
    m = cx.mark()
    NBK = 8
    xt, b_xt = cx.sb("xt", [128, NBK, 1024], F32)
    htok, b_htok = cx.sb("htok", [128, NBK, 1024], BF16)
    hT, b_hT = cx.sb("hT", [128, 8, 1024], BF16)
    wo, b_wo = cx.sb("wo", [128, 8, 1024], BF16)
    rwg, b_rwg = cx.sb("rwg", [128, NE, 1024], F32)
    gfin, b_gfin = cx.sb("gfin", [128, 1024], F32)
    gT, b_gT = cx.sb("gT", [128, 8], F32)
    ss, b_ss = cx.sb("ss", [128, 16], F32)
    lg, b_lg = cx.sb("lg", [128, NBK, 8], F32)
    m8, b_m8 = cx.sb("m8", [128, 8], F32)
    gts, b_gts = cx.sb("gts", [128, NBK, 8], F32)
    gsum, b_gsum = cx.sb("gsum", [128, 2], F32)
    g8T, b_g8T = cx.sb("g8T", [8, 1024], F32)
    sel, b_sel = cx.sb("sel", [8, NE, 128], F32)
    gbc, b_gbc = cx.sb("gbc", [128, 1024], F32)
    sg = []
    b_sg = []
    tg = []
    b_tg = []
    for i in range(2):
        t, b = cx.sb(f"sg{i}", [128, 512], F32); sg.append(t); b_sg.append(b)
        t, b = cx.sb(f"tg{i}", [128, 512], F32); tg.append(t); b_tg.append(b)
    actT = []
    b_actT = []
    for i in range(2):
        t, b = cx.sb(f"actT{i}", [128, 4, 1024], BF16); actT.append(t); b_actT.append(b)
    junk, b_junk = cx.sb("junk", [128, 1024], BF16)
    ws = WStream(cx, 2, 3 * 4096, name="wm")
    rot = PsRot(cx, [2, 3, 4, 5, 6, 7])
    ot = htok

    kb.dma("pool", wo, din["w_o"].rearrange("(k p) n -> p k n", p=128), writes=[b_wo])
    kb.dma("sp", gfin, din["g_final"].partition_broadcast(128), writes=[b_gfin])
    kb.dma("sp", gT, din["gT_ffn1"], writes=[b_gT])
    kb.dma("sp", gbc, din["g_ffn1"].partition_broadcast(128), writes=[b_gbc])
    kb.dma("sp", sel, din["sel"].rearrange("k (e m) -> k e m", m=128), writes=[b_sel])
    for e in range(NE):
        kb.dma("sp", rwg[:, e, :], din["router_wT"][e].partition_broadcast(128), writes=[b_rwg])
    for e in range(NE):
        cx.tt("pool", rwg[:, e, :], rwg[:, e, :], gbc, ALU.mult, [b_rwg, b_gbc], [b_rwg])
    wg, wu, wd = din["moe_w_gate"], din["moe_w_up"], din["moe_w_down"]
    NG = MOE_FF // 512
    jobs = []
    for T in range(ntiles):
        for e in range(nexp):
            for g in range(NG):
                jobs.append([
                    (wg[e][:, g * 512:(g + 1) * 512].rearrange("(k p) n -> p k n", p=128), 8, 512),
                    (wu[e][:, g * 512:(g + 1) * 512].rearrange("(k p) n -> p k n", p=128), 8, 512),
                    (wd[e][g * 512:(g + 1) * 512, :].rearrange("(a p) d -> p a d", p=128), 4, 1024)])
    ws.plan(jobs)

    for T in range(ntiles):
        t0 = T * 1024
        kb.dma("sp", xt, xs[t0:t0 + 1024, :].rearrange("(b p) d -> p b d", p=128), reads=[b_xs], writes=[b_xt])
        kb.dma("sp", ot, OT[:, :, t0:t0 + 1024].rearrange("k p t -> p k t"), reads=[b_OT], writes=[b_htok])
        for b in range(NBK):
            for half in range(2):
                ps, bps = rot.next()
                for k in range(8):
                    cx.mm(ps, ot[:, k, b * 128:(b + 1) * 128], wo[:, k, half * 512:(half + 1) * 512], k == 0, k == 7,
                          [b_htok, b_wo], [bps])
                xv = xt[:, b, half * 512:(half + 1) * 512]
                cx.tt("dve", xv, xv, ps, ALU.add, [b_xt, bps], [b_xt])
        rms_to_hT(cx, P, xt, b_xt, NBK, gT, b_gT, htok, b_htok, hT, b_hT, ss, b_ss, [0, 1])
        for b in range(NBK):
            for e in range(NE):
                kb.op("dve", lambda g_, b=b, e=e: g_.scalar_tensor_tensor(
                    out=junk, in0=xt[:, b, :], scalar=ss[:, b:b + 1], in1=rwg[:, e, :], op0=ALU.mult, op1=ALU.mult,
                    accum_out=lg[:, b, e:e + 1]), [b_xt, b_ss, b_rwg], [b_junk, b_lg])
            kb.op("dve", lambda g_, b=b: g_.max(out=m8, in_=lg[:, b, :]), [b_lg], [b_m8])
            cx.ts("dve", gsum[:, 0:1], m8[:, 0:1], -1.0, None, ALU.mult, None, [b_m8], [b_gsum])
            cx.act(gts[:, b, :], lg[:, b, :], AF.Exp, [b_lg, b_gsum], [b_gts], bias=gsum[:, 0:1])
            cx.stt("dve", gts[:, b, :], lg[:, b, :], m8[:, 1:2], gts[:, b, :], ALU.is_ge, ALU.mult,
                   [b_lg, b_m8, b_gts], [b_gts])
            kb.op("dve", lambda g_, b=b: g_.reduce_sum(out=gsum[:, 1:2], in_=gts[:, b, :], axis=AX.X), [b_gts], [b_gsum])
            kb.op("dve", lambda g_: g_.reciprocal(out=gsum[:, 1:2], in_=gsum[:, 1:2]), [b_gsum], [b_gsum])
            cx.ts("dve", gts[:, b, :], gts[:, b, :], gsum[:, 1:2], None, ALU.mult, None, [b_gts, b_gsum], [b_gts])
            pt_, bpt = rot.next()
            cx.tr(pt_[0:8, 0:128], gts[:, b, :], identf, [b_gts, b_identf], [bpt])
            cx.cp("dve", g8T[:, b * 128:(b + 1) * 128], pt_[0:8, 0:128], [bpt], [b_g8T])
        gi = 0
        for e in range(nexp):
            for half in range(2):
                ps, bps = rot.next()
                cx.mm(ps, sel[:, e, :], g8T[:, half * 512:(half + 1) * 512], True, True, [b_sel, b_g8T], [bps])
                cx.cp("act", gbc[:, half * 512:(half + 1) * 512], ps, [bps], [b_gbc])
            for g in range(NG):
                (gv, uv, dv), b_w = ws.get()
                at, b_at = actT[gi % 2], b_actT[gi % 2]
                gi += 1
                for f4 in range(4):
                    for half in range(2):
                        hs = slice(half * 512, (half + 1) * 512)
                        pg, bpg = rot.next()
                        pu, bpu = rot.next()
                        for kc in range(8):
                            cx.mm(pg, gv[:, kc, f4 * 128:(f4 + 1) * 128], hT[:, kc, hs], kc == 0, kc == 7, [b_w, b_hT], [bpg])
                        for kc in range(8):
                            cx.mm(pu, uv[:, kc, f4 * 128:(f4 + 1) * 128], hT[:, kc, hs], kc == 0, kc == 7, [b_w, b_hT], [bpu])
                        i2 = (f4 * 2 + half) % 2
                        cx.act(sg[i2], pg, AF.Silu, [bpg], [b_sg[i2]])
                        cx.tt("dve", tg[i2], pu, gbc[:, hs], ALU.mult, [bpu, b_gbc], [b_tg[i2]])
                        cx.tt("pool", at[:, f4, hs], sg[i2], tg[i2], ALU.mult, [b_sg[i2], b_tg[i2]], [b_at])
                for b in range(NBK):
                    for half in range(2):
                        ps, bps = rot.next()
                        for f4 in range(4):
                            cx.mm(ps, at[:, f4, b * 128:(b + 1) * 128], dv[:, f4, half * 512:(half + 1) * 512],
                                  f4 == 0, f4 == 3, [b_at, b_w], [bps])
                        xv = xt[:, b, half * 512:(half + 1) * 512]
                        cx.tt("dve", xv, xv, ps, ALU.add, [b_xt, bps], [b_xt])
        for b in range(NBK):
            cx.act(junk, xt[:, b, :], AF.Square, [b_xt], [b_junk, b_ss], accum=ss[:, 8 + (b % 8):9 + (b % 8)])
        cx.ts("dve", ss[:, 8:16], ss[:, 8:16], 1.0 / D, EPS, ALU.mult, ALU.add, [b_ss], [b_ss])
        cx.act(ss[:, 8:16], ss[:, 8:16], AF.Sqrt, [b_ss], [b_ss])
        kb.op("dve", lambda g_: g_.reciprocal(out=ss[:, 8:16], in_=ss[:, 8:16]), [b_ss], [b_ss])
        for b in range(NBK):
            cx.stt("dve", xt[:, b, :], xt[:, b, :], ss[:, 8 + b:9 + b], gfin, ALU.mult, ALU.mult, [b_xt, b_ss, b_gfin], [b_xt])
        kb.dma("sp", out[t0:t0 + 1024, :].rearrange("(b p) d -> p b d", p=128), xt, reads=[b_xt], writes=[b_out])
    cx.release(m)


def build_program():
    nc = bass.Bass("TRN2", target_bir_lowering=False)
    cx = Ctx(nc)
    kb = cx.kb
    din = declare_inputs(nc, list(IN_SPECS.keys()))
    out = nc.dram_tensor("out", [L, D], F32, kind="ExternalOutput").ap()
    xs = nc.dram_tensor("xs", [L, D], F32, kind="Internal").ap()
    KT = nc.dram_tensor("KT", [NH, 97, L], BF16, kind="Internal").ap()
    QT = nc.dram_tensor("QT", [NH, 97, L], BF16, kind="Internal").ap()
    VS = nc.dram_tensor("VS", [NH, 128, NB, 65], BF16, kind="Internal").ap()
    OT = nc.dram_tensor("OT", [NH // 2, 128, L], BF16, kind="Internal").ap()
    b_out, b_xs, b_KT, b_QT, b_VS, b_OT = (kb.buf(n) for n in ("out", "xs", "KT", "QT", "VS", "OT"))
    W0 = nc.dram_tensor("W0", [N_L0_JOBS * 128, 4096], BF16, kind="Internal").ap()
    b_W0 = kb.buf("W0")
    P = setup_ident(cx, din)
    m0 = cx.mark()
    phase0(cx, P, din, pre_hook=lambda: layer0_convert(cx, din, W0, b_W0))
    phase1(cx, P, din, xs, b_xs, W0, b_W0)
    cx.release(m0)
    W16 = {k: nc.dram_tensor("W16" + k, [NE * NGRP * 128, 4096], BF16, kind="Internal").ap() for k in "gud"}
    b_W16 = {k: kb.buf("W16" + k) for k in "gud"}
    HS = nc.dram_tensor("HS", [NSLAB * SLAB, D], BF16, kind="Internal").ap()
    YS = nc.dram_tensor("YS", [NSLAB * SLAB, D], F32, kind="Internal").ap()
    b_HS, b_YS = kb.buf("HS"), kb.buf("YS")
    m1 = cx.mark()
    conv = MoeConv(cx, din, W16, b_W16)
    phase15(cx, P, din, xs, b_xs, KT, b_KT, QT, b_QT, VS, b_VS, hook=lambda: conv.step(12))
    phase2(cx, P, din, KT, b_KT, QT, b_QT, VS, b_VS, OT, b_OT, hook=lambda: conv.step(1))
    conv.finish()
    cx.release(m1)
    phase3r(cx, P, din, xs, b_xs, OT, b_OT, out, b_out, W16, b_W16, HS, b_HS, YS, b_YS)
    kb.finish([b_out])
    return nc


_NC_CACHE = {}


def kernel(**inputs):
    inp = {k: np.asarray(v) for k, v in inputs.items()}
    shared = host_shared(inp)
    if "nc" not in _NC_CACHE:
        _NC_CACHE["nc"] = build_program()
    nc = _NC_CACHE["nc"]
    ncores = 8
    in_maps = []
    for c in range(ncores):
        mp = dict(shared)
        mp["x"] = np.ascontiguousarray(inp["x"][c], dtype=np.float32)
        mp["pos"] = np.ascontiguousarray(inp["positions"][c:c + 1], dtype=np.int32)
        in_maps.append(mp)
    res = run_bass_kernel_spmd(nc, in_maps, core_ids=list(range(ncores)))
    return np.stack([np.asarray(r["out"], dtype=np.float32) for r in res.results], axis=0)


class MoeConv:
    def __init__(self, cx, din, W16, b_W16, nstage=2, nexp=NE):
        self.cx = cx
        self.W16, self.b_W16 = W16, b_W16
        self.stage = [cx.sb(f"cvst{i}", [128, 4096], BF16) for i in range(nstage)]
        self.chunks = []
        for e in range(nexp):
            for g in range(NGRP):
                cs = slice(g * 512, (g + 1) * 512)
                self.chunks.append(("g", e, g, din["moe_w_gate"][e][:, cs].rearrange("(k p) n -> p k n", p=128), 512))
                self.chunks.append(("u", e, g, din["moe_w_up"][e][:, cs].rearrange("(k p) n -> p k n", p=128), 512))
                self.chunks.append(("d", e, g, din["moe_w_down"][e][cs, :].rearrange("(a p) d -> p a d", p=128), 1024))
        self.i = 0

    def step(self, n=1):
        kb = self.cx.kb
        for _ in range(n):
            if self.i >= len(self.chunks):
                return
            kind, e, g, src, n_in = self.chunks[self.i]
            st, b_st = self.stage[self.i % len(self.stage)]
            self.i += 1
            kb.dma("pool", st.rearrange("p (a n) -> p a n", n=n_in), src, writes=[b_st])
            r0 = (e * NGRP + g) * 128
            kb.dma("sp", self.W16[kind][r0:r0 + 128, :], st, reads=[b_st], writes=[self.b_W16[kind]], disjoint=True)

    def finish(self):
        self.step(len(self.chunks))


def phase3r(cx, P, din, xs, b_xs, OT, b_OT, out, b_out, W16, b_W16, HS, b_HS, YS, b_YS, nslab=NSLAB):
    nc, kb = cx.nc, cx.kb
    B = P["bufs"]
    identb, b_identb = P["identb"], B["identb"]
    identf, b_identf = P["identf"], B["identf"]
    NTOT = NE * NGRP * 128
    mp = cx.mark()
    ss, b_ss = cx.sb("ss", [128, 8], F32)
    LG, b_LG = cx.sb("LG", [128, NB, 8], F32)
    GTS, b_GTS = cx.sb("GTS", [128, NB, 8], F32)
    TOP, b_TOP = cx.sb("TOP", [128, NB, 2], F32)
    m8, b_m8 = cx.sb("m8", [128, 8], F32)
    gsum, b_gsum = cx.sb("gsum", [128, 2], F32)
    junk, b_junk = cx.sb("junk", [128, 1024], BF16)
    M1, b_M1 = cx.sb("M1", [128, NB, 8], F32)
    M2, b_M2 = cx.sb("M2", [128, NB, 8], F32)
    MSK, b_MSK = cx.sb("MSK", [128, NB, 8], BF16)
    ltri, b_ltri = cx.sb("ltri", [128, 128], BF16)
    onesb, b_onesb = cx.sb("onesb", [128, 128], BF16)
    ones32, b_ones32 = cx.sb("ones32", [128, NB], F32)
    tmpf, b_tmpf = cx.sb("tmpf", [128, 128], F32)
    TOT, b_TOT = cx.sb("TOT", [128, NB, 8], F32)
    INC, b_INC = cx.sb("INC", [128, NB, 8], F32)
    WIN, b_WIN = cx.sb("WIN", [128, NB, 8], F32)
    SL, b_SL = cx.sb("SL", [128, NB, 8], F32)
    NSL, b_NSL = cx.sb("NSL", [128, 8], F32)
    NSLi, b_NSLi = cx.sb("NSLi", [128, 8], I32)
    SEND, b_SEND = cx.sb("SEND", [128, 8], F32)
    OFF, b_OFF = cx.sb("OFF", [128, 8], F32)
    one8, b_one8 = cx.sb("one8", [128, 8], F32)
    SF, b_SF = cx.sb("SF", [128, 2, NB], F32)
    SLOT, b_SLOT = cx.sb("SLOT", [128, 2, NB], I32)
    WGT, b_WGT = cx.sb("WGT", [128, 2, NB], F32)
    wtab, b_wtab = cx.sb("wtab", [128, NSLAB, 8], F32)
    CMP, b_CMP = cx.sb("CMP", [128, NSLAB, 8], F32)
    EW, b_EW = cx.sb("EW", [128, NSLAB], F32)
    ctab, b_ctab = cx.sb("ctab", [128, 7], F32)
    IDXf, b_IDXf = cx.sb("IDXf", [128, NSLAB, 7], F32)
    IDX, b_IDX = cx.sb("IDX", [128, NSLAB, 7], I32)
    m = cx.mark()
    xt, b_xt = cx.sb("xt", [128, 8, 1024], F32)
    ot, b_ot = cx.sb("ot", [128, 8, 1024], BF16)
    wo, b_wo = cx.sb("wo", [128, 8, 1024], BF16)
    rwT, b_rwT = cx.sb("rwT", [128, 8, NE], F32)
    gT1, b_gT1 = cx.sb("gT1", [128, 8], F32)
    xT32, b_xT32 = cx.sb("xT32", [128, 8, 128], F32)
    gff, b_gff = cx.sb("gff", [128, 1024], F32)
    hall, _ = cx.sb("hall", [128, NB, 1024], BF16)
    b_hall = kb.bufs_n("hall", NB)
    rot = PsRot(cx, [2, 3, 4, 5, 6, 7])
    kb.dma("pool", wo, din["w_o"].rearrange("(k p) n -> p k n", p=128), writes=[b_wo])
    kb.dma("sp", gff, din["g_ffn1"].partition_broadcast(128), writes=[b_gff])
    kb.dma("sp", rwT, din["router_w"].rearrange("(k p) e -> p k e", p=128), writes=[b_rwT])
    kb.dma("sp", gT1, din["gT_ffn1"], writes=[b_gT1])
    cx.tt("dve", rwT, rwT, gT1.unsqueeze(2).to_broadcast([128, 8, NE]), ALU.mult, [b_rwT, b_gT1], [b_rwT])
    for T in range(4):
        t0 = T * 1024
        kb.dma("sp", xt, xs[t0:t0 + 1024, :].rearrange("(b p) d -> p b d", p=128), reads=[b_xs], writes=[b_xt])
        kb.dma("sp", ot, OT[:, :, t0:t0 + 1024].rearrange("k p t -> p k t"), reads=[b_OT], writes=[b_ot])
        for b in range(8):
            for half in range(2):
                ps, bps = rot.next()
                for k in range(8):
                    cx.mm(ps, ot[:, k, b * 128:(b + 1) * 128], wo[:, k, half * 512:(half + 1) * 512], k == 0, k == 7,
                          [b_ot, b_wo], [bps])
                xv = xt[:, b, half * 512:(half + 1) * 512]
                cx.tt("dve", xv, xv, ps, ALU.add, [b_xt, bps], [b_xt])
        kb.dma("sp", xs[t0:t0 + 1024, :].rearrange("(b p) d -> p b d", p=128), xt, reads=[b_xt], writes=[b_xs])
        for b in range(8):
            cx.act(junk, xt[:, b, :], AF.Square, [b_xt], [b_junk, b_ss], accum=ss[:, b:b + 1])
        cx.ts("dve", ss, ss, 1.0 / D, EPS, ALU.mult, ALU.add, [b_ss], [b_ss])
        cx.act(ss, ss, AF.Sqrt, [b_ss], [b_ss])
        kb.op("dve", lambda g_: g_.reciprocal(out=ss, in_=ss), [b_ss], [b_ss])
        for b in range(8):
            gb = T * 8 + b
            cx.stt("dve", hall[:, gb, :], xt[:, b, :], ss[:, b:b + 1], gff, ALU.mult, ALU.mult,
                   [b_xt, b_ss, b_gff], [b_hall[gb]])
            pA, bpA = rot.next()
            pB, bpB = rot.next()
            for kc in range(8):
                pp, bpp = (pA, bpA) if kc < 4 else (pB, bpB)
                cx.tr(pp[:, (kc % 4) * 128:(kc % 4 + 1) * 128], xt[:, b, kc * 128:(kc + 1) * 128], identf, [b_xt, b_identf], [bpp])
            cx.cp("act", xT32[:, 0:4, :], pA.rearrange("p (k c) -> p k c", c=128), [bpA], [b_xT32])
            cx.cp("act", xT32[:, 4:8, :], pB.rearrange("p (k c) -> p k c", c=128), [bpB], [b_xT32])
            pL, bpL = rot.next()
            for kc in range(8):
                cx.mm(pL[:, 0:NE], xT32[:, kc, :], rwT[:, kc, :], kc == 0, kc == 7, [b_xT32, b_rwT], [bpL])
            cx.ts("dve", LG[:, gb, :], pL[:, 0:NE], ss[:, b:b + 1], None, ALU.mult, None, [bpL, b_ss], [b_LG])
            kb.op("dve", lambda g_, gb=gb: g_.max(out=m8, in_=LG[:, gb, :]), [b_LG], [b_m8])
            cx.cp("dve", TOP[:, gb, :], m8[:, 0:2], [b_m8], [b_TOP])
            cx.ts("dve", gsum[:, 0:1], m8[:, 0:1], -1.0, None, ALU.mult, None, [b_m8], [b_gsum])
            cx.act(GTS[:, gb, :], LG[:, gb, :], AF.Exp, [b_LG, b_gsum], [b_GTS], bias=gsum[:, 0:1])
            cx.stt("dve", GTS[:, gb, :], LG[:, gb, :], m8[:, 1:2], GTS[:, gb, :], ALU.is_ge, ALU.mult,
                   [b_LG, b_m8, b_GTS], [b_GTS])
            kb.op("dve", lambda g_, gb=gb: g_.reduce_sum(out=gsum[:, 1:2], in_=GTS[:, gb, :], axis=AX.X), [b_GTS], [b_gsum])
            kb.op("dve", lambda g_: g_.reciprocal(out=gsum[:, 1:2], in_=gsum[:, 1:2]), [b_gsum], [b_gsum])
            cx.ts("dve", GTS[:, gb, :], GTS[:, gb, :], gsum[:, 1:2], None, ALU.mult, None, [b_GTS, b_gsum], [b_GTS])
    kb.dma("sp", tmpf, din["ltri"], writes=[b_tmpf])
    cx.cp("dve", ltri, tmpf, [b_tmpf], [b_ltri])
    kb.dma("sp", wtab, din["wtab"].rearrange("p (w e) -> p w e", e=8), writes=[b_wtab])
    kb.dma("sp", ctab, din["ctab"], writes=[b_ctab])
    cx.memset("dve", onesb, 1.0, [b_onesb])
    cx.memset("dve", ones32, 1.0, [b_ones32])
    cx.memset("dve", one8, 1.0, [b_one8])
    bce = lambda t: t.unsqueeze(2).to_broadcast([128, NB, 8])
    cx.tt("dve", M1, LG, bce(TOP[:, :, 0]), ALU.is_equal, [b_LG, b_TOP], [b_M1])
    cx.tt("dve", M2, LG, bce(TOP[:, :, 1]), ALU.is_equal, [b_LG, b_TOP], [b_M2])
    cx.tt("dve", MSK, M1, M2, ALU.add, [b_M1, b_M2], [b_MSK])
    mskf = MSK.rearrange("p b e -> p (b e)")
    ps, bps = rot.next()
    cx.mm(ps[:, 0:256], ltri, mskf, True, True, [b_ltri, b_MSK], [bps])
    cx.cp("dve", WIN.rearrange("p b e -> p (b e)"), ps[:, 0:256], [bps], [b_WIN])
    ps, bps = rot.next()
    cx.mm(ps[:, 0:256], onesb, mskf, True, True, [b_onesb, b_MSK], [bps])
    cx.cp("dve", TOT.rearrange("p b e -> p (b e)"), ps[:, 0:256], [bps], [b_TOT])
    for e in range(NE):
        kb.op("dve", lambda g_, e=e: g_.tensor_tensor_scan(out=INC[:, :, e], data0=ones32, data1=TOT[:, :, e], initial=0.0,
                                                           op0=ALU.mult, op1=ALU.add), [b_ones32, b_TOT], [b_INC])
    cx.ts("dve", NSL, INC[:, NB - 1, :], float(SLAB - 1), 1.0 / SLAB, ALU.add, ALU.mult, [b_INC], [b_NSL])
    cx.ts("dve", NSL, NSL, -0.4995, None, ALU.add, None, [b_NSL], [b_NSL])
    cx.cp("dve", NSLi, NSL, [b_NSL], [b_NSLi])
    cx.cp("dve", NSL, NSLi, [b_NSLi], [b_NSL])
    kb.op("dve", lambda g_: g_.tensor_tensor_scan(out=SEND, data0=one8, data1=NSL, initial=0.0, op0=ALU.mult, op1=ALU.add),
          [b_one8, b_NSL], [b_SEND])
    cx.tt("dve", OFF, SEND, NSL, ALU.subtract, [b_SEND, b_NSL], [b_OFF])
    cx.ts("dve", OFF, OFF, float(SLAB), None, ALU.mult, None, [b_OFF], [b_OFF])
    cx.tt("dve", SL, INC, TOT, ALU.subtract, [b_INC, b_TOT], [b_SL])
    cx.tt("dve", SL, SL, WIN, ALU.add, [b_SL, b_WIN], [b_SL])
    cx.tt("dve", SL, SL, OFF.unsqueeze(1).to_broadcast([128, NB, 8]), ALU.add, [b_SL, b_OFF], [b_SL])
    for k, (Mk, b_Mk) in enumerate(((M1, b_M1), (M2, b_M2))):
        cx.tt("dve", TOT, Mk, SL, ALU.mult, [b_Mk, b_SL], [b_TOT])
        kb.op("dve", lambda g_, k=k: g_.reduce_sum(out=SF[:, k, :], in_=TOT, axis=AX.X), [b_TOT], [b_SF])
        cx.tt("dve", TOT, Mk, GTS, ALU.mult, [b_Mk, b_GTS], [b_TOT])
        kb.op("dve", lambda g_, k=k: g_.reduce_sum(out=WGT[:, k, :], in_=TOT, axis=AX.X), [b_TOT], [b_WGT])
    cx.cp("dve", SLOT, SF, [b_SF], [b_SLOT])
    cx.tt("dve", CMP, SEND.unsqueeze(1).to_broadcast([128, NSLAB, 8]), wtab, ALU.is_le, [b_SEND, b_wtab], [b_CMP])
    kb.op("dve", lambda g_: g_.reduce_sum(out=EW, in_=CMP, axis=AX.X), [b_CMP], [b_EW])
    cx.ts("dve", EW, EW, float(NE - 1), float(NGRP * 128), ALU.min, ALU.mult, [b_EW], [b_EW])
    cx.tt("dve", IDXf, EW.unsqueeze(2).to_broadcast([128, NSLAB, 7]), ctab.unsqueeze(1).to_broadcast([128, NSLAB, 7]),
          ALU.add, [b_EW, b_ctab], [b_IDXf])
    cx.cp("dve", IDX, IDXf, [b_IDXf], [b_IDX])
    for gb in range(NB):
        for k in range(2):
            kb.idma(HS, hall[:, gb, :], out_idx=SLOT[:, k, gb:gb + 1], bound=NSLAB * SLAB - 1,
                    reads=[b_hall[gb], b_SLOT], writes=[b_HS], disjoint=True)
    cx.release(m)
    m2 = cx.mark()
    NBK = SLAB // 128
    hsls = [cx.sb(f"hsl{i}", [128, NBK, 1024], BF16) for i in range(2)]
    hTs = [cx.sb(f"hTs{i}", [128, 8, SLAB], BF16) for i in range(2)]
    yacc, b_yacc = cx.sb("yacc", [128, NBK, 1024], F32)
    wsl = [cx.sb(f"wsl{i}", [128, 3 * 4096], BF16) for i in range(2)]
    actT = [cx.sb(f"actT{i}", [128, 4, SLAB], BF16) for i in range(2)]
    sg = [cx.sb(f"sg{i}", [128, 512], F32) for i in range(2)]
    NH2 = SLAB // 512
    wi = 0
    gi = 0

    def issue_w(w, g, slot):
        t, b = slot
        for j, kind in enumerate(("g", "u", "d")):
            kb.idma(t[:, j * 4096:(j + 1) * 4096], W16[kind], in_idx=IDX[:, w, g:g + 1], bound=NTOT - 1,
                    reads=[b_W16[kind], b_IDX], writes=[b], lane=b.name)

    def load_slab_dma(w):
        hsl, b_hsl = hsls[w % 2]
        kb.dma("sp", hsl, HS[w * SLAB:(w + 1) * SLAB, :].rearrange("(b p) d -> p b d", p=128), reads=[b_HS], writes=[b_hsl])

    def load_slab(w):
        hsl, b_hsl = hsls[w % 2]
        hT_, b_hT_ = hTs[w % 2]
        for b in range(NBK):
            pi = b % 2
            p16 = cx.ps[pi].bitcast(BF16).rearrange("p (k c) -> p k c", c=128)
            for kc in range(8):
                cx.tr(p16[:, kc, :], hsl[:, b, kc * 128:(kc + 1) * 128], identb, [b_hsl, b_identb], [cx.psb[pi]])
            cx.cp("act" if b % 2 else "dve", hT_[:, :, b * 128:(b + 1) * 128], p16, [cx.psb[pi]], [b_hT_])

    seq = [(w, g) for w in range(nslab) for g in range(NGRP)]
    issue_w(seq[0][0], seq[0][1], wsl[0])
    load_slab_dma(0)
    load_slab(0)
    for si, (w, g) in enumerate(seq):
        if si + 1 < len(seq):
            issue_w(seq[si + 1][0], seq[si + 1][1], wsl[(si + 1) % 2])
        wt, b_w = wsl[si % 2]
        gv = wt[:, 0:4096].rearrange("p (k n) -> p k n", n=512)
        uv = wt[:, 4096:8192].rearrange("p (k n) -> p k n", n=512)
        dv = wt[:, 8192:12288].rearrange("p (a d) -> p a d", d=1024)
        hT, b_hT = hTs[w % 2]
        if g == NGRP - 3 and w + 1 < nslab:
            load_slab_dma(w + 1)
        if g == NGRP - 1 and w + 1 < nslab:
            load_slab(w + 1)
        at, b_at = actT[gi % 2]
        gi += 1
        for f4 in range(4):
            for half in range(NH2):
                hs = slice(half * 512, (half + 1) * 512)
                pg, bpg = rot.next()
                pu, bpu = rot.next()
                for kc in range(8):
                    cx.mm(pg, gv[:, kc, f4 * 128:(f4 + 1) * 128], hT[:, kc, hs], kc == 0, kc == 7, [b_w, b_hT], [bpg])
                for kc in range(8):
                    cx.mm(pu, uv[:, kc, f4 * 128:(f4 + 1) * 128], hT[:, kc, hs], kc == 0, kc == 7, [b_w, b_hT], [bpu])
                s1, b_s1 = sg[(f4 * NH2 + half) % 2]
                cx.act(s1, pg, AF.Silu, [bpg], [b_s1])
                cx.tt("dve", at[:, f4, hs], s1, pu, ALU.mult, [b_s1, bpu], [b_at])
        for b in range(NBK):
            for half in range(2):
                ps, bps = rot.next()
                for f4 in range(4):
                    cx.mm(ps, at[:, f4, b * 128:(b + 1) * 128], dv[:, f4, half * 512:(half + 1) * 512],
                          f4 == 0, f4 == 3, [b_at, b_w], [bps])
                yv = yacc[:, b, half * 512:(half + 1) * 512]
                if g == 0:
                    cx.cp("act", yv, ps, [bps], [b_yacc])
                else:
                    cx.tt("dve", yv, yv, ps, ALU.add, [b_yacc, bps], [b_yacc])
        if g == NGRP - 1:
            kb.dma("sp", YS[w * SLAB:(w + 1) * SLAB, :].rearrange("(b p) d -> p b d", p=128), yacc, reads=[b_yacc], writes=[b_YS],
                   disjoint=True)
    cx.release(m2)
    y1, b_y1 = cx.sb("y1", [128, 2, 1024], F32)
    y2, b_y2 = cx.sb("y2", [128, 2, 1024], F32)
    xb, b_xb = cx.sb("xb", [128, 2, 1024], F32)
    gfin, b_gfin = cx.sb("gfin", [128, 1024], F32)
    s2, b_s2 = cx.sb("s2", [128, 2], F32)
    kb.dma("sp", gfin, din["g_final"].partition_broadcast(128), writes=[b_gfin])
    b_y1s = kb.bufs_n("y1s", 2); b_y2s = kb.bufs_n("y2s", 2); b_xbs = kb.bufs_n("xbs", 2)
    for gb in range(NB):
        i = gb % 2
        kb.dma("sp", xb[:, i, :], xs[gb * 128:(gb + 1) * 128, :], reads=[b_xs], writes=[b_xbs[i]])
        kb.idma(y1[:, i, :], YS, in_idx=SLOT[:, 0, gb:gb + 1], bound=NSLAB * SLAB - 1, reads=[b_YS, b_SLOT], writes=[b_y1s[i]])
        kb.idma(y2[:, i, :], YS, in_idx=SLOT[:, 1, gb:gb + 1], bound=NSLAB * SLAB - 1, reads=[b_YS, b_SLOT], writes=[b_y2s[i]])
        cx.stt("dve", xb[:, i, :], y1[:, i, :], WGT[:, 0, gb:gb + 1], xb[:, i, :], ALU.mult, ALU.add,
               [b_y1s[i], b_WGT, b_xbs[i]], [b_xbs[i]])
        cx.stt("dve", xb[:, i, :], y2[:, i, :], WGT[:, 1, gb:gb + 1], xb[:, i, :], ALU.mult, ALU.add,
               [b_y2s[i], b_WGT, b_xbs[i]], [b_xbs[i]])
        cx.act(junk, xb[:, i, :], AF.Square, [b_xbs[i]], [b_junk, b_s2], accum=s2[:, i:i + 1])
        cx.ts("dve", s2[:, i:i + 1], s2[:, i:i + 1], 1.0 / D, EPS, ALU.mult, ALU.add, [b_s2], [b_s2])
        cx.act(s2[:, i:i + 1], s2[:, i:i + 1], AF.Sqrt, [b_s2], [b_s2])
        kb.op("dve", lambda g_, i=i: g_.reciprocal(out=s2[:, i:i + 1], in_=s2[:, i:i + 1]), [b_s2], [b_s2])
        cx.stt("dve", xb[:, i, :], xb[:, i, :], s2[:, i:i + 1], gfin, ALU.mult, ALU.mult, [b_xbs[i], b_s2, b_gfin], [b_xbs[i]])
        kb.dma("sp", out[gb * 128:(gb + 1) * 128, :], xb[:, i, :], reads=[b_xbs[i]], writes=[b_out], disjoint=True)
    cx.release(mp)
```

```python
import math
import numpy as np
import ml_dtypes
import concourse.bass as bass
import concourse.mybir as mybir
from concourse.bass_utils import run_bass_kernel_spmd

F32 = mybir.dt.float32
BF16 = mybir.dt.bfloat16
I32 = mybir.dt.int32
AF = mybir.ActivationFunctionType
ALU = mybir.AluOpType
AX = mybir.AxisListType

L = 4096
D = 1024
NB = L // 128
G = 64
GS = 16
PS = 64
TCH = 8
D_FF = 2688
NE = 8
MOE_FF = 3584
NH = 16
QK_NOPE = 64
QK_ROPE = 32
V_HEAD = 64
Q_LORA = 512
KV_LORA = 256
EPS = 1e-6
DT_MIN = 1e-3
DT_MAX = 1e-1
SLAB = 1024
NSLAB = (2 * L) // SLAB + NE
NGRP = MOE_FF // 512


class Buf:
    __slots__ = ("name", "w", "r")

    def __init__(self, name):
        self.name = name
        self.w = {}
        self.r = {}


class KB:
    def __init__(self, nc):
        self.nc = nc
        self.eng = {"pe": nc.tensor, "act": nc.scalar, "dve": nc.vector,
                    "pool": nc.gpsimd, "sp": nc.sync}
        self.sem = {k: nc.alloc_semaphore(name="sem_" + k) for k in self.eng}
        self.cnt = {k: 0 for k in self.eng}
        self.known = {k: {} for k in self.eng}
        self.lanes = {}
        self.bufs = []
        self.n_ins = 0

    def _lane(self, lane):
        if lane not in self.lanes:
            pool = self.__dict__.setdefault("_lane_pool", [])
            if pool:
                self.lanes[lane] = pool.pop()
            else:
                self._nl = getattr(self, "_nl", 0) + 1
                self.lanes[lane] = [self.nc.alloc_semaphore(name=f"ln{self._nl}"), 0]

    def buf(self, name):
        b = Buf(name)
        self.bufs.append(b)
        return b

    def bufs_n(self, name, n):
        return [self.buf(f"{name}{i}") for i in range(n)]

    def _semof(self, key):
        if key[0] == "e":
            return self.sem[key[1]]
        return self.lanes[key[1]][0]

    def _need(self, reads, writes):
        need = {}
        for b in reads:
            for k, v in b.w.items():
                if need.get(k, 0) < v:
                    need[k] = v
        for b in writes:
            for k, v in b.w.items():
                if need.get(k, 0) < v:
                    need[k] = v
            for k, v in b.r.items():
                if need.get(k, 0) < v:
                    need[k] = v
        return need

    def _wait(self, e, need):
        kn = self.known[e]
        for k, v in need.items():
            if e == "pe" and k == ("e", "pe"):
                continue
            if kn.get(k, 0) >= v:
                continue
            self.eng[e].wait_ge(self._semof(k), v)
            kn[k] = v

    def op(self, e, fn, reads=(), writes=()):
        self._wait(e, self._need(reads, writes))
        ins = fn(self.eng[e])
        self.cnt[e] += 1
        c = self.cnt[e]
        ins.then_inc(self.sem[e], 1)
        key = ("e", e)
        for b in reads:
            b.r[key] = c
        for b in writes:
            b.w = {key: c}
            b.r = {}
        self.n_ins += 1
        return ins

    def dma(self, q, out, in_, reads=(), writes=(), lane=None, disjoint=False, **kw):
        if lane is None:
            lane = writes[0].name
        self._lane(lane)
        need = self._need(reads, writes)
        if disjoint:
            need.pop(("l", lane), None)
        self._wait(q, need)
        ins = self.eng[q].dma_start(out=out, in_=in_, **kw)
        ln = self.lanes[lane]
        ln[1] += 16
        ins.then_inc(ln[0], 16)
        key = ("l", lane)
        for b in reads:
            b.r[key] = ln[1]
        for b in writes:
            neww = {k: v for k, v in b.w.items() if k[0] == "l" and k != key}
            neww[key] = ln[1]
            b.w = neww
            b.r = {}
        self.n_ins += 1
        return ins

    def idma(self, out, in_, out_idx=None, in_idx=None, bound=None, reads=(), writes=(), lane=None, disjoint=False):
        if lane is None:
            lane = writes[0].name
        self._lane(lane)
        need = self._need(reads, writes)
        if disjoint:
            need.pop(("l", lane), None)
        self._wait("pool", need)
        oo = bass.IndirectOffsetOnAxis(ap=out_idx, axis=0) if out_idx is not None else None
        io = bass.IndirectOffsetOnAxis(ap=in_idx, axis=0) if in_idx is not None else None
        ins = self.nc.gpsimd.indirect_dma_start(out=out, out_offset=oo, in_=in_, in_offset=io)
        ln = self.lanes[lane]
        ln[1] += 16
        ins.then_inc(ln[0], 16)
        key = ("l", lane)
        for b in reads:
            b.r[key] = ln[1]
        for b in writes:
            neww = {k: v for k, v in b.w.items() if k[0] == "l" and k != key}
            neww[key] = ln[1]
            b.w = neww
            b.r = {}
        self.n_ins += 1
        return ins

    def finish(self, bufs):
        need = {}
        for b in bufs:
            for k, v in b.w.items():
                need[k] = max(need.get(k, 0), v)
        self._wait("sp", need)
        allneed = {("l", ln): v[1] for ln, v in self.lanes.items() if v[1] > 0}
        for e in self.eng:
            if self.cnt[e] > 0:
                allneed[("e", e)] = self.cnt[e]
        allneed.pop(("e", "sp"), None)
        self._wait("sp", allneed)

    def barrier(self):
        need = {("l", ln): v[1] for ln, v in self.lanes.items() if v[1] > 0}
        for e in self.eng:
            if self.cnt[e] > 0:
                need[("e", e)] = self.cnt[e]
        for e in self.eng:
            n2 = dict(need)
            n2.pop(("e", e), None)
            self._wait(e, n2)
        for b in self.bufs:
            b.w = {}
            b.r = {}
        pool = self.__dict__.setdefault("_lane_pool", [])
        for ln, v in self.lanes.items():
            pool.append(v)
        self.lanes = {}
        for e in self.eng:
            self.known[e] = {k: v for k, v in self.known[e].items() if k[0] == "e"}


class Ctx:
    def __init__(self, nc):
        self.nc = nc
        self.kb = KB(nc)
        self.ps = []
        self.psb = []
        for i in range(8):
            self.ps.append(nc.alloc_psum_tensor(f"ps{i}", [128, 512], F32).ap())
            self.psb.append(self.kb.buf(f"ps{i}"))
        self._mark = None

    def sb(self, name, shape, dtype=F32):
        self._n = getattr(self, "_n", 0) + 1
        t = self.nc.alloc_sbuf_tensor(f"sb{self._n}_{name}", list(shape), dtype).ap()
        return t, self.kb.buf(f"sb{self._n}_{name}")

    def mark(self):
        return (self.nc.sbuf_base, self.nc.sbuf_top)

    def release(self, m):
        self.kb.barrier()
        self.nc.sbuf_base, self.nc.sbuf_top = m

    def tt(self, e, out, in0, in1, op, r, w):
        return self.kb.op(e, lambda g: g.tensor_tensor(out=out, in0=in0, in1=in1, op=op), r, w)

    def ts(self, e, out, in0, s1, s2, op0, op1, r, w):
        if s2 is None:
            return self.kb.op(e, lambda g: g.tensor_scalar(out=out, in0=in0, scalar1=s1, scalar2=None, op0=op0), r, w)
        return self.kb.op(e, lambda g: g.tensor_scalar(out=out, in0=in0, scalar1=s1, scalar2=s2, op0=op0, op1=op1), r, w)

    def stt(self, e, out, in0, scalar, in1, op0, op1, r, w):
        return self.kb.op(e, lambda g: g.scalar_tensor_tensor(out=out, in0=in0, scalar=scalar, in1=in1, op0=op0, op1=op1), r, w)

    def cp(self, e, out, in_, r, w):
        if e == "act":
            return self.kb.op(e, lambda g: g.activation(out=out, in_=in_, func=AF.Copy), r, w)
        return self.kb.op(e, lambda g: g.tensor_copy(out=out, in_=in_), r, w)

    def act(self, out, in_, func, r, w, scale=1.0, bias=None, accum=None):
        kw = {}
        if bias is not None:
            kw["bias"] = bias
        if accum is not None:
            kw["accum_out"] = accum
        return self.kb.op("act", lambda g: g.activation(out=out, in_=in_, func=func, scale=scale, **kw), r, w)

    def mm(self, out, lhsT, rhs, start, stop, r, w, **kw):
        return self.kb.op("pe", lambda g: g.matmul(out, lhsT=lhsT, rhs=rhs, start=start, stop=stop, **kw), r, w)

    def tr(self, out, in_, ident, r, w):
        return self.kb.op("pe", lambda g: g.transpose(out=out, in_=in_, identity=ident), r, w)

    def memset(self, e, out, val, w):
        return self.kb.op(e, lambda g: g.memset(out, val), (), w)


PI = math.pi


def setup_ident(cx, din):
    P = {"bufs": {}}
    P["identf"], bf = cx.sb("identf", [128, 128], F32)
    P["identb"], bb = cx.sb("identb", [128, 128], BF16)
    cx.kb.dma("sp", P["identf"], din["ident"], writes=[bf])
    cx.cp("dve", P["identb"], P["identf"], [bf], [bb])
    P["bufs"]["identf"], P["bufs"]["identb"] = bf, bb
    return P


def phase0(cx, P, din, pre_hook=None):
    nc, kb = cx.nc, cx.kb
    b_identf, b_identb = P["bufs"]["identf"], P["bufs"]["identb"]
    P["WBre"], b_WBre = cx.sb("WBre", [128, 8, 8, 128], BF16)
    P["WBim"], b_WBim = cx.sb("WBim", [128, 8, 8, 128], BF16)
    P["WCre"], b_WCre = cx.sb("WCre", [128, 32, 8, 2, 16], BF16)
    P["WCim"], b_WCim = cx.sb("WCim", [128, 32, 8, 2, 16], BF16)
    P["FIRW"], b_FIRW = cx.sb("FIRW", [128, 8, 8, 128], BF16)
    P["A0c"], b_A0c = cx.sb("A0c", [128, 32, 8], F32)
    P["A0s"], b_A0s = cx.sb("A0s", [128, 32, 8], F32)
    P["A1c"], b_A1c = cx.sb("A1c", [128, 32, 8], F32)
    P["A1s"], b_A1s = cx.sb("A1s", [128, 32, 8], F32)
    P["RM8"], b_RM8 = cx.sb("RM8", [128, 32], F32)
    P["dT"], b_dT = cx.sb("dT", [128, 8], F32)
    P["bufs"].update(dict(WBre=b_WBre, WBim=b_WBim, WCre=b_WCre,
                          WCim=b_WCim, FIRW=b_FIRW, A0c=b_A0c, A0s=b_A0s, A1c=b_A1c, A1s=b_A1s, RM8=b_RM8, dT=b_dT))
    m = cx.mark()
    if pre_hook is not None:
        pre_hook()
    LR, b_LR = cx.sb("LR", [128, 32]); LI, b_LI = cx.sb("LI", [128, 32]); LDT, b_LDT = cx.sb("LDT", [128, 32])
    TH, b_TH = cx.sb("TH", [128, 32]); LM, b_LM = cx.sb("LM", [128, 32])
    EV, b_EV = cx.sb("EV", [128, 9, 32]); ANG, b_ANG = cx.sb("ANG", [128, 9, 32]); MAG, b_MAG = cx.sb("MAG", [128, 9, 32])
    SN, b_SN = cx.sb("SN", [128, 9, 32]); CS, b_CS = cx.sb("CS", [128, 9, 32]); IT, b_IT = cx.sb("IT", [128, 9, 32], I32)
    ARE, b_ARE = cx.sb("ARE", [128, 9, 32]); AIM, b_AIM = cx.sb("AIM", [128, 9, 32])
    NR, b_NR = cx.sb("NR", [128, 32]); DEN, b_DEN = cx.sb("DEN", [128, 32]); TMPa, b_TMPa = cx.sb("TMPa", [128, 32])
    TMPb, b_TMPb = cx.sb("TMPb", [128, 32])
    CRE, b_CRE = cx.sb("CRE", [128, 32]); CIM, b_CIM = cx.sb("CIM", [128, 32])
    WRE, b_WRE = cx.sb("WRE", [128, 8, 32]); WIM, b_WIM = cx.sb("WIM", [128, 8, 32])
    W8a, b_W8a = cx.sb("W8a", [128, 8, 32]); W8b, b_W8b = cx.sb("W8b", [128, 8, 32])
    BTre, b_BTre = cx.sb("BTre", [128, 32, 16]); BTim, b_BTim = cx.sb("BTim", [128, 32, 16])
    CTre, b_CTre = cx.sb("CTre", [128, 32, 16]); CTim, b_CTim = cx.sb("CTim", [128, 32, 16])
    T1, b_T1 = cx.sb("T1", [128, 32, 16]); T2, b_T2 = cx.sb("T2", [128, 32, 16])
    T3, b_T3 = cx.sb("T3", [128, 32, 16]); T4, b_T4 = cx.sb("T4", [128, 32, 16])
    XPre, b_XPre = cx.sb("XPre", [128, 8, 32, 2, 16], BF16); XPim, b_XPim = cx.sb("XPim", [128, 8, 32, 2, 16], BF16)
    CPre, b_CPre = cx.sb("CPre", [128, 32, 2, 16], BF16); CPnim, b_CPnim = cx.sb("CPnim", [128, 32, 2, 16], BF16)
    BM, b_BM = cx.sb("BM", [128, 128], F32)
    TK, b_TK = cx.sb("TK", [128, 128], F32)

    kb.dma("sp", BM, din["bmask"], writes=[b_BM])
    kb.dma("sp", EV, din["ev"].rearrange("p (e q) -> p e q", e=9), writes=[b_EV])
    kb.dma("sp", LR, din["lamT_re"], writes=[b_LR])
    kb.dma("sp", LI, din["lamT_im"], writes=[b_LI])
    kb.dma("sp", LDT, din["ldtT"], writes=[b_LDT])
    kb.dma("sp", P["dT"], din["dT"], writes=[b_dT])
    kb.dma("sp", BTre, din["bT_re"].rearrange("p (q n) -> p q n", n=16), writes=[b_BTre])
    kb.dma("sp", BTim, din["bT_im"].rearrange("p (q n) -> p q n", n=16), writes=[b_BTim])
    kb.dma("sp", CTre, din["cT_re"].rearrange("p (q n) -> p q n", n=16), writes=[b_CTre])
    kb.dma("sp", CTim, din["cT_im"].rearrange("p (q n) -> p q n", n=16), writes=[b_CTim])

    cx.act(LDT, LDT, AF.Exp, [b_LDT], [b_LDT])
    cx.tt("dve", TH, LI, LDT, ALU.mult, [b_LI, b_LDT], [b_TH])
    cx.tt("dve", LM, LR, LDT, ALU.mult, [b_LR, b_LDT], [b_LM])
    bc9 = lambda t: t.unsqueeze(1).to_broadcast([128, 9, 32])
    bc8 = lambda t: t.unsqueeze(1).to_broadcast([128, 8, 32])
    cx.tt("dve", ANG, EV, bc9(TH), ALU.mult, [b_EV, b_TH], [b_ANG])
    cx.tt("dve", MAG, EV, bc9(LM), ALU.mult, [b_EV, b_LM], [b_MAG])
    cx.act(MAG, MAG, AF.Exp, [b_MAG], [b_MAG])
    cx.ts("dve", SN, ANG, 1.0 / (2.0 * PI), None, ALU.mult, None, [b_ANG], [b_SN])
    cx.cp("dve", IT, SN, [b_SN], [b_IT])
    cx.cp("dve", SN, IT, [b_IT], [b_SN])
    cx.stt("dve", SN, SN, -2.0 * PI, ANG, ALU.mult, ALU.add, [b_SN, b_ANG], [b_SN])
    cx.ts("dve", CS, ANG, 1.0 / (2.0 * PI), 0.25, ALU.mult, ALU.add, [b_ANG], [b_CS])
    cx.cp("dve", IT, CS, [b_CS], [b_IT])
    cx.cp("dve", CS, IT, [b_IT], [b_CS])
    cx.stt("dve", CS, CS, -2.0 * PI, ANG, ALU.mult, ALU.add, [b_CS, b_ANG], [b_CS])
    cx.ts("dve", CS, CS, 0.5 * PI, None, ALU.add, None, [b_CS], [b_CS])
    cx.ts("dve", SN, SN, -PI, PI, ALU.max, ALU.min, [b_SN], [b_SN])
    cx.ts("dve", CS, CS, -PI, PI, ALU.max, ALU.min, [b_CS], [b_CS])
    cx.act(SN, SN, AF.Sin, [b_SN], [b_SN])
    cx.act(CS, CS, AF.Sin, [b_CS], [b_CS])
    cx.tt("dve", ARE, MAG, CS, ALU.mult, [b_MAG, b_CS], [b_ARE])
    cx.tt("dve", AIM, MAG, SN, ALU.mult, [b_MAG, b_SN], [b_AIM])
    cx.ts("dve", NR, ARE[:, 1, :], -1.0, None, ALU.add, None, [b_ARE], [b_NR])
    NI = AIM[:, 1, :]
    cx.tt("dve", DEN, LR, LR, ALU.mult, [b_LR], [b_DEN])
    cx.tt("dve", TMPa, LI, LI, ALU.mult, [b_LI], [b_TMPa])
    cx.tt("dve", DEN, DEN, TMPa, ALU.add, [b_DEN, b_TMPa], [b_DEN])
    kb.op("dve", lambda g: g.reciprocal(out=DEN, in_=DEN), [b_DEN], [b_DEN])
    cx.tt("dve", TMPa, NR, LR, ALU.mult, [b_NR, b_LR], [b_TMPa])
    cx.tt("dve", TMPb, NI, LI, ALU.mult, [b_AIM, b_LI], [b_TMPb])
    cx.tt("dve", TMPa, TMPa, TMPb, ALU.add, [b_TMPa, b_TMPb], [b_TMPa])
    cx.tt("dve", CRE, TMPa, DEN, ALU.mult, [b_TMPa, b_DEN], [b_CRE])
    cx.tt("dve", TMPa, NI, LR, ALU.mult, [b_AIM, b_LR], [b_TMPa])
    cx.tt("dve", TMPb, NR, LI, ALU.mult, [b_NR, b_LI], [b_TMPb])
    cx.tt("dve", TMPa, TMPa, TMPb, ALU.subtract, [b_TMPa, b_TMPb], [b_TMPa])
    cx.tt("dve", CIM, TMPa, DEN, ALU.mult, [b_TMPa, b_DEN], [b_CIM])
    cx.tt("dve", W8a, ARE[:, 0:8, :], bc8(CRE), ALU.mult, [b_ARE, b_CRE], [b_W8a])
    cx.tt("dve", W8b, AIM[:, 0:8, :], bc8(CIM), ALU.mult, [b_AIM, b_CIM], [b_W8b])
    cx.tt("dve", WRE, W8a, W8b, ALU.subtract, [b_W8a, b_W8b], [b_WRE])
    cx.tt("dve", W8a, ARE[:, 0:8, :], bc8(CIM), ALU.mult, [b_ARE, b_CIM], [b_W8a])
    cx.tt("dve", W8b, AIM[:, 0:8, :], bc8(CRE), ALU.mult, [b_AIM, b_CRE], [b_W8b])
    cx.tt("dve", WIM, W8a, W8b, ALU.add, [b_W8a, b_W8b], [b_WIM])
    cx.cp("dve", P["RM8"], MAG[:, 8, :], [b_MAG], [b_RM8])
    A0c, A0s, A1c, A1s = P["A0c"], P["A0s"], P["A1c"], P["A1s"]
    cx.cp("dve", A0c[:, :, 0], CS[:, 8, :], [b_CS], [b_A0c])
    cx.cp("dve", A0s[:, :, 0], SN[:, 8, :], [b_SN], [b_A0s])
    for i in range(1, 8):
        cx.tt("dve", TMPa, A0c[:, :, i - 1], A0c[:, :, 0], ALU.mult, [b_A0c], [b_TMPa])
        cx.tt("dve", TMPb, A0s[:, :, i - 1], A0s[:, :, 0], ALU.mult, [b_A0s], [b_TMPb])
        cx.tt("dve", A0c[:, :, i], TMPa, TMPb, ALU.subtract, [b_TMPa, b_TMPb], [b_A0c])
        cx.tt("dve", TMPa, A0c[:, :, i - 1], A0s[:, :, 0], ALU.mult, [b_A0c, b_A0s], [b_TMPa])
        cx.tt("dve", TMPb, A0s[:, :, i - 1], A0c[:, :, 0], ALU.mult, [b_A0s, b_A0c], [b_TMPb])
        cx.tt("dve", A0s[:, :, i], TMPa, TMPb, ALU.add, [b_TMPa, b_TMPb], [b_A0s])
    cx.memset("dve", A1c[:, :, 0], 1.0, [b_A1c])
    cx.memset("dve", A1s[:, :, 0], 0.0, [b_A1s])
    for i in range(1, 8):
        cx.tt("dve", TMPa, A1c[:, :, i - 1], A0c[:, :, 7], ALU.mult, [b_A1c, b_A0c], [b_TMPa])
        cx.tt("dve", TMPb, A1s[:, :, i - 1], A0s[:, :, 7], ALU.mult, [b_A1s, b_A0s], [b_TMPb])
        cx.tt("dve", A1c[:, :, i], TMPa, TMPb, ALU.subtract, [b_TMPa, b_TMPb], [b_A1c])
        cx.tt("dve", TMPa, A1c[:, :, i - 1], A0s[:, :, 7], ALU.mult, [b_A1c, b_A0s], [b_TMPa])
        cx.tt("dve", TMPb, A1s[:, :, i - 1], A0c[:, :, 7], ALU.mult, [b_A1s, b_A0c], [b_TMPb])
        cx.tt("dve", A1s[:, :, i], TMPa, TMPb, ALU.add, [b_TMPa, b_TMPb], [b_A1s])

    cx.memset("pool", XPre, 0.0, [b_XPre])
    cx.memset("pool", XPim, 0.0, [b_XPim])
    cx.memset("pool", CPre, 0.0, [b_CPre])
    cx.memset("pool", CPnim, 0.0, [b_CPnim])
    cx.memset("pool", P["WCre"], 0.0, [b_WCre])
    cx.memset("pool", P["WCim"], 0.0, [b_WCim])
    bcn = lambda t: t.unsqueeze(2).to_broadcast([128, 32, 16])
    for s in range(8):
        e = 7 - s
        cx.tt("dve", T1, BTre, bcn(WRE[:, e, :]), ALU.mult, [b_BTre, b_WRE], [b_T1])
        cx.tt("dve", T2, BTim, bcn(WIM[:, e, :]), ALU.mult, [b_BTim, b_WIM], [b_T2])
        cx.tt("dve", T3, BTim, bcn(WRE[:, e, :]), ALU.mult, [b_BTim, b_WRE], [b_T3])
        cx.tt("dve", T4, BTre, bcn(WIM[:, e, :]), ALU.mult, [b_BTre, b_WIM], [b_T4])
        for par in range(2):
            sl = slice(64 * par, 64 * par + 64)
            cx.tt("dve", XPre[sl, s, :, par, :], T1[sl], T2[sl], ALU.subtract, [b_T1, b_T2], [b_XPre])
            cx.tt("dve", XPim[sl, s, :, par, :], T3[sl], T4[sl], ALU.add, [b_T3, b_T4], [b_XPim])
    for par in range(2):
        sl = slice(64 * par, 64 * par + 64)
        cx.cp("dve", CPre[sl, :, par, :], CTre[sl], [b_CTre], [b_CPre])
        cx.ts("dve", CPnim[sl, :, par, :], CTim[sl], -1.0, None, ALU.mult, None, [b_CTim], [b_CPnim])
    for j in range(8):
        e = j + 1
        cx.tt("dve", T1, CTre, bcn(ARE[:, e, :]), ALU.mult, [b_CTre, b_ARE], [b_T1])
        cx.tt("dve", T2, CTim, bcn(AIM[:, e, :]), ALU.mult, [b_CTim, b_AIM], [b_T2])
        cx.tt("dve", T3, CTre, bcn(AIM[:, e, :]), ALU.mult, [b_CTre, b_AIM], [b_T3])
        cx.tt("dve", T4, CTim, bcn(ARE[:, e, :]), ALU.mult, [b_CTim, b_ARE], [b_T4])
        for par in range(2):
            sl = slice(64 * par, 64 * par + 64)
            cx.tt("dve", P["WCre"][sl, :, j, par, :], T1[sl], T2[sl], ALU.subtract, [b_T1, b_T2], [b_WCre])
            cx.stt("dve", P["WCim"][sl, :, j, par, :], T3[sl], -1.0, T4[sl], ALU.mult, ALU.subtract,
                   [b_T3, b_T4], [b_WCim])
    for fc in range(8):
        pq = slice(4 * fc, 4 * fc + 4)
        for k in range(8):
            pi = (fc * 8 + k) % 4
            ps, bps = cx.ps[pi], cx.psb[pi]
            cx.mm(ps[:, 0:128], XPre[:, 7 - k, pq, :, :], CPre[:, pq, :, :], True, False, [b_XPre, b_CPre], [bps])
            cx.mm(ps[:, 0:128], XPim[:, 7 - k, pq, :, :], CPnim[:, pq, :, :], False, True, [b_XPim, b_CPnim], [bps])
            if k == 0:
                cx.tt("dve", TK, ps[:, 0:128], BM, ALU.mult, [bps, b_BM], [b_TK])
                cx.stt("dve", P["FIRW"][:, fc, 0, :], P["identf"], P["dT"][:, fc:fc + 1], TK, ALU.mult, ALU.add,
                       [b_identf, b_dT, b_TK], [b_FIRW])
            else:
                cx.tt("dve", P["FIRW"][:, fc, k, :], ps[:, 0:128], BM, ALU.mult, [bps, b_BM], [b_FIRW])
    for (XP, b_XP, WB, b_WB) in ((XPre, b_XPre, P["WBre"], b_WBre), (XPim, b_XPim, P["WBim"], b_WBim)):
        for fc in range(8):
            pq = slice(4 * fc, 4 * fc + 4)
            pi = 4 + (fc % 2)
            psb16 = cx.ps[pi].bitcast(BF16).rearrange("p (s c) -> p s c", s=8)
            for s in range(8):
                cx.tr(psb16[:, s, :], XP[:, s, pq, :, :], P["identb"], [b_XP, b_identb], [cx.psb[pi]])
            cx.cp("act" if fc % 2 else "dve", WB[:, fc, :, :], psb16, [cx.psb[pi]], [b_WB])
    cx.release(m)
    return P


class PsRot:
    def __init__(self, cx, banks):
        self.cx = cx
        self.banks = list(banks)
        self.i = 0

    def next(self):
        b = self.banks[self.i % len(self.banks)]
        self.i += 1
        return self.cx.ps[b], self.cx.psb[b]


class WStream:
    def __init__(self, cx, nslots, slot_elems, name="ws", direct=None, ahead=None):
        self.cx = cx
        self.slots = []
        for i in range(nslots):
            t, b = cx.sb(f"{name}{i}", [128, slot_elems], BF16)
            self.slots.append((t, b))
        self.jobs = []
        self.issued = 0
        self.used = 0
        self.res = {}
        self.direct = direct
        self.ahead = ahead

    def plan(self, jobs):
        self.jobs.extend(jobs)

    def _issue(self, i):
        t, b = self.slots[i % len(self.slots)]
        off = 0
        views = []
        if self.direct is not None:
            src, n = self.jobs[i]
            self.cx.kb.dma("sp", t[:, 0:n], src, reads=[self.direct], writes=[b])
            self.res[i] = (t, b)
            return
        for (src, a, n) in self.jobs[i]:
            v = t[:, off:off + a * n].rearrange("p (a n) -> p a n", n=n)
            self.cx.kb.dma("pool", v, src, writes=[b])
            views.append(v)
            off += a * n
        self.res[i] = (views, b)

    def get(self):
        i = self.used
        ahead = self.ahead if self.ahead is not None else max(1, len(self.slots) - 2)
        while self.issued < min(len(self.jobs), i + 1 + ahead):
            self._issue(self.issued)
            self.issued += 1
        self.used += 1
        return self.res.pop(i)


def rms_to_hT(cx, P, xt, b_xt, nblk, gT, b_gT, htok, b_htok, hT, b_hT, ss, b_ss, trbanks, d=D, extra=None):
    nkc = d // 128
    bx = b_xt if isinstance(b_xt, list) else [b_xt]
    for b in range(nblk):
        cx.act(htok[:, b, :], xt[:, b, :], AF.Square, bx, [b_htok, b_ss], accum=ss[:, b:b + 1])
    cx.ts("dve", ss[:, 0:nblk], ss[:, 0:nblk], 1.0 / d, EPS, ALU.mult, ALU.add, [b_ss], [b_ss])
    cx.act(ss[:, 0:nblk], ss[:, 0:nblk], AF.Sqrt, [b_ss], [b_ss])
    cx.kb.op("dve", lambda g: g.reciprocal(out=ss[:, 0:nblk], in_=ss[:, 0:nblk]), [b_ss], [b_ss])
    for b in range(nblk):
        cx.ts("dve", htok[:, b, :], xt[:, b, :], ss[:, b:b + 1], None, ALU.mult, None, bx + [b_ss], [b_htok])
    for b in range(nblk):
        pi = trbanks[b % len(trbanks)]
        p16 = cx.ps[pi].bitcast(BF16).rearrange("p (k c) -> p k c", c=128)
        for kc in range(nkc):
            cx.tr(p16[:, kc, :], htok[:, b, kc * 128:(kc + 1) * 128], P["identb"], [b_htok, P["bufs"]["identb"]], [cx.psb[pi]])
        cx.tt("dve", hT[:, 0:nkc, b * 128:(b + 1) * 128], p16[:, 0:nkc, :],
              gT[:, 0:nkc].unsqueeze(2).to_broadcast([128, nkc, 128]), ALU.mult, [cx.psb[pi], b_gT], [b_hT])
        if extra is not None:
            gT2, hT2, b_hT2 = extra
            cx.tt("dve", hT2[:, 0:nkc, b * 128:(b + 1) * 128], p16[:, 0:nkc, :],
                  gT2[:, 0:nkc].unsqueeze(2).to_broadcast([128, nkc, 128]), ALU.mult, [cx.psb[pi], b_gT], [b_hT2])


N_L0_JOBS = 6 + D_FF // 128


def layer0_convert(cx, din, W0, b_W0, nstage=4):
    kb = cx.kb
    stage = [cx.sb(f"l0st{i}", [128, 4096], BF16) for i in range(nstage)]
    j = 0
    for w in (din["s5_w_in"], din["s5_w_glu"], din["s5_w_out"]):
        for half in range(2):
            st, b_st = stage[j % nstage]
            kb.dma("pool", st.rearrange("p (k n) -> p k n", n=512),
                   w[:, half * 512:(half + 1) * 512].rearrange("(k p) n -> p k n", p=128), writes=[b_st])
            kb.dma("sp", W0[j * 128:(j + 1) * 128, :], st, reads=[b_st], writes=[b_W0], disjoint=True)
            j += 1
    for f in range(D_FF // 128):
        st, b_st = stage[j % nstage]
        cs = slice(f * 128, (f + 1) * 128)
        kb.dma("pool", st[:, 0:1024].rearrange("p (k n) -> p k n", n=128),
               din["ffn_w_gate"][:, cs].rearrange("(k p) n -> p k n", p=128), writes=[b_st])
        kb.dma("pool", st[:, 1024:2048].rearrange("p (k n) -> p k n", n=128),
               din["ffn_w_up"][:, cs].rearrange("(k p) n -> p k n", p=128), writes=[b_st])
        kb.dma("pool", st[:, 2048:3072], din["ffn_w_down"][cs, :], writes=[b_st])
        kb.dma("sp", W0[j * 128:(j + 1) * 128, 0:3072], st[:, 0:3072], reads=[b_st], writes=[b_W0], disjoint=True)
        j += 1


GELU_C = 0.044715
GELU_S = 2.0 * math.sqrt(2.0 / math.pi)


def phase1(cx, P, din, xs, b_xs, W0, b_W0, ntiles=8, hook=None):
    nc, kb = cx.nc, cx.kb
    B = P["bufs"]
    m = cx.mark()
    xt, _ = cx.sb("xt", [128, 4, 1024], F32)
    b_xt = kb.bufs_n("xt8_", 8)
    regA, b_A = cx.sb("regA", [128, 4096], F32)
    regB, b_B = cx.sb("regB", [128, 2048], F32)
    regC, b_C = cx.sb("regC", [128, 2048], F32)
    uT, _ = cx.sb("uT", [128, 8, 512], BF16)
    b_u = kb.bufs_n("uT", 8)
    yT, _ = cx.sb("yT", [128, 8, 512], BF16)
    b_y = kb.bufs_n("yT", 8)
    SR, b_SR = cx.sb("SR", [128, 32, 65], F32)
    SI, b_SI = cx.sb("SI", [128, 32, 65], F32)
    SB, b_SB = cx.sb("SB", [128, 32, 2, 64], BF16)
    ss, b_ss = cx.sb("ss", [128, 8], F32)
    gT, b_gT = cx.sb("gT", [128, 2, 8], F32)
    CAR, b_CAR = cx.sb("CAR", [128, 2, 32], F32)
    ws = WStream(cx, 3, 4096, direct=b_W0, ahead=2)
    y32 = regA.rearrange("p (f t) -> p f t", t=512)
    t3 = regA[:, 0:2048].rearrange("p (q c) -> p q c", c=64)
    t4 = regA[:, 2048:4096].rearrange("p (q c) -> p q c", c=64)
    htok = regB.bitcast(BF16).rearrange("p (b d) -> p b d", d=1024)
    t1 = regB.rearrange("p (q c) -> p q c", c=64)
    hT = regC.bitcast(BF16).rearrange("p (k t) -> p k t", t=512)
    t2 = regC.rearrange("p (q c) -> p q c", c=64)
    zT, b_z = uT, b_u
    sgs = [regB[:, i * 512:(i + 1) * 512] for i in range(2)]
    acts = [yT.rearrange("p f t -> p (f t)")[:, i * 512:(i + 1) * 512] for i in range(4)]
    rot = PsRot(cx, [2, 3, 4, 5, 6, 7])
    ytmp = [(regA[:, i * 512:(i + 1) * 512], kb.buf(f"ytmp{i}")) for i in range(4)]
    nt = 0

    kb.dma("sp", gT[:, 0, :], din["gT_mix0"], writes=[b_gT])
    kb.dma("sp", gT[:, 1, :], din["gT_ffn0"], writes=[b_gT])
    cx.memset("dve", CAR, 0.0, [b_CAR])
    w_in, w_glu, w_out = din["s5_w_in"], din["s5_w_glu"], din["s5_w_out"]
    wg, wu, wd = din["ffn_w_gate"], din["ffn_w_up"], din["ffn_w_down"]

    def wcols(w, c0, n):
        return (w[:, c0:c0 + n].rearrange("(k p) n -> p k n", p=128), 8, n)

    jobs = []
    for T in range(ntiles):
        for j in range(N_L0_JOBS):
            jobs.append((W0[j * 128:(j + 1) * 128, 0:(4096 if j < 6 else 3072)], 4096 if j < 6 else 3072))
    ws.plan(jobs)

    for T in range(ntiles):
        t0 = T * 512
        kb.dma("sp", xt, din["x"][t0:t0 + 512, :].rearrange("(b p) d -> p b d", p=128), writes=b_xt, lane="xt")
        rms_to_hT(cx, P, xt, b_xt, 4, gT[:, 0, :], b_gT, htok, b_B, hT, b_C, ss, b_ss, [0, 1])
        for half in range(2):
            wt_, b_w = ws.get()
            wv = wt_.rearrange("p (k n) -> p k n", n=512)
            for f4 in range(4):
                fc = half * 4 + f4
                ps, bps = rot.next()
                for kc in range(8):
                    cx.mm(ps, wv[:, kc, f4 * 128:(f4 + 1) * 128], hT[:, kc, :], kc == 0, kc == 7, [b_w, b_C], [bps])
                cx.cp("act", uT[:, fc, :], ps, [bps], [b_u[fc]])
        a0c = lambda: P["A0c"].unsqueeze(2).to_broadcast([128, 32, 8, 8])
        a0s = lambda: P["A0s"].unsqueeze(2).to_broadcast([128, 32, 8, 8])
        a1c = lambda: P["A1c"].unsqueeze(3).to_broadcast([128, 32, 8, 8])
        a1s = lambda: P["A1s"].unsqueeze(3).to_broadcast([128, 32, 8, 8])
        v4 = lambda t: t.rearrange("p q (a b) -> p q a b", b=8)
        SRv, SIv = SR[:, :, 0:64], SI[:, :, 0:64]
        for qb in range(4):
            psr, bpsr = rot.next()
            psi, bpsi = rot.next()
            for q8 in range(8):
                q = qb * 8 + q8
                fc, q4 = q // 4, q % 4
                rows = slice(32 * q4, 32 * q4 + 32)
                uv = uT[rows, fc, :].rearrange("p (c s) -> p c s", s=8)
                for (pp, bpp, WB, bWB) in ((psr, bpsr, P["WBre"], B["WBre"]), (psi, bpsi, P["WBim"], B["WBim"])):
                    for s in range(8):
                        cx.mm(pp[:, q8 * 64:(q8 + 1) * 64], WB[rows, fc, s, :], uv[:, :, s], s == 0, s == 7,
                              [bWB, b_u[fc]], [bpp], tile_position=(32 * q4, 0))
            qs = slice(qb * 8, (qb + 1) * 8)
            pr4 = psr.rearrange("p (q a b) -> p q a b", a=8, b=8)
            pi4 = psi.rearrange("p (q a b) -> p q a b", a=8, b=8)
            c4 = P["A0c"][:, qs, :].unsqueeze(2).to_broadcast([128, 8, 8, 8])
            s4 = P["A0s"][:, qs, :].unsqueeze(2).to_broadcast([128, 8, 8, 8])
            cx.tt("dve", v4(t3)[:, qs], pr4, c4, ALU.mult, [bpsr, B["A0c"]], [b_A])
            cx.tt("dve", v4(t4)[:, qs], pi4, s4, ALU.mult, [bpsi, B["A0s"]], [b_A])
            cx.tt("dve", v4(SRv)[:, qs], v4(t3)[:, qs], v4(t4)[:, qs], ALU.add, [b_A], [b_SR])
            cx.tt("dve", v4(t3)[:, qs], pi4, c4, ALU.mult, [bpsi, B["A0c"]], [b_A])
            cx.tt("dve", v4(t4)[:, qs], pr4, s4, ALU.mult, [bpsr, B["A0s"]], [b_A])
            cx.tt("dve", v4(SIv)[:, qs], v4(t3)[:, qs], v4(t4)[:, qs], ALU.subtract, [b_A], [b_SI])
        if hook is not None:
            hook()
        cx.tt("dve", v4(t3), v4(SRv), a1c(), ALU.mult, [b_SR, B["A1c"]], [b_A])
        cx.tt("dve", v4(t4), v4(SIv), a1s(), ALU.mult, [b_SI, B["A1s"]], [b_A])
        cx.tt("dve", v4(t1), v4(t3), v4(t4), ALU.add, [b_A], [b_B])
        cx.tt("dve", v4(t3), v4(SIv), a1c(), ALU.mult, [b_SI, B["A1c"]], [b_A])
        cx.tt("dve", v4(t4), v4(SRv), a1s(), ALU.mult, [b_SR, B["A1s"]], [b_A])
        cx.tt("dve", v4(t2), v4(t3), v4(t4), ALU.subtract, [b_A], [b_C])
        for q in range(32):
            rm = P["RM8"][:, q:q + 1].to_broadcast([128, 64])
            kb.op("dve", lambda g_, q=q, rm=rm: g_.tensor_tensor_scan(out=SRv[:, q, :], data0=rm, data1=t1[:, q, :],
                  initial=CAR[:, 0, q:q + 1], op0=ALU.mult, op1=ALU.add), [b_B, B["RM8"], b_CAR], [b_SR])
            kb.op("dve", lambda g_, q=q, rm=rm: g_.tensor_tensor_scan(out=SIv[:, q, :], data0=rm, data1=t2[:, q, :],
                  initial=CAR[:, 1, q:q + 1], op0=ALU.mult, op1=ALU.add), [b_C, B["RM8"], b_CAR], [b_SI])
        cx.tt("dve", v4(t3), v4(SRv), a1c(), ALU.mult, [b_SR, B["A1c"]], [b_A])
        cx.tt("dve", v4(t4), v4(SIv), a1s(), ALU.mult, [b_SI, B["A1s"]], [b_A])
        cx.tt("dve", v4(t1), v4(t3), v4(t4), ALU.subtract, [b_A], [b_B])
        cx.tt("dve", v4(t3), v4(SIv), a1c(), ALU.mult, [b_SI, B["A1c"]], [b_A])
        cx.tt("dve", v4(t4), v4(SRv), a1s(), ALU.mult, [b_SR, B["A1s"]], [b_A])
        cx.tt("dve", v4(t2), v4(t3), v4(t4), ALU.add, [b_A], [b_C])
        cx.cp("dve", SB[:, :, 0, 0:1], CAR[:, 0, :].unsqueeze(2), [b_CAR], [b_SB])
        cx.cp("dve", SB[:, :, 1, 0:1], CAR[:, 1, :].unsqueeze(2), [b_CAR], [b_SB])
        cx.tt("dve", v4(t3), v4(t1), a0c(), ALU.mult, [b_B, B["A0c"]], [b_A])
        cx.tt("dve", v4(t4), v4(t2), a0s(), ALU.mult, [b_C, B["A0s"]], [b_A])
        cx.tt("dve", SB[:, :, 0, 1:64], t3[:, :, 0:63], t4[:, :, 0:63], ALU.subtract, [b_A], [b_SB])
        cx.tt("dve", CAR[:, 0, :].unsqueeze(2), t3[:, :, 63:64], t4[:, :, 63:64], ALU.subtract, [b_A], [b_CAR])
        cx.tt("dve", v4(t3), v4(t2), a0c(), ALU.mult, [b_C, B["A0c"]], [b_A])
        cx.tt("dve", v4(t4), v4(t1), a0s(), ALU.mult, [b_B, B["A0s"]], [b_A])
        cx.tt("dve", SB[:, :, 1, 1:64], t3[:, :, 0:63], t4[:, :, 0:63], ALU.add, [b_A], [b_SB])
        cx.tt("dve", CAR[:, 1, :].unsqueeze(2), t3[:, :, 63:64], t4[:, :, 63:64], ALU.add, [b_A], [b_CAR])
        for fc in range(8):
            ps, bps = rot.next()
            uv = uT[:, fc, :].rearrange("p (c s) -> p c s", s=8)
            pv = ps.rearrange("p (c s) -> p c s", s=8)
            for k in range(8):
                cx.mm(pv[:, :, k:8], P["FIRW"][:, fc, k, :], uv[:, :, 0:8 - k], k == 0, False,
                      [B["FIRW"], b_u[fc]], [bps])
            for q4 in range(4):
                q = fc * 4 + q4
                rows = slice(32 * q4, 32 * q4 + 32)
                for j in range(8):
                    last = (q4 == 3 and j == 7)
                    cx.mm(pv[rows, :, j], P["WCre"][:, q, j, :, :], SB[:, q, 0, :], False, False,
                          [B["WCre"], b_SB], [bps], tile_position=(0, 32 * q4))
                    cx.mm(pv[rows, :, j], P["WCim"][:, q, j, :, :], SB[:, q, 1, :], False, last,
                          [B["WCim"], b_SB], [bps], tile_position=(0, 32 * q4))
            yv = y32[:, fc, :]
            g1 = sgs[fc % 2]
            cx.act(g1, ps, AF.Square, [bps], [b_B])
            cx.ts("dve", g1, g1, GELU_C, 1.0, ALU.mult, ALU.add, [b_B], [b_B])
            cx.tt("dve", g1, g1, ps, ALU.mult, [b_B, bps], [b_B])
            cx.act(g1, g1, AF.Sigmoid, [b_B], [b_B], scale=GELU_S)
            cx.tt("dve", yv, g1, ps, ALU.mult, [b_B, bps], [b_A])
            cx.cp("act", yT[:, fc, :], yv, [b_A], [b_y[fc]])
        for half in range(2):
            wt_, b_w = ws.get()
            wv = wt_.rearrange("p (k n) -> p k n", n=512)
            for f4 in range(4):
                fc = half * 4 + f4
                ps, bps = rot.next()
                for kc in range(8):
                    cx.mm(ps, wv[:, kc, f4 * 128:(f4 + 1) * 128], yT[:, kc, :], kc == 0, kc == 7, [b_w, b_y[kc]], [bps])
                g1 = sgs[fc % 2]
                cx.act(g1, ps, AF.Sigmoid, [bps], [b_B])
                cx.tt("dve", zT[:, fc, :], y32[:, fc, :], g1, ALU.mult, [b_A, b_B], [b_z[fc]])
        for half in range(2):
            wt_, b_w = ws.get()
            wv = wt_.rearrange("p (k n) -> p k n", n=512)
            for b in range(4):
                ps, bps = rot.next()
                for kc in range(8):
                    cx.mm(ps, zT[:, kc, b * 128:(b + 1) * 128], wv[:, kc, :], kc == 0, kc == 7, [b_z[kc], b_w], [bps])
                xv = xt[:, b, half * 512:(half + 1) * 512]
                cx.tt("dve", xv, xv, ps, ALU.add, [b_xt[b * 2 + half], bps], [b_xt[b * 2 + half]])
        rms_to_hT(cx, P, xt, b_xt, 4, gT[:, 1, :], b_gT, htok, b_B, hT, b_C, ss, b_ss, [0, 1])
        for f in range(D_FF // 128):
            wt_, b_w = ws.get()
            gv = wt_[:, 0:1024].rearrange("p (k n) -> p k n", n=128)
            uvw = wt_[:, 1024:2048].rearrange("p (k n) -> p k n", n=128)
            dv = wt_[:, 2048:3072].rearrange("p (a d) -> p a d", d=1024)
            pg, bpg = rot.next()
            pu, bpu = rot.next()
            for kc in range(8):
                cx.mm(pg, gv[:, kc, :], hT[:, kc, :], kc == 0, kc == 7, [b_w, b_C], [bpg])
            for kc in range(8):
                cx.mm(pu, uvw[:, kc, :], hT[:, kc, :], kc == 0, kc == 7, [b_w, b_C], [bpu])
            g1 = sgs[f % 2]
            a1 = acts[f % 4]
            b_a = b_y[(f % 4)]
            cx.act(g1, pg, AF.Silu, [bpg], [b_B])
            cx.tt("dve", a1, g1, pu, ALU.mult, [b_B, bpu], [b_a])
            for b in range(4):
                for half in range(2):
                    ps, bps = rot.next()
                    cx.mm(ps, a1[:, b * 128:(b + 1) * 128], dv[:, 0, half * 512:(half + 1) * 512], True, True,
                          [b_a, b_w], [bps])
                    xv = xt[:, b, half * 512:(half + 1) * 512]
                    bx = b_xt[b * 2 + half]
                    if (b * 2 + half) % 2 == 0:
                        cx.tt("dve", xv, xv, ps, ALU.add, [bx, bps], [bx])
                    else:
                        tb, b_tb = ytmp[nt % 4]
                        nt += 1
                        cx.cp("act", tb, ps, [bps], [b_tb])
                        cx.tt("pool", xv, xv, tb, ALU.add, [bx, b_tb], [bx])
        kb.dma("sp", xs[t0:t0 + 512, :].rearrange("(b p) d -> p b d", p=128), xt, reads=b_xt, writes=[b_xs], disjoint=True)
    cx.release(m)


IN_SPECS = {
    "x": ([L, D], F32), "pos": ([1, L], I32),
    "ident": ([128, 128], F32), "bmask": ([128, 128], F32), "ev": ([128, 9 * 32], F32),
    "ltri": ([128, 128], F32), "wtab": ([128, NSLAB * NE], F32), "ctab": ([128, 7], F32),
    "invf_t": ([128, 16], F32), "invf_f": ([128, 1], F32), "esel": ([128, 31], F32), "dmask": ([128, 128], F32),
    "lamT_re": ([128, 32], F32), "lamT_im": ([128, 32], F32), "ldtT": ([128, 32], F32),
    "bT_re": ([128, 512], F32), "bT_im": ([128, 512], F32),
    "cT_re": ([128, 512], F32), "cT_im": ([128, 512], F32), "dT": ([128, 8], F32),
    "gT_mix0": ([128, 8], F32), "gT_ffn0": ([128, 8], F32), "gT_kv": ([128, 8], F32),
    "gT_mix1": ([128, 8], F32), "gT_ffn1": ([128, 8], F32), "g_final": ([D], F32),
    "g_kvlat": ([KV_LORA], F32), "g_qlat": ([Q_LORA], F32),
    "s5_w_in": ([D, D], F32), "s5_w_glu": ([D, D], F32), "s5_w_out": ([D, D], F32),
    "ffn_w_gate": ([D, D_FF], F32), "ffn_w_up": ([D, D_FF], F32), "ffn_w_down": ([D_FF, D], F32),
    "w_dkv": ([D, KV_LORA + QK_ROPE], F32), "w_ukv": ([KV_LORA, NH * 128], F32),
    "w_dq": ([D, Q_LORA], F32), "w_uq": ([Q_LORA, NH * 96], F32), "w_o": ([NH * V_HEAD, D], F32),
    "router_w": ([D, NE], F32), "router_wT": ([NE, D], F32), "g_ffn1": ([D], F32), "sel": ([8, NE * 128], F32),
    "moe_w_gate": ([NE, D, MOE_FF], F32), "moe_w_up": ([NE, D, MOE_FF], F32),
    "moe_w_down": ([NE, MOE_FF, D], F32),
}


def host_consts():
    c = {}
    c["ident"] = np.eye(128, dtype=np.float32)
    blk = np.arange(128) // 16
    c["bmask"] = (blk[:, None] == blk[None, :]).astype(np.float32)
    c["ev"] = np.broadcast_to(np.arange(9, dtype=np.float32)[None, :, None], (128, 9, 32)).reshape(128, 288).copy()
    invf = (np.float32(10000.0) ** (-np.arange(16, dtype=np.float32) * np.float32(2.0 / QK_ROPE))).astype(np.float32)
    c["invf_t"] = np.broadcast_to(invf[None, :], (128, 16)).copy()
    ff = np.zeros((128, 1), np.float32)
    ff[64:80, 0] = invf
    ff[80:96, 0] = invf
    c["invf_f"] = ff
    es = np.zeros((128, 31), np.float32)
    es[:, 15] = 1.0
    c["esel"] = es
    kk = np.arange(128)[:, None]
    qq = np.arange(128)[None, :]
    c["dmask"] = ((kk < 64) | (qq >= 64)).astype(np.float32)
    se = np.zeros((8, NE, 128), np.float32)
    for e in range(NE):
        se[e, e, :] = 1.0
    c["sel"] = se.reshape(8, NE * 128)
    c["ltri"] = (np.arange(128)[:, None] < np.arange(128)[None, :]).astype(np.float32)
    c["wtab"] = np.broadcast_to(np.arange(NSLAB, dtype=np.float32)[None, :, None], (128, NSLAB, NE)).reshape(128, NSLAB * NE).copy()
    c["ctab"] = (np.arange(7, dtype=np.float32)[None, :] * 128.0 + np.arange(128, dtype=np.float32)[:, None]).copy()
    return c


def _gT(g):
    return np.ascontiguousarray(g.reshape(8, 128).T)


def host_shared(inp):
    f = lambda a: np.ascontiguousarray(a, dtype=np.float32)
    pair = lambda a: f(a.reshape(32, 2, 64).transpose(1, 2, 0).reshape(128, 32))
    s = dict(host_consts())
    s["lamT_re"] = pair(inp["s5_lambda_re"][0])
    s["lamT_im"] = pair(inp["s5_lambda_im"][0])
    s["ldtT"] = pair(np.broadcast_to(inp["s5_log_dt"][0][:, None], (64, 64)))
    bt = lambda b: f(b.reshape(32, 2, 64, 16).transpose(1, 2, 0, 3).reshape(128, 512))
    ct = lambda c: f(c.reshape(32, 2, 16, 64).transpose(1, 3, 0, 2).reshape(128, 512))
    s["bT_re"], s["bT_im"] = bt(inp["s5_b_re"][0]), bt(inp["s5_b_im"][0])
    s["cT_re"], s["cT_im"] = ct(inp["s5_c_re"][0]), ct(inp["s5_c_im"][0])
    s["dT"] = _gT(f(inp["s5_d"][0]))
    s["gT_mix0"], s["gT_mix1"] = _gT(f(inp["norm_mix"][0])), _gT(f(inp["norm_mix"][1]))
    s["gT_ffn0"], s["gT_ffn1"] = _gT(f(inp["norm_ffn"][0])), _gT(f(inp["norm_ffn"][1]))
    s["gT_kv"] = _gT(f(inp["kv_norm"]))
    s["g_final"] = f(inp["final_norm"])
    s["g_kvlat"] = f(inp["kv_latent_norm"])
    s["g_qlat"] = f(inp["q_latent_norm"][0])
    for k in ("s5_w_in", "s5_w_glu", "s5_w_out", "ffn_w_gate", "ffn_w_up", "ffn_w_down",
              "w_dq", "w_uq", "w_o", "moe_w_gate", "moe_w_up", "moe_w_down"):
        s[k] = f(inp[k][0])
    s["router_wT"] = f(inp["router_w"][0].T)
    s["router_w"] = f(inp["router_w"][0])
    s["g_ffn1"] = f(inp["norm_ffn"][1])
    s["w_dkv"] = f(inp["w_dkv"])
    s["w_ukv"] = f(inp["w_ukv"])
    return s


def declare_inputs(nc, names):
    din = {}
    for k in names:
        shape, dt = IN_SPECS[k]
        din[k] = nc.dram_tensor(k, list(shape), dt, kind="ExternalInput").ap()
    return din


ATT_SCALE = (QK_NOPE + QK_ROPE) ** -0.5
KMAX_MARGIN = 1.02


def range_reduce_sin(cx, out, ang, it, b_out, b_ang, b_it, shift):
    cx.ts("dve", out, ang, 1.0 / (2.0 * PI), shift / (2.0 * PI), ALU.mult, ALU.add, [b_ang], [b_out])
    cx.cp("dve", it, out, [b_out], [b_it])
    cx.cp("dve", out, it, [b_it], [b_out])
    cx.stt("dve", out, out, -2.0 * PI, ang, ALU.mult, ALU.add, [b_out, b_ang], [b_out])
    cx.ts("dve", out, out, shift, -PI, ALU.add, ALU.max, [b_out], [b_out])
    cx.ts("dve", out, out, PI, None, ALU.min, None, [b_out], [b_out])
    cx.act(out, out, AF.Sin, [b_out], [b_out])


def phase15(cx, P, din, xs, b_xs, KT, b_KT, QT, b_QT, VS, b_VS, ntiles=8, hook=None):
    nc, kb = cx.nc, cx.kb
    B = P["bufs"]
    identb, b_identb = P["identb"], B["identb"]
    m = cx.mark()
    xts = [cx.sb(f"xt{i}", [128, 4, 1024], F32) for i in range(2)]
    htok, b_htok = cx.sb("htok", [128, 4, 1024], BF16)
    hT, b_hT = cx.sb("hT", [128, 8, 512], BF16)
    ss, b_ss = cx.sb("ss", [128, 8], F32)
    gT, b_gT = cx.sb("gT", [128, 2, 8], F32)
    wdkv, b_wdkv = cx.sb("wdkv", [128, 8, 288], BF16)
    wukv, b_wukv = cx.sb("wukv", [128, 2, 2048], BF16)
    wdq, b_wdq = cx.sb("wdq", [128, 8, 512], BF16)
    wuq, b_wuq = cx.sb("wuq", [128, 4, 1536], BF16)
    wrot, b_wrot = cx.sb("wrot", [128, 4, 16, 32], BF16)
    gkv, b_gkv = cx.sb("gkv", [128, 256], F32)
    gq, b_gq = cx.sb("gq", [128, 512], F32)
    invf_t, b_invf_t = cx.sb("invf_t", [128, 16], F32)
    invf_f, b_invf_f = cx.sb("invf_f", [128, 1], F32)
    esel, b_esel = cx.sb("esel", [128, 31], BF16)
    eself, b_eself = cx.sb("eself", [128, 31], F32)
    posis = [cx.sb(f"posi{i}", [128, 512], I32) for i in range(2)]
    posf, b_posf = cx.sb("posf", [128, 512], F32)
    ptok_is = [cx.sb(f"ptok_i{i}", [128, 4], I32) for i in range(2)]
    ptok, b_ptok = cx.sb("ptok", [128, 4], F32)
    angf, b_angf = cx.sb("angf", [128, 512], F32)
    cosf, b_cosf = cx.sb("cosf", [128, 512], F32)
    sinf, b_sinf = cx.sb("sinf", [128, 512], F32)
    itf, b_itf = cx.sb("itf", [128, 512], I32)
    angt, b_angt = cx.sb("angt", [128, 16], F32)
    cost, b_cost = cx.sb("cost", [128, 16], F32)
    sint, b_sint = cx.sb("sint", [128, 16], F32)
    itt, b_itt = cx.sb("itt", [128, 16], I32)
    ckvn, b_ckvn = cx.sb("ckvn", [128, 256], BF16)
    kro, b_kro = cx.sb("kro", [128, 32], BF16)
    krt, b_krt = cx.sb("krt", [128, 4, 16], F32)
    ckvT, b_ckvT = cx.sb("ckvT", [128, 2, 512], BF16)
    krT, b_krT = cx.sb("krT", [128, 512], BF16)
    cqn, b_cqn = cx.sb("cqn", [128, 512], BF16)
    cqnT, b_cqnT = cx.sb("cqnT", [128, 4, 512], BF16)
    KTt, b_KTt = cx.sb("KTt", [128, 16, 512], BF16)
    QTt, b_QTt = cx.sb("QTt", [128, 16, 512], BF16)
    SQ, b_SQ = cx.sb("SQ", [128, 16, 512], BF16)
    VA, b_VA = cx.sb("VA", [128, 16, 4, 65], BF16)
    nrm, b_nrm = cx.sb("nrm", [16, 512], F32)
    nrmb, b_nrmb = cx.sb("nrmb", [16, 512], BF16)
    rmax, b_rmax = cx.sb("rmax", [16, 2], F32)
    KM, b_KM = cx.sb("KM", [16, 4096], BF16)
    t96a, b_t96a = cx.sb("t96a", [128, 512], F32)
    t96b, b_t96b = cx.sb("t96b", [128, 512], F32)
    rot = PsRot(cx, [2, 3, 4, 5, 6, 7])

    kb.dma("sp", gT[:, 0, :], din["gT_kv"], writes=[b_gT])
    kb.dma("sp", gT[:, 1, :], din["gT_mix1"], writes=[b_gT])
    kb.dma("sp", gkv, din["g_kvlat"].partition_broadcast(128), writes=[b_gkv])
    kb.dma("sp", gq, din["g_qlat"].partition_broadcast(128), writes=[b_gq])
    kb.dma("sp", invf_t, din["invf_t"], writes=[b_invf_t])
    kb.dma("sp", invf_f, din["invf_f"], writes=[b_invf_f])
    kb.dma("sp", eself, din["esel"], writes=[b_eself])
    cx.cp("dve", esel, eself, [b_eself], [b_esel])
    kb.dma("pool", wdkv, din["w_dkv"].rearrange("(k p) n -> p k n", p=128), writes=[b_wdkv])
    kb.dma("pool", wukv, din["w_ukv"].rearrange("(k p) n -> p k n", p=128), writes=[b_wukv])
    kb.dma("pool", wdq, din["w_dq"].rearrange("(k p) n -> p k n", p=128), writes=[b_wdq])
    kb.dma("pool", wuq, din["w_uq"].rearrange("(k p) n -> p k n", p=128), writes=[b_wuq])
    wuq4 = wuq.rearrange("p k (h c) -> p k h c", c=96)
    for kc in range(4):
        cx.ts("dve", wrot[:, kc, :, 0:16], wuq4[:, kc, :, 80:96], -1.0, None, ALU.mult, None, [b_wuq], [b_wrot])
        cx.cp("dve", wrot[:, kc, :, 16:32], wuq4[:, kc, :, 64:80], [b_wuq], [b_wrot])
    cx.memset("dve", rmax, 0.0, [b_rmax])
    cx.memset("dve", VA, 1.0, [b_VA])
    cx.memset("pool", kro, 0.0, [b_kro])

    def head_norms(Tt, b_Tt, dst_row96, b_dst, t0, is_k):
        cx.act(SQ[0:96], Tt[0:96], AF.Square, [b_Tt], [b_SQ])
        ps, bps = rot.next()
        for h in range(16):
            cx.mm(ps[0:16, :], esel[0:96, 15 - h:31 - h], SQ[0:96, h, :], h == 0, h == 15, [b_esel, b_SQ], [bps])
        if is_k:
            cx.kb.op("dve", lambda g: g.reduce_max(out=rmax[:, 1:2], in_=ps[0:16, :], axis=AX.X), [bps], [b_rmax])
            cx.tt("dve", rmax[:, 0:1], rmax[:, 0:1], rmax[:, 1:2], ALU.max, [b_rmax], [b_rmax])
        else:
            cx.act(nrm, ps[0:16, :], AF.Sqrt, [bps], [b_nrm])
            cx.ts("dve", nrmb, nrm, -1.0, None, ALU.mult, None, [b_nrm], [b_nrmb])
            kb.dma("sp", dst_row96[:, t0:t0 + 512], nrmb, reads=[b_nrmb], writes=[b_dst])

    def load_tile(TT):
        tt0 = TT * 512
        xt_, b_xt_ = xts[TT % 2]
        posi_, b_posi_ = posis[TT % 2]
        ptok_i_, b_ptok_i_ = ptok_is[TT % 2]
        kb.dma("sp", xt_, xs[tt0:tt0 + 512, :].rearrange("(b p) d -> p b d", p=128), reads=[b_xs], writes=[b_xt_])
        kb.dma("sp", posi_, din["pos"][0, tt0:tt0 + 512].partition_broadcast(128), writes=[b_posi_])
        kb.dma("sp", ptok_i_, din["pos"][0, tt0:tt0 + 512].rearrange("(b p) -> p b", p=128), writes=[b_ptok_i_],
               allow_slow_non_contiguous=True)

    load_tile(0)
    for T in range(ntiles):
        t0 = T * 512
        xt, b_xt = xts[T % 2]
        posi, b_posi = posis[T % 2]
        ptok_i, b_ptok_i = ptok_is[T % 2]
        if T + 1 < ntiles:
            load_tile(T + 1)
        cx.cp("dve", posf, posi, [b_posi], [b_posf])
        cx.cp("dve", ptok, ptok_i, [b_ptok_i], [b_ptok])
        cx.ts("dve", angf, posf, invf_f[:, 0:1], None, ALU.mult, None, [b_posf, b_invf_f], [b_angf])
        range_reduce_sin(cx, sinf, angf, itf, b_sinf, b_angf, b_itf, 0.0)
        range_reduce_sin(cx, cosf, angf, itf, b_cosf, b_angf, b_itf, 0.5 * PI)
        cx.ts("dve", sinf, sinf, ATT_SCALE, None, ALU.mult, None, [b_sinf], [b_sinf])
        cx.ts("dve", cosf, cosf, ATT_SCALE, None, ALU.mult, None, [b_cosf], [b_cosf])

        rms_to_hT(cx, P, xt, b_xt, 4, gT[:, 0, :], b_gT, htok, b_htok, hT, b_hT, ss, b_ss, [0, 1])
        for b in range(4):
            ps, bps = rot.next()
            for kc in range(8):
                cx.mm(ps[:, 0:288], hT[:, kc, b * 128:(b + 1) * 128], wdkv[:, kc, :], kc == 0, kc == 7, [b_hT, b_wdkv], [bps])
            cx.act(ckvn, ps[:, 0:256], AF.Square, [bps], [b_ckvn, b_ss], accum=ss[:, 4:5])
            cx.ts("dve", ss[:, 4:5], ss[:, 4:5], 1.0 / KV_LORA, EPS, ALU.mult, ALU.add, [b_ss], [b_ss])
            cx.act(ss[:, 4:5], ss[:, 4:5], AF.Sqrt, [b_ss], [b_ss])
            cx.kb.op("dve", lambda g: g.reciprocal(out=ss[:, 4:5], in_=ss[:, 4:5]), [b_ss], [b_ss])
            cx.stt("dve", ckvn, ps[:, 0:256], ss[:, 4:5], gkv, ALU.mult, ALU.mult, [bps, b_ss, b_gkv], [b_ckvn])
            cx.ts("dve", angt, invf_t, ptok[:, b:b + 1], None, ALU.mult, None, [b_invf_t, b_ptok], [b_angt])
            range_reduce_sin(cx, sint, angt, itt, b_sint, b_angt, b_itt, 0.0)
            range_reduce_sin(cx, cost, angt, itt, b_cost, b_angt, b_itt, 0.5 * PI)
            x1, x2 = ps[:, 256:272], ps[:, 272:288]
            cx.tt("dve", krt[:, 0, :], x1, cost, ALU.mult, [bps, b_cost], [b_krt])
            cx.tt("dve", krt[:, 1, :], x2, sint, ALU.mult, [bps, b_sint], [b_krt])
            cx.tt("dve", krt[:, 2, :], x2, cost, ALU.mult, [bps, b_cost], [b_krt])
            cx.tt("dve", krt[:, 3, :], x1, sint, ALU.mult, [bps, b_sint], [b_krt])
            cx.tt("dve", kro[:, 0:16], krt[:, 0, :], krt[:, 1, :], ALU.subtract, [b_krt], [b_kro])
            cx.tt("dve", kro[:, 16:32], krt[:, 2, :], krt[:, 3, :], ALU.add, [b_krt], [b_kro])
            p16 = cx.ps[b % 2].bitcast(BF16).rearrange("p (k c) -> p k c", c=128)
            bp16 = cx.psb[b % 2]
            for k2 in range(2):
                cx.tr(p16[:, k2, :], ckvn[:, k2 * 128:(k2 + 1) * 128], identb, [b_ckvn, b_identb], [bp16])
            cx.tr(p16[0:32, 2, :], kro, identb, [b_kro, b_identb], [bp16])
            cx.cp("act", ckvT[:, :, b * 128:(b + 1) * 128], p16[:, 0:2, :], [bp16], [b_ckvT])
            cx.cp("dve", krT[64:96, b * 128:(b + 1) * 128], p16[0:32, 2, :], [bp16], [b_krT])
            w4 = wukv.rearrange("p k (h c) -> p k h c", c=128)
            for hh in range(2):
                pv_, bpv = rot.next()
                for k2 in range(2):
                    cx.mm(pv_, ckvT[:, k2, b * 128:(b + 1) * 128], w4[:, k2, hh * 8:(hh + 1) * 8, 64:128],
                          k2 == 0, k2 == 1, [b_ckvT, b_wukv], [bpv])
                cx.cp("act" if hh else "dve", VA[:, hh * 8:(hh + 1) * 8, b, 0:64],
                      pv_.rearrange("p (h c) -> p h c", c=64), [bpv], [b_VA])
        if hook is not None:
            hook()
        for h in range(16):
            ps, bps = rot.next()
            for k2 in range(2):
                cx.mm(ps[0:64, :], wukv[:, k2, h * 128:h * 128 + 64], ckvT[:, k2, :], k2 == 0, k2 == 1, [b_wukv, b_ckvT], [bps])
            cx.cp("act" if h % 2 else "dve", KTt[0:64, h, :], ps[0:64, :], [bps], [b_KTt])
        cx.cp("dve", KTt[64:96, :, :], krT[64:96, :].unsqueeze(1).to_broadcast([32, 16, 512]), [b_krT], [b_KTt])
        kb.dma("sp", KT[:, 0:96, t0:t0 + 512].rearrange("h r t -> r h t"), KTt[0:96], reads=[b_KTt], writes=[b_KT])
        head_norms(KTt, b_KTt, None, None, t0, True)
        kb.dma("sp", VS[:, :, 4 * T:4 * T + 4, :].rearrange("h p b e -> p h b e"), VA, reads=[b_VA], writes=[b_VS])

        if hook is not None:
            hook()
        rms_to_hT(cx, P, xt, b_xt, 4, gT[:, 1, :], b_gT, htok, b_htok, hT, b_hT, ss, b_ss, [0, 1])
        for b in range(4):
            ps, bps = rot.next()
            for kc in range(8):
                cx.mm(ps, hT[:, kc, b * 128:(b + 1) * 128], wdq[:, kc, :], kc == 0, kc == 7, [b_hT, b_wdq], [bps])
            cx.act(cqn, ps, AF.Square, [bps], [b_cqn, b_ss], accum=ss[:, 5:6])
            cx.ts("dve", ss[:, 5:6], ss[:, 5:6], 1.0 / Q_LORA, EPS, ALU.mult, ALU.add, [b_ss], [b_ss])
            cx.act(ss[:, 5:6], ss[:, 5:6], AF.Sqrt, [b_ss], [b_ss])
            cx.kb.op("dve", lambda g: g.reciprocal(out=ss[:, 5:6], in_=ss[:, 5:6]), [b_ss], [b_ss])
            cx.stt("dve", cqn, ps, ss[:, 5:6], gq, ALU.mult, ALU.mult, [bps, b_ss, b_gq], [b_cqn])
            p16 = cx.ps[b % 2].bitcast(BF16).rearrange("p (k c) -> p k c", c=128)
            bp16 = cx.psb[b % 2]
            for k4 in range(4):
                cx.tr(p16[:, k4, :], cqn[:, k4 * 128:(k4 + 1) * 128], identb, [b_cqn, b_identb], [bp16])
            cx.cp("act", cqnT[:, :, b * 128:(b + 1) * 128], p16[:, 0:4, :], [bp16], [b_cqnT])
        if hook is not None:
            hook()
        for h in range(16):
            pa, bpa = rot.next()
            pb_, bpb = rot.next()
            for k4 in range(4):
                cx.mm(pa[0:96, :], wuq[:, k4, h * 96:(h + 1) * 96], cqnT[:, k4, :], k4 == 0, k4 == 3, [b_wuq, b_cqnT], [bpa])
            for k4 in range(4):
                cx.mm(pb_[64:96, :], wrot[:, k4, h, :], cqnT[:, k4, :], k4 == 0, k4 == 3, [b_wrot, b_cqnT], [bpb],
                      tile_position=(0, 64))
            cx.act(QTt[0:64, h, :], pa[0:64, :], AF.Copy, [bpa], [b_QTt], scale=ATT_SCALE)
            cx.tt("dve", t96a[64:96], pa[64:96, :], cosf[64:96], ALU.mult, [bpa, b_cosf], [b_t96a])
            cx.tt("dve", t96b[64:96], pb_[64:96, :], sinf[64:96], ALU.mult, [bpb, b_sinf], [b_t96b])
            cx.tt("pool", QTt[64:96, h, :], t96a[64:96], t96b[64:96], ALU.add, [b_t96a, b_t96b], [b_QTt])
        kb.dma("sp", QT[:, 0:96, t0:t0 + 512].rearrange("h r t -> r h t"), QTt[0:96], reads=[b_QTt], writes=[b_QT])
        if hook is not None:
            hook()
        head_norms(QTt, b_QTt, QT[:, 96, :], b_QT, t0, False)
    cx.act(rmax[:, 1:2], rmax[:, 0:1], AF.Sqrt, [b_rmax], [b_rmax])
    cx.memset("pool", KM, 0.0, [b_KM])
    cx.ts("dve", KM, KM, rmax[:, 1:2], KMAX_MARGIN, ALU.add, ALU.mult, [b_KM, b_rmax], [b_KM])
    kb.dma("sp", KT[:, 96, :], KM, reads=[b_KM], writes=[b_KT])
    cx.release(m)


def phase2(cx, P, din, KT, b_KT, QT, b_QT, VS, b_VS, OT, b_OT, nheads=16, ngroups=8, hook=None):
    nc, kb = cx.nc, cx.kb
    m = cx.mark()
    KTh, QTh, VAh, b_KTh, b_QTh, b_VAh = [], [], [], [], [], []
    for i in range(2):
        t, b = cx.sb(f"KTh{i}", [128, 4096], BF16); KTh.append(t); b_KTh.append(b)
        t, b = cx.sb(f"QTh{i}", [128, 4096], BF16); QTh.append(t); b_QTh.append(b)
        t, b = cx.sb(f"VAh{i}", [128, 32, 65], BF16); VAh.append(t); b_VAh.append(b)
    PT, b_PT = [], []
    for i in range(2):
        t, _ = cx.sb(f"PT{i}", [128, 32, 512], BF16)
        PT.append(t)
        b_PT.append(kb.bufs_n(f"PT{i}_", 32))
    dmask, b_dmask = cx.sb("dmask", [128, 128], F32)
    onesr, b_onesr = cx.sb("onesr", [128, 64], F32)
    R, b_R = cx.sb("R", [128, 512], F32)
    RB, b_RB = cx.sb("RB", [128, 512], F32)
    OTs, b_OTs = [], []
    for i in range(2):
        t, b = cx.sb(f"OTs{i}", [128, 4096], BF16); OTs.append(t); b_OTs.append(b)
    kb.dma("sp", dmask, din["dmask"], writes=[b_dmask])
    cx.memset("dve", onesr, 1.0, [b_onesr])
    rot = PsRot(cx, [0, 1, 2, 3, 4])
    rot_o = PsRot(cx, [5, 6])

    def emit_pv(st, j):
        nkt = st["nkt"]
        c0 = max(0, j - 4 * st["G"]) * 128
        s_ = st["s"]
        cx.mm(st["po"][0:65, c0:512], VAh[s_][:, j, :], st["pt"][:, j, c0:512], j == 0, j == nkt - 1,
              [b_VAh[s_], st["b_pt"][j]], [st["bpo"]])

    def emit_fin(st):
        po, bpo, s_, G = st["po"], st["bpo"], st["s"], st["G"]
        ot, b_ot = st["ot"], st["b_ot"]
        cx.kb.op("dve", lambda g: g.reciprocal(out=R[64:65, :], in_=po[64:65, :]), [bpo], [b_R])
        pb_, bpb = cx.ps[7], cx.psb[7]
        cx.mm(pb_[0:64, :], onesr[64:65, :], R[64:65, :], True, True, [b_onesr, b_R], [bpb])
        cx.cp("act", RB[0:64, :], pb_[0:64, :], [bpb], [b_RB])
        dst = ot[64 * s_:64 * s_ + 64, G * 512:(G + 1) * 512]
        cx.tt("dve", dst, po[0:64, :], RB[0:64, :], ALU.mult, [bpo, b_RB], [b_ot])
        if st["store"] is not None:
            kb.dma("sp", OT[st["store"]], ot, reads=[b_ot], writes=[b_OT])

    prev = None
    fin_q = None
    gi = 0
    def load_head(hh):
        ss_ = hh % 2
        kb.dma("sp", KTh[ss_][0:97, :], KT[hh], reads=[b_KT], writes=[b_KTh[ss_]])
        kb.dma("sp", QTh[ss_][0:97, :], QT[hh], reads=[b_QT], writes=[b_QTh[ss_]])
        kb.dma("sp", VAh[ss_], VS[hh], reads=[b_VS], writes=[b_VAh[ss_]])

    load_head(0)
    for h in range(nheads):
        s = h % 2
        pr = h // 2
        ot, b_ot = OTs[pr % 2], b_OTs[pr % 2]
        for G in range(ngroups):
            if G == 1 and h + 1 < nheads:
                load_head(h + 1)
            if hook is not None:
                hook()
            pt, b_pt = PT[gi % 2], b_PT[gi % 2]
            gi += 1
            nkt = 4 * G + 4
            if prev is not None:
                prev["po"], prev["bpo"] = rot_o.next()
            npv = prev["nkt"] if prev is not None else 0
            for j in range(max(nkt, npv)):
                if j < nkt:
                    c0 = max(0, j - 4 * G) * 128
                    ps, bps = rot.next()
                    cx.mm(ps[:, c0:512], KTh[s][0:97, j * 128:(j + 1) * 128], QTh[s][0:97, G * 512 + c0:(G + 1) * 512],
                          True, True, [b_KTh[s], b_QTh[s]], [bps])
                    cx.act(pt[:, j, c0:512], ps[:, c0:512], AF.Exp, [bps], [b_pt[j]])
                    if j >= 4 * G:
                        cx.tt("pool", pt[:, j, c0:c0 + 128], pt[:, j, c0:c0 + 128], dmask, ALU.mult, [b_pt[j], b_dmask], [b_pt[j]])
                if j < npv:
                    emit_pv(prev, j)
                if j == 1 and fin_q is not None:
                    emit_fin(fin_q)
                    fin_q = None
            if fin_q is not None:
                emit_fin(fin_q)
                fin_q = None
            fin_q = prev
            last_of_pair = (G == ngroups - 1) and (s == 1 or h == nheads - 1)
            prev = dict(pt=pt, b_pt=b_pt, nkt=nkt, G=G, s=s, ot=ot, b_ot=b_ot, store=(pr if last_of_pair else None))
    if prev is not None:
        prev["po"], prev["bpo"] = rot_o.next()
        for j in range(prev["nkt"]):
            emit_pv(prev, j)
    if fin_q is not None:
        emit_fin(fin_q)
    if prev is not None:
        emit_fin(prev)
    cx.release(m)


def phase3a(cx, P, din, xs, b_xs, OT, b_OT, out, b_out, ntiles=8):
    nc, kb = cx.nc, cx.kb
    m = cx.mark()
    xt, b_xt = cx.sb("xt", [128, 4, 1024], F32)
    ot, b_ot = cx.sb("ot", [128, 8, 512], BF16)
    wo, b_wo = cx.sb("wo", [128, 8, 1024], BF16)
    kb.dma("pool", wo, din["w_o"].rearrange("(k p) n -> p k n", p=128), writes=[b_wo])
    rot = PsRot(cx, [0, 1, 2, 3])
    for T in range(ntiles):
        t0 = T * 512
        kb.dma("sp", xt, xs[t0:t0 + 512, :].rearrange("(b p) d -> p b d", p=128), reads=[b_xs], writes=[b_xt])
        kb.dma("sp", ot, OT[:, :, t0:t0 + 512].rearrange("k p t -> p k t"), reads=[b_OT], writes=[b_ot])
        for b in range(4):
            for half in range(2):
                ps, bps = rot.next()
                for k in range(8):
                    cx.mm(ps, ot[:, k, b * 128:(b + 1) * 128], wo[:, k, half * 512:(half + 1) * 512], k == 0, k == 7, [b_ot, b_wo], [bps])
                xv = xt[:, b, half * 512:(half + 1) * 512]
                cx.tt("dve", xv, xv, ps, ALU.add, [b_xt, bps], [b_xt])
        kb.dma("sp", out[t0:t0 + 512, :].rearrange("(b p) d -> p b d", p=128), xt, reads=[b_xt], writes=[b_out])
    cx.release(m)


def phase3(cx, P, din, xs, b_xs, OT, b_OT, out, b_out, ntiles=4, nexp=NE):
    nc, kb = cx.nc, cx.kb
    B = P["bufs"]
    identf, b_identf = P["identf"], B["identf"]
    m = cx.mark()
    NBK = 8
    xt, b_xt = cx.sb("xt", [128, NBK, 1024], F32)
    htok, b_htok = cx.sb("htok", [128, NBK, 1024], BF16)
    hT, b_hT = cx.sb("hT", [128, 8, 1024], BF16)
    wo, b_wo = cx.sb("wo", [128, 8, 1024], BF16)
    rwg, b_rwg = cx.sb("rwg", [128, NE, 1024], F32)
    gfin, b_gfin = cx.sb("gfin", [128, 1024], F32)
    gT, b_gT = cx.sb("gT", [128, 8], F32)
    ss, b_ss = cx.sb("ss", [128, 16], F32)
    lg, b_lg = cx.sb("lg", [128, NBK, 8], F32)
    m8, b_m8 = cx.sb("m8", [128, 8], F32)
    gts, b_gts = cx.sb("gts", [128, NBK, 8], F32)
    gsum, b_gsum = cx.sb("gsum", [128, 2], F32)
    g8T, b_g8T = cx.sb("g8T", [8, 1024], F32)
    sel, b_sel = cx.sb("sel", [8, NE, 128], F32)
    gbc, b_gbc = cx.sb("gbc", [128, 1024], F32)
    sg = []
    b_sg = []
    tg = []
    b_tg = []
    for i in range(2):
        t, b = cx.sb(f"sg{i}", [128, 512], F32); sg.append(t); b_sg.append(b)
        t, b = cx.sb(f"tg{i}", [128, 512], F32); tg.append(t); b_tg.append(b)
    actT = []
    b_actT = []
    for i in range(2):
        t, b = cx.sb(f"actT{i}", [128, 4, 1024], BF16); actT.append(t); b_actT.append(b)
    junk, b_junk = cx.sb("junk", [128, 1024], BF16)
    ws = WStream(cx, 2, 3 * 4096, name="wm")
    rot = PsRot(cx, [2, 3, 4, 5, 6, 7])
    ot = htok

    kb.dma("pool", wo, din["w_o"].rearrange("(k p) n -> p k n", p=128), writes=[b_wo])
    kb.dma("sp", gfin, din["g_final"].partition_broadcast(128), writes=[b_gfin])
    kb.dma("sp", gT, din["gT_ffn1"], writes=[b_gT])
    kb.dma("sp", gbc, din["g_ffn1"].partition_broadcast(128), writes=[b_gbc])
    kb.dma("sp", sel, din["sel"].rearrange("k (e m) -> k e m", m=128), writes=[b_sel])
    for e in range(NE):
        kb.dma("sp", rwg[:, e, :], din["router_wT"][e].partition_broadcast(128), writes=[b_rwg])
    for e in range(NE):
        cx.tt("pool", rwg[:, e, :], rwg[:, e, :], gbc, ALU.mult, [b_rwg, b_gbc], [b_rwg])
    wg, wu, wd = din["moe_w_gate"], din["moe_w_up"], din["moe_w_down"]
    NG = MOE_FF // 512
    jobs = []
    for T in range(ntiles):
        for e in range(nexp):
            for g in range(NG):
                jobs.append([
                    (wg[e][:, g * 512:(g + 1) * 512].rearrange("(k p) n -> p k n", p=128), 8, 512),
                    (wu[e][:, g * 512:(g + 1) * 512].rearrange("(k p) n -> p k n", p=128), 8, 512),
                    (wd[e][g * 512:(g + 1) * 512, :].rearrange("(a p) d -> p a d", p=128), 4, 1024)])
    ws.plan(jobs)

    for T in range(ntiles):
        t0 = T * 1024
        kb.dma("sp", xt, xs[t0:t0 + 1024, :].rearrange("(b p) d -> p b d", p=128), reads=[b_xs], writes=[b_xt])
        kb.dma("sp", ot, OT[:, :, t0:t0 + 1024].rearrange("k p t -> p k t"), reads=[b_OT], writes=[b_htok])
        for b in range(NBK):
            for half in range(2):
                ps, bps = rot.next()
                for k in range(8):
                    cx.mm(ps, ot[:, k, b * 128:(b + 1) * 128], wo[:, k, half * 512:(half + 1) * 512], k == 0, k == 7,
                          [b_htok, b_wo], [bps])
                xv = xt[:, b, half * 512:(half + 1) * 512]
                cx.tt("dve", xv, xv, ps, ALU.add, [b_xt, bps], [b_xt])
        rms_to_hT(cx, P, xt, b_xt, NBK, gT, b_gT, htok, b_htok, hT, b_hT, ss, b_ss, [0, 1])
        for b in range(NBK):
            for e in range(NE):
                kb.op("dve", lambda g_, b=b, e=e: g_.scalar_tensor_tensor(
                    out=junk, in0=xt[:, b, :], scalar=ss[:, b:b + 1], in1=rwg[:, e, :], op0=ALU.mult, op1=ALU.mult,
                    accum_out=lg[:, b, e:e + 1]), [b_xt, b_ss, b_rwg], [b_junk, b_lg])
            kb.op("dve", lambda g_, b=b: g_.max(out=m8, in_=lg[:, b, :]), [b_lg], [b_m8])
            cx.ts("dve", gsum[:, 0:1], m8[:, 0:1], -1.0, None, ALU.mult, None, [b_m8], [b_gsum])
            cx.act(gts[:, b, :], lg[:, b, :], AF.Exp, [b_lg, b_gsum], [b_gts], bias=gsum[:, 0:1])
            cx.stt("dve", gts[:, b, :], lg[:, b, :], m8[:, 1:2], gts[:, b, :], ALU.is_ge, ALU.mult,
                   [b_lg, b_m8, b_gts], [b_gts])
            kb.op("dve", lambda g_, b=b: g_.reduce_sum(out=gsum[:, 1:2], in_=gts[:, b, :], axis=AX.X), [b_gts], [b_gsum])
            kb.op("dve", lambda g_: g_.reciprocal(out=gsum[:, 1:2], in_=gsum[:, 1:2]), [b_gsum], [b_gsum])
            cx.ts("dve", gts[:, b, :], gts[:, b, :], gsum[:, 1:2], None, ALU.mult, None, [b_gts, b_gsum], [b_gts])
            pt_, bpt = rot.next()
            cx.tr(pt_[0:8, 0:128], gts[:, b, :], identf, [b_gts, b_identf], [bpt])
            cx.cp("dve", g8T[:, b * 128:(b + 1) * 128], pt_[0:8, 0:128], [bpt], [b_g8T])
        gi = 0
        for e in range(nexp):
            for half in range(2):
                ps, bps = rot.next()
                cx.mm(ps, sel[:, e, :], g8T[:, half * 512:(half + 1) * 512], True, True, [b_sel, b_g8T], [bps])
                cx.cp("act", gbc[:, half * 512:(half + 1) * 512], ps, [bps], [b_gbc])
            for g in range(NG):
                (gv, uv, dv), b_w = ws.get()
                at, b_at = actT[gi % 2], b_actT[gi % 2]
                gi += 1
                for f4 in range(4):
                    for half in range(2):
                        hs = slice(half * 512, (half + 1) * 512)
                        pg, bpg = rot.next()
                        pu, bpu = rot.next()
                        for kc in range(8):
                            cx.mm(pg, gv[:, kc, f4 * 128:(f4 + 1) * 128], hT[:, kc, hs], kc == 0, kc == 7, [b_w, b_hT], [bpg])
                        for kc in range(8):
                            cx.mm(pu, uv[:, kc, f4 * 128:(f4 + 1) * 128], hT[:, kc, hs], kc == 0, kc == 7, [b_w, b_hT], [bpu])
                        i2 = (f4 * 2 + half) % 2
                        cx.act(sg[i2], pg, AF.Silu, [bpg], [b_sg[i2]])
                        cx.tt("dve", tg[i2], pu, gbc[:, hs], ALU.mult, [bpu, b_gbc], [b_tg[i2]])
                        cx.tt("pool", at[:, f4, hs], sg[i2], tg[i2], ALU.mult, [b_sg[i2], b_tg[i2]], [b_at])
                for b in range(NBK):
                    for half in range(2):
                        ps, bps = rot.next()
                        for f4 in range(4):
                            cx.mm(ps, at[:, f4, b * 128:(b + 1) * 128], dv[:, f4, half * 512:(half + 1) * 512],
                                  f4 == 0, f4 == 3, [b_at, b_w], [bps])
                        xv = xt[:, b, half * 512:(half + 1) * 512]
                        cx.tt("dve", xv, xv, ps, ALU.add, [b_xt, bps], [b_xt])
        for b in range(NBK):
            cx.act(junk, xt[:, b, :], AF.Square, [b_xt], [b_junk, b_ss], accum=ss[:, 8 + (b % 8):9 + (b % 8)])
        cx.ts("dve", ss[:, 8:16], ss[:, 8:16], 1.0 / D, EPS, ALU.mult, ALU.add, [b_ss], [b_ss])
        cx.act(ss[:, 8:16], ss[:, 8:16], AF.Sqrt, [b_ss], [b_ss])
        kb.op("dve", lambda g_: g_.reciprocal(out=ss[:, 8:16], in_=ss[:, 8:16]), [b_ss], [b_ss])
        for b in range(NBK):
            cx.stt("dve", xt[:, b, :], xt[:, b, :], ss[:, 8 + b:9 + b], gfin, ALU.mult, ALU.mult, [b_xt, b_ss, b_gfin], [b_xt])
        kb.dma("sp", out[t0:t0 + 1024, :].rearrange("(b p) d -> p b d", p=128), xt, reads=[b_xt], writes=[b_out])
    cx.release(m)


def build_program():
    nc = bass.Bass("TRN2", target_bir_lowering=False)
    cx = Ctx(nc)
    kb = cx.kb
    din = declare_inputs(nc, list(IN_SPECS.keys()))
    out = nc.dram_tensor("out", [L, D], F32, kind="ExternalOutput").ap()
    xs = nc.dram_tensor("xs", [L, D], F32, kind="Internal").ap()
    KT = nc.dram_tensor("KT", [NH, 97, L], BF16, kind="Internal").ap()
    QT = nc.dram_tensor("QT", [NH, 97, L], BF16, kind="Internal").ap()
    VS = nc.dram_tensor("VS", [NH, 128, NB, 65], BF16, kind="Internal").ap()
    OT = nc.dram_tensor("OT", [NH // 2, 128, L], BF16, kind="Internal").ap()
    b_out, b_xs, b_KT, b_QT, b_VS, b_OT = (kb.buf(n) for n in ("out", "xs", "KT", "QT", "VS", "OT"))
    W0 = nc.dram_tensor("W0", [N_L0_JOBS * 128, 4096], BF16, kind="Internal").ap()
    b_W0 = kb.buf("W0")
    W16 = {k: nc.dram_tensor("W16" + k, [NE * NGRP * 128, 4096], BF16, kind="Internal").ap() for k in "gud"}
    b_W16 = {k: kb.buf("W16" + k) for k in "gud"}
    HS = nc.dram_tensor("HS", [NSLAB * SLAB, D], BF16, kind="Internal").ap()
    YS = nc.dram_tensor("YS", [NSLAB * SLAB, D], F32, kind="Internal").ap()
    b_HS, b_YS = kb.buf("HS"), kb.buf("YS")
    conv = MoeConv(cx, din, W16, b_W16, nstage=0)
    P = setup_ident(cx, din)
    m0 = cx.mark()
    phase0(cx, P, din, pre_hook=lambda: layer0_convert(cx, din, W0, b_W0))
    phase1(cx, P, din, xs, b_xs, W0, b_W0)
    cx.release(m0)
    m1 = cx.mark()
    conv.stage = [cx.sb(f"cvB{i}", [128, 4096], BF16) for i in range(2)]
    phase15(cx, P, din, xs, b_xs, KT, b_KT, QT, b_QT, VS, b_VS, hook=lambda: conv.step(3))
    phase2(cx, P, din, KT, b_KT, QT, b_QT, VS, b_VS, OT, b_OT, hook=lambda: conv.step(1))
    conv.finish()
    cx.release(m1)
    phase3r(cx, P, din, xs, b_xs, OT, b_OT, out, b_out, W16, b_W16, HS, b_HS, YS, b_YS)
    kb.finish([b_out])
    return nc


_NC_CACHE = {}


def kernel(**inputs):
    inp = {k: np.asarray(v) for k, v in inputs.items()}
    shared = host_shared(inp)
    if "nc" not in _NC_CACHE:
        _NC_CACHE["nc"] = build_program()
    nc = _NC_CACHE["nc"]
    ncores = 8
    in_maps = []
    for c in range(ncores):
        mp = dict(shared)
        mp["x"] = np.ascontiguousarray(inp["x"][c], dtype=np.float32)
        mp["pos"] = np.ascontiguousarray(inp["positions"][c:c + 1], dtype=np.int32)
        in_maps.append(mp)
    res = run_bass_kernel_spmd(nc, in_maps, core_ids=list(range(ncores)))
    return np.stack([np.asarray(r["out"], dtype=np.float32) for r in res.results], axis=0)


class MoeConv:
    def __init__(self, cx, din, W16, b_W16, nstage=2, nexp=NE):
        self.cx = cx
        self.W16, self.b_W16 = W16, b_W16
        self.stage = [cx.sb(f"cvst{i}", [128, 4096], BF16) for i in range(nstage)] if nstage else []
        self.chunks = []
        for e in range(nexp):
            for g in range(NGRP):
                cs = slice(g * 512, (g + 1) * 512)
                self.chunks.append(("g", e, g, din["moe_w_gate"][e][:, cs].rearrange("(k p) n -> p k n", p=128), 512))
                self.chunks.append(("u", e, g, din["moe_w_up"][e][:, cs].rearrange("(k p) n -> p k n", p=128), 512))
                self.chunks.append(("d", e, g, din["moe_w_down"][e][cs, :].rearrange("(a p) d -> p a d", p=128), 1024))
        self.i = 0

    def step(self, n=1):
        kb = self.cx.kb
        for _ in range(n):
            if self.i >= len(self.chunks):
                return
            kind, e, g, src, n_in = self.chunks[self.i]
            st, b_st = self.stage[self.i % len(self.stage)]
            self.i += 1
            kb.dma("pool", st.rearrange("p (a n) -> p a n", n=n_in), src, writes=[b_st])
            r0 = (e * NGRP + g) * 128
            kb.dma("sp", self.W16[kind][r0:r0 + 128, :], st, reads=[b_st], writes=[self.b_W16[kind]], disjoint=True)

    def finish(self):
        self.step(len(self.chunks))


def phase3r(cx, P, din, xs, b_xs, OT, b_OT, out, b_out, W16, b_W16, HS, b_HS, YS, b_YS, nslab=NSLAB):
    nc, kb = cx.nc, cx.kb
    B = P["bufs"]
    identb, b_identb = P["identb"], B["identb"]
    identf, b_identf = P["identf"], B["identf"]
    NTOT = NE * NGRP * 128
    mp = cx.mark()
    ss, b_ss = cx.sb("ss", [128, 8], F32)
    LG, b_LG = cx.sb("LG", [128, NB, 8], F32)
    GTS, b_GTS = cx.sb("GTS", [128, NB, 8], F32)
    TOP, b_TOP = cx.sb("TOP", [128, NB, 2], F32)
    m8, b_m8 = cx.sb("m8", [128, 8], F32)
    gsum, b_gsum = cx.sb("gsum", [128, 2], F32)
    junk, b_junk = cx.sb("junk", [128, 1024], BF16)
    M1, b_M1 = cx.sb("M1", [128, NB, 8], F32)
    M2, b_M2 = cx.sb("M2", [128, NB, 8], F32)
    MSK, b_MSK = cx.sb("MSK", [128, NB, 8], BF16)
    ltri, b_ltri = cx.sb("ltri", [128, 128], BF16)
    onesb, b_onesb = cx.sb("onesb", [128, 128], BF16)
    ones32, b_ones32 = cx.sb("ones32", [128, NB], F32)
    tmpf, b_tmpf = cx.sb("tmpf", [128, 128], F32)
    TOT, b_TOT = cx.sb("TOT", [128, NB, 8], F32)
    INC, b_INC = cx.sb("INC", [128, NB, 8], F32)
    WIN, b_WIN = cx.sb("WIN", [128, NB, 8], F32)
    SL, b_SL = cx.sb("SL", [128, NB, 8], F32)
    NSL, b_NSL = cx.sb("NSL", [128, 8], F32)
    NSLi, b_NSLi = cx.sb("NSLi", [128, 8], I32)
    SEND, b_SEND = cx.sb("SEND", [128, 8], F32)
    OFF, b_OFF = cx.sb("OFF", [128, 8], F32)
    one8, b_one8 = cx.sb("one8", [128, 8], F32)
    SF, b_SF = cx.sb("SF", [128, 2, NB], F32)
    SLOT, b_SLOT = cx.sb("SLOT", [128, 2, NB], I32)
    WGT, b_WGT = cx.sb("WGT", [128, 2, NB], F32)
    wtab, b_wtab = cx.sb("wtab", [128, NSLAB, 8], F32)
    CMP, b_CMP = cx.sb("CMP", [128, NSLAB, 8], F32)
    EW, b_EW = cx.sb("EW", [128, NSLAB], F32)
    ctab, b_ctab = cx.sb("ctab", [128, 7], F32)
    IDXf, b_IDXf = cx.sb("IDXf", [128, NSLAB, 7], F32)
    IDX, b_IDX = cx.sb("IDX", [128, NSLAB, 7], I32)
    m = cx.mark()
    xt, b_xt = cx.sb("xt", [128, 8, 1024], F32)
    ot, b_ot = cx.sb("ot", [128, 8, 1024], BF16)
    wo, b_wo = cx.sb("wo", [128, 8, 1024], BF16)
    rwT, b_rwT = cx.sb("rwT", [128, 8, NE], F32)
    gT1, b_gT1 = cx.sb("gT1", [128, 8], F32)
    xT32, b_xT32 = cx.sb("xT32", [128, 8, 128], F32)
    gff, b_gff = cx.sb("gff", [128, 1024], F32)
    hall, _ = cx.sb("hall", [128, NB, 1024], BF16)
    b_hall = kb.bufs_n("hall", NB)
    rot = PsRot(cx, [2, 3, 4, 5, 6, 7])
    kb.dma("pool", wo, din["w_o"].rearrange("(k p) n -> p k n", p=128), writes=[b_wo])
    kb.dma("sp", gff, din["g_ffn1"].partition_broadcast(128), writes=[b_gff])
    kb.dma("sp", rwT, din["router_w"].rearrange("(k p) e -> p k e", p=128), writes=[b_rwT])
    kb.dma("sp", gT1, din["gT_ffn1"], writes=[b_gT1])
    cx.tt("dve", rwT, rwT, gT1.unsqueeze(2).to_broadcast([128, 8, NE]), ALU.mult, [b_rwT, b_gT1], [b_rwT])
    for T in range(4):
        t0 = T * 1024
        kb.dma("sp", xt, xs[t0:t0 + 1024, :].rearrange("(b p) d -> p b d", p=128), reads=[b_xs], writes=[b_xt])
        kb.dma("sp", ot, OT[:, :, t0:t0 + 1024].rearrange("k p t -> p k t"), reads=[b_OT], writes=[b_ot])
        for b in range(8):
            for half in range(2):
                ps, bps = rot.next()
                for k in range(8):
                    cx.mm(ps, ot[:, k, b * 128:(b + 1) * 128], wo[:, k, half * 512:(half + 1) * 512], k == 0, k == 7,
                          [b_ot, b_wo], [bps])
                xv = xt[:, b, half * 512:(half + 1) * 512]
                cx.tt("dve", xv, xv, ps, ALU.add, [b_xt, bps], [b_xt])
        kb.dma("sp", xs[t0:t0 + 1024, :].rearrange("(b p) d -> p b d", p=128), xt, reads=[b_xt], writes=[b_xs])
        for b in range(8):
            cx.act(junk, xt[:, b, :], AF.Square, [b_xt], [b_junk, b_ss], accum=ss[:, b:b + 1])
        cx.ts("dve", ss, ss, 1.0 / D, EPS, ALU.mult, ALU.add, [b_ss], [b_ss])
        cx.act(ss, ss, AF.Sqrt, [b_ss], [b_ss])
        kb.op("dve", lambda g_: g_.reciprocal(out=ss, in_=ss), [b_ss], [b_ss])
        for b in range(8):
            gb = T * 8 + b
            cx.stt("dve", hall[:, gb, :], xt[:, b, :], ss[:, b:b + 1], gff, ALU.mult, ALU.mult,
                   [b_xt, b_ss, b_gff], [b_hall[gb]])
            pA, bpA = rot.next()
            pB, bpB = rot.next()
            for kc in range(8):
                pp, bpp = (pA, bpA) if kc < 4 else (pB, bpB)
                cx.tr(pp[:, (kc % 4) * 128:(kc % 4 + 1) * 128], xt[:, b, kc * 128:(kc + 1) * 128], identf, [b_xt, b_identf], [bpp])
            cx.cp("act", xT32[:, 0:4, :], pA.rearrange("p (k c) -> p k c", c=128), [bpA], [b_xT32])
            cx.cp("act", xT32[:, 4:8, :], pB.rearrange("p (k c) -> p k c", c=128), [bpB], [b_xT32])
            pL, bpL = rot.next()
            for kc in range(8):
                cx.mm(pL[:, 0:NE], xT32[:, kc, :], rwT[:, kc, :], kc == 0, kc == 7, [b_xT32, b_rwT], [bpL])
            cx.ts("dve", LG[:, gb, :], pL[:, 0:NE], ss[:, b:b + 1], None, ALU.mult, None, [bpL, b_ss], [b_LG])
            kb.op("dve", lambda g_, gb=gb: g_.max(out=m8, in_=LG[:, gb, :]), [b_LG], [b_m8])
            cx.cp("dve", TOP[:, gb, :], m8[:, 0:2], [b_m8], [b_TOP])
            cx.ts("dve", gsum[:, 0:1], m8[:, 0:1], -1.0, None, ALU.mult, None, [b_m8], [b_gsum])
            cx.act(GTS[:, gb, :], LG[:, gb, :], AF.Exp, [b_LG, b_gsum], [b_GTS], bias=gsum[:, 0:1])
            cx.stt("dve", GTS[:, gb, :], LG[:, gb, :], m8[:, 1:2], GTS[:, gb, :], ALU.is_ge, ALU.mult,
                   [b_LG, b_m8, b_GTS], [b_GTS])
            kb.op("dve", lambda g_, gb=gb: g_.reduce_sum(out=gsum[:, 1:2], in_=GTS[:, gb, :], axis=AX.X), [b_GTS], [b_gsum])
            kb.op("dve", lambda g_: g_.reciprocal(out=gsum[:, 1:2], in_=gsum[:, 1:2]), [b_gsum], [b_gsum])
            cx.ts("dve", GTS[:, gb, :], GTS[:, gb, :], gsum[:, 1:2], None, ALU.mult, None, [b_GTS, b_gsum], [b_GTS])
    kb.dma("sp", tmpf, din["ltri"], writes=[b_tmpf])
    cx.cp("dve", ltri, tmpf, [b_tmpf], [b_ltri])
    kb.dma("sp", wtab, din["wtab"].rearrange("p (w e) -> p w e", e=8), writes=[b_wtab])
    kb.dma("sp", ctab, din["ctab"], writes=[b_ctab])
    cx.memset("dve", onesb, 1.0, [b_onesb])
    cx.memset("dve", ones32, 1.0, [b_ones32])
    cx.memset("dve", one8, 1.0, [b_one8])
    bce = lambda t: t.unsqueeze(2).to_broadcast([128, NB, 8])
    cx.tt("dve", M1, LG, bce(TOP[:, :, 0]), ALU.is_equal, [b_LG, b_TOP], [b_M1])
    cx.tt("dve", M2, LG, bce(TOP[:, :, 1]), ALU.is_equal, [b_LG, b_TOP], [b_M2])
    cx.tt("dve", MSK, M1, M2, ALU.add, [b_M1, b_M2], [b_MSK])
    mskf = MSK.rearrange("p b e -> p (b e)")
    ps, bps = rot.next()
    cx.mm(ps[:, 0:256], ltri, mskf, True, True, [b_ltri, b_MSK], [bps])
    cx.cp("dve", WIN.rearrange("p b e -> p (b e)"), ps[:, 0:256], [bps], [b_WIN])
    ps, bps = rot.next()
    cx.mm(ps[:, 0:256], onesb, mskf, True, True, [b_onesb, b_MSK], [bps])
    cx.cp("dve", TOT.rearrange("p b e -> p (b e)"), ps[:, 0:256], [bps], [b_TOT])
    for e in range(NE):
        kb.op("dve", lambda g_, e=e: g_.tensor_tensor_scan(out=INC[:, :, e], data0=ones32, data1=TOT[:, :, e], initial=0.0,
                                                           op0=ALU.mult, op1=ALU.add), [b_ones32, b_TOT], [b_INC])
    cx.ts("dve", NSL, INC[:, NB - 1, :], float(SLAB - 1), 1.0 / SLAB, ALU.add, ALU.mult, [b_INC], [b_NSL])
    cx.ts("dve", NSL, NSL, -0.4995, None, ALU.add, None, [b_NSL], [b_NSL])
    cx.cp("dve", NSLi, NSL, [b_NSL], [b_NSLi])
    cx.cp("dve", NSL, NSLi, [b_NSLi], [b_NSL])
    kb.op("dve", lambda g_: g_.tensor_tensor_scan(out=SEND, data0=one8, data1=NSL, initial=0.0, op0=ALU.mult, op1=ALU.add),
          [b_one8, b_NSL], [b_SEND])
    cx.tt("dve", OFF, SEND, NSL, ALU.subtract, [b_SEND, b_NSL], [b_OFF])
    cx.ts("dve", OFF, OFF, float(SLAB), None, ALU.mult, None, [b_OFF], [b_OFF])
    cx.tt("dve", SL, INC, TOT, ALU.subtract, [b_INC, b_TOT], [b_SL])
    cx.tt("dve", SL, SL, WIN, ALU.add, [b_SL, b_WIN], [b_SL])
    cx.tt("dve", SL, SL, OFF.unsqueeze(1).to_broadcast([128, NB, 8]), ALU.add, [b_SL, b_OFF], [b_SL])
    for k, (Mk, b_Mk) in enumerate(((M1, b_M1), (M2, b_M2))):
        cx.tt("dve", TOT, Mk, SL, ALU.mult, [b_Mk, b_SL], [b_TOT])
        kb.op("dve", lambda g_, k=k: g_.reduce_sum(out=SF[:, k, :], in_=TOT, axis=AX.X), [b_TOT], [b_SF])
        cx.tt("dve", TOT, Mk, GTS, ALU.mult, [b_Mk, b_GTS], [b_TOT])
        kb.op("dve", lambda g_, k=k: g_.reduce_sum(out=WGT[:, k, :], in_=TOT, axis=AX.X), [b_TOT], [b_WGT])
    cx.cp("dve", SLOT, SF, [b_SF], [b_SLOT])
    cx.tt("dve", CMP, SEND.unsqueeze(1).to_broadcast([128, NSLAB, 8]), wtab, ALU.is_le, [b_SEND, b_wtab], [b_CMP])
    kb.op("dve", lambda g_: g_.reduce_sum(out=EW, in_=CMP, axis=AX.X), [b_CMP], [b_EW])
    cx.ts("dve", EW, EW, float(NE - 1), float(NGRP * 128), ALU.min, ALU.mult, [b_EW], [b_EW])
    cx.tt("dve", IDXf, EW.unsqueeze(2).to_broadcast([128, NSLAB, 7]), ctab.unsqueeze(1).to_broadcast([128, NSLAB, 7]),
          ALU.add, [b_EW, b_ctab], [b_IDXf])
    cx.cp("dve", IDX, IDXf, [b_IDXf], [b_IDX])
    for gb in range(NB):
        for k in range(2):
            kb.idma(HS, hall[:, gb, :], out_idx=SLOT[:, k, gb:gb + 1], bound=NSLAB * SLAB - 1,
                    reads=[b_hall[gb], b_SLOT], writes=[b_HS], disjoint=True)
    cx.release(m)
    m2 = cx.mark()
    NBK = SLAB // 128
    hsls = [cx.sb(f"hsl{i}", [128, NBK, 1024], BF16) for i in range(2)]
    hTs = [cx.sb(f"hTs{i}", [128, 8, SLAB], BF16) for i in range(2)]
    yacc, b_yacc = cx.sb("yacc", [128, NBK, 1024], F32)
    wsl = [cx.sb(f"wsl{i}", [128, 3 * 4096], BF16) for i in range(2)]
    actT = [cx.sb(f"actT{i}", [128, 4, SLAB], BF16) for i in range(2)]
    sg = [cx.sb(f"sg{i}", [128, 512], F32) for i in range(2)]
    NH2 = SLAB // 512
    wi = 0
    gi = 0

    def issue_w(w, g, slot):
        t, b = slot
        for j, kind in enumerate(("g", "u", "d")):
            kb.idma(t[:, j * 4096:(j + 1) * 4096], W16[kind], in_idx=IDX[:, w, g:g + 1], bound=NTOT - 1,
                    reads=[b_W16[kind], b_IDX], writes=[b], lane=b.name)

    def load_slab_dma(w):
        hsl, b_hsl = hsls[w % 2]
        kb.dma("sp", hsl, HS[w * SLAB:(w + 1) * SLAB, :].rearrange("(b p) d -> p b d", p=128), reads=[b_HS], writes=[b_hsl])

    def load_slab(w):
        hsl, b_hsl = hsls[w % 2]
        hT_, b_hT_ = hTs[w % 2]
        for b in range(NBK):
            pi = b % 2
            p16 = cx.ps[pi].bitcast(BF16).rearrange("p (k c) -> p k c", c=128)
            for kc in range(8):
                cx.tr(p16[:, kc, :], hsl[:, b, kc * 128:(kc + 1) * 128], identb, [b_hsl, b_identb], [cx.psb[pi]])
            cx.cp("act" if b % 2 else "dve", hT_[:, :, b * 128:(b + 1) * 128], p16, [cx.psb[pi]], [b_hT_])

    seq = [(w, g) for w in range(nslab) for g in range(NGRP)]
    issue_w(seq[0][0], seq[0][1], wsl[0])
    load_slab_dma(0)
    load_slab(0)
    for si, (w, g) in enumerate(seq):
        if si + 1 < len(seq):
            issue_w(seq[si + 1][0], seq[si + 1][1], wsl[(si + 1) % 2])
        wt, b_w = wsl[si % 2]
        gv = wt[:, 0:4096].rearrange("p (k n) -> p k n", n=512)
        uv = wt[:, 4096:8192].rearrange("p (k n) -> p k n", n=512)
        dv = wt[:, 8192:12288].rearrange("p (a d) -> p a d", d=1024)
        hT, b_hT = hTs[w % 2]
        if g == NGRP - 3 and w + 1 < nslab:
            load_slab_dma(w + 1)
        if g == NGRP - 1 and w + 1 < nslab:
            load_slab(w + 1)
        at, b_at = actT[gi % 2]
        gi += 1
        for f4 in range(4):
            for half in range(NH2):
                hs = slice(half * 512, (half + 1) * 512)
                pg, bpg = rot.next()
                pu, bpu = rot.next()
                for kc in range(8):
                    cx.mm(pg, gv[:, kc, f4 * 128:(f4 + 1) * 128], hT[:, kc, hs], kc == 0, kc == 7, [b_w, b_hT], [bpg])
                for kc in range(8):
                    cx.mm(pu, uv[:, kc, f4 * 128:(f4 + 1) * 128], hT[:, kc, hs], kc == 0, kc == 7, [b_w, b_hT], [bpu])
                s1, b_s1 = sg[(f4 * NH2 + half) % 2]
                cx.act(s1, pg, AF.Silu, [bpg], [b_s1])
                cx.tt("dve", at[:, f4, hs], s1, pu, ALU.mult, [b_s1, bpu], [b_at])
        for b in range(NBK):
            for half in range(2):
                ps, bps = rot.next()
                for f4 in range(4):
                    cx.mm(ps, at[:, f4, b * 128:(b + 1) * 128], dv[:, f4, half * 512:(half + 1) * 512],
                          f4 == 0, f4 == 3, [b_at, b_w], [bps])
                yv = yacc[:, b, half * 512:(half + 1) * 512]
                if g == 0:
                    cx.cp("act", yv, ps, [bps], [b_yacc])
                else:
                    cx.tt("dve", yv, yv, ps, ALU.add, [b_yacc, bps], [b_yacc])
        if g == NGRP - 1:
            kb.dma("sp", YS[w * SLAB:(w + 1) * SLAB, :].rearrange("(b p) d -> p b d", p=128), yacc, reads=[b_yacc], writes=[b_YS],
                   disjoint=True)
    cx.release(m2)
    NBUF3 = 4
    y1, b_y1 = cx.sb("y1", [128, NBUF3, 1024], F32)
    y2, b_y2 = cx.sb("y2", [128, NBUF3, 1024], F32)
    xb, b_xb = cx.sb("xb", [128, NBUF3, 1024], F32)
    gfin, b_gfin = cx.sb("gfin", [128, 1024], F32)
    s2, _ = cx.sb("s2", [128, NBUF3], F32)
    b_s2s = kb.bufs_n("s2_", NBUF3)
    kb.dma("sp", gfin, din["g_final"].partition_broadcast(128), writes=[b_gfin])
    b_y1s = kb.bufs_n("y1s", NBUF3); b_y2s = kb.bufs_n("y2s", NBUF3); b_xbs = kb.bufs_n("xbs", NBUF3)
    def fetch3(g2):
        i2 = g2 % NBUF3
        kb.dma("sp", xb[:, i2, :], xs[g2 * 128:(g2 + 1) * 128, :], reads=[b_xs], writes=[b_xbs[i2]])
        kb.idma(y1[:, i2, :], YS, in_idx=SLOT[:, 0, g2:g2 + 1], bound=NSLAB * SLAB - 1, reads=[b_YS, b_SLOT], writes=[b_y1s[i2]])
        kb.idma(y2[:, i2, :], YS, in_idx=SLOT[:, 1, g2:g2 + 1], bound=NSLAB * SLAB - 1, reads=[b_YS, b_SLOT], writes=[b_y2s[i2]])

    for g2 in range(NBUF3 - 1):
        fetch3(g2)
    for gb in range(NB):
        i = gb % NBUF3
        b_s2 = b_s2s[i]
        if gb + NBUF3 - 1 < NB:
            fetch3(gb + NBUF3 - 1)
        cx.stt("dve", xb[:, i, :], y1[:, i, :], WGT[:, 0, gb:gb + 1], xb[:, i, :], ALU.mult, ALU.add,
               [b_y1s[i], b_WGT, b_xbs[i]], [b_xbs[i]])
        cx.stt("dve", xb[:, i, :], y2[:, i, :], WGT[:, 1, gb:gb + 1], xb[:, i, :], ALU.mult, ALU.add,
               [b_y2s[i], b_WGT, b_xbs[i]], [b_xbs[i]])
        cx.act(junk, xb[:, i, :], AF.Square, [b_xbs[i]], [b_junk, b_s2], accum=s2[:, i:i + 1])
        cx.ts("dve", s2[:, i:i + 1], s2[:, i:i + 1], 1.0 / D, EPS, ALU.mult, ALU.add, [b_s2], [b_s2])
        cx.act(s2[:, i:i + 1], s2[:, i:i + 1], AF.Sqrt, [b_s2], [b_s2])
        kb.op("dve", lambda g_, i=i: g_.reciprocal(out=s2[:, i:i + 1], in_=s2[:, i:i + 1]), [b_s2], [b_s2])
        cx.stt("dve", xb[:, i, :], xb[:, i, :], s2[:, i:i + 1], gfin, ALU.mult, ALU.mult, [b_xbs[i], b_s2, b_gfin], [b_xbs[i]])
        kb.dma("sp", out[gb * 128:(gb + 1) * 128, :], xb[:, i, :], reads=[b_xbs[i]], writes=[b_out], disjoint=True)
    cx.release(mp)
```

```python
import math
import numpy as np
import ml_dtypes
import concourse.bass as bass
import concourse.mybir as mybir
from concourse.bass_utils import run_bass_kernel_spmd

F32 = mybir.dt.float32
BF16 = mybir.dt.bfloat16
I32 = mybir.dt.int32
AF = mybir.ActivationFunctionType
ALU = mybir.AluOpType
AX = mybir.AxisListType

L = 4096
D = 1024
NB = L // 128
G = 64
GS = 16
PS = 64
TCH = 8
D_FF = 2688
NE = 8
MOE_FF = 3584
NH = 16
QK_NOPE = 64
QK_ROPE = 32
V_HEAD = 64
Q_LORA = 512
KV_LORA = 256
EPS = 1e-6
DT_MIN = 1e-3
DT_MAX = 1e-1
SLAB = 1024
NSLAB = (2 * L) // SLAB + NE
NGRP = MOE_FF // 512


class Buf:
    __slots__ = ("name", "w", "r")

    def __init__(self, name):
        self.name = name
        self.w = {}
        self.r = {}


class KB:
    def __init__(self, nc):
        self.nc = nc
        self.eng = {"pe": nc.tensor, "act": nc.scalar, "dve": nc.vector,
                    "pool": nc.gpsimd, "sp": nc.sync}
        self.sem = {k: nc.alloc_semaphore(name="sem_" + k) for k in self.eng}
        self.cnt = {k: 0 for k in self.eng}
        self.known = {k: {} for k in self.eng}
        self.lanes = {}
        self.bufs = []
        self.n_ins = 0

    def _lane(self, lane):
        if lane not in self.lanes:
            pool = self.__dict__.setdefault("_lane_pool", [])
            if pool:
                self.lanes[lane] = pool.pop()
            else:
                self._nl = getattr(self, "_nl", 0) + 1
                self.lanes[lane] = [self.nc.alloc_semaphore(name=f"ln{self._nl}"), 0]

    def buf(self, name):
        b = Buf(name)
        self.bufs.append(b)
        return b

    def bufs_n(self, name, n):
        return [self.buf(f"{name}{i}") for i in range(n)]

    def _semof(self, key):
        if key[0] == "e":
            return self.sem[key[1]]
        return self.lanes[key[1]][0]

    def _need(self, reads, writes):
        need = {}
        for b in reads:
            for k, v in b.w.items():
                if need.get(k, 0) < v:
                    need[k] = v
        for b in writes:
            for k, v in b.w.items():
                if need.get(k, 0) < v:
                    need[k] = v
            for k, v in b.r.items():
                if need.get(k, 0) < v:
                    need[k] = v
        return need

    def _wait(self, e, need):
        kn = self.known[e]
        for k, v in need.items():
            if e == "pe" and k == ("e", "pe"):
                continue
            if kn.get(k, 0) >= v:
                continue
            self.eng[e].wait_ge(self._semof(k), v)
            kn[k] = v

    def op(self, e, fn, reads=(), writes=()):
        self._wait(e, self._need(reads, writes))
        ins = fn(self.eng[e])
        self.cnt[e] += 1
        c = self.cnt[e]
        ins.then_inc(self.sem[e], 1)
        key = ("e", e)
        for b in reads:
            b.r[key] = c
        for b in writes:
            b.w = {key: c}
            b.r = {}
        self.n_ins += 1
        return ins

    def dma(self, q, out, in_, reads=(), writes=(), lane=None, disjoint=False, **kw):
        if lane is None:
            lane = writes[0].name
        self._lane(lane)
        need = self._need(reads, writes)
        if disjoint:
            need.pop(("l", lane), None)
        self._wait(q, need)
        ins = self.eng[q].dma_start(out=out, in_=in_, **kw)
        ln = self.lanes[lane]
        ln[1] += 16
        ins.then_inc(ln[0], 16)
        key = ("l", lane)
        for b in reads:
            b.r[key] = ln[1]
        for b in writes:
            neww = {k: v for k, v in b.w.items() if k[0] == "l" and k != key}
            neww[key] = ln[1]
            b.w = neww
            b.r = {}
        self.n_ins += 1
        return ins

    def idma(self, out, in_, out_idx=None, in_idx=None, bound=None, reads=(), writes=(), lane=None, disjoint=False):
        if lane is None:
            lane = writes[0].name
        self._lane(lane)
        need = self._need(reads, writes)
        if disjoint:
            need.pop(("l", lane), None)
        self._wait("pool", need)
        oo = bass.IndirectOffsetOnAxis(ap=out_idx, axis=0) if out_idx is not None else None
        io = bass.IndirectOffsetOnAxis(ap=in_idx, axis=0) if in_idx is not None else None
        ins = self.nc.gpsimd.indirect_dma_start(out=out, out_offset=oo, in_=in_, in_offset=io)
        ln = self.lanes[lane]
        ln[1] += 16
        ins.then_inc(ln[0], 16)
        key = ("l", lane)
        for b in reads:
            b.r[key] = ln[1]
        for b in writes:
            neww = {k: v for k, v in b.w.items() if k[0] == "l" and k != key}
            neww[key] = ln[1]
            b.w = neww
            b.r = {}
        self.n_ins += 1
        return ins

    def finish(self, bufs):
        need = {}
        for b in bufs:
            for k, v in b.w.items():
                need[k] = max(need.get(k, 0), v)
        self._wait("sp", need)
        allneed = {("l", ln): v[1] for ln, v in self.lanes.items() if v[1] > 0}
        for e in self.eng:
            if self.cnt[e] > 0:
                allneed[("e", e)] = self.cnt[e]
        allneed.pop(("e", "sp"), None)
        self._wait("sp", allneed)

    def barrier(self):
        need = {("l", ln): v[1] for ln, v in self.lanes.items() if v[1] > 0}
        for e in self.eng:
            if self.cnt[e] > 0:
                need[("e", e)] = self.cnt[e]
        for e in self.eng:
            n2 = dict(need)
            n2.pop(("e", e), None)
            self._wait(e, n2)
        for b in self.bufs:
            b.w = {}
            b.r = {}
        pool = self.__dict__.setdefault("_lane_pool", [])
        for ln, v in self.lanes.items():
            pool.append(v)
        self.lanes = {}
        for e in self.eng:
            self.known[e] = {k: v for k, v in self.known[e].items() if k[0] == "e"}


class Ctx:
    def __init__(self, nc):
        self.nc = nc
        self.kb = KB(nc)
        self.ps = []
        self.psb = []
        for i in range(8):
            self.ps.append(nc.alloc_psum_tensor(f"ps{i}", [128, 512], F32).ap())
            self.psb.append(self.kb.buf(f"ps{i}"))
        self._mark = None

    def sb(self, name, shape, dtype=F32):
        self._n = getattr(self, "_n", 0) + 1
        t = self.nc.alloc_sbuf_tensor(f"sb{self._n}_{name}", list(shape), dtype).ap()
        return t, self.kb.buf(f"sb{self._n}_{name}")

    def mark(self):
        return (self.nc.sbuf_base, self.nc.sbuf_top)

    def release(self, m):
        self.kb.barrier()
        self.nc.sbuf_base, self.nc.sbuf_top = m

    def tt(self, e, out, in0, in1, op, r, w):
        return self.kb.op(e, lambda g: g.tensor_tensor(out=out, in0=in0, in1=in1, op=op), r, w)

    def ts(self, e, out, in0, s1, s2, op0, op1, r, w):
        if s2 is None:
            return self.kb.op(e, lambda g: g.tensor_scalar(out=out, in0=in0, scalar1=s1, scalar2=None, op0=op0), r, w)
        return self.kb.op(e, lambda g: g.tensor_scalar(out=out, in0=in0, scalar1=s1, scalar2=s2, op0=op0, op1=op1), r, w)

    def stt(self, e, out, in0, scalar, in1, op0, op1, r, w):
        return self.kb.op(e, lambda g: g.scalar_tensor_tensor(out=out, in0=in0, scalar=scalar, in1=in1, op0=op0, op1=op1), r, w)

    def cp(self, e, out, in_, r, w):
        if e == "act":
            return self.kb.op(e, lambda g: g.activation(out=out, in_=in_, func=AF.Copy), r, w)
        return self.kb.op(e, lambda g: g.tensor_copy(out=out, in_=in_), r, w)

    def act(self, out, in_, func, r, w, scale=1.0, bias=None, accum=None):
        kw = {}
        if bias is not None:
            kw["bias"] = bias
        if accum is not None:
            kw["accum_out"] = accum
        return self.kb.op("act", lambda g: g.activation(out=out, in_=in_, func=func, scale=scale, **kw), r, w)

    def mm(self, out, lhsT, rhs, start, stop, r, w, **kw):
        return self.kb.op("pe", lambda g: g.matmul(out, lhsT=lhsT, rhs=rhs, start=start, stop=stop, **kw), r, w)

    def tr(self, out, in_, ident, r, w):
        return self.kb.op("pe", lambda g: g.transpose(out=out, in_=in_, identity=ident), r, w)

    def memset(self, e, out, val, w):
        return self.kb.op(e, lambda g: g.memset(out, val), (), w)


PI = math.pi


def setup_ident(cx, din):
    P = {"bufs": {}}
    P["identf"], bf = cx.sb("identf", [128, 128], F32)
    P["identb"], bb = cx.sb("identb", [128, 128], BF16)
    cx.kb.dma("sp", P["identf"], din["ident"], writes=[bf])
    cx.cp("dve", P["identb"], P["identf"], [bf], [bb])
    P["bufs"]["identf"], P["bufs"]["identb"] = bf, bb
    return P


def phase0(cx, P, din, pre_hook=None):
    nc, kb = cx.nc, cx.kb
    b_identf, b_identb = P["bufs"]["identf"], P["bufs"]["identb"]
    P["WBre"], b_WBre = cx.sb("WBre", [128, 8, 8, 128], BF16)
    P["WBim"], b_WBim = cx.sb("WBim", [128, 8, 8, 128], BF16)
    P["WCre"], b_WCre = cx.sb("WCre", [128, 32, 8, 2, 16], BF16)
    P["WCim"], b_WCim = cx.sb("WCim", [128, 32, 8, 2, 16], BF16)
    P["FIRW"], b_FIRW = cx.sb("FIRW", [128, 8, 8, 128], BF16)
    P["A0c"], b_A0c = cx.sb("A0c", [128, 32, 8], F32)
    P["A0s"], b_A0s = cx.sb("A0s", [128, 32, 8], F32)
    P["A1c"], b_A1c = cx.sb("A1c", [128, 32, 8], F32)
    P["A1s"], b_A1s = cx.sb("A1s", [128, 32, 8], F32)
    P["RM8"], b_RM8 = cx.sb("RM8", [128, 32], F32)
    P["dT"], b_dT = cx.sb("dT", [128, 8], F32)
    P["bufs"].update(dict(WBre=b_WBre, WBim=b_WBim, WCre=b_WCre,
                          WCim=b_WCim, FIRW=b_FIRW, A0c=b_A0c, A0s=b_A0s, A1c=b_A1c, A1s=b_A1s, RM8=b_RM8, dT=b_dT))
    m = cx.mark()
    if pre_hook is not None:
        pre_hook()
    LR, b_LR = cx.sb("LR", [128, 32]); LI, b_LI = cx.sb("LI", [128, 32]); LDT, b_LDT = cx.sb("LDT", [128, 32])
    TH, b_TH = cx.sb("TH", [128, 32]); LM, b_LM = cx.sb("LM", [128, 32])
    EV, b_EV = cx.sb("EV", [128, 9, 32]); ANG, b_ANG = cx.sb("ANG", [128, 9, 32]); MAG, b_MAG = cx.sb("MAG", [128, 9, 32])
    SN, b_SN = cx.sb("SN", [128, 9, 32]); CS, b_CS = cx.sb("CS", [128, 9, 32]); IT, b_IT = cx.sb("IT", [128, 9, 32], I32)
    ARE, b_ARE = cx.sb("ARE", [128, 9, 32]); AIM, b_AIM = cx.sb("AIM", [128, 9, 32])
    NR, b_NR = cx.sb("NR", [128, 32]); DEN, b_DEN = cx.sb("DEN", [128, 32]); TMPa, b_TMPa = cx.sb("TMPa", [128, 32])
    TMPb, b_TMPb = cx.sb("TMPb", [128, 32])
    CRE, b_CRE = cx.sb("CRE", [128, 32]); CIM, b_CIM = cx.sb("CIM", [128, 32])
    WRE, b_WRE = cx.sb("WRE", [128, 8, 32]); WIM, b_WIM = cx.sb("WIM", [128, 8, 32])
    W8a, b_W8a = cx.sb("W8a", [128, 8, 32]); W8b, b_W8b = cx.sb("W8b", [128, 8, 32])
    BTre, b_BTre = cx.sb("BTre", [128, 32, 16]); BTim, b_BTim = cx.sb("BTim", [128, 32, 16])
    CTre, b_CTre = cx.sb("CTre", [128, 32, 16]); CTim, b_CTim = cx.sb("CTim", [128, 32, 16])
    T1, b_T1 = cx.sb("T1", [128, 32, 16]); T2, b_T2 = cx.sb("T2", [128, 32, 16])
    T3, b_T3 = cx.sb("T3", [128, 32, 16]); T4, b_T4 = cx.sb("T4", [128, 32, 16])
    XPre, b_XPre = cx.sb("XPre", [128, 8, 32, 2, 16], BF16); XPim, b_XPim = cx.sb("XPim", [128, 8, 32, 2, 16], BF16)
    CPre, b_CPre = cx.sb("CPre", [128, 32, 2, 16], BF16); CPnim, b_CPnim = cx.sb("CPnim", [128, 32, 2, 16], BF16)
    BM, b_BM = cx.sb("BM", [128, 128], F32)
    TK, b_TK = cx.sb("TK", [128, 128], F32)

    kb.dma("sp", BM, din["bmask"], writes=[b_BM])
    kb.dma("sp", EV, din["ev"].rearrange("p (e q) -> p e q", e=9), writes=[b_EV])
    kb.dma("sp", LR, din["lamT_re"], writes=[b_LR])
    kb.dma("sp", LI, din["lamT_im"], writes=[b_LI])
    kb.dma("sp", LDT, din["ldtT"], writes=[b_LDT])
    kb.dma("sp", P["dT"], din["dT"], writes=[b_dT])
    kb.dma("sp", BTre, din["bT_re"].rearrange("p (q n) -> p q n", n=16), writes=[b_BTre])
    kb.dma("sp", BTim, din["bT_im"].rearrange("p (q n) -> p q n", n=16), writes=[b_BTim])
    kb.dma("sp", CTre, din["cT_re"].rearrange("p (q n) -> p q n", n=16), writes=[b_CTre])
    kb.dma("sp", CTim, din["cT_im"].rearrange("p (q n) -> p q n", n=16), writes=[b_CTim])

    cx.act(LDT, LDT, AF.Exp, [b_LDT], [b_LDT])
    cx.tt("dve", TH, LI, LDT, ALU.mult, [b_LI, b_LDT], [b_TH])
    cx.tt("dve", LM, LR, LDT, ALU.mult, [b_LR, b_LDT], [b_LM])
    bc9 = lambda t: t.unsqueeze(1).to_broadcast([128, 9, 32])
    bc8 = lambda t: t.unsqueeze(1).to_broadcast([128, 8, 32])
    cx.tt("dve", ANG, EV, bc9(TH), ALU.mult, [b_EV, b_TH], [b_ANG])
    cx.tt("dve", MAG, EV, bc9(LM), ALU.mult, [b_EV, b_LM], [b_MAG])
    cx.act(MAG, MAG, AF.Exp, [b_MAG], [b_MAG])
    cx.ts("dve", SN, ANG, 1.0 / (2.0 * PI), None, ALU.mult, None, [b_ANG], [b_SN])
    cx.cp("dve", IT, SN, [b_SN], [b_IT])
    cx.cp("dve", SN, IT, [b_IT], [b_SN])
    cx.stt("dve", SN, SN, -2.0 * PI, ANG, ALU.mult, ALU.add, [b_SN, b_ANG], [b_SN])
    cx.ts("dve", CS, ANG, 1.0 / (2.0 * PI), 0.25, ALU.mult, ALU.add, [b_ANG], [b_CS])
    cx.cp("dve", IT, CS, [b_CS], [b_IT])
    cx.cp("dve", CS, IT, [b_IT], [b_CS])
    cx.stt("dve", CS, CS, -2.0 * PI, ANG, ALU.mult, ALU.add, [b_CS, b_ANG], [b_CS])
    cx.ts("dve", CS, CS, 0.5 * PI, None, ALU.add, None, [b_CS], [b_CS])
    cx.ts("dve", SN, SN, -PI, PI, ALU.max, ALU.min, [b_SN], [b_SN])
    cx.ts("dve", CS, CS, -PI, PI, ALU.max, ALU.min, [b_CS], [b_CS])
    cx.act(SN, SN, AF.Sin, [b_SN], [b_SN])
    cx.act(CS, CS, AF.Sin, [b_CS], [b_CS])
    cx.tt("dve", ARE, MAG, CS, ALU.mult, [b_MAG, b_CS], [b_ARE])
    cx.tt("dve", AIM, MAG, SN, ALU.mult, [b_MAG, b_SN], [b_AIM])
    cx.ts("dve", NR, ARE[:, 1, :], -1.0, None, ALU.add, None, [b_ARE], [b_NR])
    NI = AIM[:, 1, :]
    cx.tt("dve", DEN, LR, LR, ALU.mult, [b_LR], [b_DEN])
    cx.tt("dve", TMPa, LI, LI, ALU.mult, [b_LI], [b_TMPa])
    cx.tt("dve", DEN, DEN, TMPa, ALU.add, [b_DEN, b_TMPa], [b_DEN])
    kb.op("dve", lambda g: g.reciprocal(out=DEN, in_=DEN), [b_DEN], [b_DEN])
    cx.tt("dve", TMPa, NR, LR, ALU.mult, [b_NR, b_LR], [b_TMPa])
    cx.tt("dve", TMPb, NI, LI, ALU.mult, [b_AIM, b_LI], [b_TMPb])
    cx.tt("dve", TMPa, TMPa, TMPb, ALU.add, [b_TMPa, b_TMPb], [b_TMPa])
    cx.tt("dve", CRE, TMPa, DEN, ALU.mult, [b_TMPa, b_DEN], [b_CRE])
    cx.tt("dve", TMPa, NI, LR, ALU.mult, [b_AIM, b_LR], [b_TMPa])
    cx.tt("dve", TMPb, NR, LI, ALU.mult, [b_NR, b_LI], [b_TMPb])
    cx.tt("dve", TMPa, TMPa, TMPb, ALU.subtract, [b_TMPa, b_TMPb], [b_TMPa])
    cx.tt("dve", CIM, TMPa, DEN, ALU.mult, [b_TMPa, b_DEN], [b_CIM])
    cx.tt("dve", W8a, ARE[:, 0:8, :], bc8(CRE), ALU.mult, [b_ARE, b_CRE], [b_W8a])
    cx.tt("dve", W8b, AIM[:, 0:8, :], bc8(CIM), ALU.mult, [b_AIM, b_CIM], [b_W8b])
    cx.tt("dve", WRE, W8a, W8b, ALU.subtract, [b_W8a, b_W8b], [b_WRE])
    cx.tt("dve", W8a, ARE[:, 0:8, :], bc8(CIM), ALU.mult, [b_ARE, b_CIM], [b_W8a])
    cx.tt("dve", W8b, AIM[:, 0:8, :], bc8(CRE), ALU.mult, [b_AIM, b_CRE], [b_W8b])
    cx.tt("dve", WIM, W8a, W8b, ALU.add, [b_W8a, b_W8b], [b_WIM])
    cx.cp("dve", P["RM8"], MAG[:, 8, :], [b_MAG], [b_RM8])
    A0c, A0s, A1c, A1s = P["A0c"], P["A0s"], P["A1c"], P["A1s"]
    cx.cp("dve", A0c[:, :, 0], CS[:, 8, :], [b_CS], [b_A0c])
    cx.cp("dve", A0s[:, :, 0], SN[:, 8, :], [b_SN], [b_A0s])
    for i in range(1, 8):
        cx.tt("dve", TMPa, A0c[:, :, i - 1], A0c[:, :, 0], ALU.mult, [b_A0c], [b_TMPa])
        cx.tt("dve", TMPb, A0s[:, :, i - 1], A0s[:, :, 0], ALU.mult, [b_A0s], [b_TMPb])
        cx.tt("dve", A0c[:, :, i], TMPa, TMPb, ALU.subtract, [b_TMPa, b_TMPb], [b_A0c])
        cx.tt("dve", TMPa, A0c[:, :, i - 1], A0s[:, :, 0], ALU.mult, [b_A0c, b_A0s], [b_TMPa])
        cx.tt("dve", TMPb, A0s[:, :, i - 1], A0c[:, :, 0], ALU.mult, [b_A0s, b_A0c], [b_TMPb])
        cx.tt("dve", A0s[:, :, i], TMPa, TMPb, ALU.add, [b_TMPa, b_TMPb], [b_A0s])
    cx.memset("dve", A1c[:, :, 0], 1.0, [b_A1c])
    cx.memset("dve", A1s[:, :, 0], 0.0, [b_A1s])
    for i in range(1, 8):
        cx.tt("dve", TMPa, A1c[:, :, i - 1], A0c[:, :, 7], ALU.mult, [b_A1c, b_A0c], [b_TMPa])
        cx.tt("dve", TMPb, A1s[:, :, i - 1], A0s[:, :, 7], ALU.mult, [b_A1s, b_A0s], [b_TMPb])
        cx.tt("dve", A1c[:, :, i], TMPa, TMPb, ALU.subtract, [b_TMPa, b_TMPb], [b_A1c])
        cx.tt("dve", TMPa, A1c[:, :, i - 1], A0s[:, :, 7], ALU.mult, [b_A1c, b_A0s], [b_TMPa])
        cx.tt("dve", TMPb, A1s[:, :, i - 1], A0c[:, :, 7], ALU.mult, [b_A1s, b_A0c], [b_TMPb])
        cx.tt("dve", A1s[:, :, i], TMPa, TMPb, ALU.add, [b_TMPa, b_TMPb], [b_A1s])

    cx.memset("pool", XPre, 0.0, [b_XPre])
    cx.memset("pool", XPim, 0.0, [b_XPim])
    cx.memset("pool", CPre, 0.0, [b_CPre])
    cx.memset("pool", CPnim, 0.0, [b_CPnim])
    cx.memset("pool", P["WCre"], 0.0, [b_WCre])
    cx.memset("pool", P["WCim"], 0.0, [b_WCim])
    bcn = lambda t: t.unsqueeze(2).to_broadcast([128, 32, 16])
    for s in range(8):
        e = 7 - s
        cx.tt("dve", T1, BTre, bcn(WRE[:, e, :]), ALU.mult, [b_BTre, b_WRE], [b_T1])
        cx.tt("dve", T2, BTim, bcn(WIM[:, e, :]), ALU.mult, [b_BTim, b_WIM], [b_T2])
        cx.tt("dve", T3, BTim, bcn(WRE[:, e, :]), ALU.mult, [b_BTim, b_WRE], [b_T3])
        cx.tt("dve", T4, BTre, bcn(WIM[:, e, :]), ALU.mult, [b_BTre, b_WIM], [b_T4])
        for par in range(2):
            sl = slice(64 * par, 64 * par + 64)
            cx.tt("dve", XPre[sl, s, :, par, :], T1[sl], T2[sl], ALU.subtract, [b_T1, b_T2], [b_XPre])
            cx.tt("dve", XPim[sl, s, :, par, :], T3[sl], T4[sl], ALU.add, [b_T3, b_T4], [b_XPim])
    for par in range(2):
        sl = slice(64 * par, 64 * par + 64)
        cx.cp("dve", CPre[sl, :, par, :], CTre[sl], [b_CTre], [b_CPre])
        cx.ts("dve", CPnim[sl, :, par, :], CTim[sl], -1.0, None, ALU.mult, None, [b_CTim], [b_CPnim])
    for j in range(8):
        e = j + 1
        cx.tt("dve", T1, CTre, bcn(ARE[:, e, :]), ALU.mult, [b_CTre, b_ARE], [b_T1])
        cx.tt("dve", T2, CTim, bcn(AIM[:, e, :]), ALU.mult, [b_CTim, b_AIM], [b_T2])
        cx.tt("dve", T3, CTre, bcn(AIM[:, e, :]), ALU.mult, [b_CTre, b_AIM], [b_T3])
        cx.tt("dve", T4, CTim, bcn(ARE[:, e, :]), ALU.mult, [b_CTim, b_ARE], [b_T4])
        for par in range(2):
            sl = slice(64 * par, 64 * par + 64)
            cx.tt("dve", P["WCre"][sl, :, j, par, :], T1[sl], T2[sl], ALU.subtract, [b_T1, b_T2], [b_WCre])
            cx.stt("dve", P["WCim"][sl, :, j, par, :], T3[sl], -1.0, T4[sl], ALU.mult, ALU.subtract,
                   [b_T3, b_T4], [b_WCim])
    for fc in range(8):
        pq = slice(4 * fc, 4 * fc + 4)
        for k in range(8):
            pi = (fc * 8 + k) % 4
            ps, bps = cx.ps[pi], cx.psb[pi]
            cx.mm(ps[:, 0:128], XPre[:, 7 - k, pq, :, :], CPre[:, pq, :, :], True, False, [b_XPre, b_CPre], [bps])
            cx.mm(ps[:, 0:128], XPim[:, 7 - k, pq, :, :], CPnim[:, pq, :, :], False, True, [b_XPim, b_CPnim], [bps])
            if k == 0:
                cx.tt("dve", TK, ps[:, 0:128], BM, ALU.mult, [bps, b_BM], [b_TK])
                cx.stt("dve", P["FIRW"][:, fc, 0, :], P["identf"], P["dT"][:, fc:fc + 1], TK, ALU.mult, ALU.add,
                       [b_identf, b_dT, b_TK], [b_FIRW])
            else:
                cx.tt("dve", P["FIRW"][:, fc, k, :], ps[:, 0:128], BM, ALU.mult, [bps, b_BM], [b_FIRW])
    for (XP, b_XP, WB, b_WB) in ((XPre, b_XPre, P["WBre"], b_WBre), (XPim, b_XPim, P["WBim"], b_WBim)):
        for fc in range(8):
            pq = slice(4 * fc, 4 * fc + 4)
            pi = 4 + (fc % 2)
            psb16 = cx.ps[pi].bitcast(BF16).rearrange("p (s c) -> p s c", s=8)
            for s in range(8):
                cx.tr(psb16[:, s, :], XP[:, s, pq, :, :], P["identb"], [b_XP, b_identb], [cx.psb[pi]])
            cx.cp("act" if fc % 2 else "dve", WB[:, fc, :, :], psb16, [cx.psb[pi]], [b_WB])
    cx.release(m)
    return P


class PsRot:
    def __init__(self, cx, banks):
        self.cx = cx
        self.banks = list(banks)
        self.i = 0

    def next(self):
        b = self.banks[self.i % len(self.banks)]
        self.i += 1
        return self.cx.ps[b], self.cx.psb[b]


class WStream:
    def __init__(self, cx, nslots, slot_elems, name="ws", direct=None, ahead=None):
        self.cx = cx
        self.slots = []
        for i in range(nslots):
            t, b = cx.sb(f"{name}{i}", [128, slot_elems], BF16)
            self.slots.append((t, b))
        self.jobs = []
        self.issued = 0
        self.used = 0
        self.res = {}
        self.direct = direct
        self.ahead = ahead

    def plan(self, jobs):
        self.jobs.extend(jobs)

    def _issue(self, i):
        t, b = self.slots[i % len(self.slots)]
        off = 0
        views = []
        if self.direct is not None:
            src, n = self.jobs[i]
            self.cx.kb.dma("sp", t[:, 0:n], src, reads=[self.direct], writes=[b])
            self.res[i] = (t, b)
            return
        for (src, a, n) in self.jobs[i]:
            v = t[:, off:off + a * n].rearrange("p (a n) -> p a n", n=n)
            self.cx.kb.dma("pool", v, src, writes=[b])
            views.append(v)
            off += a * n
        self.res[i] = (views, b)

    def get(self):
        i = self.used
        ahead = self.ahead if self.ahead is not None else max(1, len(self.slots) - 2)
        while self.issued < min(len(self.jobs), i + 1 + ahead):
            self._issue(self.issued)
            self.issued += 1
        self.used += 1
        return self.res.pop(i)


def rms_to_hT(cx, P, xt, b_xt, nblk, gT, b_gT, htok, b_htok, hT, b_hT, ss, b_ss, trbanks, d=D, extra=None):
    nkc = d // 128
    bx = b_xt if isinstance(b_xt, list) else [b_xt]
    for b in range(nblk):
        cx.act(htok[:, b, :], xt[:, b, :], AF.Square, bx, [b_htok, b_ss], accum=ss[:, b:b + 1])
    cx.ts("dve", ss[:, 0:nblk], ss[:, 0:nblk], 1.0 / d, EPS, ALU.mult, ALU.add, [b_ss], [b_ss])
    cx.act(ss[:, 0:nblk], ss[:, 0:nblk], AF.Sqrt, [b_ss], [b_ss])
    cx.kb.op("dve", lambda g: g.reciprocal(out=ss[:, 0:nblk], in_=ss[:, 0:nblk]), [b_ss], [b_ss])
    for b in range(nblk):
        cx.ts("dve", htok[:, b, :], xt[:, b, :], ss[:, b:b + 1], None, ALU.mult, None, bx + [b_ss], [b_htok])
    for b in range(nblk):
        pi = trbanks[b % len(trbanks)]
        p16 = cx.ps[pi].bitcast(BF16).rearrange("p (k c) -> p k c", c=128)
        for kc in range(nkc):
            cx.tr(p16[:, kc, :], htok[:, b, kc * 128:(kc + 1) * 128], P["identb"], [b_htok, P["bufs"]["identb"]], [cx.psb[pi]])
        cx.tt("dve", hT[:, 0:nkc, b * 128:(b + 1) * 128], p16[:, 0:nkc, :],
              gT[:, 0:nkc].unsqueeze(2).to_broadcast([128, nkc, 128]), ALU.mult, [cx.psb[pi], b_gT], [b_hT])
        if extra is not None:
            gT2, hT2, b_hT2 = extra
            cx.tt("dve", hT2[:, 0:nkc, b * 128:(b + 1) * 128], p16[:, 0:nkc, :],
                  gT2[:, 0:nkc].unsqueeze(2).to_broadcast([128, nkc, 128]), ALU.mult, [cx.psb[pi], b_gT], [b_hT2])


N_L0_JOBS = 6 + D_FF // 128


def layer0_convert(cx, din, W0, b_W0, nstage=4):
    kb = cx.kb
    stage = [cx.sb(f"l0st{i}", [128, 4096], BF16) for i in range(nstage)]
    j = 0
    for w in (din["s5_w_in"], din["s5_w_glu"], din["s5_w_out"]):
        for half in range(2):
            st, b_st = stage[j % nstage]
            kb.dma("pool", st.rearrange("p (k n) -> p k n", n=512),
                   w[:, half * 512:(half + 1) * 512].rearrange("(k p) n -> p k n", p=128), writes=[b_st])
            kb.dma("sp", W0[j * 128:(j + 1) * 128, :], st, reads=[b_st], writes=[b_W0], disjoint=True)
            j += 1
    for f in range(D_FF // 128):
        st, b_st = stage[j % nstage]
        cs = slice(f * 128, (f + 1) * 128)
        kb.dma("pool", st[:, 0:1024].rearrange("p (k n) -> p k n", n=128),
               din["ffn_w_gate"][:, cs].rearrange("(k p) n -> p k n", p=128), writes=[b_st])
        kb.dma("pool", st[:, 1024:2048].rearrange("p (k n) -> p k n", n=128),
               din["ffn_w_up"][:, cs].rearrange("(k p) n -> p k n", p=128), writes=[b_st])
        kb.dma("pool", st[:, 2048:3072], din["ffn_w_down"][cs, :], writes=[b_st])
        kb.dma("sp", W0[j * 128:(j + 1) * 128, 0:3072], st[:, 0:3072], reads=[b_st], writes=[b_W0], disjoint=True)
        j += 1


GELU_C = 0.044715
GELU_S = 2.0 * math.sqrt(2.0 / math.pi)


def phase1(cx, P, din, xs, b_xs, W0, b_W0, ntiles=8, hook=None):
    nc, kb = cx.nc, cx.kb
    B = P["bufs"]
    m = cx.mark()
    xt, _ = cx.sb("xt", [128, 4, 1024], F32)
    b_xt = kb.bufs_n("xt8_", 8)
    regA, b_A = cx.sb("regA", [128, 4096], F32)
    regB, b_B = cx.sb("regB", [128, 2048], F32)
    regC, b_C = cx.sb("regC", [128, 2048], F32)
    uT, _ = cx.sb("uT", [128, 8, 512], BF16)
    b_u = kb.bufs_n("uT", 8)
    yT, _ = cx.sb("yT", [128, 8, 512], BF16)
    b_y = kb.bufs_n("yT", 8)
    SR, b_SR = cx.sb("SR", [128, 32, 65], F32)
    SI, b_SI = cx.sb("SI", [128, 32, 65], F32)
    SB, b_SB = cx.sb("SB", [128, 32, 2, 64], BF16)
    ss, b_ss = cx.sb("ss", [128, 8], F32)
    gT, b_gT = cx.sb("gT", [128, 2, 8], F32)
    CAR, b_CAR = cx.sb("CAR", [128, 2, 32], F32)
    ws = WStream(cx, 3, 4096, direct=b_W0, ahead=2)
    y32 = regA.rearrange("p (f t) -> p f t", t=512)
    t3 = regA[:, 0:2048].rearrange("p (q c) -> p q c", c=64)
    t4 = regA[:, 2048:4096].rearrange("p (q c) -> p q c", c=64)
    htok = regB.bitcast(BF16).rearrange("p (b d) -> p b d", d=1024)
    t1 = regB.rearrange("p (q c) -> p q c", c=64)
    hT = regC.bitcast(BF16).rearrange("p (k t) -> p k t", t=512)
    t2 = regC.rearrange("p (q c) -> p q c", c=64)
    zT, b_z = uT, b_u
    sgs = [regB[:, i * 512:(i + 1) * 512] for i in range(2)]
    acts = [yT.rearrange("p f t -> p (f t)")[:, i * 512:(i + 1) * 512] for i in range(4)]
    rot = PsRot(cx, [2, 3, 4, 5, 6, 7])
    ytmp = [(regA[:, i * 512:(i + 1) * 512], kb.buf(f"ytmp{i}")) for i in range(4)]
    nt = 0

    kb.dma("sp", gT[:, 0, :], din["gT_mix0"], writes=[b_gT])
    kb.dma("sp", gT[:, 1, :], din["gT_ffn0"], writes=[b_gT])
    cx.memset("dve", CAR, 0.0, [b_CAR])
    w_in, w_glu, w_out = din["s5_w_in"], din["s5_w_glu"], din["s5_w_out"]
    wg, wu, wd = din["ffn_w_gate"], din["ffn_w_up"], din["ffn_w_down"]

    def wcols(w, c0, n):
        return (w[:, c0:c0 + n].rearrange("(k p) n -> p k n", p=128), 8, n)

    jobs = []
    for T in range(ntiles):
        for j in range(N_L0_JOBS):
            jobs.append((W0[j * 128:(j + 1) * 128, 0:(4096 if j < 6 else 3072)], 4096 if j < 6 else 3072))
    ws.plan(jobs)

    for T in range(ntiles):
        t0 = T * 512
        kb.dma("sp", xt, din["x"][t0:t0 + 512, :].rearrange("(b p) d -> p b d", p=128), writes=b_xt, lane="xt")
        rms_to_hT(cx, P, xt, b_xt, 4, gT[:, 0, :], b_gT, htok, b_B, hT, b_C, ss, b_ss, [0, 1])
        for half in range(2):
            wt_, b_w = ws.get()
            wv = wt_.rearrange("p (k n) -> p k n", n=512)
            for f4 in range(4):
                fc = half * 4 + f4
                ps, bps = rot.next()
                for kc in range(8):
                    cx.mm(ps, wv[:, kc, f4 * 128:(f4 + 1) * 128], hT[:, kc, :], kc == 0, kc == 7, [b_w, b_C], [bps])
                cx.cp("act", uT[:, fc, :], ps, [bps], [b_u[fc]])
        a0c = lambda: P["A0c"].unsqueeze(2).to_broadcast([128, 32, 8, 8])
        a0s = lambda: P["A0s"].unsqueeze(2).to_broadcast([128, 32, 8, 8])
        a1c = lambda: P["A1c"].unsqueeze(3).to_broadcast([128, 32, 8, 8])
        a1s = lambda: P["A1s"].unsqueeze(3).to_broadcast([128, 32, 8, 8])
        v4 = lambda t: t.rearrange("p q (a b) -> p q a b", b=8)
        SRv, SIv = SR[:, :, 0:64], SI[:, :, 0:64]
        for qb in range(4):
            psr, bpsr = rot.next()
            psi, bpsi = rot.next()
            for q8 in range(8):
                q = qb * 8 + q8
                fc, q4 = q // 4, q % 4
                rows = slice(32 * q4, 32 * q4 + 32)
                uv = uT[rows, fc, :].rearrange("p (c s) -> p c s", s=8)
                for (pp, bpp, WB, bWB) in ((psr, bpsr, P["WBre"], B["WBre"]), (psi, bpsi, P["WBim"], B["WBim"])):
                    for s in range(8):
                        cx.mm(pp[:, q8 * 64:(q8 + 1) * 64], WB[rows, fc, s, :], uv[:, :, s], s == 0, s == 7,
                              [bWB, b_u[fc]], [bpp], tile_position=(32 * q4, 0))
            qs = slice(qb * 8, (qb + 1) * 8)
            pr4 = psr.rearrange("p (q a b) -> p q a b", a=8, b=8)
            pi4 = psi.rearrange("p (q a b) -> p q a b", a=8, b=8)
            c4 = P["A0c"][:, qs, :].unsqueeze(2).to_broadcast([128, 8, 8, 8])
            s4 = P["A0s"][:, qs, :].unsqueeze(2).to_broadcast([128, 8, 8, 8])
            cx.tt("dve", v4(t3)[:, qs], pr4, c4, ALU.mult, [bpsr, B["A0c"]], [b_A])
            cx.tt("dve", v4(t4)[:, qs], pi4, s4, ALU.mult, [bpsi, B["A0s"]], [b_A])
            cx.tt("dve", v4(SRv)[:, qs], v4(t3)[:, qs], v4(t4)[:, qs], ALU.add, [b_A], [b_SR])
            cx.tt("dve", v4(t3)[:, qs], pi4, c4, ALU.mult, [bpsi, B["A0c"]], [b_A])
            cx.tt("dve", v4(t4)[:, qs], pr4, s4, ALU.mult, [bpsr, B["A0s"]], [b_A])
            cx.tt("dve", v4(SIv)[:, qs], v4(t3)[:, qs], v4(t4)[:, qs], ALU.subtract, [b_A], [b_SI])
        if hook is not None:
            hook()
        cx.tt("dve", v4(t3), v4(SRv), a1c(), ALU.mult, [b_SR, B["A1c"]], [b_A])
        cx.tt("dve", v4(t4), v4(SIv), a1s(), ALU.mult, [b_SI, B["A1s"]], [b_A])
        cx.tt("dve", v4(t1), v4(t3), v4(t4), ALU.add, [b_A], [b_B])
        cx.tt("dve", v4(t3), v4(SIv), a1c(), ALU.mult, [b_SI, B["A1c"]], [b_A])
        cx.tt("dve", v4(t4), v4(SRv), a1s(), ALU.mult, [b_SR, B["A1s"]], [b_A])
        cx.tt("dve", v4(t2), v4(t3), v4(t4), ALU.subtract, [b_A], [b_C])
        for q in range(32):
            rm = P["RM8"][:, q:q + 1].to_broadcast([128, 64])
            kb.op("dve", lambda g_, q=q, rm=rm: g_.tensor_tensor_scan(out=SRv[:, q, :], data0=rm, data1=t1[:, q, :],
                  initial=CAR[:, 0, q:q + 1], op0=ALU.mult, op1=ALU.add), [b_B, B["RM8"], b_CAR], [b_SR])
            kb.op("dve", lambda g_, q=q, rm=rm: g_.tensor_tensor_scan(out=SIv[:, q, :], data0=rm, data1=t2[:, q, :],
                  initial=CAR[:, 1, q:q + 1], op0=ALU.mult, op1=ALU.add), [b_C, B["RM8"], b_CAR], [b_SI])
        cx.tt("dve", v4(t3), v4(SRv), a1c(), ALU.mult, [b_SR, B["A1c"]], [b_A])
        cx.tt("dve", v4(t4), v4(SIv), a1s(), ALU.mult, [b_SI, B["A1s"]], [b_A])
        cx.tt("dve", v4(t1), v4(t3), v4(t4), ALU.subtract, [b_A], [b_B])
        cx.tt("dve", v4(t3), v4(SIv), a1c(), ALU.mult, [b_SI, B["A1c"]], [b_A])
        cx.tt("dve", v4(t4), v4(SRv), a1s(), ALU.mult, [b_SR, B["A1s"]], [b_A])
        cx.tt("dve", v4(t2), v4(t3), v4(t4), ALU.add, [b_A], [b_C])
        cx.cp("dve", SB[:, :, 0, 0:1], CAR[:, 0, :].unsqueeze(2), [b_CAR], [b_SB])
        cx.cp("dve", SB[:, :, 1, 0:1], CAR[:, 1, :].unsqueeze(2), [b_CAR], [b_SB])
        cx.tt("dve", v4(t3), v4(t1), a0c(), ALU.mult, [b_B, B["A0c"]], [b_A])
        cx.tt("dve", v4(t4), v4(t2), a0s(), ALU.mult, [b_C, B["A0s"]], [b_A])
        cx.tt("dve", SB[:, :, 0, 1:64], t3[:, :, 0:63], t4[:, :, 0:63], ALU.subtract, [b_A], [b_SB])
        cx.tt("dve", CAR[:, 0, :].unsqueeze(2), t3[:, :, 63:64], t4[:, :, 63:64], ALU.subtract, [b_A], [b_CAR])
        cx.tt("dve", v4(t3), v4(t2), a0c(), ALU.mult, [b_C, B["A0c"]], [b_A])
        cx.tt("dve", v4(t4), v4(t1), a0s(), ALU.mult, [b_B, B["A0s"]], [b_A])
        cx.tt("dve", SB[:, :, 1, 1:64], t3[:, :, 0:63], t4[:, :, 0:63], ALU.add, [b_A], [b_SB])
        cx.tt("dve", CAR[:, 1, :].unsqueeze(2), t3[:, :, 63:64], t4[:, :, 63:64], ALU.add, [b_A], [b_CAR])
        for fc in range(8):
            ps, bps = rot.next()
            uv = uT[:, fc, :].rearrange("p (c s) -> p c s", s=8)
            pv = ps.rearrange("p (c s) -> p c s", s=8)
            for k in range(8):
                cx.mm(pv[:, :, k:8], P["FIRW"][:, fc, k, :], uv[:, :, 0:8 - k], k == 0, False,
                      [B["FIRW"], b_u[fc]], [bps])
            for q4 in range(4):
                q = fc * 4 + q4
                rows = slice(32 * q4, 32 * q4 + 32)
                for j in range(8):
                    last = (q4 == 3 and j == 7)
                    cx.mm(pv[rows, :, j], P["WCre"][:, q, j, :, :], SB[:, q, 0, :], False, False,
                          [B["WCre"], b_SB], [bps], tile_position=(0, 32 * q4))
                    cx.mm(pv[rows, :, j], P["WCim"][:, q, j, :, :], SB[:, q, 1, :], False, last,
                          [B["WCim"], b_SB], [bps], tile_position=(0, 32 * q4))
            yv = y32[:, fc, :]
            g1 = sgs[fc % 2]
            cx.act(g1, ps, AF.Square, [bps], [b_B])
            cx.ts("dve", g1, g1, GELU_C, 1.0, ALU.mult, ALU.add, [b_B], [b_B])
            cx.tt("dve", g1, g1, ps, ALU.mult, [b_B, bps], [b_B])
            cx.act(g1, g1, AF.Sigmoid, [b_B], [b_B], scale=GELU_S)
            cx.tt("dve", yv, g1, ps, ALU.mult, [b_B, bps], [b_A])
            cx.cp("act", yT[:, fc, :], yv, [b_A], [b_y[fc]])
        for half in range(2):
            wt_, b_w = ws.get()
            wv = wt_.rearrange("p (k n) -> p k n", n=512)
            for f4 in range(4):
                fc = half * 4 + f4
                ps, bps = rot.next()
                for kc in range(8):
                    cx.mm(ps, wv[:, kc, f4 * 128:(f4 + 1) * 128], yT[:, kc, :], kc == 0, kc == 7, [b_w, b_y[kc]], [bps])
                g1 = sgs[fc % 2]
                cx.act(g1, ps, AF.Sigmoid, [bps], [b_B])
                cx.tt("dve", zT[:, fc, :], y32[:, fc, :], g1, ALU.mult, [b_A, b_B], [b_z[fc]])
        for half in range(2):
            wt_, b_w = ws.get()
            wv = wt_.rearrange("p (k n) -> p k n", n=512)
            for b in range(4):
                ps, bps = rot.next()
                for kc in range(8):
                    cx.mm(ps, zT[:, kc, b * 128:(b + 1) * 128], wv[:, kc, :], kc == 0, kc == 7, [b_z[kc], b_w], [bps])
                xv = xt[:, b, half * 512:(half + 1) * 512]
                cx.tt("dve", xv, xv, ps, ALU.add, [b_xt[b * 2 + half], bps], [b_xt[b * 2 + half]])
        rms_to_hT(cx, P, xt, b_xt, 4, gT[:, 1, :], b_gT, htok, b_B, hT, b_C, ss, b_ss, [0, 1])
        for f in range(D_FF // 128):
            wt_, b_w = ws.get()
            gv = wt_[:, 0:1024].rearrange("p (k n) -> p k n", n=128)
            uvw = wt_[:, 1024:2048].rearrange("p (k n) -> p k n", n=128)
            dv = wt_[:, 2048:3072].rearrange("p (a d) -> p a d", d=1024)
            pg, bpg = rot.next()
            pu, bpu = rot.next()
            for kc in range(8):
                cx.mm(pg, gv[:, kc, :], hT[:, kc, :], kc == 0, kc == 7, [b_w, b_C], [bpg])
            for kc in range(8):
                cx.mm(pu, uvw[:, kc, :], hT[:, kc, :], kc == 0, kc == 7, [b_w, b_C], [bpu])
            g1 = sgs[f % 2]
            a1 = acts[f % 4]
            b_a = b_y[(f % 4)]
            cx.act(g1, pg, AF.Silu, [bpg], [b_B])
            cx.tt("dve", a1, g1, pu, ALU.mult, [b_B, bpu], [b_a])
            for b in range(4):
                for half in range(2):
                    ps, bps = rot.next()
                    cx.mm(ps, a1[:, b * 128:(b + 1) * 128], dv[:, 0, half * 512:(half + 1) * 512], True, True,
                          [b_a, b_w], [bps])
                    xv = xt[:, b, half * 512:(half + 1) * 512]
                    bx = b_xt[b * 2 + half]
                    if (b * 2 + half) % 2 == 0:
                        cx.tt("dve", xv, xv, ps, ALU.add, [bx, bps], [bx])
                    else:
                        tb, b_tb = ytmp[nt % 4]
                        nt += 1
                        cx.cp("act", tb, ps, [bps], [b_tb])
                        cx.tt("pool", xv, xv, tb, ALU.add, [bx, b_tb], [bx])
        kb.dma("sp", xs[t0:t0 + 512, :].rearrange("(b p) d -> p b d", p=128), xt, reads=b_xt, writes=[b_xs], disjoint=True)
    cx.release(m)


IN_SPECS = {
    "x": ([L, D], F32), "pos": ([1, L], I32),
    "ident": ([128, 128], F32), "bmask": ([128, 128], F32), "ev": ([128, 9 * 32], F32),
    "ltri": ([128, 128], F32), "wtab": ([128, NSLAB * NE], F32), "ctab": ([128, 7], F32),
    "invf_t": ([128, 16], F32), "invf_f": ([128, 1], F32), "esel": ([128, 31], F32), "dmask": ([128, 128], F32),
    "lamT_re": ([128, 32], F32), "lamT_im": ([128, 32], F32), "ldtT": ([128, 32], F32),
    "bT_re": ([128, 512], F32), "bT_im": ([128, 512], F32),
    "cT_re": ([128, 512], F32), "cT_im": ([128, 512], F32), "dT": ([128, 8], F32),
    "gT_mix0": ([128, 8], F32), "gT_ffn0": ([128, 8], F32), "gT_kv": ([128, 8], F32),
    "gT_mix1": ([128, 8], F32), "gT_ffn1": ([128, 8], F32), "g_final": ([D], F32),
    "g_kvlat": ([KV_LORA], F32), "g_qlat": ([Q_LORA], F32),
    "s5_w_in": ([D, D], F32), "s5_w_glu": ([D, D], F32), "s5_w_out": ([D, D], F32),
    "ffn_w_gate": ([D, D_FF], F32), "ffn_w_up": ([D, D_FF], F32), "ffn_w_down": ([D_FF, D], F32),
    "w_dkv": ([D, KV_LORA + QK_ROPE], F32), "w_ukv": ([KV_LORA, NH * 128], F32),
    "w_dq": ([D, Q_LORA], F32), "w_uq": ([Q_LORA, NH * 96], F32), "w_o": ([NH * V_HEAD, D], F32),
    "router_w": ([D, NE], F32), "router_wT": ([NE, D], F32), "g_ffn1": ([D], F32), "sel": ([8, NE * 128], F32),
    "moe_w_gate": ([NE, D, MOE_FF], F32), "moe_w_up": ([NE, D, MOE_FF], F32),
    "moe_w_down": ([NE, MOE_FF, D], F32),
}


def host_consts():
    c = {}
    c["ident"] = np.eye(128, dtype=np.float32)
    blk = np.arange(128) // 16
    c["bmask"] = (blk[:, None] == blk[None, :]).astype(np.float32)
    c["ev"] = np.broadcast_to(np.arange(9, dtype=np.float32)[None, :, None], (128, 9, 32)).reshape(128, 288).copy()
    invf = (np.float32(10000.0) ** (-np.arange(16, dtype=np.float32) * np.float32(2.0 / QK_ROPE))).astype(np.float32)
    c["invf_t"] = np.broadcast_to(invf[None, :], (128, 16)).copy()
    ff = np.zeros((128, 1), np.float32)
    ff[64:80, 0] = invf
    ff[80:96, 0] = invf
    c["invf_f"] = ff
    es = np.zeros((128, 31), np.float32)
    es[:, 15] = 1.0
    c["esel"] = es
    kk = np.arange(128)[:, None]
    qq = np.arange(128)[None, :]
    c["dmask"] = ((kk < 64) | (qq >= 64)).astype(np.float32)
    se = np.zeros((8, NE, 128), np.float32)
    for e in range(NE):
        se[e, e, :] = 1.0
    c["sel"] = se.reshape(8, NE * 128)
    c["ltri"] = (np.arange(128)[:, None] < np.arange(128)[None, :]).astype(np.float32)
    c["wtab"] = np.broadcast_to(np.arange(NSLAB, dtype=np.float32)[None, :, None], (128, NSLAB, NE)).reshape(128, NSLAB * NE).copy()
    c["ctab"] = (np.arange(7, dtype=np.float32)[None, :] * 128.0 + np.arange(128, dtype=np.float32)[:, None]).copy()
    return c


def _gT(g):
    return np.ascontiguousarray(g.reshape(8, 128).T)


def host_shared(inp):
    f = lambda a: np.ascontiguousarray(a, dtype=np.float32)
    pair = lambda a: f(a.reshape(32, 2, 64).transpose(1, 2, 0).reshape(128, 32))
    s = dict(host_consts())
    s["lamT_re"] = pair(inp["s5_lambda_re"][0])
    s["lamT_im"] = pair(inp["s5_lambda_im"][0])
    s["ldtT"] = pair(np.broadcast_to(inp["s5_log_dt"][0][:, None], (64, 64)))
    bt = lambda b: f(b.reshape(32, 2, 64, 16).transpose(1, 2, 0, 3).reshape(128, 512))
    ct = lambda c: f(c.reshape(32, 2, 16, 64).transpose(1, 3, 0, 2).reshape(128, 512))
    s["bT_re"], s["bT_im"] = bt(inp["s5_b_re"][0]), bt(inp["s5_b_im"][0])
    s["cT_re"], s["cT_im"] = ct(inp["s5_c_re"][0]), ct(inp["s5_c_im"][0])
    s["dT"] = _gT(f(inp["s5_d"][0]))
    s["gT_mix0"], s["gT_mix1"] = _gT(f(inp["norm_mix"][0])), _gT(f(inp["norm_mix"][1]))
    s["gT_ffn0"], s["gT_ffn1"] = _gT(f(inp["norm_ffn"][0])), _gT(f(inp["norm_ffn"][1]))
    s["gT_kv"] = _gT(f(inp["kv_norm"]))
    s["g_final"] = f(inp["final_norm"])
    s["g_kvlat"] = f(inp["kv_latent_norm"])
    s["g_qlat"] = f(inp["q_latent_norm"][0])
    for k in ("s5_w_in", "s5_w_glu", "s5_w_out", "ffn_w_gate", "ffn_w_up", "ffn_w_down",
              "w_dq", "w_uq", "w_o", "moe_w_gate", "moe_w_up", "moe_w_down"):
        s[k] = f(inp[k][0])
    s["router_wT"] = f(inp["router_w"][0].T)
    s["router_w"] = f(inp["router_w"][0])
    s["g_ffn1"] = f(inp["norm_ffn"][1])
    s["w_dkv"] = f(inp["w_dkv"])
    s["w_ukv"] = f(inp["w_ukv"])
    return s


def declare_inputs(nc, names):
    din = {}
    for k in names:
        shape, dt = IN_SPECS[k]
        din[k] = nc.dram_tensor(k, list(shape), dt, kind="ExternalInput").ap()
    return din


ATT_SCALE = (QK_NOPE + QK_ROPE) ** -0.5
KMAX_MARGIN = 1.02


def range_reduce_sin(cx, out, ang, it, b_out, b_ang, b_it, shift):
    cx.ts("dve", out, ang, 1.0 / (2.0 * PI), shift / (2.0 * PI), ALU.mult, ALU.add, [b_ang], [b_out])
    cx.cp("dve", it, out, [b_out], [b_it])
    cx.cp("dve", out, it, [b_it], [b_out])
    cx.stt("dve", out, out, -2.0 * PI, ang, ALU.mult, ALU.add, [b_out, b_ang], [b_out])
    cx.ts("dve", out, out, shift, -PI, ALU.add, ALU.max, [b_out], [b_out])
    cx.ts("dve", out, out, PI, None, ALU.min, None, [b_out], [b_out])
    cx.act(out, out, AF.Sin, [b_out], [b_out])


def phase15(cx, P, din, xs, b_xs, KT, b_KT, QT, b_QT, VS, b_VS, ntiles=8, hook=None):
    nc, kb = cx.nc, cx.kb
    B = P["bufs"]
    identb, b_identb = P["identb"], B["identb"]
    m = cx.mark()
    xts = [cx.sb(f"xt{i}", [128, 4, 1024], F32) for i in range(2)]
    htok, b_htok = cx.sb("htok", [128, 4, 1024], BF16)
    hT, b_hT = cx.sb("hT", [128, 8, 512], BF16)
    ss, b_ss = cx.sb("ss", [128, 8], F32)
    gT, b_gT = cx.sb("gT", [128, 2, 8], F32)
    wdkv, b_wdkv = cx.sb("wdkv", [128, 8, 288], BF16)
    wukv, b_wukv = cx.sb("wukv", [128, 2, 2048], BF16)
    wdq, b_wdq = cx.sb("wdq", [128, 8, 512], BF16)
    wuq, b_wuq = cx.sb("wuq", [128, 4, 1536], BF16)
    wrot, b_wrot = cx.sb("wrot", [128, 4, 16, 32], BF16)
    gkv, b_gkv = cx.sb("gkv", [128, 256], F32)
    gq, b_gq = cx.sb("gq", [128, 512], F32)
    invf_t, b_invf_t = cx.sb("invf_t", [128, 16], F32)
    invf_f, b_invf_f = cx.sb("invf_f", [128, 1], F32)
    esel, b_esel = cx.sb("esel", [128, 31], BF16)
    eself, b_eself = cx.sb("eself", [128, 31], F32)
    posis = [cx.sb(f"posi{i}", [128, 512], I32) for i in range(2)]
    posf, b_posf = cx.sb("posf", [128, 512], F32)
    ptok_is = [cx.sb(f"ptok_i{i}", [128, 4], I32) for i in range(2)]
    ptok, b_ptok = cx.sb("ptok", [128, 4], F32)
    angf, b_angf = cx.sb("angf", [128, 512], F32)
    cosf, b_cosf = cx.sb("cosf", [128, 512], F32)
    sinf, b_sinf = cx.sb("sinf", [128, 512], F32)
    itf, b_itf = cx.sb("itf", [128, 512], I32)
    angt, b_angt = cx.sb("angt", [128, 16], F32)
    cost, b_cost = cx.sb("cost", [128, 16], F32)
    sint, b_sint = cx.sb("sint", [128, 16], F32)
    itt, b_itt = cx.sb("itt", [128, 16], I32)
    ckvn, b_ckvn = cx.sb("ckvn", [128, 256], BF16)
    kro, b_kro = cx.sb("kro", [128, 32], BF16)
    krt, b_krt = cx.sb("krt", [128, 4, 16], F32)
    ckvT, b_ckvT = cx.sb("ckvT", [128, 2, 512], BF16)
    krT, b_krT = cx.sb("krT", [128, 512], BF16)
    cqn, b_cqn = cx.sb("cqn", [128, 512], BF16)
    cqnT, b_cqnT = cx.sb("cqnT", [128, 4, 512], BF16)
    KTt, b_KTt = cx.sb("KTt", [128, 16, 512], BF16)
    QTt, b_QTt = cx.sb("QTt", [128, 16, 512], BF16)
    SQ, b_SQ = cx.sb("SQ", [128, 16, 512], BF16)
    VA, b_VA = cx.sb("VA", [128, 16, 4, 65], BF16)
    nrm, b_nrm = cx.sb("nrm", [16, 512], F32)
    nrmb, b_nrmb = cx.sb("nrmb", [16, 512], BF16)
    rmax, b_rmax = cx.sb("rmax", [16, 2], F32)
    KM, b_KM = cx.sb("KM", [16, 4096], BF16)
    t96a, b_t96a = cx.sb("t96a", [128, 512], F32)
    t96b, b_t96b = cx.sb("t96b", [128, 512], F32)
    rot = PsRot(cx, [2, 3, 4, 5, 6, 7])

    kb.dma("sp", gT[:, 0, :], din["gT_kv"], writes=[b_gT])
    kb.dma("sp", gT[:, 1, :], din["gT_mix1"], writes=[b_gT])
    kb.dma("sp", gkv, din["g_kvlat"].partition_broadcast(128), writes=[b_gkv])
    kb.dma("sp", gq, din["g_qlat"].partition_broadcast(128), writes=[b_gq])
    kb.dma("sp", invf_t, din["invf_t"], writes=[b_invf_t])
    kb.dma("sp", invf_f, din["invf_f"], writes=[b_invf_f])
    kb.dma("sp", eself, din["esel"], writes=[b_eself])
    cx.cp("dve", esel, eself, [b_eself], [b_esel])
    kb.dma("pool", wdkv, din["w_dkv"].rearrange("(k p) n -> p k n", p=128), writes=[b_wdkv])
    kb.dma("pool", wukv, din["w_ukv"].rearrange("(k p) n -> p k n", p=128), writes=[b_wukv])
    kb.dma("pool", wdq, din["w_dq"].rearrange("(k p) n -> p k n", p=128), writes=[b_wdq])
    kb.dma("pool", wuq, din["w_uq"].rearrange("(k p) n -> p k n", p=128), writes=[b_wuq])
    wuq4 = wuq.rearrange("p k (h c) -> p k h c", c=96)
    for kc in range(4):
        cx.ts("dve", wrot[:, kc, :, 0:16], wuq4[:, kc, :, 80:96], -1.0, None, ALU.mult, None, [b_wuq], [b_wrot])
        cx.cp("dve", wrot[:, kc, :, 16:32], wuq4[:, kc, :, 64:80], [b_wuq], [b_wrot])
    cx.memset("dve", rmax, 0.0, [b_rmax])
    cx.memset("dve", VA, 1.0, [b_VA])
    cx.memset("pool", kro, 0.0, [b_kro])

    def head_norms(Tt, b_Tt, dst_row96, b_dst, t0, is_k):
        cx.act(SQ[0:96], Tt[0:96], AF.Square, [b_Tt], [b_SQ])
        ps, bps = rot.next()
        for h in range(16):
            cx.mm(ps[0:16, :], esel[0:96, 15 - h:31 - h], SQ[0:96, h, :], h == 0, h == 15, [b_esel, b_SQ], [bps])
        if is_k:
            cx.kb.op("dve", lambda g: g.reduce_max(out=rmax[:, 1:2], in_=ps[0:16, :], axis=AX.X), [bps], [b_rmax])
            cx.tt("dve", rmax[:, 0:1], rmax[:, 0:1], rmax[:, 1:2], ALU.max, [b_rmax], [b_rmax])
        else:
            cx.act(nrm, ps[0:16, :], AF.Sqrt, [bps], [b_nrm])
            cx.ts("dve", nrmb, nrm, -1.0, None, ALU.mult, None, [b_nrm], [b_nrmb])
            kb.dma("sp", dst_row96[:, t0:t0 + 512], nrmb, reads=[b_nrmb], writes=[b_dst])

    def load_tile(TT):
        tt0 = TT * 512
        xt_, b_xt_ = xts[TT % 2]
        posi_, b_posi_ = posis[TT % 2]
        ptok_i_, b_ptok_i_ = ptok_is[TT % 2]
        kb.dma("sp", xt_, xs[tt0:tt0 + 512, :].rearrange("(b p) d -> p b d", p=128), reads=[b_xs], writes=[b_xt_])
        kb.dma("sp", posi_, din["pos"][0, tt0:tt0 + 512].partition_broadcast(128), writes=[b_posi_])
        kb.dma("sp", ptok_i_, din["pos"][0, tt0:tt0 + 512].rearrange("(b p) -> p b", p=128), writes=[b_ptok_i_],
               allow_slow_non_contiguous=True)

    load_tile(0)
    for T in range(ntiles):
        t0 = T * 512
        xt, b_xt = xts[T % 2]
        posi, b_posi = posis[T % 2]
        ptok_i, b_ptok_i = ptok_is[T % 2]
        if T + 1 < ntiles:
            load_tile(T + 1)
        cx.cp("dve", posf, posi, [b_posi], [b_posf])
        cx.cp("dve", ptok, ptok_i, [b_ptok_i], [b_ptok])
        cx.ts("dve", angf, posf, invf_f[:, 0:1], None, ALU.mult, None, [b_posf, b_invf_f], [b_angf])
        range_reduce_sin(cx, sinf, angf, itf, b_sinf, b_angf, b_itf, 0.0)
        range_reduce_sin(cx, cosf, angf, itf, b_cosf, b_angf, b_itf, 0.5 * PI)
        cx.ts("dve", sinf, sinf, ATT_SCALE, None, ALU.mult, None, [b_sinf], [b_sinf])
        cx.ts("dve", cosf, cosf, ATT_SCALE, None, ALU.mult, None, [b_cosf], [b_cosf])

        rms_to_hT(cx, P, xt, b_xt, 4, gT[:, 0, :], b_gT, htok, b_htok, hT, b_hT, ss, b_ss, [0, 1])
        for b in range(4):
            ps, bps = rot.next()
            for kc in range(8):
                cx.mm(ps[:, 0:288], hT[:, kc, b * 128:(b + 1) * 128], wdkv[:, kc, :], kc == 0, kc == 7, [b_hT, b_wdkv], [bps])
            cx.act(ckvn, ps[:, 0:256], AF.Square, [bps], [b_ckvn, b_ss], accum=ss[:, 4:5])
            cx.ts("dve", ss[:, 4:5], ss[:, 4:5], 1.0 / KV_LORA, EPS, ALU.mult, ALU.add, [b_ss], [b_ss])
            cx.act(ss[:, 4:5], ss[:, 4:5], AF.Sqrt, [b_ss], [b_ss])
            cx.kb.op("dve", lambda g: g.reciprocal(out=ss[:, 4:5], in_=ss[:, 4:5]), [b_ss], [b_ss])
            cx.stt("dve", ckvn, ps[:, 0:256], ss[:, 4:5], gkv, ALU.mult, ALU.mult, [bps, b_ss, b_gkv], [b_ckvn])
            cx.ts("dve", angt, invf_t, ptok[:, b:b + 1], None, ALU.mult, None, [b_invf_t, b_ptok], [b_angt])
            range_reduce_sin(cx, sint, angt, itt, b_sint, b_angt, b_itt, 0.0)
            range_reduce_sin(cx, cost, angt, itt, b_cost, b_angt, b_itt, 0.5 * PI)
            x1, x2 = ps[:, 256:272], ps[:, 272:288]
            cx.tt("dve", krt[:, 0, :], x1, cost, ALU.mult, [bps, b_cost], [b_krt])
            cx.tt("dve", krt[:, 1, :], x2, sint, ALU.mult, [bps, b_sint], [b_krt])
            cx.tt("dve", krt[:, 2, :], x2, cost, ALU.mult, [bps, b_cost], [b_krt])
            cx.tt("dve", krt[:, 3, :], x1, sint, ALU.mult, [bps, b_sint], [b_krt])
            cx.tt("dve", kro[:, 0:16], krt[:, 0, :], krt[:, 1, :], ALU.subtract, [b_krt], [b_kro])
            cx.tt("dve", kro[:, 16:32], krt[:, 2, :], krt[:, 3, :], ALU.add, [b_krt], [b_kro])
            p16 = cx.ps[b % 2].bitcast(BF16).rearrange("p (k c) -> p k c", c=128)
            bp16 = cx.psb[b % 2]
            for k2 in range(2):
                cx.tr(p16[:, k2, :], ckvn[:, k2 * 128:(k2 + 1) * 128], identb, [b_ckvn, b_identb], [bp16])
            cx.tr(p16[0:32, 2, :], kro, identb, [b_kro, b_identb], [bp16])
            cx.cp("act", ckvT[:, :, b * 128:(b + 1) * 128], p16[:, 0:2, :], [bp16], [b_ckvT])
            cx.cp("dve", krT[64:96, b * 128:(b + 1) * 128], p16[0:32, 2, :], [bp16], [b_krT])
            w4 = wukv.rearrange("p k (h c) -> p k h c", c=128)
            for hh in range(2):
                pv_, bpv = rot.next()
                for k2 in range(2):
                    cx.mm(pv_, ckvT[:, k2, b * 128:(b + 1) * 128], w4[:, k2, hh * 8:(hh + 1) * 8, 64:128],
                          k2 == 0, k2 == 1, [b_ckvT, b_wukv], [bpv])
                cx.cp("act" if hh else "dve", VA[:, hh * 8:(hh + 1) * 8, b, 0:64],
                      pv_.rearrange("p (h c) -> p h c", c=64), [bpv], [b_VA])
        if hook is not None:
            hook()
        for h in range(16):
            ps, bps = rot.next()
            for k2 in range(2):
                cx.mm(ps[0:64, :], wukv[:, k2, h * 128:h * 128 + 64], ckvT[:, k2, :], k2 == 0, k2 == 1, [b_wukv, b_ckvT], [bps])
            cx.cp("act" if h % 2 else "dve", KTt[0:64, h, :], ps[0:64, :], [bps], [b_KTt])
        cx.cp("dve", KTt[64:96, :, :], krT[64:96, :].unsqueeze(1).to_broadcast([32, 16, 512]), [b_krT], [b_KTt])
        kb.dma("sp", KT[:, 0:96, t0:t0 + 512].rearrange("h r t -> r h t"), KTt[0:96], reads=[b_KTt], writes=[b_KT])
        head_norms(KTt, b_KTt, None, None, t0, True)
        kb.dma("sp", VS[:, :, 4 * T:4 * T + 4, :].rearrange("h p b e -> p h b e"), VA, reads=[b_VA], writes=[b_VS])

        if hook is not None:
            hook()
        rms_to_hT(cx, P, xt, b_xt, 4, gT[:, 1, :], b_gT, htok, b_htok, hT, b_hT, ss, b_ss, [0, 1])
        for b in range(4):
            ps, bps = rot.next()
            for kc in range(8):
                cx.mm(ps, hT[:, kc, b * 128:(b + 1) * 128], wdq[:, kc, :], kc == 0, kc == 7, [b_hT, b_wdq], [bps])
            cx.act(cqn, ps, AF.Square, [bps], [b_cqn, b_ss], accum=ss[:, 5:6])
            cx.ts("dve", ss[:, 5:6], ss[:, 5:6], 1.0 / Q_LORA, EPS, ALU.mult, ALU.add, [b_ss], [b_ss])
            cx.act(ss[:, 5:6], ss[:, 5:6], AF.Sqrt, [b_ss], [b_ss])
            cx.kb.op("dve", lambda g: g.reciprocal(out=ss[:, 5:6], in_=ss[:, 5:6]), [b_ss], [b_ss])
            cx.stt("dve", cqn, ps, ss[:, 5:6], gq, ALU.mult, ALU.mult, [bps, b_ss, b_gq], [b_cqn])
            p16 = cx.ps[b % 2].bitcast(BF16).rearrange("p (k c) -> p k c", c=128)
            bp16 = cx.psb[b % 2]
            for k4 in range(4):
                cx.tr(p16[:, k4, :], cqn[:, k4 * 128:(k4 + 1) * 128], identb, [b_cqn, b_identb], [bp16])
            cx.cp("act", cqnT[:, :, b * 128:(b + 1) * 128], p16[:, 0:4, :], [bp16], [b_cqnT])
        if hook is not None:
            hook()
        for h in range(16):
            pa, bpa = rot.next()
            pb_, bpb = rot.next()
            for k4 in range(4):
                cx.mm(pa[0:96, :], wuq[:, k4, h * 96:(h + 1) * 96], cqnT[:, k4, :], k4 == 0, k4 == 3, [b_wuq, b_cqnT], [bpa])
            for k4 in range(4):
                cx.mm(pb_[64:96, :], wrot[:, k4, h, :], cqnT[:, k4, :], k4 == 0, k4 == 3, [b_wrot, b_cqnT], [bpb],
                      tile_position=(0, 64))
            cx.act(QTt[0:64, h, :], pa[0:64, :], AF.Copy, [bpa], [b_QTt], scale=ATT_SCALE)
            cx.tt("dve", t96a[64:96], pa[64:96, :], cosf[64:96], ALU.mult, [bpa, b_cosf], [b_t96a])
            cx.tt("dve", t96b[64:96], pb_[64:96, :], sinf[64:96], ALU.mult, [bpb, b_sinf], [b_t96b])
            cx.tt("pool", QTt[64:96, h, :], t96a[64:96], t96b[64:96], ALU.add, [b_t96a, b_t96b], [b_QTt])
        kb.dma("sp", QT[:, 0:96, t0:t0 + 512].rearrange("h r t -> r h t"), QTt[0:96], reads=[b_QTt], writes=[b_QT])
        if hook is not None:
            hook()
        head_norms(QTt, b_QTt, QT[:, 96, :], b_QT, t0, False)
    cx.act(rmax[:, 1:2], rmax[:, 0:1], AF.Sqrt, [b_rmax], [b_rmax])
    cx.memset("pool", KM, 0.0, [b_KM])
    cx.ts("dve", KM, KM, rmax[:, 1:2], KMAX_MARGIN, ALU.add, ALU.mult, [b_KM, b_rmax], [b_KM])
    kb.dma("sp", KT[:, 96, :], KM, reads=[b_KM], writes=[b_KT])
    cx.release(m)


def phase2(cx, P, din, KT, b_KT, QT, b_QT, VS, b_VS, OT, b_OT, nheads=16, ngroups=8, hook=None):
    nc, kb = cx.nc, cx.kb
    m = cx.mark()
    KTh, QTh, VAh, b_KTh, b_QTh, b_VAh = [], [], [], [], [], []
    for i in range(2):
        t, b = cx.sb(f"KTh{i}", [128, 4096], BF16); KTh.append(t); b_KTh.append(b)
        t, b = cx.sb(f"QTh{i}", [128, 4096], BF16); QTh.append(t); b_QTh.append(b)
        t, b = cx.sb(f"VAh{i}", [128, 32, 65], BF16); VAh.append(t); b_VAh.append(b)
    PT, b_PT = [], []
    for i in range(2):
        t, _ = cx.sb(f"PT{i}", [128, 32, 512], BF16)
        PT.append(t)
        b_PT.append(kb.bufs_n(f"PT{i}_", 32))
    dmask, b_dmask = cx.sb("dmask", [128, 128], F32)
    onesr, b_onesr = cx.sb("onesr", [128, 64], F32)
    R, b_R = cx.sb("R", [128, 512], F32)
    RB, b_RB = cx.sb("RB", [128, 512], F32)
    OTs, b_OTs = [], []
    for i in range(2):
        t, b = cx.sb(f"OTs{i}", [128, 4096], BF16); OTs.append(t); b_OTs.append(b)
    kb.dma("sp", dmask, din["dmask"], writes=[b_dmask])
    cx.memset("dve", onesr, 1.0, [b_onesr])
    rot = PsRot(cx, [0, 1, 2, 3, 4])
    rot_o = PsRot(cx, [5, 6])

    def emit_pv(st, j):
        nkt = st["nkt"]
        c0 = max(0, j - 4 * st["G"]) * 128
        s_ = st["s"]
        cx.mm(st["po"][0:65, c0:512], VAh[s_][:, j, :], st["pt"][:, j, c0:512], j == 0, j == nkt - 1,
              [b_VAh[s_], st["b_pt"][j]], [st["bpo"]])

    def emit_fin(st):
        po, bpo, s_, G = st["po"], st["bpo"], st["s"], st["G"]
        ot, b_ot = st["ot"], st["b_ot"]
        cx.kb.op("dve", lambda g: g.reciprocal(out=R[64:65, :], in_=po[64:65, :]), [bpo], [b_R])
        pb_, bpb = cx.ps[7], cx.psb[7]
        cx.mm(pb_[0:64, :], onesr[64:65, :], R[64:65, :], True, True, [b_onesr, b_R], [bpb])
        cx.cp("act", RB[0:64, :], pb_[0:64, :], [bpb], [b_RB])
        dst = ot[64 * s_:64 * s_ + 64, G * 512:(G + 1) * 512]
        cx.tt("dve", dst, po[0:64, :], RB[0:64, :], ALU.mult, [bpo, b_RB], [b_ot])
        if st["store"] is not None:
            kb.dma("sp", OT[st["store"]], ot, reads=[b_ot], writes=[b_OT])

    prev = None
    fin_q = None
    gi = 0
    def load_head(hh):
        ss_ = hh % 2
        kb.dma("sp", KTh[ss_][0:97, :], KT[hh], reads=[b_KT], writes=[b_KTh[ss_]])
        kb.dma("sp", QTh[ss_][0:97, :], QT[hh], reads=[b_QT], writes=[b_QTh[ss_]])
        kb.dma("sp", VAh[ss_], VS[hh], reads=[b_VS], writes=[b_VAh[ss_]])

    load_head(0)
    for h in range(nheads):
        s = h % 2
        pr = h // 2
        ot, b_ot = OTs[pr % 2], b_OTs[pr % 2]
        for G in range(ngroups):
            if G == 1 and h + 1 < nheads:
                load_head(h + 1)
            if hook is not None:
                hook()
            pt, b_pt = PT[gi % 2], b_PT[gi % 2]
            gi += 1
            nkt = 4 * G + 4
            if prev is not None:
                prev["po"], prev["bpo"] = rot_o.next()
            npv = prev["nkt"] if prev is not None else 0
            for j in range(max(nkt, npv)):
                if j < nkt:
                    c0 = max(0, j - 4 * G) * 128
                    ps, bps = rot.next()
                    cx.mm(ps[:, c0:512], KTh[s][0:97, j * 128:(j + 1) * 128], QTh[s][0:97, G * 512 + c0:(G + 1) * 512],
                          True, True, [b_KTh[s], b_QTh[s]], [bps])
                    cx.act(pt[:, j, c0:512], ps[:, c0:512], AF.Exp, [bps], [b_pt[j]])
                    if j >= 4 * G:
                        cx.tt("pool", pt[:, j, c0:c0 + 128], pt[:, j, c0:c0 + 128], dmask, ALU.mult, [b_pt[j], b_dmask], [b_pt[j]])
                if j < npv:
                    emit_pv(prev, j)
                if j == 1 and fin_q is not None:
                    emit_fin(fin_q)
                    fin_q = None
            if fin_q is not None:
                emit_fin(fin_q)
                fin_q = None
            fin_q = prev
            last_of_pair = (G == ngroups - 1) and (s == 1 or h == nheads - 1)
            prev = dict(pt=pt, b_pt=b_pt, nkt=nkt, G=G, s=s, ot=ot, b_ot=b_ot, store=(pr if last_of_pair else None))
    if prev is not None:
        prev["po"], prev["bpo"] = rot_o.next()
        for j in range(prev["nkt"]):
            emit_pv(prev, j)
    if fin_q is not None:
        emit_fin(fin_q)
    if prev is not None:
        emit_fin(prev)
    cx.release(m)


def phase3a(cx, P, din, xs, b_xs, OT, b_OT, out, b_out, ntiles=8):
    nc, kb = cx.nc, cx.kb
    m = cx.mark()
    xt, b_xt = cx.sb("xt", [128, 4, 1024], F32)
    ot, b_ot = cx.sb("ot", [128, 8, 512], BF16)
    wo, b_wo = cx.sb("wo", [128, 8, 1024], BF16)
    kb.dma("pool", wo, din["w_o"].rearrange("(k p) n -> p k n", p=128), writes=[b_wo])
    rot = PsRot(cx, [0, 1, 2, 3])
    for T in range(ntiles):
        t0 = T * 512
        kb.dma("sp", xt, xs[t0:t0 + 512, :].rearrange("(b p) d -> p b d", p=128), reads=[b_xs], writes=[b_xt])
        kb.dma("sp", ot, OT[:, :, t0:t0 + 512].rearrange("k p t -> p k t"), reads=[b_OT], writes=[b_ot])
        for b in range(4):
            for half in range(2):
                ps, bps = rot.next()
                for k in range(8):
                    cx.mm(ps, ot[:, k, b * 128:(b + 1) * 128], wo[:, k, half * 512:(half + 1) * 512], k == 0, k == 7, [b_ot, b_wo], [bps])
                xv = xt[:, b, half * 512:(half + 1) * 512]
                cx.tt("dve", xv, xv, ps, ALU.add, [b_xt, bps], [b_xt])
        kb.dma("sp", out[t0:t0 + 512, :].rearrange("(b p) d -> p b d", p=128), xt, reads=[b_xt], writes=[b_out])
    cx.release(m)


def phase3(cx, P, din, xs, b_xs, OT, b_OT, out, b_out, ntiles=4, nexp=NE):
    nc, kb = cx.nc, cx.kb
    B = P["bufs"]
    identf, b_identf = P["identf"], B["identf"]
    m = cx.mark()
    NBK = 8
    xt, b_xt = cx.sb("xt", [128, NBK, 1024], F32)
    htok, b_htok = cx.sb("htok", [128, NBK, 1024], BF16)
    hT, b_hT = cx.sb("hT", [128, 8, 1024], BF16)
    wo, b_wo = cx.sb("wo", [128, 8, 1024], BF16)
    rwg, b_rwg = cx.sb("rwg", [128, NE, 1024], F32)
    gfin, b_gfin = cx.sb("gfin", [128, 1024], F32)
    gT, b_gT = cx.sb("gT", [128, 8], F32)
    ss, b_ss = cx.sb("ss", [128, 16], F32)
    lg, b_lg = cx.sb("lg", [128, NBK, 8], F32)
    m8, b_m8 = cx.sb("m8", [128, 8], F32)
    gts, b_gts = cx.sb("gts", [128, NBK, 8], F32)
    gsum, b_gsum = cx.sb("gsum", [128, 2], F32)
    g8T, b_g8T = cx.sb("g8T", [8, 1024], F32)
    sel, b_sel = cx.sb("sel", [8, NE, 128], F32)
    gbc, b_gbc = cx.sb("gbc", [128, 1024], F32)
    sg = []
    b_sg = []
    tg = []
    b_tg = []
    for i in range(2):
        t, b = cx.sb(f"sg{i}", [128, 512], F32); sg.append(t); b_sg.append(b)
        t, b = cx.sb(f"tg{i}", [128, 512], F32); tg.append(t); b_tg.append(b)
    actT = []
    b_actT = []
    for i in range(2):
        t, b = cx.sb(f"actT{i}", [128, 4, 1024], BF16); actT.append(t); b_actT.append(b)
    junk, b_junk = cx.sb("junk", [128, 1024], BF16)
    ws = WStream(cx, 2, 3 * 4096, name="wm")
    rot = PsRot(cx, [2, 3, 4, 5, 6, 7])
    ot = htok

    kb.dma("pool", wo, din["w_o"].rearrange("(k p) n -> p k n", p=128), writes=[b_wo])
    kb.dma("sp", gfin, din["g_final"].partition_broadcast(128), writes=[b_gfin])
    kb.dma("sp", gT, din["gT_ffn1"], writes=[b_gT])
    kb.dma("sp", gbc, din["g_ffn1"].partition_broadcast(128), writes=[b_gbc])
    kb.dma("sp", sel, din["sel"].rearrange("k (e m) -> k e m", m=128), writes=[b_sel])
    for e in range(NE):
        kb.dma("sp", rwg[:, e, :], din["router_wT"][e].partition_broadcast(128), writes=[b_rwg])
    for e in range(NE):
        cx.tt("pool", rwg[:, e, :], rwg[:, e, :], gbc, ALU.mult, [b_rwg, b_gbc], [b_rwg])
    wg, wu, wd = din["moe_w_gate"], din["moe_w_up"], din["moe_w_down"]
    NG = MOE_FF // 512
    jobs = []
    for T in range(ntiles):
        for e in range(nexp):
            for g in range(NG):
                jobs.append([
                    (wg[e][:, g * 512:(g + 1) * 512].rearrange("(k p) n -> p k n", p=128), 8, 512),
                    (wu[e][:, g * 512:(g + 1) * 512].rearrange("(k p) n -> p k n", p=128), 8, 512),
                    (wd[e][g * 512:(g + 1) * 512, :].rearrange("(a p) d -> p a d", p=128), 4, 1024)])
    ws.plan(jobs)

    for T in range(ntiles):
        t0 = T * 1024
        kb.dma("sp", xt, xs[t0:t0 + 1024, :].rearrange("(b p) d -> p b d", p=128), reads=[b_xs], writes=[b_xt])
        kb.dma("sp", ot, OT[:, :, t0:t0 + 1024].rearrange("k p t -> p k t"), reads=[b_OT], writes=[b_htok])
        for b in range(NBK):
            for half in range(2):
                ps, bps = rot.next()
                for k in range(8):
                    cx.mm(ps, ot[:, k, b * 128:(b + 1) * 128], wo[:, k, half * 512:(half + 1) * 512], k == 0, k == 7,
                          [b_htok, b_wo], [bps])
                xv = xt[:, b, half * 512:(half + 1) * 512]
                cx.tt("dve", xv, xv, ps, ALU.add, [b_xt, bps], [b_xt])
        rms_to_hT(cx, P, xt, b_xt, NBK, gT, b_gT, htok, b_htok, hT, b_hT, ss, b_ss, [0, 1])
        for b in range(NBK):
            for e in range(NE):
                kb.op("dve", lambda g_, b=b, e=e: g_.scalar_tensor_tensor(
                    out=junk, in0=xt[:, b, :], scalar=ss[:, b:b + 1], in1=rwg[:, e, :], op0=ALU.mult, op1=ALU.mult,
                    accum_out=lg[:, b, e:e + 1]), [b_xt, b_ss, b_rwg], [b_junk, b_lg])
            kb.op("dve", lambda g_, b=b: g_.max(out=m8, in_=lg[:, b, :]), [b_lg], [b_m8])
            cx.ts("dve", gsum[:, 0:1], m8[:, 0:1], -1.0, None, ALU.mult, None, [b_m8], [b_gsum])
            cx.act(gts[:, b, :], lg[:, b, :], AF.Exp, [b_lg, b_gsum], [b_gts], bias=gsum[:, 0:1])
            cx.stt("dve", gts[:, b, :], lg[:, b, :], m8[:, 1:2], gts[:, b, :], ALU.is_ge, ALU.mult,
                   [b_lg, b_m8, b_gts], [b_gts])
            kb.op("dve", lambda g_, b=b: g_.reduce_sum(out=gsum[:, 1:2], in_=gts[:, b, :], axis=AX.X), [b_gts], [b_gsum])
            kb.op("dve", lambda g_: g_.reciprocal(out=gsum[:, 1:2], in_=gsum[:, 1:2]), [b_gsum], [b_gsum])
            cx.ts("dve", gts[:, b, :], gts[:, b, :], gsum[:, 1:2], None, ALU.mult, None, [b_gts, b_gsum], [b_gts])
            pt_, bpt = rot.next()
            cx.tr(pt_[0:8, 0:128], gts[:, b, :], identf, [b_gts, b_identf], [bpt])
            cx.cp("dve", g8T[:, b * 128:(b + 1) * 128], pt_[0:8, 0:128], [bpt], [b_g8T])
        gi = 0
        for e in range(nexp):
            for half in range(2):
                ps, bps = rot.next()
                cx.mm(ps, sel[:, e, :], g8T[:, half * 512:(half + 1) * 512], True, True, [b_sel, b_g8T], [bps])
                cx.cp("act", gbc[:, half * 512:(half + 1) * 512], ps, [bps], [b_gbc])
            for g in range(NG):
                (gv, uv, dv), b_w = ws.get()
                at, b_at = actT[gi % 2], b_actT[gi % 2]
                gi += 1
                for f4 in range(4):
                    for half in range(2):
                        hs = slice(half * 512, (half + 1) * 512)
                        pg, bpg = rot.next()
                        pu, bpu = rot.next()
                        for kc in range(8):
                            cx.mm(pg, gv[:, kc, f4 * 128:(f4 + 1) * 128], hT[:, kc, hs], kc == 0, kc == 7, [b_w, b_hT], [bpg])
                        for kc in range(8):
                            cx.mm(pu, uv[:, kc, f4 * 128:(f4 + 1) * 128], hT[:, kc, hs], kc == 0, kc == 7, [b_w, b_hT], [bpu])
                        i2 = (f4 * 2 + half) % 2
                        cx.act(sg[i2], pg, AF.Silu, [bpg], [b_sg[i2]])
                        cx.tt("dve", tg[i2], pu, gbc[:, hs], ALU.mult, [bpu, b_gbc], [b_tg[i2]])
                        cx.tt("pool", at[:, f4, hs], sg[i2], tg[i2], ALU.mult, [b_sg[i2], b_tg[i2]], [b_at])
                for b in range(NBK):
                    for half in range(2):
                        ps, bps = rot.next()
                        for f4 in range(4):
                            cx.mm(ps, at[:, f4, b * 128:(b + 1) * 128], dv[:, f4, half * 512:(half + 1) * 512],
                                  f4 == 0, f4 == 3, [b_at, b_w], [bps])
                        xv = xt[:, b, half * 512:(half + 1) * 512]
                        cx.tt("dve", xv, xv, ps, ALU.add, [b_xt, bps], [b_xt])
        for b in range(NBK):
            cx.act(junk, xt[:, b, :], AF.Square, [b_xt], [b_junk, b_ss], accum=ss[:, 8 + (b % 8):9 + (b % 8)])
        cx.ts("dve", ss[:, 8:16], ss[:, 8:16], 1.0 / D, EPS, ALU.mult, ALU.add, [b_ss], [b_ss])
        cx.act(ss[:, 8:16], ss[:, 8:16], AF.Sqrt, [b_ss], [b_ss])
        kb.op("dve", lambda g_: g_.reciprocal(out=ss[:, 8:16], in_=ss[:, 8:16]), [b_ss], [b_ss])
        for b in range(NBK):
            cx.stt("dve", xt[:, b, :], xt[:, b, :], ss[:, 8 + b:9 + b], gfin, ALU.mult, ALU.mult, [b_xt, b_ss, b_gfin], [b_xt])
        kb.dma("sp", out[t0:t0 + 1024, :].rearrange("(b p) d -> p b d", p=128), xt, reads=[b_xt], writes=[b_out])
    cx.release(m)


def build_program():
    nc = bass.Bass("TRN2", target_bir_lowering=False)
    cx = Ctx(nc)
    kb = cx.kb
    din = declare_inputs(nc, list(IN_SPECS.keys()))
    out = nc.dram_tensor("out", [L, D], F32, kind="ExternalOutput").ap()
    xs = nc.dram_tensor("xs", [L, D], F32, kind="Internal").ap()
    KT = nc.dram_tensor("KT", [NH, 97, L], BF16, kind="Internal").ap()
    QT = nc.dram_tensor("QT", [NH, 97, L], BF16, kind="Internal").ap()
    VS = nc.dram_tensor("VS", [NH, 128, NB, 65], BF16, kind="Internal").ap()
    OT = nc.dram_tensor("OT", [NH // 2, 128, L], BF16, kind="Internal").ap()
    b_out, b_xs, b_KT, b_QT, b_VS, b_OT = (kb.buf(n) for n in ("out", "xs", "KT", "QT", "VS", "OT"))
    W0 = nc.dram_tensor("W0", [N_L0_JOBS * 128, 4096], BF16, kind="Internal").ap()
    b_W0 = kb.buf("W0")
    W16 = {k: nc.dram_tensor("W16" + k, [NE * NGRP * 128, 4096], BF16, kind="Internal").ap() for k in "gud"}
    b_W16 = {k: kb.buf("W16" + k) for k in "gud"}
    HS = nc.dram_tensor("HS", [NSLAB * SLAB, D], BF16, kind="Internal").ap()
    YS = nc.dram_tensor("YS", [NSLAB * SLAB, D], F32, kind="Internal").ap()
    b_HS, b_YS = kb.buf("HS"), kb.buf("YS")
    conv = MoeConv(cx, din, W16, b_W16, nstage=0)
    P = setup_ident(cx, din)
    m0 = cx.mark()
    phase0(cx, P, din, pre_hook=lambda: layer0_convert(cx, din, W0, b_W0))
    phase1(cx, P, din, xs, b_xs, W0, b_W0)
    cx.release(m0)
    m1 = cx.mark()
    conv.stage = [cx.sb(f"cvB{i}", [128, 4096], BF16) for i in range(2)]
    phase15(cx, P, din, xs, b_xs, KT, b_KT, QT, b_QT, VS, b_VS, hook=lambda: conv.step(3))
    cnt2 = [0]

    def hook2():
        cnt2[0] += 1
        rem_iters = 128 - cnt2[0] + 1
        rem_chunks = len(conv.chunks) - conv.i
        if rem_chunks > 0 and (rem_chunks >= rem_iters or (cnt2[0] * 72) // 128 > ((cnt2[0] - 1) * 72) // 128):
            conv.step(1)

    phase2(cx, P, din, KT, b_KT, QT, b_QT, VS, b_VS, OT, b_OT, hook=hook2)
    conv.finish()
    cx.release(m1)
    phase3r(cx, P, din, xs, b_xs, OT, b_OT, out, b_out, W16, b_W16, HS, b_HS, YS, b_YS)
    kb.finish([b_out])
    return nc


_NC_CACHE = {}


def kernel(**inputs):
    inp = {k: np.asarray(v) for k, v in inputs.items()}
    shared = host_shared(inp)
    if "nc" not in _NC_CACHE:
        _NC_CACHE["nc"] = build_program()
    nc = _NC_CACHE["nc"]
    ncores = 8
    in_maps = []
    for c in range(ncores):
        mp = dict(shared)
        mp["x"] = np.ascontiguousarray(inp["x"][c], dtype=np.float32)
        mp["pos"] = np.ascontiguousarray(inp["positions"][c:c + 1], dtype=np.int32)
        in_maps.append(mp)
    res = run_bass_kernel_spmd(nc, in_maps, core_ids=list(range(ncores)))
    return np.stack([np.asarray(r["out"], dtype=np.float32) for r in res.results], axis=0)


class MoeConv:
    def __init__(self, cx, din, W16, b_W16, nstage=2, nexp=NE):
        self.cx = cx
        self.W16, self.b_W16 = W16, b_W16
        self.stage = [cx.sb(f"cvst{i}", [128, 4096], BF16) for i in range(nstage)] if nstage else []
        self.chunks = []
        for e in range(nexp):
            for g in range(NGRP):
                cs = slice(g * 512, (g + 1) * 512)
                self.chunks.append(("g", e, g, din["moe_w_gate"][e][:, cs].rearrange("(k p) n -> p k n", p=128), 512))
                self.chunks.append(("u", e, g, din["moe_w_up"][e][:, cs].rearrange("(k p) n -> p k n", p=128), 512))
                self.chunks.append(("d", e, g, din["moe_w_down"][e][cs, :].rearrange("(a p) d -> p a d", p=128), 1024))
        self.i = 0

    def step(self, n=1):
        kb = self.cx.kb
        for _ in range(n):
            if self.i >= len(self.chunks):
                return
            kind, e, g, src, n_in = self.chunks[self.i]
            st, b_st = self.stage[self.i % len(self.stage)]
            self.i += 1
            kb.dma("pool", st.rearrange("p (a n) -> p a n", n=n_in), src, writes=[b_st])
            r0 = (e * NGRP + g) * 128
            kb.dma("sp", self.W16[kind][r0:r0 + 128, :], st, reads=[b_st], writes=[self.b_W16[kind]], disjoint=True)

    def finish(self):
        self.step(len(self.chunks))


def phase3r(cx, P, din, xs, b_xs, OT, b_OT, out, b_out, W16, b_W16, HS, b_HS, YS, b_YS, nslab=NSLAB):
    nc, kb = cx.nc, cx.kb
    B = P["bufs"]
    identb, b_identb = P["identb"], B["identb"]
    identf, b_identf = P["identf"], B["identf"]
    NTOT = NE * NGRP * 128
    mp = cx.mark()
    ss, b_ss = cx.sb("ss", [128, 8], F32)
    LG, b_LG = cx.sb("LG", [128, NB, 8], F32)
    GTS, b_GTS = cx.sb("GTS", [128, NB, 8], F32)
    TOP, b_TOP = cx.sb("TOP", [128, NB, 2], F32)
    m8, b_m8 = cx.sb("m8", [128, 8], F32)
    gsum, b_gsum = cx.sb("gsum", [128, 2], F32)
    junk, b_junk = cx.sb("junk", [128, 1024], BF16)
    M1, b_M1 = cx.sb("M1", [128, NB, 8], F32)
    M2, b_M2 = cx.sb("M2", [128, NB, 8], F32)
    MSK, b_MSK = cx.sb("MSK", [128, NB, 8], BF16)
    ltri, b_ltri = cx.sb("ltri", [128, 128], BF16)
    onesb, b_onesb = cx.sb("onesb", [128, 128], BF16)
    ones32, b_ones32 = cx.sb("ones32", [128, NB], F32)
    tmpf, b_tmpf = cx.sb("tmpf", [128, 128], F32)
    TOT, b_TOT = cx.sb("TOT", [128, NB, 8], F32)
    INC, b_INC = cx.sb("INC", [128, NB, 8], F32)
    WIN, b_WIN = cx.sb("WIN", [128, NB, 8], F32)
    SL, b_SL = cx.sb("SL", [128, NB, 8], F32)
    NSL, b_NSL = cx.sb("NSL", [128, 8], F32)
    NSLi, b_NSLi = cx.sb("NSLi", [128, 8], I32)
    SEND, b_SEND = cx.sb("SEND", [128, 8], F32)
    OFF, b_OFF = cx.sb("OFF", [128, 8], F32)
    one8, b_one8 = cx.sb("one8", [128, 8], F32)
    SF, b_SF = cx.sb("SF", [128, 2, NB], F32)
    SLOT, b_SLOT = cx.sb("SLOT", [128, 2, NB], I32)
    WGT, b_WGT = cx.sb("WGT", [128, 2, NB], F32)
    wtab, b_wtab = cx.sb("wtab", [128, NSLAB, 8], F32)
    CMP, b_CMP = cx.sb("CMP", [128, NSLAB, 8], F32)
    EW, b_EW = cx.sb("EW", [128, NSLAB], F32)
    ctab, b_ctab = cx.sb("ctab", [128, 7], F32)
    IDXf, b_IDXf = cx.sb("IDXf", [128, NSLAB, 7], F32)
    IDX, b_IDX = cx.sb("IDX", [128, NSLAB, 7], I32)
    m = cx.mark()
    xt, b_xt = cx.sb("xt", [128, 8, 1024], F32)
    ot, b_ot = cx.sb("ot", [128, 8, 1024], BF16)
    wo, b_wo = cx.sb("wo", [128, 8, 1024], BF16)
    rwT, b_rwT = cx.sb("rwT", [128, 8, NE], F32)
    gT1, b_gT1 = cx.sb("gT1", [128, 8], F32)
    xT32, b_xT32 = cx.sb("xT32", [128, 8, 128], F32)
    gff, b_gff = cx.sb("gff", [128, 1024], F32)
    hall, _ = cx.sb("hall", [128, NB, 1024], BF16)
    b_hall = kb.bufs_n("hall", NB)
    rot = PsRot(cx, [2, 3, 4, 5, 6, 7])
    kb.dma("pool", wo, din["w_o"].rearrange("(k p) n -> p k n", p=128), writes=[b_wo])
    kb.dma("sp", gff, din["g_ffn1"].partition_broadcast(128), writes=[b_gff])
    kb.dma("sp", rwT, din["router_w"].rearrange("(k p) e -> p k e", p=128), writes=[b_rwT])
    kb.dma("sp", gT1, din["gT_ffn1"], writes=[b_gT1])
    cx.tt("dve", rwT, rwT, gT1.unsqueeze(2).to_broadcast([128, 8, NE]), ALU.mult, [b_rwT, b_gT1], [b_rwT])
    for T in range(4):
        t0 = T * 1024
        kb.dma("sp", xt, xs[t0:t0 + 1024, :].rearrange("(b p) d -> p b d", p=128), reads=[b_xs], writes=[b_xt])
        kb.dma("sp", ot, OT[:, :, t0:t0 + 1024].rearrange("k p t -> p k t"), reads=[b_OT], writes=[b_ot])
        for b in range(8):
            for half in range(2):
                ps, bps = rot.next()
                for k in range(8):
                    cx.mm(ps, ot[:, k, b * 128:(b + 1) * 128], wo[:, k, half * 512:(half + 1) * 512], k == 0, k == 7,
                          [b_ot, b_wo], [bps])
                xv = xt[:, b, half * 512:(half + 1) * 512]
                cx.tt("dve", xv, xv, ps, ALU.add, [b_xt, bps], [b_xt])
        kb.dma("sp", xs[t0:t0 + 1024, :].rearrange("(b p) d -> p b d", p=128), xt, reads=[b_xt], writes=[b_xs])
        for b in range(8):
            cx.act(junk, xt[:, b, :], AF.Square, [b_xt], [b_junk, b_ss], accum=ss[:, b:b + 1])
        cx.ts("dve", ss, ss, 1.0 / D, EPS, ALU.mult, ALU.add, [b_ss], [b_ss])
        cx.act(ss, ss, AF.Sqrt, [b_ss], [b_ss])
        kb.op("dve", lambda g_: g_.reciprocal(out=ss, in_=ss), [b_ss], [b_ss])
        for b in range(8):
            gb = T * 8 + b
            cx.stt("dve", hall[:, gb, :], xt[:, b, :], ss[:, b:b + 1], gff, ALU.mult, ALU.mult,
                   [b_xt, b_ss, b_gff], [b_hall[gb]])
            pA, bpA = rot.next()
            pB, bpB = rot.next()
            for kc in range(8):
                pp, bpp = (pA, bpA) if kc < 4 else (pB, bpB)
                cx.tr(pp[:, (kc % 4) * 128:(kc % 4 + 1) * 128], xt[:, b, kc * 128:(kc + 1) * 128], identf, [b_xt, b_identf], [bpp])
            cx.cp("act", xT32[:, 0:4, :], pA.rearrange("p (k c) -> p k c", c=128), [bpA], [b_xT32])
            cx.cp("act", xT32[:, 4:8, :], pB.rearrange("p (k c) -> p k c", c=128), [bpB], [b_xT32])
            pL, bpL = rot.next()
            for kc in range(8):
                cx.mm(pL[:, 0:NE], xT32[:, kc, :], rwT[:, kc, :], kc == 0, kc == 7, [b_xT32, b_rwT], [bpL])
            cx.ts("dve", LG[:, gb, :], pL[:, 0:NE], ss[:, b:b + 1], None, ALU.mult, None, [bpL, b_ss], [b_LG])
            kb.op("dve", lambda g_, gb=gb: g_.max(out=m8, in_=LG[:, gb, :]), [b_LG], [b_m8])
            cx.cp("dve", TOP[:, gb, :], m8[:, 0:2], [b_m8], [b_TOP])
            cx.ts("dve", gsum[:, 0:1], m8[:, 0:1], -1.0, None, ALU.mult, None, [b_m8], [b_gsum])
            cx.act(GTS[:, gb, :], LG[:, gb, :], AF.Exp, [b_LG, b_gsum], [b_GTS], bias=gsum[:, 0:1])
            cx.stt("dve", GTS[:, gb, :], LG[:, gb, :], m8[:, 1:2], GTS[:, gb, :], ALU.is_ge, ALU.mult,
                   [b_LG, b_m8, b_GTS], [b_GTS])
            kb.op("dve", lambda g_, gb=gb: g_.reduce_sum(out=gsum[:, 1:2], in_=GTS[:, gb, :], axis=AX.X), [b_GTS], [b_gsum])
            kb.op("dve", lambda g_: g_.reciprocal(out=gsum[:, 1:2], in_=gsum[:, 1:2]), [b_gsum], [b_gsum])
            cx.ts("dve", GTS[:, gb, :], GTS[:, gb, :], gsum[:, 1:2], None, ALU.mult, None, [b_GTS, b_gsum], [b_GTS])
    kb.dma("sp", tmpf, din["ltri"], writes=[b_tmpf])
    cx.cp("dve", ltri, tmpf, [b_tmpf], [b_ltri])
    kb.dma("sp", wtab, din["wtab"].rearrange("p (w e) -> p w e", e=8), writes=[b_wtab])
    kb.dma("sp", ctab, din["ctab"], writes=[b_ctab])
    cx.memset("dve", onesb, 1.0, [b_onesb])
    cx.memset("dve", ones32, 1.0, [b_ones32])
    cx.memset("dve", one8, 1.0, [b_one8])
    bce = lambda t: t.unsqueeze(2).to_broadcast([128, NB, 8])
    cx.tt("dve", M1, LG, bce(TOP[:, :, 0]), ALU.is_equal, [b_LG, b_TOP], [b_M1])
    cx.tt("dve", M2, LG, bce(TOP[:, :, 1]), ALU.is_equal, [b_LG, b_TOP], [b_M2])
    cx.tt("dve", MSK, M1, M2, ALU.add, [b_M1, b_M2], [b_MSK])
    mskf = MSK.rearrange("p b e -> p (b e)")
    ps, bps = rot.next()
    cx.mm(ps[:, 0:256], ltri, mskf, True, True, [b_ltri, b_MSK], [bps])
    cx.cp("dve", WIN.rearrange("p b e -> p (b e)"), ps[:, 0:256], [bps], [b_WIN])
    ps, bps = rot.next()
    cx.mm(ps[:, 0:256], onesb, mskf, True, True, [b_onesb, b_MSK], [bps])
    cx.cp("dve", TOT.rearrange("p b e -> p (b e)"), ps[:, 0:256], [bps], [b_TOT])
    for e in range(NE):
        kb.op("dve", lambda g_, e=e: g_.tensor_tensor_scan(out=INC[:, :, e], data0=ones32, data1=TOT[:, :, e], initial=0.0,
                                                           op0=ALU.mult, op1=ALU.add), [b_ones32, b_TOT], [b_INC])
    cx.ts("dve", NSL, INC[:, NB - 1, :], float(SLAB - 1), 1.0 / SLAB, ALU.add, ALU.mult, [b_INC], [b_NSL])
    cx.ts("dve", NSL, NSL, -0.4995, None, ALU.add, None, [b_NSL], [b_NSL])
    cx.cp("dve", NSLi, NSL, [b_NSL], [b_NSLi])
    cx.cp("dve", NSL, NSLi, [b_NSLi], [b_NSL])
    kb.op("dve", lambda g_: g_.tensor_tensor_scan(out=SEND, data0=one8, data1=NSL, initial=0.0, op0=ALU.mult, op1=ALU.add),
          [b_one8, b_NSL], [b_SEND])
    cx.tt("dve", OFF, SEND, NSL, ALU.subtract, [b_SEND, b_NSL], [b_OFF])
    cx.ts("dve", OFF, OFF, float(SLAB), None, ALU.mult, None, [b_OFF], [b_OFF])
    cx.tt("dve", SL, INC, TOT, ALU.subtract, [b_INC, b_TOT], [b_SL])
    cx.tt("dve", SL, SL, WIN, ALU.add, [b_SL, b_WIN], [b_SL])
    cx.tt("dve", SL, SL, OFF.unsqueeze(1).to_broadcast([128, NB, 8]), ALU.add, [b_SL, b_OFF], [b_SL])
    for k, (Mk, b_Mk) in enumerate(((M1, b_M1), (M2, b_M2))):
        cx.tt("dve", TOT, Mk, SL, ALU.mult, [b_Mk, b_SL], [b_TOT])
        kb.op("dve", lambda g_, k=k: g_.reduce_sum(out=SF[:, k, :], in_=TOT, axis=AX.X), [b_TOT], [b_SF])
        cx.tt("dve", TOT, Mk, GTS, ALU.mult, [b_Mk, b_GTS], [b_TOT])
        kb.op("dve", lambda g_, k=k: g_.reduce_sum(out=WGT[:, k, :], in_=TOT, axis=AX.X), [b_TOT], [b_WGT])
    cx.cp("dve", SLOT, SF, [b_SF], [b_SLOT])
    cx.tt("dve", CMP, SEND.unsqueeze(1).to_broadcast([128, NSLAB, 8]), wtab, ALU.is_le, [b_SEND, b_wtab], [b_CMP])
    kb.op("dve", lambda g_: g_.reduce_sum(out=EW, in_=CMP, axis=AX.X), [b_CMP], [b_EW])
    cx.ts("dve", EW, EW, float(NE - 1), float(NGRP * 128), ALU.min, ALU.mult, [b_EW], [b_EW])
    cx.tt("dve", IDXf, EW.unsqueeze(2).to_broadcast([128, NSLAB, 7]), ctab.unsqueeze(1).to_broadcast([128, NSLAB, 7]),
          ALU.add, [b_EW, b_ctab], [b_IDXf])
    cx.cp("dve", IDX, IDXf, [b_IDXf], [b_IDX])
    for gb in range(NB):
        for k in range(2):
            kb.idma(HS, hall[:, gb, :], out_idx=SLOT[:, k, gb:gb + 1], bound=NSLAB * SLAB - 1,
                    reads=[b_hall[gb], b_SLOT], writes=[b_HS], disjoint=True)
    cx.release(m)
    m2 = cx.mark()
    NBK = SLAB // 128
    hsls = [cx.sb(f"hsl{i}", [128, NBK, 1024], BF16) for i in range(2)]
    hTs = [cx.sb(f"hTs{i}", [128, 8, SLAB], BF16) for i in range(2)]
    yacc, b_yacc = cx.sb("yacc", [128, NBK, 1024], F32)
    wsl = [cx.sb(f"wsl{i}", [128, 3 * 4096], BF16) for i in range(2)]
    actT = [cx.sb(f"actT{i}", [128, 4, SLAB], BF16) for i in range(2)]
    sg = [cx.sb(f"sg{i}", [128, 512], F32) for i in range(2)]
    NH2 = SLAB // 512
    wi = 0
    gi = 0

    def issue_w(w, g, slot):
        t, b = slot
        for j, kind in enumerate(("g", "u", "d")):
            kb.idma(t[:, j * 4096:(j + 1) * 4096], W16[kind], in_idx=IDX[:, w, g:g + 1], bound=NTOT - 1,
                    reads=[b_W16[kind], b_IDX], writes=[b], lane=b.name)

    def load_slab_dma(w):
        hsl, b_hsl = hsls[w % 2]
        kb.dma("sp", hsl, HS[w * SLAB:(w + 1) * SLAB, :].rearrange("(b p) d -> p b d", p=128), reads=[b_HS], writes=[b_hsl])

    def load_slab(w):
        hsl, b_hsl = hsls[w % 2]
        hT_, b_hT_ = hTs[w % 2]
        for b in range(NBK):
            pi = b % 2
            p16 = cx.ps[pi].bitcast(BF16).rearrange("p (k c) -> p k c", c=128)
            for kc in range(8):
                cx.tr(p16[:, kc, :], hsl[:, b, kc * 128:(kc + 1) * 128], identb, [b_hsl, b_identb], [cx.psb[pi]])
            cx.cp("act" if b % 2 else "dve", hT_[:, :, b * 128:(b + 1) * 128], p16, [cx.psb[pi]], [b_hT_])

    seq = [(w, g) for w in range(nslab) for g in range(NGRP)]
    issue_w(seq[0][0], seq[0][1], wsl[0])
    load_slab_dma(0)
    load_slab(0)
    for si, (w, g) in enumerate(seq):
        if si + 1 < len(seq):
            issue_w(seq[si + 1][0], seq[si + 1][1], wsl[(si + 1) % 2])
        wt, b_w = wsl[si % 2]
        gv = wt[:, 0:4096].rearrange("p (k n) -> p k n", n=512)
        uv = wt[:, 4096:8192].rearrange("p (k n) -> p k n", n=512)
        dv = wt[:, 8192:12288].rearrange("p (a d) -> p a d", d=1024)
        hT, b_hT = hTs[w % 2]
        if g == NGRP - 3 and w + 1 < nslab:
            load_slab_dma(w + 1)
        if g == NGRP - 1 and w + 1 < nslab:
            load_slab(w + 1)
        at, b_at = actT[gi % 2]
        gi += 1
        for f4 in range(4):
            for half in range(NH2):
                hs = slice(half * 512, (half + 1) * 512)
                pg, bpg = rot.next()
                pu, bpu = rot.next()
                for kc in range(8):
                    cx.mm(pg, gv[:, kc, f4 * 128:(f4 + 1) * 128], hT[:, kc, hs], kc == 0, kc == 7, [b_w, b_hT], [bpg])
                for kc in range(8):
                    cx.mm(pu, uv[:, kc, f4 * 128:(f4 + 1) * 128], hT[:, kc, hs], kc == 0, kc == 7, [b_w, b_hT], [bpu])
                s1, b_s1 = sg[(f4 * NH2 + half) % 2]
                cx.act(s1, pg, AF.Silu, [bpg], [b_s1])
                cx.tt("dve", at[:, f4, hs], s1, pu, ALU.mult, [b_s1, bpu], [b_at])
        for b in range(NBK):
            for half in range(2):
                ps, bps = rot.next()
                for f4 in range(4):
                    cx.mm(ps, at[:, f4, b * 128:(b + 1) * 128], dv[:, f4, half * 512:(half + 1) * 512],
                          f4 == 0, f4 == 3, [b_at, b_w], [bps])
                yv = yacc[:, b, half * 512:(half + 1) * 512]
                if g == 0:
                    cx.cp("act", yv, ps, [bps], [b_yacc])
                else:
                    cx.tt("dve", yv, yv, ps, ALU.add, [b_yacc, bps], [b_yacc])
        if g == NGRP - 1:
            kb.dma("sp", YS[w * SLAB:(w + 1) * SLAB, :].rearrange("(b p) d -> p b d", p=128), yacc, reads=[b_yacc], writes=[b_YS],
                   disjoint=True)
    cx.release(m2)
    NBUF3 = 4
    y1, b_y1 = cx.sb("y1", [128, NBUF3, 1024], F32)
    y2, b_y2 = cx.sb("y2", [128, NBUF3, 1024], F32)
    xb, b_xb = cx.sb("xb", [128, NBUF3, 1024], F32)
    gfin, b_gfin = cx.sb("gfin", [128, 1024], F32)
    s2, _ = cx.sb("s2", [128, NBUF3], F32)
    b_s2s = kb.bufs_n("s2_", NBUF3)
    kb.dma("sp", gfin, din["g_final"].partition_broadcast(128), writes=[b_gfin])
    b_y1s = kb.bufs_n("y1s", NBUF3); b_y2s = kb.bufs_n("y2s", NBUF3); b_xbs = kb.bufs_n("xbs", NBUF3)
    def fetch3(g2):
        i2 = g2 % NBUF3
        kb.dma("sp", xb[:, i2, :], xs[g2 * 128:(g2 + 1) * 128, :], reads=[b_xs], writes=[b_xbs[i2]])
        kb.idma(y1[:, i2, :], YS, in_idx=SLOT[:, 0, g2:g2 + 1], bound=NSLAB * SLAB - 1, reads=[b_YS, b_SLOT], writes=[b_y1s[i2]])
        kb.idma(y2[:, i2, :], YS, in_idx=SLOT[:, 1, g2:g2 + 1], bound=NSLAB * SLAB - 1, reads=[b_YS, b_SLOT], writes=[b_y2s[i2]])

    for g2 in range(NBUF3 - 1):
        fetch3(g2)
    for gb in range(NB):
        i = gb % NBUF3
        b_s2 = b_s2s[i]
        if gb + NBUF3 - 1 < NB:
            fetch3(gb + NBUF3 - 1)
        cx.stt("dve", xb[:, i, :], y1[:, i, :], WGT[:, 0, gb:gb + 1], xb[:, i, :], ALU.mult, ALU.add,
               [b_y1s[i], b_WGT, b_xbs[i]], [b_xbs[i]])
        cx.stt("dve", xb[:, i, :], y2[:, i, :], WGT[:, 1, gb:gb + 1], xb[:, i, :], ALU.mult, ALU.add,
               [b_y2s[i], b_WGT, b_xbs[i]], [b_xbs[i]])
        cx.act(junk, xb[:, i, :], AF.Square, [b_xbs[i]], [b_junk, b_s2], accum=s2[:, i:i + 1])
        cx.ts("dve", s2[:, i:i + 1], s2[:, i:i + 1], 1.0 / D, EPS, ALU.mult, ALU.add, [b_s2], [b_s2])
        cx.act(s2[:, i:i + 1], s2[:, i:i + 1], AF.Sqrt, [b_s2], [b_s2])
        kb.op("dve", lambda g_, i=i: g_.reciprocal(out=s2[:, i:i + 1], in_=s2[:, i:i + 1]), [b_s2], [b_s2])
        cx.stt("dve", xb[:, i, :], xb[:, i, :], s2[:, i:i + 1], gfin, ALU.mult, ALU.mult, [b_xbs[i], b_s2, b_gfin], [b_xbs[i]])
        kb.dma("sp", out[gb * 128:(gb + 1) * 128, :], xb[:, i, :], reads=[b_xbs[i]], writes=[b_out], disjoint=True)
    cx.release(mp)
```

```python
import math
import numpy as np
import ml_dtypes
import concourse.bass as bass
import concourse.mybir as mybir
from concourse.bass_utils import run_bass_kernel_spmd

F32 = mybir.dt.float32
BF16 = mybir.dt.bfloat16
I32 = mybir.dt.int32
AF = mybir.ActivationFunctionType
ALU = mybir.AluOpType
AX = mybir.AxisListType

L = 4096
D = 1024
NB = L // 128
G = 64
GS = 16
PS = 64
TCH = 8
D_FF = 2688
NE = 8
MOE_FF = 3584
NH = 16
QK_NOPE = 64
QK_ROPE = 32
V_HEAD = 64
Q_LORA = 512
KV_LORA = 256
EPS = 1e-6
DT_MIN = 1e-3
DT_MAX = 1e-1
SLAB = 1024
NSLAB = (2 * L) // SLAB + NE - 1
NGRP = MOE_FF // 512


class Buf:
    __slots__ = ("name", "w", "r")

    def __init__(self, name):
        self.name = name
        self.w = {}
        self.r = {}


class KB:
    def __init__(self, nc):
        self.nc = nc
        self.eng = {"pe": nc.tensor, "act": nc.scalar, "dve": nc.vector,
                    "pool": nc.gpsimd, "sp": nc.sync}
        self.sem = {k: nc.alloc_semaphore(name="sem_" + k) for k in self.eng}
        self.cnt = {k: 0 for k in self.eng}
        self.known = {k: {} for k in self.eng}
        self.lanes = {}
        self.bufs = []
        self.n_ins = 0

    def _lane(self, lane):
        if lane not in self.lanes:
            pool = self.__dict__.setdefault("_lane_pool", [])
            if pool:
                self.lanes[lane] = pool.pop()
            else:
                self._nl = getattr(self, "_nl", 0) + 1
                self.lanes[lane] = [self.nc.alloc_semaphore(name=f"ln{self._nl}"), 0]

    def buf(self, name):
        b = Buf(name)
        self.bufs.append(b)
        return b

    def bufs_n(self, name, n):
        return [self.buf(f"{name}{i}") for i in range(n)]

    def _semof(self, key):
        if key[0] == "e":
            return self.sem[key[1]]
        return self.lanes[key[1]][0]

    def _need(self, reads, writes):
        need = {}
        for b in reads:
            for k, v in b.w.items():
                if need.get(k, 0) < v:
                    need[k] = v
        for b in writes:
            for k, v in b.w.items():
                if need.get(k, 0) < v:
                    need[k] = v
            for k, v in b.r.items():
                if need.get(k, 0) < v:
                    need[k] = v
        return need

    def _wait(self, e, need):
        kn = self.known[e]
        for k, v in need.items():
            if e == "pe" and k == ("e", "pe"):
                continue
            if kn.get(k, 0) >= v:
                continue
            self.eng[e].wait_ge(self._semof(k), v)
            kn[k] = v

    def op(self, e, fn, reads=(), writes=()):
        self._wait(e, self._need(reads, writes))
        ins = fn(self.eng[e])
        self.cnt[e] += 1
        c = self.cnt[e]
        ins.then_inc(self.sem[e], 1)
        key = ("e", e)
        for b in reads:
            b.r[key] = c
        for b in writes:
            b.w = {key: c}
            b.r = {}
        self.n_ins += 1
        return ins

    def dma(self, q, out, in_, reads=(), writes=(), lane=None, disjoint=False, **kw):
        if lane is None:
            lane = writes[0].name
        self._lane(lane)
        need = self._need(reads, writes)
        if disjoint:
            need.pop(("l", lane), None)
        self._wait(q, need)
        ins = self.eng[q].dma_start(out=out, in_=in_, **kw)
        ln = self.lanes[lane]
        ln[1] += 16
        ins.then_inc(ln[0], 16)
        key = ("l", lane)
        for b in reads:
            b.r[key] = ln[1]
        for b in writes:
            neww = {k: v for k, v in b.w.items() if k[0] == "l" and k != key}
            neww[key] = ln[1]
            b.w = neww
            b.r = {}
        self.n_ins += 1
        return ins

    def idma(self, out, in_, out_idx=None, in_idx=None, bound=None, reads=(), writes=(), lane=None, disjoint=False):
        if lane is None:
            lane = writes[0].name
        self._lane(lane)
        need = self._need(reads, writes)
        if disjoint:
            need.pop(("l", lane), None)
        self._wait("pool", need)
        oo = bass.IndirectOffsetOnAxis(ap=out_idx, axis=0) if out_idx is not None else None
        io = bass.IndirectOffsetOnAxis(ap=in_idx, axis=0) if in_idx is not None else None
        ins = self.nc.gpsimd.indirect_dma_start(out=out, out_offset=oo, in_=in_, in_offset=io)
        ln = self.lanes[lane]
        ln[1] += 16
        ins.then_inc(ln[0], 16)
        key = ("l", lane)
        for b in reads:
            b.r[key] = ln[1]
        for b in writes:
            neww = {k: v for k, v in b.w.items() if k[0] == "l" and k != key}
            neww[key] = ln[1]
            b.w = neww
            b.r = {}
        self.n_ins += 1
        return ins

    def finish(self, bufs):
        need = {}
        for b in bufs:
            for k, v in b.w.items():
                need[k] = max(need.get(k, 0), v)
        self._wait("sp", need)
        allneed = {("l", ln): v[1] for ln, v in self.lanes.items() if v[1] > 0}
        for e in self.eng:
            if self.cnt[e] > 0:
                allneed[("e", e)] = self.cnt[e]
        allneed.pop(("e", "sp"), None)
        self._wait("sp", allneed)

    def barrier(self):
        need = {("l", ln): v[1] for ln, v in self.lanes.items() if v[1] > 0}
        for e in self.eng:
            if self.cnt[e] > 0:
                need[("e", e)] = self.cnt[e]
        for e in self.eng:
            n2 = dict(need)
            n2.pop(("e", e), None)
            self._wait(e, n2)
        for b in self.bufs:
            b.w = {}
            b.r = {}
        pool = self.__dict__.setdefault("_lane_pool", [])
        for ln, v in self.lanes.items():
            pool.append(v)
        self.lanes = {}
        for e in self.eng:
            self.known[e] = {k: v for k, v in self.known[e].items() if k[0] == "e"}


class Ctx:
    def __init__(self, nc):
        self.nc = nc
        self.kb = KB(nc)
        self.ps = []
        self.psb = []
        for i in range(8):
            self.ps.append(nc.alloc_psum_tensor(f"ps{i}", [128, 512], F32).ap())
            self.psb.append(self.kb.buf(f"ps{i}"))
        self._mark = None

    def sb(self, name, shape, dtype=F32):
        self._n = getattr(self, "_n", 0) + 1
        t = self.nc.alloc_sbuf_tensor(f"sb{self._n}_{name}", list(shape), dtype).ap()
        return t, self.kb.buf(f"sb{self._n}_{name}")

    def mark(self):
        return (self.nc.sbuf_base, self.nc.sbuf_top)

    def release(self, m):
        self.kb.barrier()
        self.nc.sbuf_base, self.nc.sbuf_top = m

    def tt(self, e, out, in0, in1, op, r, w):
        return self.kb.op(e, lambda g: g.tensor_tensor(out=out, in0=in0, in1=in1, op=op), r, w)

    def ts(self, e, out, in0, s1, s2, op0, op1, r, w):
        if s2 is None:
            return self.kb.op(e, lambda g: g.tensor_scalar(out=out, in0=in0, scalar1=s1, scalar2=None, op0=op0), r, w)
        return self.kb.op(e, lambda g: g.tensor_scalar(out=out, in0=in0, scalar1=s1, scalar2=s2, op0=op0, op1=op1), r, w)

    def stt(self, e, out, in0, scalar, in1, op0, op1, r, w):
        return self.kb.op(e, lambda g: g.scalar_tensor_tensor(out=out, in0=in0, scalar=scalar, in1=in1, op0=op0, op1=op1), r, w)

    def cp(self, e, out, in_, r, w):
        if e == "act":
            return self.kb.op(e, lambda g: g.activation(out=out, in_=in_, func=AF.Copy), r, w)
        return self.kb.op(e, lambda g: g.tensor_copy(out=out, in_=in_), r, w)

    def act(self, out, in_, func, r, w, scale=1.0, bias=None, accum=None):
        kw = {}
        if bias is not None:
            kw["bias"] = bias
        if accum is not None:
            kw["accum_out"] = accum
        return self.kb.op("act", lambda g: g.activation(out=out, in_=in_, func=func, scale=scale, **kw), r, w)

    def mm(self, out, lhsT, rhs, start, stop, r, w, **kw):
        return self.kb.op("pe", lambda g: g.matmul(out, lhsT=lhsT, rhs=rhs, start=start, stop=stop, **kw), r, w)

    def tr(self, out, in_, ident, r, w):
        return self.kb.op("pe", lambda g: g.transpose(out=out, in_=in_, identity=ident), r, w)

    def memset(self, e, out, val, w):
        return self.kb.op(e, lambda g: g.memset(out, val), (), w)


PI = math.pi


def setup_ident(cx, din):
    P = {"bufs": {}}
    P["identf"], bf = cx.sb("identf", [128, 128], F32)
    P["identb"], bb = cx.sb("identb", [128, 128], BF16)
    cx.kb.dma("sp", P["identf"], din["ident"], writes=[bf])
    cx.cp("dve", P["identb"], P["identf"], [bf], [bb])
    P["bufs"]["identf"], P["bufs"]["identb"] = bf, bb
    return P


def phase0(cx, P, din, pre_hook=None):
    nc, kb = cx.nc, cx.kb
    b_identf, b_identb = P["bufs"]["identf"], P["bufs"]["identb"]
    P["WBre"], b_WBre = cx.sb("WBre", [128, 8, 8, 128], BF16)
    P["WBim"], b_WBim = cx.sb("WBim", [128, 8, 8, 128], BF16)
    P["WCre"], b_WCre = cx.sb("WCre", [128, 32, 8, 2, 16], BF16)
    P["WCim"], b_WCim = cx.sb("WCim", [128, 32, 8, 2, 16], BF16)
    P["FIRW"], b_FIRW = cx.sb("FIRW", [128, 8, 8, 128], BF16)
    P["A0c"], b_A0c = cx.sb("A0c", [128, 32, 8], F32)
    P["A0s"], b_A0s = cx.sb("A0s", [128, 32, 8], F32)
    P["A1c"], b_A1c = cx.sb("A1c", [128, 32, 8], F32)
    P["A1s"], b_A1s = cx.sb("A1s", [128, 32, 8], F32)
    P["RM8"], b_RM8 = cx.sb("RM8", [128, 32], F32)
    P["dT"], b_dT = cx.sb("dT", [128, 8], F32)
    P["bufs"].update(dict(WBre=b_WBre, WBim=b_WBim, WCre=b_WCre,
                          WCim=b_WCim, FIRW=b_FIRW, A0c=b_A0c, A0s=b_A0s, A1c=b_A1c, A1s=b_A1s, RM8=b_RM8, dT=b_dT))
    m = cx.mark()
    if pre_hook is not None:
        pre_hook()
    LR, b_LR = cx.sb("LR", [128, 32]); LI, b_LI = cx.sb("LI", [128, 32]); LDT, b_LDT = cx.sb("LDT", [128, 32])
    TH, b_TH = cx.sb("TH", [128, 32]); LM, b_LM = cx.sb("LM", [128, 32])
    EV, b_EV = cx.sb("EV", [128, 9, 32]); ANG, b_ANG = cx.sb("ANG", [128, 9, 32]); MAG, b_MAG = cx.sb("MAG", [128, 9, 32])
    SN, b_SN = cx.sb("SN", [128, 9, 32]); CS, b_CS = cx.sb("CS", [128, 9, 32]); IT, b_IT = cx.sb("IT", [128, 9, 32], I32)
    ARE, b_ARE = cx.sb("ARE", [128, 9, 32]); AIM, b_AIM = cx.sb("AIM", [128, 9, 32])
    NR, b_NR = cx.sb("NR", [128, 32]); DEN, b_DEN = cx.sb("DEN", [128, 32]); TMPa, b_TMPa = cx.sb("TMPa", [128, 32])
    TMPb, b_TMPb = cx.sb("TMPb", [128, 32])
    CRE, b_CRE = cx.sb("CRE", [128, 32]); CIM, b_CIM = cx.sb("CIM", [128, 32])
    WRE, b_WRE = cx.sb("WRE", [128, 8, 32]); WIM, b_WIM = cx.sb("WIM", [128, 8, 32])
    W8a, b_W8a = cx.sb("W8a", [128, 8, 32]); W8b, b_W8b = cx.sb("W8b", [128, 8, 32])
    BTre, b_BTre = cx.sb("BTre", [128, 32, 16]); BTim, b_BTim = cx.sb("BTim", [128, 32, 16])
    CTre, b_CTre = cx.sb("CTre", [128, 32, 16]); CTim, b_CTim = cx.sb("CTim", [128, 32, 16])
    T1, b_T1 = cx.sb("T1", [128, 32, 16]); T2, b_T2 = cx.sb("T2", [128, 32, 16])
    T3, b_T3 = cx.sb("T3", [128, 32, 16]); T4, b_T4 = cx.sb("T4", [128, 32, 16])
    XPre, b_XPre = cx.sb("XPre", [128, 8, 32, 2, 16], BF16); XPim, b_XPim = cx.sb("XPim", [128, 8, 32, 2, 16], BF16)
    CPre, b_CPre = cx.sb("CPre", [128, 32, 2, 16], BF16); CPnim, b_CPnim = cx.sb("CPnim", [128, 32, 2, 16], BF16)
    BM, b_BM = cx.sb("BM", [128, 128], F32)
    TK, b_TK = cx.sb("TK", [128, 128], F32)

    kb.dma("sp", BM, din["bmask"], writes=[b_BM])
    kb.dma("sp", EV, din["ev"].rearrange("p (e q) -> p e q", e=9), writes=[b_EV])
    kb.dma("sp", LR, din["lamT_re"], writes=[b_LR])
    kb.dma("sp", LI, din["lamT_im"], writes=[b_LI])
    kb.dma("sp", LDT, din["ldtT"], writes=[b_LDT])
    kb.dma("sp", P["dT"], din["dT"], writes=[b_dT])
    kb.dma("sp", BTre, din["bT_re"].rearrange("p (q n) -> p q n", n=16), writes=[b_BTre])
    kb.dma("sp", BTim, din["bT_im"].rearrange("p (q n) -> p q n", n=16), writes=[b_BTim])
    kb.dma("sp", CTre, din["cT_re"].rearrange("p (q n) -> p q n", n=16), writes=[b_CTre])
    kb.dma("sp", CTim, din["cT_im"].rearrange("p (q n) -> p q n", n=16), writes=[b_CTim])

    cx.act(LDT, LDT, AF.Exp, [b_LDT], [b_LDT])
    cx.tt("dve", TH, LI, LDT, ALU.mult, [b_LI, b_LDT], [b_TH])
    cx.tt("dve", LM, LR, LDT, ALU.mult, [b_LR, b_LDT], [b_LM])
    bc9 = lambda t: t.unsqueeze(1).to_broadcast([128, 9, 32])
    bc8 = lambda t: t.unsqueeze(1).to_broadcast([128, 8, 32])
    cx.tt("dve", ANG, EV, bc9(TH), ALU.mult, [b_EV, b_TH], [b_ANG])
    cx.tt("dve", MAG, EV, bc9(LM), ALU.mult, [b_EV, b_LM], [b_MAG])
    cx.act(MAG, MAG, AF.Exp, [b_MAG], [b_MAG])
    cx.ts("dve", SN, ANG, 1.0 / (2.0 * PI), None, ALU.mult, None, [b_ANG], [b_SN])
    cx.cp("dve", IT, SN, [b_SN], [b_IT])
    cx.cp("dve", SN, IT, [b_IT], [b_SN])
    cx.stt("dve", SN, SN, -2.0 * PI, ANG, ALU.mult, ALU.add, [b_SN, b_ANG], [b_SN])
    cx.ts("dve", CS, ANG, 1.0 / (2.0 * PI), 0.25, ALU.mult, ALU.add, [b_ANG], [b_CS])
    cx.cp("dve", IT, CS, [b_CS], [b_IT])
    cx.cp("dve", CS, IT, [b_IT], [b_CS])
    cx.stt("dve", CS, CS, -2.0 * PI, ANG, ALU.mult, ALU.add, [b_CS, b_ANG], [b_CS])
    cx.ts("dve", CS, CS, 0.5 * PI, None, ALU.add, None, [b_CS], [b_CS])
    cx.ts("dve", SN, SN, -PI, PI, ALU.max, ALU.min, [b_SN], [b_SN])
    cx.ts("dve", CS, CS, -PI, PI, ALU.max, ALU.min, [b_CS], [b_CS])
    cx.act(SN, SN, AF.Sin, [b_SN], [b_SN])
    cx.act(CS, CS, AF.Sin, [b_CS], [b_CS])
    cx.tt("dve", ARE, MAG, CS, ALU.mult, [b_MAG, b_CS], [b_ARE])
    cx.tt("dve", AIM, MAG, SN, ALU.mult, [b_MAG, b_SN], [b_AIM])
    cx.ts("dve", NR, ARE[:, 1, :], -1.0, None, ALU.add, None, [b_ARE], [b_NR])
    NI = AIM[:, 1, :]
    cx.tt("dve", DEN, LR, LR, ALU.mult, [b_LR], [b_DEN])
    cx.tt("dve", TMPa, LI, LI, ALU.mult, [b_LI], [b_TMPa])
    cx.tt("dve", DEN, DEN, TMPa, ALU.add, [b_DEN, b_TMPa], [b_DEN])
    kb.op("dve", lambda g: g.reciprocal(out=DEN, in_=DEN), [b_DEN], [b_DEN])
    cx.tt("dve", TMPa, NR, LR, ALU.mult, [b_NR, b_LR], [b_TMPa])
    cx.tt("dve", TMPb, NI, LI, ALU.mult, [b_AIM, b_LI], [b_TMPb])
    cx.tt("dve", TMPa, TMPa, TMPb, ALU.add, [b_TMPa, b_TMPb], [b_TMPa])
    cx.tt("dve", CRE, TMPa, DEN, ALU.mult, [b_TMPa, b_DEN], [b_CRE])
    cx.tt("dve", TMPa, NI, LR, ALU.mult, [b_AIM, b_LR], [b_TMPa])
    cx.tt("dve", TMPb, NR, LI, ALU.mult, [b_NR, b_LI], [b_TMPb])
    cx.tt("dve", TMPa, TMPa, TMPb, ALU.subtract, [b_TMPa, b_TMPb], [b_TMPa])
    cx.tt("dve", CIM, TMPa, DEN, ALU.mult, [b_TMPa, b_DEN], [b_CIM])
    cx.tt("dve", W8a, ARE[:, 0:8, :], bc8(CRE), ALU.mult, [b_ARE, b_CRE], [b_W8a])
    cx.tt("dve", W8b, AIM[:, 0:8, :], bc8(CIM), ALU.mult, [b_AIM, b_CIM], [b_W8b])
    cx.tt("dve", WRE, W8a, W8b, ALU.subtract, [b_W8a, b_W8b], [b_WRE])
    cx.tt("dve", W8a, ARE[:, 0:8, :], bc8(CIM), ALU.mult, [b_ARE, b_CIM], [b_W8a])
    cx.tt("dve", W8b, AIM[:, 0:8, :], bc8(CRE), ALU.mult, [b_AIM, b_CRE], [b_W8b])
    cx.tt("dve", WIM, W8a, W8b, ALU.add, [b_W8a, b_W8b], [b_WIM])
    cx.cp("dve", P["RM8"], MAG[:, 8, :], [b_MAG], [b_RM8])
    A0c, A0s, A1c, A1s = P["A0c"], P["A0s"], P["A1c"], P["A1s"]
    cx.cp("dve", A0c[:, :, 0], CS[:, 8, :], [b_CS], [b_A0c])
    cx.cp("dve", A0s[:, :, 0], SN[:, 8, :], [b_SN], [b_A0s])
    for i in range(1, 8):
        cx.tt("dve", TMPa, A0c[:, :, i - 1], A0c[:, :, 0], ALU.mult, [b_A0c], [b_TMPa])
        cx.tt("dve", TMPb, A0s[:, :, i - 1], A0s[:, :, 0], ALU.mult, [b_A0s], [b_TMPb])
        cx.tt("dve", A0c[:, :, i], TMPa, TMPb, ALU.subtract, [b_TMPa, b_TMPb], [b_A0c])
        cx.tt("dve", TMPa, A0c[:, :, i - 1], A0s[:, :, 0], ALU.mult, [b_A0c, b_A0s], [b_TMPa])
        cx.tt("dve", TMPb, A0s[:, :, i - 1], A0c[:, :, 0], ALU.mult, [b_A0s, b_A0c], [b_TMPb])
        cx.tt("dve", A0s[:, :, i], TMPa, TMPb, ALU.add, [b_TMPa, b_TMPb], [b_A0s])
    cx.memset("dve", A1c[:, :, 0], 1.0, [b_A1c])
    cx.memset("dve", A1s[:, :, 0], 0.0, [b_A1s])
    for i in range(1, 8):
        cx.tt("dve", TMPa, A1c[:, :, i - 1], A0c[:, :, 7], ALU.mult, [b_A1c, b_A0c], [b_TMPa])
        cx.tt("dve", TMPb, A1s[:, :, i - 1], A0s[:, :, 7], ALU.mult, [b_A1s, b_A0s], [b_TMPb])
        cx.tt("dve", A1c[:, :, i], TMPa, TMPb, ALU.subtract, [b_TMPa, b_TMPb], [b_A1c])
        cx.tt("dve", TMPa, A1c[:, :, i - 1], A0s[:, :, 7], ALU.mult, [b_A1c, b_A0s], [b_TMPa])
        cx.tt("dve", TMPb, A1s[:, :, i - 1], A0c[:, :, 7], ALU.mult, [b_A1s, b_A0c], [b_TMPb])
        cx.tt("dve", A1s[:, :, i], TMPa, TMPb, ALU.add, [b_TMPa, b_TMPb], [b_A1s])

    cx.memset("pool", XPre, 0.0, [b_XPre])
    cx.memset("pool", XPim, 0.0, [b_XPim])
    cx.memset("pool", CPre, 0.0, [b_CPre])
    cx.memset("pool", CPnim, 0.0, [b_CPnim])
    cx.memset("pool", P["WCre"], 0.0, [b_WCre])
    cx.memset("pool", P["WCim"], 0.0, [b_WCim])
    bcn = lambda t: t.unsqueeze(2).to_broadcast([128, 32, 16])
    for s in range(8):
        e = 7 - s
        cx.tt("dve", T1, BTre, bcn(WRE[:, e, :]), ALU.mult, [b_BTre, b_WRE], [b_T1])
        cx.tt("dve", T2, BTim, bcn(WIM[:, e, :]), ALU.mult, [b_BTim, b_WIM], [b_T2])
        cx.tt("dve", T3, BTim, bcn(WRE[:, e, :]), ALU.mult, [b_BTim, b_WRE], [b_T3])
        cx.tt("dve", T4, BTre, bcn(WIM[:, e, :]), ALU.mult, [b_BTre, b_WIM], [b_T4])
        for par in range(2):
            sl = slice(64 * par, 64 * par + 64)
            cx.tt("dve", XPre[sl, s, :, par, :], T1[sl], T2[sl], ALU.subtract, [b_T1, b_T2], [b_XPre])
            cx.tt("dve", XPim[sl, s, :, par, :], T3[sl], T4[sl], ALU.add, [b_T3, b_T4], [b_XPim])
    for par in range(2):
        sl = slice(64 * par, 64 * par + 64)
        cx.cp("dve", CPre[sl, :, par, :], CTre[sl], [b_CTre], [b_CPre])
        cx.ts("dve", CPnim[sl, :, par, :], CTim[sl], -1.0, None, ALU.mult, None, [b_CTim], [b_CPnim])
    for j in range(8):
        e = j + 1
        cx.tt("dve", T1, CTre, bcn(ARE[:, e, :]), ALU.mult, [b_CTre, b_ARE], [b_T1])
        cx.tt("dve", T2, CTim, bcn(AIM[:, e, :]), ALU.mult, [b_CTim, b_AIM], [b_T2])
        cx.tt("dve", T3, CTre, bcn(AIM[:, e, :]), ALU.mult, [b_CTre, b_AIM], [b_T3])
        cx.tt("dve", T4, CTim, bcn(ARE[:, e, :]), ALU.mult, [b_CTim, b_ARE], [b_T4])
        for par in range(2):
            sl = slice(64 * par, 64 * par + 64)
            cx.tt("dve", P["WCre"][sl, :, j, par, :], T1[sl], T2[sl], ALU.subtract, [b_T1, b_T2], [b_WCre])
            cx.stt("dve", P["WCim"][sl, :, j, par, :], T3[sl], -1.0, T4[sl], ALU.mult, ALU.subtract,
                   [b_T3, b_T4], [b_WCim])
    for fc in range(8):
        pq = slice(4 * fc, 4 * fc + 4)
        for k in range(8):
            pi = (fc * 8 + k) % 4
            ps, bps = cx.ps[pi], cx.psb[pi]
            cx.mm(ps[:, 0:128], XPre[:, 7 - k, pq, :, :], CPre[:, pq, :, :], True, False, [b_XPre, b_CPre], [bps])
            cx.mm(ps[:, 0:128], XPim[:, 7 - k, pq, :, :], CPnim[:, pq, :, :], False, True, [b_XPim, b_CPnim], [bps])
            if k == 0:
                cx.tt("dve", TK, ps[:, 0:128], BM, ALU.mult, [bps, b_BM], [b_TK])
                cx.stt("dve", P["FIRW"][:, fc, 0, :], P["identf"], P["dT"][:, fc:fc + 1], TK, ALU.mult, ALU.add,
                       [b_identf, b_dT, b_TK], [b_FIRW])
            else:
                cx.tt("dve", P["FIRW"][:, fc, k, :], ps[:, 0:128], BM, ALU.mult, [bps, b_BM], [b_FIRW])
    for (XP, b_XP, WB, b_WB) in ((XPre, b_XPre, P["WBre"], b_WBre), (XPim, b_XPim, P["WBim"], b_WBim)):
        for fc in range(8):
            pq = slice(4 * fc, 4 * fc + 4)
            pi = 4 + (fc % 2)
            psb16 = cx.ps[pi].bitcast(BF16).rearrange("p (s c) -> p s c", s=8)
            for s in range(8):
                cx.tr(psb16[:, s, :], XP[:, s, pq, :, :], P["identb"], [b_XP, b_identb], [cx.psb[pi]])
            cx.cp("act" if fc % 2 else "dve", WB[:, fc, :, :], psb16, [cx.psb[pi]], [b_WB])
    cx.release(m)
    return P


class PsRot:
    def __init__(self, cx, banks):
        self.cx = cx
        self.banks = list(banks)
        self.i = 0

    def next(self):
        b = self.banks[self.i % len(self.banks)]
        self.i += 1
        return self.cx.ps[b], self.cx.psb[b]


class WStream:
    def __init__(self, cx, nslots, slot_elems, name="ws", direct=None, ahead=None):
        self.cx = cx
        self.slots = []
        for i in range(nslots):
            t, b = cx.sb(f"{name}{i}", [128, slot_elems], BF16)
            self.slots.append((t, b))
        self.jobs = []
        self.issued = 0
        self.used = 0
        self.res = {}
        self.direct = direct
        self.ahead = ahead

    def plan(self, jobs):
        self.jobs.extend(jobs)

    def _issue(self, i):
        t, b = self.slots[i % len(self.slots)]
        off = 0
        views = []
        if self.direct is not None:
            src, n = self.jobs[i]
            self.cx.kb.dma("sp", t[:, 0:n], src, reads=[self.direct], writes=[b])
            self.res[i] = (t, b)
            return
        for (src, a, n) in self.jobs[i]:
            v = t[:, off:off + a * n].rearrange("p (a n) -> p a n", n=n)
            self.cx.kb.dma("pool", v, src, writes=[b])
            views.append(v)
            off += a * n
        self.res[i] = (views, b)

    def get(self):
        i = self.used
        ahead = self.ahead if self.ahead is not None else max(1, len(self.slots) - 2)
        while self.issued < min(len(self.jobs), i + 1 + ahead):
            self._issue(self.issued)
            self.issued += 1
        self.used += 1
        return self.res.pop(i)


def rms_to_hT(cx, P, xt, b_xt, nblk, gT, b_gT, htok, b_htok, hT, b_hT, ss, b_ss, trbanks, d=D, extra=None):
    nkc = d // 128
    bx = b_xt if isinstance(b_xt, list) else [b_xt]
    for b in range(nblk):
        cx.act(htok[:, b, :], xt[:, b, :], AF.Square, bx, [b_htok, b_ss], accum=ss[:, b:b + 1])
    cx.ts("dve", ss[:, 0:nblk], ss[:, 0:nblk], 1.0 / d, EPS, ALU.mult, ALU.add, [b_ss], [b_ss])
    cx.act(ss[:, 0:nblk], ss[:, 0:nblk], AF.Sqrt, [b_ss], [b_ss])
    cx.kb.op("dve", lambda g: g.reciprocal(out=ss[:, 0:nblk], in_=ss[:, 0:nblk]), [b_ss], [b_ss])
    for b in range(nblk):
        cx.ts("dve", htok[:, b, :], xt[:, b, :], ss[:, b:b + 1], None, ALU.mult, None, bx + [b_ss], [b_htok])
    for b in range(nblk):
        pi = trbanks[b % len(trbanks)]
        p16 = cx.ps[pi].bitcast(BF16).rearrange("p (k c) -> p k c", c=128)
        for kc in range(nkc):
            cx.tr(p16[:, kc, :], htok[:, b, kc * 128:(kc + 1) * 128], P["identb"], [b_htok, P["bufs"]["identb"]], [cx.psb[pi]])
        cx.tt("dve", hT[:, 0:nkc, b * 128:(b + 1) * 128], p16[:, 0:nkc, :],
              gT[:, 0:nkc].unsqueeze(2).to_broadcast([128, nkc, 128]), ALU.mult, [cx.psb[pi], b_gT], [b_hT])
        if extra is not None:
            gT2, hT2, b_hT2 = extra
            cx.tt("dve", hT2[:, 0:nkc, b * 128:(b + 1) * 128], p16[:, 0:nkc, :],
                  gT2[:, 0:nkc].unsqueeze(2).to_broadcast([128, nkc, 128]), ALU.mult, [cx.psb[pi], b_gT], [b_hT2])


N_L0_JOBS = 6 + D_FF // 128


def layer0_convert(cx, din, W0, b_W0, nstage=4):
    kb = cx.kb
    stage = [cx.sb(f"l0st{i}", [128, 4096], BF16) for i in range(nstage)]
    j = 0
    for w in (din["s5_w_in"], din["s5_w_glu"], din["s5_w_out"]):
        for half in range(2):
            st, b_st = stage[j % nstage]
            kb.dma("pool", st.rearrange("p (k n) -> p k n", n=512),
                   w[:, half * 512:(half + 1) * 512].rearrange("(k p) n -> p k n", p=128), writes=[b_st])
            kb.dma("sp", W0[j * 128:(j + 1) * 128, :], st, reads=[b_st], writes=[b_W0], disjoint=True)
            j += 1
    for f in range(D_FF // 128):
        st, b_st = stage[j % nstage]
        cs = slice(f * 128, (f + 1) * 128)
        kb.dma("pool", st[:, 0:1024].rearrange("p (k n) -> p k n", n=128),
               din["ffn_w_gate"][:, cs].rearrange("(k p) n -> p k n", p=128), writes=[b_st])
        kb.dma("pool", st[:, 1024:2048].rearrange("p (k n) -> p k n", n=128),
               din["ffn_w_up"][:, cs].rearrange("(k p) n -> p k n", p=128), writes=[b_st])
        kb.dma("pool", st[:, 2048:3072], din["ffn_w_down"][cs, :], writes=[b_st])
        kb.dma("sp", W0[j * 128:(j + 1) * 128, 0:3072], st[:, 0:3072], reads=[b_st], writes=[b_W0], disjoint=True)
        j += 1


GELU_C = 0.044715
GELU_S = 2.0 * math.sqrt(2.0 / math.pi)


def phase1(cx, P, din, xs, b_xs, W0, b_W0, ntiles=8, hook=None):
    nc, kb = cx.nc, cx.kb
    B = P["bufs"]
    m = cx.mark()
    xt, _ = cx.sb("xt", [128, 4, 1024], F32)
    b_xt = kb.bufs_n("xt8_", 8)
    regA, b_A = cx.sb("regA", [128, 4096], F32)
    regB, b_B = cx.sb("regB", [128, 2048], F32)
    regC, b_C = cx.sb("regC", [128, 2048], F32)
    uT, _ = cx.sb("uT", [128, 8, 512], BF16)
    b_u = kb.bufs_n("uT", 8)
    yT, _ = cx.sb("yT", [128, 8, 512], BF16)
    b_y = kb.bufs_n("yT", 8)
    SR, b_SR = cx.sb("SR", [128, 32, 65], F32)
    SI, b_SI = cx.sb("SI", [128, 32, 65], F32)
    SB, b_SB = cx.sb("SB", [128, 32, 2, 64], BF16)
    ss, b_ss = cx.sb("ss", [128, 8], F32)
    gT, b_gT = cx.sb("gT", [128, 2, 8], F32)
    CAR, b_CAR = cx.sb("CAR", [128, 2, 32], F32)
    ws = WStream(cx, 3, 4096, direct=b_W0, ahead=2)
    y32 = regA.rearrange("p (f t) -> p f t", t=512)
    t3 = regA[:, 0:2048].rearrange("p (q c) -> p q c", c=64)
    t4 = regA[:, 2048:4096].rearrange("p (q c) -> p q c", c=64)
    htok = regB.bitcast(BF16).rearrange("p (b d) -> p b d", d=1024)
    t1 = regB.rearrange("p (q c) -> p q c", c=64)
    hT = regC.bitcast(BF16).rearrange("p (k t) -> p k t", t=512)
    t2 = regC.rearrange("p (q c) -> p q c", c=64)
    zT, b_z = uT, b_u
    sgs = [regB[:, i * 512:(i + 1) * 512] for i in range(2)]
    acts = [yT.rearrange("p f t -> p (f t)")[:, i * 512:(i + 1) * 512] for i in range(4)]
    rot = PsRot(cx, [2, 3, 4, 5, 6, 7])
    ytmp = [(regA[:, i * 512:(i + 1) * 512], kb.buf(f"ytmp{i}")) for i in range(4)]
    nt = 0

    kb.dma("sp", gT[:, 0, :], din["gT_mix0"], writes=[b_gT])
    kb.dma("sp", gT[:, 1, :], din["gT_ffn0"], writes=[b_gT])
    cx.memset("dve", CAR, 0.0, [b_CAR])
    w_in, w_glu, w_out = din["s5_w_in"], din["s5_w_glu"], din["s5_w_out"]
    wg, wu, wd = din["ffn_w_gate"], din["ffn_w_up"], din["ffn_w_down"]

    def wcols(w, c0, n):
        return (w[:, c0:c0 + n].rearrange("(k p) n -> p k n", p=128), 8, n)

    jobs = []
    for T in range(ntiles):
        for j in range(N_L0_JOBS):
            jobs.append((W0[j * 128:(j + 1) * 128, 0:(4096 if j < 6 else 3072)], 4096 if j < 6 else 3072))
    ws.plan(jobs)

    for T in range(ntiles):
        t0 = T * 512
        kb.dma("sp", xt, din["x"][t0:t0 + 512, :].rearrange("(b p) d -> p b d", p=128), writes=b_xt, lane="xt")
        rms_to_hT(cx, P, xt, b_xt, 4, gT[:, 0, :], b_gT, htok, b_B, hT, b_C, ss, b_ss, [0, 1])
        for half in range(2):
            wt_, b_w = ws.get()
            wv = wt_.rearrange("p (k n) -> p k n", n=512)
            for f4 in range(4):
                fc = half * 4 + f4
                ps, bps = rot.next()
                for kc in range(8):
                    cx.mm(ps, wv[:, kc, f4 * 128:(f4 + 1) * 128], hT[:, kc, :], kc == 0, kc == 7, [b_w, b_C], [bps])
                cx.cp("act", uT[:, fc, :], ps, [bps], [b_u[fc]])
        a0c = lambda: P["A0c"].unsqueeze(2).to_broadcast([128, 32, 8, 8])
        a0s = lambda: P["A0s"].unsqueeze(2).to_broadcast([128, 32, 8, 8])
        a1c = lambda: P["A1c"].unsqueeze(3).to_broadcast([128, 32, 8, 8])
        a1s = lambda: P["A1s"].unsqueeze(3).to_broadcast([128, 32, 8, 8])
        v4 = lambda t: t.rearrange("p q (a b) -> p q a b", b=8)
        SRv, SIv = SR[:, :, 0:64], SI[:, :, 0:64]
        for qb in range(4):
            psr, bpsr = rot.next()
            psi, bpsi = rot.next()
            for q8 in range(8):
                q = qb * 8 + q8
                fc, q4 = q // 4, q % 4
                rows = slice(32 * q4, 32 * q4 + 32)
                uv = uT[rows, fc, :].rearrange("p (c s) -> p c s", s=8)
                for (pp, bpp, WB, bWB) in ((psr, bpsr, P["WBre"], B["WBre"]), (psi, bpsi, P["WBim"], B["WBim"])):
                    for s in range(8):
                        cx.mm(pp[:, q8 * 64:(q8 + 1) * 64], WB[rows, fc, s, :], uv[:, :, s], s == 0, s == 7,
                              [bWB, b_u[fc]], [bpp], tile_position=(32 * q4, 0))
            qs = slice(qb * 8, (qb + 1) * 8)
            pr4 = psr.rearrange("p (q a b) -> p q a b", a=8, b=8)
            pi4 = psi.rearrange("p (q a b) -> p q a b", a=8, b=8)
            c4 = P["A0c"][:, qs, :].unsqueeze(2).to_broadcast([128, 8, 8, 8])
            s4 = P["A0s"][:, qs, :].unsqueeze(2).to_broadcast([128, 8, 8, 8])
            cx.tt("dve", v4(t3)[:, qs], pr4, c4, ALU.mult, [bpsr, B["A0c"]], [b_A])
            cx.tt("dve", v4(t4)[:, qs], pi4, s4, ALU.mult, [bpsi, B["A0s"]], [b_A])
            cx.tt("dve", v4(SRv)[:, qs], v4(t3)[:, qs], v4(t4)[:, qs], ALU.add, [b_A], [b_SR])
            cx.tt("dve", v4(t3)[:, qs], pi4, c4, ALU.mult, [bpsi, B["A0c"]], [b_A])
            cx.tt("dve", v4(t4)[:, qs], pr4, s4, ALU.mult, [bpsr, B["A0s"]], [b_A])
            cx.tt("dve", v4(SIv)[:, qs], v4(t3)[:, qs], v4(t4)[:, qs], ALU.subtract, [b_A], [b_SI])
        if hook is not None:
            hook()
        cx.tt("dve", v4(t3), v4(SRv), a1c(), ALU.mult, [b_SR, B["A1c"]], [b_A])
        cx.tt("dve", v4(t4), v4(SIv), a1s(), ALU.mult, [b_SI, B["A1s"]], [b_A])
        cx.tt("dve", v4(t1), v4(t3), v4(t4), ALU.add, [b_A], [b_B])
        cx.tt("dve", v4(t3), v4(SIv), a1c(), ALU.mult, [b_SI, B["A1c"]], [b_A])
        cx.tt("dve", v4(t4), v4(SRv), a1s(), ALU.mult, [b_SR, B["A1s"]], [b_A])
        cx.tt("dve", v4(t2), v4(t3), v4(t4), ALU.subtract, [b_A], [b_C])
        for q in range(32):
            rm = P["RM8"][:, q:q + 1].to_broadcast([128, 64])
            kb.op("dve", lambda g_, q=q, rm=rm: g_.tensor_tensor_scan(out=SRv[:, q, :], data0=rm, data1=t1[:, q, :],
                  initial=CAR[:, 0, q:q + 1], op0=ALU.mult, op1=ALU.add), [b_B, B["RM8"], b_CAR], [b_SR])
            kb.op("dve", lambda g_, q=q, rm=rm: g_.tensor_tensor_scan(out=SIv[:, q, :], data0=rm, data1=t2[:, q, :],
                  initial=CAR[:, 1, q:q + 1], op0=ALU.mult, op1=ALU.add), [b_C, B["RM8"], b_CAR], [b_SI])
        cx.tt("dve", v4(t3), v4(SRv), a1c(), ALU.mult, [b_SR, B["A1c"]], [b_A])
        cx.tt("dve", v4(t4), v4(SIv), a1s(), ALU.mult, [b_SI, B["A1s"]], [b_A])
        cx.tt("dve", v4(t1), v4(t3), v4(t4), ALU.subtract, [b_A], [b_B])
        cx.tt("dve", v4(t3), v4(SIv), a1c(), ALU.mult, [b_SI, B["A1c"]], [b_A])
        cx.tt("dve", v4(t4), v4(SRv), a1s(), ALU.mult, [b_SR, B["A1s"]], [b_A])
        cx.tt("dve", v4(t2), v4(t3), v4(t4), ALU.add, [b_A], [b_C])
        cx.cp("dve", SB[:, :, 0, 0:1], CAR[:, 0, :].unsqueeze(2), [b_CAR], [b_SB])
        cx.cp("dve", SB[:, :, 1, 0:1], CAR[:, 1, :].unsqueeze(2), [b_CAR], [b_SB])
        cx.tt("dve", v4(t3), v4(t1), a0c(), ALU.mult, [b_B, B["A0c"]], [b_A])
        cx.tt("dve", v4(t4), v4(t2), a0s(), ALU.mult, [b_C, B["A0s"]], [b_A])
        cx.tt("dve", SB[:, :, 0, 1:64], t3[:, :, 0:63], t4[:, :, 0:63], ALU.subtract, [b_A], [b_SB])
        cx.tt("dve", CAR[:, 0, :].unsqueeze(2), t3[:, :, 63:64], t4[:, :, 63:64], ALU.subtract, [b_A], [b_CAR])
        cx.tt("dve", v4(t3), v4(t2), a0c(), ALU.mult, [b_C, B["A0c"]], [b_A])
        cx.tt("dve", v4(t4), v4(t1), a0s(), ALU.mult, [b_B, B["A0s"]], [b_A])
        cx.tt("dve", SB[:, :, 1, 1:64], t3[:, :, 0:63], t4[:, :, 0:63], ALU.add, [b_A], [b_SB])
        cx.tt("dve", CAR[:, 1, :].unsqueeze(2), t3[:, :, 63:64], t4[:, :, 63:64], ALU.add, [b_A], [b_CAR])
        for fc in range(8):
            ps, bps = rot.next()
            uv = uT[:, fc, :].rearrange("p (c s) -> p c s", s=8)
            pv = ps.rearrange("p (c s) -> p c s", s=8)
            for k in range(8):
                cx.mm(pv[:, :, k:8], P["FIRW"][:, fc, k, :], uv[:, :, 0:8 - k], k == 0, False,
                      [B["FIRW"], b_u[fc]], [bps])
            for q4 in range(4):
                q = fc * 4 + q4
                rows = slice(32 * q4, 32 * q4 + 32)
                for j in range(8):
                    last = (q4 == 3 and j == 7)
                    cx.mm(pv[rows, :, j], P["WCre"][:, q, j, :, :], SB[:, q, 0, :], False, False,
                          [B["WCre"], b_SB], [bps], tile_position=(0, 32 * q4))
                    cx.mm(pv[rows, :, j], P["WCim"][:, q, j, :, :], SB[:, q, 1, :], False, last,
                          [B["WCim"], b_SB], [bps], tile_position=(0, 32 * q4))
            yv = y32[:, fc, :]
            g1 = sgs[fc % 2]
            cx.act(g1, ps, AF.Square, [bps], [b_B])
            cx.ts("dve", g1, g1, GELU_C, 1.0, ALU.mult, ALU.add, [b_B], [b_B])
            cx.tt("dve", g1, g1, ps, ALU.mult, [b_B, bps], [b_B])
            cx.act(g1, g1, AF.Sigmoid, [b_B], [b_B], scale=GELU_S)
            cx.tt("dve", yv, g1, ps, ALU.mult, [b_B, bps], [b_A])
            cx.cp("act", yT[:, fc, :], yv, [b_A], [b_y[fc]])
        for half in range(2):
            wt_, b_w = ws.get()
            wv = wt_.rearrange("p (k n) -> p k n", n=512)
            for f4 in range(4):
                fc = half * 4 + f4
                ps, bps = rot.next()
                for kc in range(8):
                    cx.mm(ps, wv[:, kc, f4 * 128:(f4 + 1) * 128], yT[:, kc, :], kc == 0, kc == 7, [b_w, b_y[kc]], [bps])
                g1 = sgs[fc % 2]
                cx.act(g1, ps, AF.Sigmoid, [bps], [b_B])
                cx.tt("dve", zT[:, fc, :], y32[:, fc, :], g1, ALU.mult, [b_A, b_B], [b_z[fc]])
        for half in range(2):
            wt_, b_w = ws.get()
            wv = wt_.rearrange("p (k n) -> p k n", n=512)
            for b in range(4):
                ps, bps = rot.next()
                for kc in range(8):
                    cx.mm(ps, zT[:, kc, b * 128:(b + 1) * 128], wv[:, kc, :], kc == 0, kc == 7, [b_z[kc], b_w], [bps])
                xv = xt[:, b, half * 512:(half + 1) * 512]
                cx.tt("dve", xv, xv, ps, ALU.add, [b_xt[b * 2 + half], bps], [b_xt[b * 2 + half]])
        rms_to_hT(cx, P, xt, b_xt, 4, gT[:, 1, :], b_gT, htok, b_B, hT, b_C, ss, b_ss, [0, 1])
        for f in range(D_FF // 128):
            wt_, b_w = ws.get()
            gv = wt_[:, 0:1024].rearrange("p (k n) -> p k n", n=128)
            uvw = wt_[:, 1024:2048].rearrange("p (k n) -> p k n", n=128)
            dv = wt_[:, 2048:3072].rearrange("p (a d) -> p a d", d=1024)
            pg, bpg = rot.next()
            pu, bpu = rot.next()
            for kc in range(8):
                cx.mm(pg, gv[:, kc, :], hT[:, kc, :], kc == 0, kc == 7, [b_w, b_C], [bpg])
            for kc in range(8):
                cx.mm(pu, uvw[:, kc, :], hT[:, kc, :], kc == 0, kc == 7, [b_w, b_C], [bpu])
            g1 = sgs[f % 2]
            a1 = acts[f % 4]
            b_a = b_y[(f % 4)]
            cx.act(g1, pg, AF.Silu, [bpg], [b_B])
            cx.tt("dve", a1, g1, pu, ALU.mult, [b_B, bpu], [b_a])
            for b in range(4):
                for half in range(2):
                    ps, bps = rot.next()
                    cx.mm(ps, a1[:, b * 128:(b + 1) * 128], dv[:, 0, half * 512:(half + 1) * 512], True, True,
                          [b_a, b_w], [bps])
                    xv = xt[:, b, half * 512:(half + 1) * 512]
                    bx = b_xt[b * 2 + half]
                    if (b * 2 + half) % 2 == 0:
                        cx.tt("dve", xv, xv, ps, ALU.add, [bx, bps], [bx])
                    else:
                        tb, b_tb = ytmp[nt % 4]
                        nt += 1
                        cx.cp("act", tb, ps, [bps], [b_tb])
                        cx.tt("pool", xv, xv, tb, ALU.add, [bx, b_tb], [bx])
        kb.dma("sp", xs[t0:t0 + 512, :].rearrange("(b p) d -> p b d", p=128), xt, reads=b_xt, writes=[b_xs], disjoint=True)
    cx.release(m)


IN_SPECS = {
    "x": ([L, D], F32), "pos": ([1, L], I32),
    "ident": ([128, 128], F32), "bmask": ([128, 128], F32), "ev": ([128, 9 * 32], F32),
    "ltri": ([128, 128], F32), "wtab": ([128, NSLAB * NE], F32), "ctab": ([128, 7], F32),
    "invf_t": ([128, 16], F32), "invf_f": ([128, 1], F32), "esel": ([128, 31], F32), "dmask": ([128, 128], F32),
    "lamT_re": ([128, 32], F32), "lamT_im": ([128, 32], F32), "ldtT": ([128, 32], F32),
    "bT_re": ([128, 512], F32), "bT_im": ([128, 512], F32),
    "cT_re": ([128, 512], F32), "cT_im": ([128, 512], F32), "dT": ([128, 8], F32),
    "gT_mix0": ([128, 8], F32), "gT_ffn0": ([128, 8], F32), "gT_kv": ([128, 8], F32),
    "gT_mix1": ([128, 8], F32), "gT_ffn1": ([128, 8], F32), "g_final": ([D], F32),
    "g_kvlat": ([KV_LORA], F32), "g_qlat": ([Q_LORA], F32),
    "s5_w_in": ([D, D], F32), "s5_w_glu": ([D, D], F32), "s5_w_out": ([D, D], F32),
    "ffn_w_gate": ([D, D_FF], F32), "ffn_w_up": ([D, D_FF], F32), "ffn_w_down": ([D_FF, D], F32),
    "w_dkv": ([D, KV_LORA + QK_ROPE], F32), "w_ukv": ([KV_LORA, NH * 128], F32),
    "w_dq": ([D, Q_LORA], F32), "w_uq": ([Q_LORA, NH * 96], F32), "w_o": ([NH * V_HEAD, D], F32),
    "router_w": ([D, NE], F32), "router_wT": ([NE, D], F32), "g_ffn1": ([D], F32), "sel": ([8, NE * 128], F32),
    "moe_w_gate": ([NE, D, MOE_FF], F32), "moe_w_up": ([NE, D, MOE_FF], F32),
    "moe_w_down": ([NE, MOE_FF, D], F32),
}


def host_consts():
    c = {}
    c["ident"] = np.eye(128, dtype=np.float32)
    blk = np.arange(128) // 16
    c["bmask"] = (blk[:, None] == blk[None, :]).astype(np.float32)
    c["ev"] = np.broadcast_to(np.arange(9, dtype=np.float32)[None, :, None], (128, 9, 32)).reshape(128, 288).copy()
    invf = (np.float32(10000.0) ** (-np.arange(16, dtype=np.float32) * np.float32(2.0 / QK_ROPE))).astype(np.float32)
    c["invf_t"] = np.broadcast_to(invf[None, :], (128, 16)).copy()
    ff = np.zeros((128, 1), np.float32)
    ff[64:80, 0] = invf
    ff[80:96, 0] = invf
    c["invf_f"] = ff
    es = np.zeros((128, 31), np.float32)
    es[:, 15] = 1.0
    c["esel"] = es
    kk = np.arange(128)[:, None]
    qq = np.arange(128)[None, :]
    c["dmask"] = ((kk < 64) | (qq >= 64)).astype(np.float32)
    se = np.zeros((8, NE, 128), np.float32)
    for e in range(NE):
        se[e, e, :] = 1.0
    c["sel"] = se.reshape(8, NE * 128)
    c["ltri"] = (np.arange(128)[:, None] < np.arange(128)[None, :]).astype(np.float32)
    c["wtab"] = np.broadcast_to(np.arange(NSLAB, dtype=np.float32)[None, :, None], (128, NSLAB, NE)).reshape(128, NSLAB * NE).copy()
    c["ctab"] = (np.arange(7, dtype=np.float32)[None, :] * 128.0 + np.arange(128, dtype=np.float32)[:, None]).copy()
    return c


def _gT(g):
    return np.ascontiguousarray(g.reshape(8, 128).T)


def host_shared(inp):
    f = lambda a: np.ascontiguousarray(a, dtype=np.float32)
    pair = lambda a: f(a.reshape(32, 2, 64).transpose(1, 2, 0).reshape(128, 32))
    s = dict(host_consts())
    s["lamT_re"] = pair(inp["s5_lambda_re"][0])
    s["lamT_im"] = pair(inp["s5_lambda_im"][0])
    s["ldtT"] = pair(np.broadcast_to(inp["s5_log_dt"][0][:, None], (64, 64)))
    bt = lambda b: f(b.reshape(32, 2, 64, 16).transpose(1, 2, 0, 3).reshape(128, 512))
    ct = lambda c: f(c.reshape(32, 2, 16, 64).transpose(1, 3, 0, 2).reshape(128, 512))
    s["bT_re"], s["bT_im"] = bt(inp["s5_b_re"][0]), bt(inp["s5_b_im"][0])
    s["cT_re"], s["cT_im"] = ct(inp["s5_c_re"][0]), ct(inp["s5_c_im"][0])
    s["dT"] = _gT(f(inp["s5_d"][0]))
    s["gT_mix0"], s["gT_mix1"] = _gT(f(inp["norm_mix"][0])), _gT(f(inp["norm_mix"][1]))
    s["gT_ffn0"], s["gT_ffn1"] = _gT(f(inp["norm_ffn"][0])), _gT(f(inp["norm_ffn"][1]))
    s["gT_kv"] = _gT(f(inp["kv_norm"]))
    s["g_final"] = f(inp["final_norm"])
    s["g_kvlat"] = f(inp["kv_latent_norm"])
    s["g_qlat"] = f(inp["q_latent_norm"][0])
    for k in ("s5_w_in", "s5_w_glu", "s5_w_out", "ffn_w_gate", "ffn_w_up", "ffn_w_down",
              "w_dq", "w_uq", "w_o", "moe_w_gate", "moe_w_up", "moe_w_down"):
        s[k] = f(inp[k][0])
    s["router_wT"] = f(inp["router_w"][0].T)
    s["router_w"] = f(inp["router_w"][0])
    s["g_ffn1"] = f(inp["norm_ffn"][1])
    s["w_dkv"] = f(inp["w_dkv"])
    s["w_ukv"] = f(inp["w_ukv"])
    return s


def declare_inputs(nc, names):
    din = {}
    for k in names:
        shape, dt = IN_SPECS[k]
        din[k] = nc.dram_tensor(k, list(shape), dt, kind="ExternalInput").ap()
    return din


ATT_SCALE = (QK_NOPE + QK_ROPE) ** -0.5
KMAX_MARGIN = 1.02


def range_reduce_sin(cx, out, ang, it, b_out, b_ang, b_it, shift):
    cx.ts("dve", out, ang, 1.0 / (2.0 * PI), shift / (2.0 * PI), ALU.mult, ALU.add, [b_ang], [b_out])
    cx.cp("dve", it, out, [b_out], [b_it])
    cx.cp("dve", out, it, [b_it], [b_out])
    cx.stt("dve", out, out, -2.0 * PI, ang, ALU.mult, ALU.add, [b_out, b_ang], [b_out])
    cx.ts("dve", out, out, shift, -PI, ALU.add, ALU.max, [b_out], [b_out])
    cx.ts("dve", out, out, PI, None, ALU.min, None, [b_out], [b_out])
    cx.act(out, out, AF.Sin, [b_out], [b_out])


def phase15(cx, P, din, xs, b_xs, KT, b_KT, QT, b_QT, VS, b_VS, ntiles=8, hook=None):
    nc, kb = cx.nc, cx.kb
    B = P["bufs"]
    identb, b_identb = P["identb"], B["identb"]
    m = cx.mark()
    xts = [cx.sb(f"xt{i}", [128, 4, 1024], F32) for i in range(2)]
    htok, b_htok = cx.sb("htok", [128, 4, 1024], BF16)
    hT, b_hT = cx.sb("hT", [128, 8, 512], BF16)
    ss, b_ss = cx.sb("ss", [128, 8], F32)
    gT, b_gT = cx.sb("gT", [128, 2, 8], F32)
    wdkv, b_wdkv = cx.sb("wdkv", [128, 8, 288], BF16)
    wukv, b_wukv = cx.sb("wukv", [128, 2, 2048], BF16)
    wdq, b_wdq = cx.sb("wdq", [128, 8, 512], BF16)
    wuq, b_wuq = cx.sb("wuq", [128, 4, 1536], BF16)
    wrot, b_wrot = cx.sb("wrot", [128, 4, 16, 32], BF16)
    gkv, b_gkv = cx.sb("gkv", [128, 256], F32)
    gq, b_gq = cx.sb("gq", [128, 512], F32)
    invf_t, b_invf_t = cx.sb("invf_t", [128, 16], F32)
    invf_f, b_invf_f = cx.sb("invf_f", [128, 1], F32)
    esel, b_esel = cx.sb("esel", [128, 31], BF16)
    eself, b_eself = cx.sb("eself", [128, 31], F32)
    posis = [cx.sb(f"posi{i}", [128, 512], I32) for i in range(2)]
    posf, b_posf = cx.sb("posf", [128, 512], F32)
    ptok_is = [cx.sb(f"ptok_i{i}", [128, 4], I32) for i in range(2)]
    ptok, b_ptok = cx.sb("ptok", [128, 4], F32)
    angf, b_angf = cx.sb("angf", [128, 512], F32)
    cosf, b_cosf = cx.sb("cosf", [128, 512], F32)
    sinf, b_sinf = cx.sb("sinf", [128, 512], F32)
    itf, b_itf = cx.sb("itf", [128, 512], I32)
    angt, b_angt = cx.sb("angt", [128, 16], F32)
    cost, b_cost = cx.sb("cost", [128, 16], F32)
    sint, b_sint = cx.sb("sint", [128, 16], F32)
    itt, b_itt = cx.sb("itt", [128, 16], I32)
    ckvn, b_ckvn = cx.sb("ckvn", [128, 256], BF16)
    kro, b_kro = cx.sb("kro", [128, 32], BF16)
    krt, b_krt = cx.sb("krt", [128, 4, 16], F32)
    ckvT, b_ckvT = cx.sb("ckvT", [128, 2, 512], BF16)
    krT, b_krT = cx.sb("krT", [128, 512], BF16)
    cqn, b_cqn = cx.sb("cqn", [128, 512], BF16)
    cqnT, b_cqnT = cx.sb("cqnT", [128, 4, 512], BF16)
    KTt, b_KTt = cx.sb("KTt", [128, 16, 512], BF16)
    QTt, b_QTt = cx.sb("QTt", [128, 16, 512], BF16)
    SQ, b_SQ = cx.sb("SQ", [128, 16, 512], BF16)
    VA, b_VA = cx.sb("VA", [128, 16, 4, 65], BF16)
    nrm, b_nrm = cx.sb("nrm", [16, 512], F32)
    nrmb, b_nrmb = cx.sb("nrmb", [16, 512], BF16)
    rmax, b_rmax = cx.sb("rmax", [16, 2], F32)
    KM, b_KM = cx.sb("KM", [16, 4096], BF16)
    t96a, b_t96a = cx.sb("t96a", [128, 512], F32)
    t96b, b_t96b = cx.sb("t96b", [128, 512], F32)
    rot = PsRot(cx, [2, 3, 4, 5, 6, 7])

    kb.dma("sp", gT[:, 0, :], din["gT_kv"], writes=[b_gT])
    kb.dma("sp", gT[:, 1, :], din["gT_mix1"], writes=[b_gT])
    kb.dma("sp", gkv, din["g_kvlat"].partition_broadcast(128), writes=[b_gkv])
    kb.dma("sp", gq, din["g_qlat"].partition_broadcast(128), writes=[b_gq])
    kb.dma("sp", invf_t, din["invf_t"], writes=[b_invf_t])
    kb.dma("sp", invf_f, din["invf_f"], writes=[b_invf_f])
    kb.dma("sp", eself, din["esel"], writes=[b_eself])
    cx.cp("dve", esel, eself, [b_eself], [b_esel])
    kb.dma("pool", wdkv, din["w_dkv"].rearrange("(k p) n -> p k n", p=128), writes=[b_wdkv])
    kb.dma("pool", wukv, din["w_ukv"].rearrange("(k p) n -> p k n", p=128), writes=[b_wukv])
    kb.dma("pool", wdq, din["w_dq"].rearrange("(k p) n -> p k n", p=128), writes=[b_wdq])
    kb.dma("pool", wuq, din["w_uq"].rearrange("(k p) n -> p k n", p=128), writes=[b_wuq])
    wuq4 = wuq.rearrange("p k (h c) -> p k h c", c=96)
    for kc in range(4):
        cx.ts("dve", wrot[:, kc, :, 0:16], wuq4[:, kc, :, 80:96], -1.0, None, ALU.mult, None, [b_wuq], [b_wrot])
        cx.cp("dve", wrot[:, kc, :, 16:32], wuq4[:, kc, :, 64:80], [b_wuq], [b_wrot])
    cx.memset("dve", rmax, 0.0, [b_rmax])
    cx.memset("dve", VA, 1.0, [b_VA])
    cx.memset("pool", kro, 0.0, [b_kro])

    def head_norms(Tt, b_Tt, dst_row96, b_dst, t0, is_k):
        cx.act(SQ[0:96], Tt[0:96], AF.Square, [b_Tt], [b_SQ])
        ps, bps = rot.next()
        for h in range(16):
            cx.mm(ps[0:16, :], esel[0:96, 15 - h:31 - h], SQ[0:96, h, :], h == 0, h == 15, [b_esel, b_SQ], [bps])
        if is_k:
            cx.kb.op("dve", lambda g: g.reduce_max(out=rmax[:, 1:2], in_=ps[0:16, :], axis=AX.X), [bps], [b_rmax])
            cx.tt("dve", rmax[:, 0:1], rmax[:, 0:1], rmax[:, 1:2], ALU.max, [b_rmax], [b_rmax])
        else:
            cx.act(nrm, ps[0:16, :], AF.Sqrt, [bps], [b_nrm])
            cx.ts("dve", nrmb, nrm, -1.0, None, ALU.mult, None, [b_nrm], [b_nrmb])
            kb.dma("sp", dst_row96[:, t0:t0 + 512], nrmb, reads=[b_nrmb], writes=[b_dst])

    def load_tile(TT):
        tt0 = TT * 512
        xt_, b_xt_ = xts[TT % 2]
        posi_, b_posi_ = posis[TT % 2]
        ptok_i_, b_ptok_i_ = ptok_is[TT % 2]
        kb.dma("sp", xt_, xs[tt0:tt0 + 512, :].rearrange("(b p) d -> p b d", p=128), reads=[b_xs], writes=[b_xt_])
        kb.dma("sp", posi_, din["pos"][0, tt0:tt0 + 512].partition_broadcast(128), writes=[b_posi_])
        kb.dma("sp", ptok_i_, din["pos"][0, tt0:tt0 + 512].rearrange("(b p) -> p b", p=128), writes=[b_ptok_i_],
               allow_slow_non_contiguous=True)

    load_tile(0)
    for T in range(ntiles):
        t0 = T * 512
        xt, b_xt = xts[T % 2]
        posi, b_posi = posis[T % 2]
        ptok_i, b_ptok_i = ptok_is[T % 2]
        if T + 1 < ntiles:
            load_tile(T + 1)
        cx.cp("dve", posf, posi, [b_posi], [b_posf])
        cx.cp("dve", ptok, ptok_i, [b_ptok_i], [b_ptok])
        cx.ts("dve", angf, posf, invf_f[:, 0:1], None, ALU.mult, None, [b_posf, b_invf_f], [b_angf])
        range_reduce_sin(cx, sinf, angf, itf, b_sinf, b_angf, b_itf, 0.0)
        range_reduce_sin(cx, cosf, angf, itf, b_cosf, b_angf, b_itf, 0.5 * PI)
        cx.ts("dve", sinf, sinf, ATT_SCALE, None, ALU.mult, None, [b_sinf], [b_sinf])
        cx.ts("dve", cosf, cosf, ATT_SCALE, None, ALU.mult, None, [b_cosf], [b_cosf])

        rms_to_hT(cx, P, xt, b_xt, 4, gT[:, 0, :], b_gT, htok, b_htok, hT, b_hT, ss, b_ss, [0, 1])
        for b in range(4):
            ps, bps = rot.next()
            for kc in range(8):
                cx.mm(ps[:, 0:288], hT[:, kc, b * 128:(b + 1) * 128], wdkv[:, kc, :], kc == 0, kc == 7, [b_hT, b_wdkv], [bps])
            cx.act(ckvn, ps[:, 0:256], AF.Square, [bps], [b_ckvn, b_ss], accum=ss[:, 4:5])
            cx.ts("dve", ss[:, 4:5], ss[:, 4:5], 1.0 / KV_LORA, EPS, ALU.mult, ALU.add, [b_ss], [b_ss])
            cx.act(ss[:, 4:5], ss[:, 4:5], AF.Sqrt, [b_ss], [b_ss])
            cx.kb.op("dve", lambda g: g.reciprocal(out=ss[:, 4:5], in_=ss[:, 4:5]), [b_ss], [b_ss])
            cx.stt("dve", ckvn, ps[:, 0:256], ss[:, 4:5], gkv, ALU.mult, ALU.mult, [bps, b_ss, b_gkv], [b_ckvn])
            cx.ts("dve", angt, invf_t, ptok[:, b:b + 1], None, ALU.mult, None, [b_invf_t, b_ptok], [b_angt])
            range_reduce_sin(cx, sint, angt, itt, b_sint, b_angt, b_itt, 0.0)
            range_reduce_sin(cx, cost, angt, itt, b_cost, b_angt, b_itt, 0.5 * PI)
            x1, x2 = ps[:, 256:272], ps[:, 272:288]
            cx.tt("dve", krt[:, 0, :], x1, cost, ALU.mult, [bps, b_cost], [b_krt])
            cx.tt("dve", krt[:, 1, :], x2, sint, ALU.mult, [bps, b_sint], [b_krt])
            cx.tt("dve", krt[:, 2, :], x2, cost, ALU.mult, [bps, b_cost], [b_krt])
            cx.tt("dve", krt[:, 3, :], x1, sint, ALU.mult, [bps, b_sint], [b_krt])
            cx.tt("dve", kro[:, 0:16], krt[:, 0, :], krt[:, 1, :], ALU.subtract, [b_krt], [b_kro])
            cx.tt("dve", kro[:, 16:32], krt[:, 2, :], krt[:, 3, :], ALU.add, [b_krt], [b_kro])
            p16 = cx.ps[b % 2].bitcast(BF16).rearrange("p (k c) -> p k c", c=128)
            bp16 = cx.psb[b % 2]
            for k2 in range(2):
                cx.tr(p16[:, k2, :], ckvn[:, k2 * 128:(k2 + 1) * 128], identb, [b_ckvn, b_identb], [bp16])
            cx.tr(p16[0:32, 2, :], kro, identb, [b_kro, b_identb], [bp16])
            cx.cp("act", ckvT[:, :, b * 128:(b + 1) * 128], p16[:, 0:2, :], [bp16], [b_ckvT])
            cx.cp("dve", krT[64:96, b * 128:(b + 1) * 128], p16[0:32, 2, :], [bp16], [b_krT])
            w4 = wukv.rearrange("p k (h c) -> p k h c", c=128)
            for hh in range(2):
                pv_, bpv = rot.next()
                for k2 in range(2):
                    cx.mm(pv_, ckvT[:, k2, b * 128:(b + 1) * 128], w4[:, k2, hh * 8:(hh + 1) * 8, 64:128],
                          k2 == 0, k2 == 1, [b_ckvT, b_wukv], [bpv])
                cx.cp("act" if hh else "dve", VA[:, hh * 8:(hh + 1) * 8, b, 0:64],
                      pv_.rearrange("p (h c) -> p h c", c=64), [bpv], [b_VA])
        if hook is not None:
            hook()
        for h in range(16):
            ps, bps = rot.next()
            for k2 in range(2):
                cx.mm(ps[0:64, :], wukv[:, k2, h * 128:h * 128 + 64], ckvT[:, k2, :], k2 == 0, k2 == 1, [b_wukv, b_ckvT], [bps])
            cx.cp("act" if h % 2 else "dve", KTt[0:64, h, :], ps[0:64, :], [bps], [b_KTt])
        cx.cp("dve", KTt[64:96, :, :], krT[64:96, :].unsqueeze(1).to_broadcast([32, 16, 512]), [b_krT], [b_KTt])
        kb.dma("sp", KT[:, 0:96, t0:t0 + 512].rearrange("h r t -> r h t"), KTt[0:96], reads=[b_KTt], writes=[b_KT])
        head_norms(KTt, b_KTt, None, None, t0, True)
        kb.dma("sp", VS[:, :, 4 * T:4 * T + 4, :].rearrange("h p b e -> p h b e"), VA, reads=[b_VA], writes=[b_VS])

        if hook is not None:
            hook()
        rms_to_hT(cx, P, xt, b_xt, 4, gT[:, 1, :], b_gT, htok, b_htok, hT, b_hT, ss, b_ss, [0, 1])
        for b in range(4):
            ps, bps = rot.next()
            for kc in range(8):
                cx.mm(ps, hT[:, kc, b * 128:(b + 1) * 128], wdq[:, kc, :], kc == 0, kc == 7, [b_hT, b_wdq], [bps])
            cx.act(cqn, ps, AF.Square, [bps], [b_cqn, b_ss], accum=ss[:, 5:6])
            cx.ts("dve", ss[:, 5:6], ss[:, 5:6], 1.0 / Q_LORA, EPS, ALU.mult, ALU.add, [b_ss], [b_ss])
            cx.act(ss[:, 5:6], ss[:, 5:6], AF.Sqrt, [b_ss], [b_ss])
            cx.kb.op("dve", lambda g: g.reciprocal(out=ss[:, 5:6], in_=ss[:, 5:6]), [b_ss], [b_ss])
            cx.stt("dve", cqn, ps, ss[:, 5:6], gq, ALU.mult, ALU.mult, [bps, b_ss, b_gq], [b_cqn])
            p16 = cx.ps[b % 2].bitcast(BF16).rearrange("p (k c) -> p k c", c=128)
            bp16 = cx.psb[b % 2]
            for k4 in range(4):
                cx.tr(p16[:, k4, :], cqn[:, k4 * 128:(k4 + 1) * 128], identb, [b_cqn, b_identb], [bp16])
            cx.cp("act", cqnT[:, :, b * 128:(b + 1) * 128], p16[:, 0:4, :], [bp16], [b_cqnT])
        if hook is not None:
            hook()
        for h in range(16):
            pa, bpa = rot.next()
            pb_, bpb = rot.next()
            for k4 in range(4):
                cx.mm(pa[0:96, :], wuq[:, k4, h * 96:(h + 1) * 96], cqnT[:, k4, :], k4 == 0, k4 == 3, [b_wuq, b_cqnT], [bpa])
            for k4 in range(4):
                cx.mm(pb_[64:96, :], wrot[:, k4, h, :], cqnT[:, k4, :], k4 == 0, k4 == 3, [b_wrot, b_cqnT], [bpb],
                      tile_position=(0, 64))
            cx.act(QTt[0:64, h, :], pa[0:64, :], AF.Copy, [bpa], [b_QTt], scale=ATT_SCALE)
            cx.tt("dve", t96a[64:96], pa[64:96, :], cosf[64:96], ALU.mult, [bpa, b_cosf], [b_t96a])
            cx.tt("dve", t96b[64:96], pb_[64:96, :], sinf[64:96], ALU.mult, [bpb, b_sinf], [b_t96b])
            cx.tt("pool", QTt[64:96, h, :], t96a[64:96], t96b[64:96], ALU.add, [b_t96a, b_t96b], [b_QTt])
        kb.dma("sp", QT[:, 0:96, t0:t0 + 512].rearrange("h r t -> r h t"), QTt[0:96], reads=[b_QTt], writes=[b_QT])
        if hook is not None:
            hook()
        head_norms(QTt, b_QTt, QT[:, 96, :], b_QT, t0, False)
    cx.act(rmax[:, 1:2], rmax[:, 0:1], AF.Sqrt, [b_rmax], [b_rmax])
    cx.memset("pool", KM, 0.0, [b_KM])
    cx.ts("dve", KM, KM, rmax[:, 1:2], KMAX_MARGIN, ALU.add, ALU.mult, [b_KM, b_rmax], [b_KM])
    kb.dma("sp", KT[:, 96, :], KM, reads=[b_KM], writes=[b_KT])
    cx.release(m)


def phase2(cx, P, din, KT, b_KT, QT, b_QT, VS, b_VS, OT, b_OT, nheads=16, ngroups=8, hook=None):
    nc, kb = cx.nc, cx.kb
    m = cx.mark()
    KTh, QTh, VAh, b_KTh, b_QTh, b_VAh = [], [], [], [], [], []
    for i in range(2):
        t, b = cx.sb(f"KTh{i}", [128, 4096], BF16); KTh.append(t); b_KTh.append(b)
        t, b = cx.sb(f"QTh{i}", [128, 4096], BF16); QTh.append(t); b_QTh.append(b)
        t, b = cx.sb(f"VAh{i}", [128, 32, 65], BF16); VAh.append(t); b_VAh.append(b)
    PT, b_PT = [], []
    for i in range(2):
        t, _ = cx.sb(f"PT{i}", [128, 32, 512], BF16)
        PT.append(t)
        b_PT.append(kb.bufs_n(f"PT{i}_", 32))
    dmask, b_dmask = cx.sb("dmask", [128, 128], F32)
    onesr, b_onesr = cx.sb("onesr", [128, 64], F32)
    R, b_R = cx.sb("R", [128, 512], F32)
    RB, b_RB = cx.sb("RB", [128, 512], F32)
    OTs, b_OTs = [], []
    for i in range(2):
        t, b = cx.sb(f"OTs{i}", [128, 4096], BF16); OTs.append(t); b_OTs.append(b)
    kb.dma("sp", dmask, din["dmask"], writes=[b_dmask])
    cx.memset("dve", onesr, 1.0, [b_onesr])
    rot = PsRot(cx, [0, 1, 2, 3, 4])
    rot_o = PsRot(cx, [5, 6])

    def emit_pv(st, j):
        nkt = st["nkt"]
        c0 = max(0, j - 4 * st["G"]) * 128
        s_ = st["s"]
        cx.mm(st["po"][0:65, c0:512], VAh[s_][:, j, :], st["pt"][:, j, c0:512], j == 0, j == nkt - 1,
              [b_VAh[s_], st["b_pt"][j]], [st["bpo"]])

    def emit_fin(st):
        po, bpo, s_, G = st["po"], st["bpo"], st["s"], st["G"]
        ot, b_ot = st["ot"], st["b_ot"]
        cx.kb.op("dve", lambda g: g.reciprocal(out=R[64:65, :], in_=po[64:65, :]), [bpo], [b_R])
        pb_, bpb = cx.ps[7], cx.psb[7]
        cx.mm(pb_[0:64, :], onesr[64:65, :], R[64:65, :], True, True, [b_onesr, b_R], [bpb])
        cx.cp("act", RB[0:64, :], pb_[0:64, :], [bpb], [b_RB])
        dst = ot[64 * s_:64 * s_ + 64, G * 512:(G + 1) * 512]
        cx.tt("dve", dst, po[0:64, :], RB[0:64, :], ALU.mult, [bpo, b_RB], [b_ot])
        if st["store"] is not None:
            kb.dma("sp", OT[st["store"]], ot, reads=[b_ot], writes=[b_OT])

    prev = None
    fin_q = None
    gi = 0
    def load_head(hh):
        ss_ = hh % 2
        kb.dma("sp", KTh[ss_][0:97, :], KT[hh], reads=[b_KT], writes=[b_KTh[ss_]])
        kb.dma("sp", QTh[ss_][0:97, :], QT[hh], reads=[b_QT], writes=[b_QTh[ss_]])
        kb.dma("sp", VAh[ss_], VS[hh], reads=[b_VS], writes=[b_VAh[ss_]])

    load_head(0)
    for h in range(nheads):
        s = h % 2
        pr = h // 2
        ot, b_ot = OTs[pr % 2], b_OTs[pr % 2]
        for G in range(ngroups):
            if G == 1 and h + 1 < nheads:
                load_head(h + 1)
            if hook is not None:
                hook()
            pt, b_pt = PT[gi % 2], b_PT[gi % 2]
            gi += 1
            nkt = 4 * G + 4
            if prev is not None:
                prev["po"], prev["bpo"] = rot_o.next()
            npv = prev["nkt"] if prev is not None else 0
            for j in range(max(nkt, npv)):
                if j < nkt:
                    c0 = max(0, j - 4 * G) * 128
                    ps, bps = rot.next()
                    cx.mm(ps[:, c0:512], KTh[s][0:97, j * 128:(j + 1) * 128], QTh[s][0:97, G * 512 + c0:(G + 1) * 512],
                          True, True, [b_KTh[s], b_QTh[s]], [bps])
                    cx.act(pt[:, j, c0:512], ps[:, c0:512], AF.Exp, [bps], [b_pt[j]])
                    if j >= 4 * G:
                        cx.tt("pool", pt[:, j, c0:c0 + 128], pt[:, j, c0:c0 + 128], dmask, ALU.mult, [b_pt[j], b_dmask], [b_pt[j]])
                if j < npv:
                    emit_pv(prev, j)
                if j == 1 and fin_q is not None:
                    emit_fin(fin_q)
                    fin_q = None
            if fin_q is not None:
                emit_fin(fin_q)
                fin_q = None
            fin_q = prev
            last_of_pair = (G == ngroups - 1) and (s == 1 or h == nheads - 1)
            prev = dict(pt=pt, b_pt=b_pt, nkt=nkt, G=G, s=s, ot=ot, b_ot=b_ot, store=(pr if last_of_pair else None))
    if prev is not None:
        prev["po"], prev["bpo"] = rot_o.next()
        for j in range(prev["nkt"]):
            emit_pv(prev, j)
    if fin_q is not None:
        emit_fin(fin_q)
    if prev is not None:
        emit_fin(prev)
    cx.release(m)


def phase3a(cx, P, din, xs, b_xs, OT, b_OT, out, b_out, ntiles=8):
    nc, kb = cx.nc, cx.kb
    m = cx.mark()
    xt, b_xt = cx.sb("xt", [128, 4, 1024], F32)
    ot, b_ot = cx.sb("ot", [128, 8, 512], BF16)
    wo, b_wo = cx.sb("wo", [128, 8, 1024], BF16)
    kb.dma("pool", wo, din["w_o"].rearrange("(k p) n -> p k n", p=128), writes=[b_wo])
    rot = PsRot(cx, [0, 1, 2, 3])
    for T in range(ntiles):
        t0 = T * 512
        kb.dma("sp", xt, xs[t0:t0 + 512, :].rearrange("(b p) d -> p b d", p=128), reads=[b_xs], writes=[b_xt])
        kb.dma("sp", ot, OT[:, :, t0:t0 + 512].rearrange("k p t -> p k t"), reads=[b_OT], writes=[b_ot])
        for b in range(4):
            for half in range(2):
                ps, bps = rot.next()
                for k in range(8):
                    cx.mm(ps, ot[:, k, b * 128:(b + 1) * 128], wo[:, k, half * 512:(half + 1) * 512], k == 0, k == 7, [b_ot, b_wo], [bps])
                xv = xt[:, b, half * 512:(half + 1) * 512]
                cx.tt("dve", xv, xv, ps, ALU.add, [b_xt, bps], [b_xt])
        kb.dma("sp", out[t0:t0 + 512, :].rearrange("(b p) d -> p b d", p=128), xt, reads=[b_xt], writes=[b_out])
    cx.release(m)


def phase3(cx, P, din, xs, b_xs, OT, b_OT, out, b_out, ntiles=4, nexp=NE):
    nc, kb = cx.nc, cx.kb
    B = P["bufs"]
    identf, b_identf = P["identf"], B["identf"]
    m = cx.mark()
    NBK = 8
    xt, b_xt = cx.sb("xt", [128, NBK, 1024], F32)
    htok, b_htok = cx.sb("htok", [128, NBK, 1024], BF16)
    hT, b_hT = cx.sb("hT", [128, 8, 1024], BF16)
    wo, b_wo = cx.sb("wo", [128, 8, 1024], BF16)
    rwg, b_rwg = cx.sb("rwg", [128, NE, 1024], F32)
    gfin, b_gfin = cx.sb("gfin", [128, 1024], F32)
    gT, b_gT = cx.sb("gT", [128, 8], F32)
    ss, b_ss = cx.sb("ss", [128, 16], F32)
    lg, b_lg = cx.sb("lg", [128, NBK, 8], F32)
    m8, b_m8 = cx.sb("m8", [128, 8], F32)
    gts, b_gts = cx.sb("gts", [128, NBK, 8], F32)
    gsum, b_gsum = cx.sb("gsum", [128, 2], F32)
    g8T, b_g8T = cx.sb("g8T", [8, 1024], F32)
    sel, b_sel = cx.sb("sel", [8, NE, 128], F32)
    gbc, b_gbc = cx.sb("gbc", [128, 1024], F32)
    sg = []
    b_sg = []
    tg = []
    b_tg = []
    for i in range(2):
        t, b = cx.sb(f"sg{i}", [128, 512], F32); sg.append(t); b_sg.append(b)
        t, b = cx.sb(f"tg{i}", [128, 512], F32); tg.append(t); b_tg.append(b)
    actT = []
    b_actT = []
    for i in range(2):
        t, b = cx.sb(f"actT{i}", [128, 4, 1024], BF16); actT.append(t); b_actT.append(b)
    junk, b_junk = cx.sb("junk", [128, 1024], BF16)
    ws = WStream(cx, 2, 3 * 4096, name="wm")
    rot = PsRot(cx, [2, 3, 4, 5, 6, 7])
    ot = htok

    kb.dma("pool", wo, din["w_o"].rearrange("(k p) n -> p k n", p=128), writes=[b_wo])
    kb.dma("sp", gfin, din["g_final"].partition_broadcast(128), writes=[b_gfin])
    kb.dma("sp", gT, din["gT_ffn1"], writes=[b_gT])
    kb.dma("sp", gbc, din["g_ffn1"].partition_broadcast(128), writes=[b_gbc])
    kb.dma("sp", sel, din["sel"].rearrange("k (e m) -> k e m", m=128), writes=[b_sel])
    for e in range(NE):
        kb.dma("sp", rwg[:, e, :], din["router_wT"][e].partition_broadcast(128), writes=[b_rwg])
    for e in range(NE):
        cx.tt("pool", rwg[:, e, :], rwg[:, e, :], gbc, ALU.mult, [b_rwg, b_gbc], [b_rwg])
    wg, wu, wd = din["moe_w_gate"], din["moe_w_up"], din["moe_w_down"]
    NG = MOE_FF // 512
    jobs = []
    for T in range(ntiles):
        for e in range(nexp):
            for g in range(NG):
                jobs.append([
                    (wg[e][:, g * 512:(g + 1) * 512].rearrange("(k p) n -> p k n", p=128), 8, 512),
                    (wu[e][:, g * 512:(g + 1) * 512].rearrange("(k p) n -> p k n", p=128), 8, 512),
                    (wd[e][g * 512:(g + 1) * 512, :].rearrange("(a p) d -> p a d", p=128), 4, 1024)])
    ws.plan(jobs)

    for T in range(ntiles):
        t0 = T * 1024
        kb.dma("sp", xt, xs[t0:t0 + 1024, :].rearrange("(b p) d -> p b d", p=128), reads=[b_xs], writes=[b_xt])
        kb.dma("sp", ot, OT[:, :, t0:t0 + 1024].rearrange("k p t -> p k t"), reads=[b_OT], writes=[b_htok])
        for b in range(NBK):
            for half in range(2):
                ps, bps = rot.next()
                for k in range(8):
                    cx.mm(ps, ot[:, k, b * 128:(b + 1) * 128], wo[:, k, half * 512:(half + 1) * 512], k == 0, k == 7,
                          [b_htok, b_wo], [bps])
                xv = xt[:, b, half * 512:(half + 1) * 512]
                cx.tt("dve", xv, xv, ps, ALU.add, [b_xt, bps], [b_xt])
        rms_to_hT(cx, P, xt, b_xt, NBK, gT, b_gT, htok, b_htok, hT, b_hT, ss, b_ss, [0, 1])
        for b in range(NBK):
            for e in range(NE):
                kb.op("dve", lambda g_, b=b, e=e: g_.scalar_tensor_tensor(
                    out=junk, in0=xt[:, b, :], scalar=ss[:, b:b + 1], in1=rwg[:, e, :], op0=ALU.mult, op1=ALU.mult,
                    accum_out=lg[:, b, e:e + 1]), [b_xt, b_ss, b_rwg], [b_junk, b_lg])
            kb.op("dve", lambda g_, b=b: g_.max(out=m8, in_=lg[:, b, :]), [b_lg], [b_m8])
            cx.ts("dve", gsum[:, 0:1], m8[:, 0:1], -1.0, None, ALU.mult, None, [b_m8], [b_gsum])
            cx.act(gts[:, b, :], lg[:, b, :], AF.Exp, [b_lg, b_gsum], [b_gts], bias=gsum[:, 0:1])
            cx.stt("dve", gts[:, b, :], lg[:, b, :], m8[:, 1:2], gts[:, b, :], ALU.is_ge, ALU.mult,
                   [b_lg, b_m8, b_gts], [b_gts])
            kb.op("dve", lambda g_, b=b: g_.reduce_sum(out=gsum[:, 1:2], in_=gts[:, b, :], axis=AX.X), [b_gts], [b_gsum])
            kb.op("dve", lambda g_: g_.reciprocal(out=gsum[:, 1:2], in_=gsum[:, 1:2]), [b_gsum], [b_gsum])
            cx.ts("dve", gts[:, b, :], gts[:, b, :], gsum[:, 1:2], None, ALU.mult, None, [b_gts, b_gsum], [b_gts])
            pt_, bpt = rot.next()
            cx.tr(pt_[0:8, 0:128], gts[:, b, :], identf, [b_gts, b_identf], [bpt])
            cx.cp("dve", g8T[:, b * 128:(b + 1) * 128], pt_[0:8, 0:128], [bpt], [b_g8T])
        gi = 0
        for e in range(nexp):
            for half in range(2):
                ps, bps = rot.next()
                cx.mm(ps, sel[:, e, :], g8T[:, half * 512:(half + 1) * 512], True, True, [b_sel, b_g8T], [bps])
                cx.cp("act", gbc[:, half * 512:(half + 1) * 512], ps, [bps], [b_gbc])
            for g in range(NG):
                (gv, uv, dv), b_w = ws.get()
                at, b_at = actT[gi % 2], b_actT[gi % 2]
                gi += 1
                for f4 in range(4):
                    for half in range(2):
                        hs = slice(half * 512, (half + 1) * 512)
                        pg, bpg = rot.next()
                        pu, bpu = rot.next()
                        for kc in range(8):
                            cx.mm(pg, gv[:, kc, f4 * 128:(f4 + 1) * 128], hT[:, kc, hs], kc == 0, kc == 7, [b_w, b_hT], [bpg])
                        for kc in range(8):
                            cx.mm(pu, uv[:, kc, f4 * 128:(f4 + 1) * 128], hT[:, kc, hs], kc == 0, kc == 7, [b_w, b_hT], [bpu])
                        i2 = (f4 * 2 + half) % 2
                        cx.act(sg[i2], pg, AF.Silu, [bpg], [b_sg[i2]])
                        cx.tt("dve", tg[i2], pu, gbc[:, hs], ALU.mult, [bpu, b_gbc], [b_tg[i2]])
                        cx.tt("pool", at[:, f4, hs], sg[i2], tg[i2], ALU.mult, [b_sg[i2], b_tg[i2]], [b_at])
                for b in range(NBK):
                    for half in range(2):
                        ps, bps = rot.next()
                        for f4 in range(4):
                            cx.mm(ps, at[:, f4, b * 128:(b + 1) * 128], dv[:, f4, half * 512:(half + 1) * 512],
                                  f4 == 0, f4 == 3, [b_at, b_w], [bps])
                        xv = xt[:, b, half * 512:(half + 1) * 512]
                        cx.tt("dve", xv, xv, ps, ALU.add, [b_xt, bps], [b_xt])
        for b in range(NBK):
            cx.act(junk, xt[:, b, :], AF.Square, [b_xt], [b_junk, b_ss], accum=ss[:, 8 + (b % 8):9 + (b % 8)])
        cx.ts("dve", ss[:, 8:16], ss[:, 8:16], 1.0 / D, EPS, ALU.mult, ALU.add, [b_ss], [b_ss])
        cx.act(ss[:, 8:16], ss[:, 8:16], AF.Sqrt, [b_ss], [b_ss])
        kb.op("dve", lambda g_: g_.reciprocal(out=ss[:, 8:16], in_=ss[:, 8:16]), [b_ss], [b_ss])
        for b in range(NBK):
            cx.stt("dve", xt[:, b, :], xt[:, b, :], ss[:, 8 + b:9 + b], gfin, ALU.mult, ALU.mult, [b_xt, b_ss, b_gfin], [b_xt])
        kb.dma("sp", out[t0:t0 + 1024, :].rearrange("(b p) d -> p b d", p=128), xt, reads=[b_xt], writes=[b_out])
    cx.release(m)


def build_program():
    nc = bass.Bass("TRN2", target_bir_lowering=False)
    cx = Ctx(nc)
    kb = cx.kb
    din = declare_inputs(nc, list(IN_SPECS.keys()))
    out = nc.dram_tensor("out", [L, D], F32, kind="ExternalOutput").ap()
    xs = nc.dram_tensor("xs", [L, D], F32, kind="Internal").ap()
    KT = nc.dram_tensor("KT", [NH, 97, L], BF16, kind="Internal").ap()
    QT = nc.dram_tensor("QT", [NH, 97, L], BF16, kind="Internal").ap()
    VS = nc.dram_tensor("VS", [NH, 128, NB, 65], BF16, kind="Internal").ap()
    OT = nc.dram_tensor("OT", [NH // 2, 128, L], BF16, kind="Internal").ap()
    b_out, b_xs, b_KT, b_QT, b_VS, b_OT = (kb.buf(n) for n in ("out", "xs", "KT", "QT", "VS", "OT"))
    W0 = nc.dram_tensor("W0", [N_L0_JOBS * 128, 4096], BF16, kind="Internal").ap()
    b_W0 = kb.buf("W0")
    W16 = {k: nc.dram_tensor("W16" + k, [NE * NGRP * 128, 4096], BF16, kind="Internal").ap() for k in "gud"}
    b_W16 = {k: kb.buf("W16" + k) for k in "gud"}
    HS = nc.dram_tensor("HS", [NSLAB * SLAB, D], BF16, kind="Internal").ap()
    YS = nc.dram_tensor("YS", [NSLAB * SLAB, D], F32, kind="Internal").ap()
    b_HS, b_YS = kb.buf("HS"), kb.buf("YS")
    conv = MoeConv(cx, din, W16, b_W16, nstage=0)
    P = setup_ident(cx, din)
    m0 = cx.mark()
    phase0(cx, P, din, pre_hook=lambda: layer0_convert(cx, din, W0, b_W0))
    phase1(cx, P, din, xs, b_xs, W0, b_W0)
    cx.release(m0)
    m1 = cx.mark()
    conv.stage = [cx.sb(f"cvB{i}", [128, 4096], BF16) for i in range(2)]
    phase15(cx, P, din, xs, b_xs, KT, b_KT, QT, b_QT, VS, b_VS, hook=lambda: conv.step(3))
    phase2(cx, P, din, KT, b_KT, QT, b_QT, VS, b_VS, OT, b_OT, hook=lambda: conv.step(1))
    conv.finish()
    cx.release(m1)
    phase3r(cx, P, din, xs, b_xs, OT, b_OT, out, b_out, W16, b_W16, HS, b_HS, YS, b_YS)
    kb.finish([b_out])
    return nc


_NC_CACHE = {}


def kernel(**inputs):
    inp = {k: np.asarray(v) for k, v in inputs.items()}
    shared = host_shared(inp)
    if "nc" not in _NC_CACHE:
        _NC_CACHE["nc"] = build_program()
    nc = _NC_CACHE["nc"]
    ncores = 8
    in_maps = []
    for c in range(ncores):
        mp = dict(shared)
        mp["x"] = np.ascontiguousarray(inp["x"][c], dtype=np.float32)
        mp["pos"] = np.ascontiguousarray(inp["positions"][c:c + 1], dtype=np.int32)
        in_maps.append(mp)
    res = run_bass_kernel_spmd(nc, in_maps, core_ids=list(range(ncores)))
    return np.stack([np.asarray(r["out"], dtype=np.float32) for r in res.results], axis=0)


class MoeConv:
    def __init__(self, cx, din, W16, b_W16, nstage=2, nexp=NE):
        self.cx = cx
        self.W16, self.b_W16 = W16, b_W16
        self.stage = [cx.sb(f"cvst{i}", [128, 4096], BF16) for i in range(nstage)] if nstage else []
        self.chunks = []
        for e in range(nexp):
            for g in range(NGRP):
                cs = slice(g * 512, (g + 1) * 512)
                self.chunks.append(("g", e, g, din["moe_w_gate"][e][:, cs].rearrange("(k p) n -> p k n", p=128), 512))
                self.chunks.append(("u", e, g, din["moe_w_up"][e][:, cs].rearrange("(k p) n -> p k n", p=128), 512))
                self.chunks.append(("d", e, g, din["moe_w_down"][e][cs, :].rearrange("(a p) d -> p a d", p=128), 1024))
        self.i = 0

    def step(self, n=1):
        kb = self.cx.kb
        for _ in range(n):
            if self.i >= len(self.chunks):
                return
            kind, e, g, src, n_in = self.chunks[self.i]
            st, b_st = self.stage[self.i % len(self.stage)]
            self.i += 1
            kb.dma("pool", st.rearrange("p (a n) -> p a n", n=n_in), src, writes=[b_st])
            r0 = (e * NGRP + g) * 128
            kb.dma("sp", self.W16[kind][r0:r0 + 128, :], st, reads=[b_st], writes=[self.b_W16[kind]], disjoint=True)

    def finish(self):
        self.step(len(self.chunks))


def phase3r(cx, P, din, xs, b_xs, OT, b_OT, out, b_out, W16, b_W16, HS, b_HS, YS, b_YS, nslab=NSLAB):
    nc, kb = cx.nc, cx.kb
    B = P["bufs"]
    identb, b_identb = P["identb"], B["identb"]
    identf, b_identf = P["identf"], B["identf"]
    NTOT = NE * NGRP * 128
    mp = cx.mark()
    ss, b_ss = cx.sb("ss", [128, 8], F32)
    LG, b_LG = cx.sb("LG", [128, NB, 8], F32)
    GTS, b_GTS = cx.sb("GTS", [128, NB, 8], F32)
    TOP, b_TOP = cx.sb("TOP", [128, NB, 2], F32)
    m8, b_m8 = cx.sb("m8", [128, 8], F32)
    gsum, b_gsum = cx.sb("gsum", [128, 2], F32)
    junk, b_junk = cx.sb("junk", [128, 1024], BF16)
    M1, b_M1 = cx.sb("M1", [128, NB, 8], F32)
    M2, b_M2 = cx.sb("M2", [128, NB, 8], F32)
    MSK, b_MSK = cx.sb("MSK", [128, NB, 8], BF16)
    ltri, b_ltri = cx.sb("ltri", [128, 128], BF16)
    onesb, b_onesb = cx.sb("onesb", [128, 128], BF16)
    ones32, b_ones32 = cx.sb("ones32", [128, NB], F32)
    tmpf, b_tmpf = cx.sb("tmpf", [128, 128], F32)
    TOT, b_TOT = cx.sb("TOT", [128, NB, 8], F32)
    INC, b_INC = cx.sb("INC", [128, NB, 8], F32)
    WIN, b_WIN = cx.sb("WIN", [128, NB, 8], F32)
    SL, b_SL = cx.sb("SL", [128, NB, 8], F32)
    NSL, b_NSL = cx.sb("NSL", [128, 8], F32)
    NSLi, b_NSLi = cx.sb("NSLi", [128, 8], I32)
    SEND, b_SEND = cx.sb("SEND", [128, 8], F32)
    OFF, b_OFF = cx.sb("OFF", [128, 8], F32)
    one8, b_one8 = cx.sb("one8", [128, 8], F32)
    SF, b_SF = cx.sb("SF", [128, 2, NB], F32)
    SLOT, b_SLOT = cx.sb("SLOT", [128, 2, NB], I32)
    WGT, b_WGT = cx.sb("WGT", [128, 2, NB], F32)
    wtab, b_wtab = cx.sb("wtab", [128, NSLAB, 8], F32)
    CMP, b_CMP = cx.sb("CMP", [128, NSLAB, 8], F32)
    EW, b_EW = cx.sb("EW", [128, NSLAB], F32)
    ctab, b_ctab = cx.sb("ctab", [128, 7], F32)
    IDXf, b_IDXf = cx.sb("IDXf", [128, NSLAB, 7], F32)
    IDX, b_IDX = cx.sb("IDX", [128, NSLAB, 7], I32)
    m = cx.mark()
    xt, b_xt = cx.sb("xt", [128, 8, 1024], F32)
    ot, b_ot = cx.sb("ot", [128, 8, 1024], BF16)
    wo, b_wo = cx.sb("wo", [128, 8, 1024], BF16)
    rwT, b_rwT = cx.sb("rwT", [128, 8, NE], F32)
    gT1, b_gT1 = cx.sb("gT1", [128, 8], F32)
    xT32, b_xT32 = cx.sb("xT32", [128, 8, 128], F32)
    gff, b_gff = cx.sb("gff", [128, 1024], F32)
    hall, _ = cx.sb("hall", [128, NB, 1024], BF16)
    b_hall = kb.bufs_n("hall", NB)
    rot = PsRot(cx, [2, 3, 4, 5, 6, 7])
    kb.dma("pool", wo, din["w_o"].rearrange("(k p) n -> p k n", p=128), writes=[b_wo])
    kb.dma("sp", gff, din["g_ffn1"].partition_broadcast(128), writes=[b_gff])
    kb.dma("sp", rwT, din["router_w"].rearrange("(k p) e -> p k e", p=128), writes=[b_rwT])
    kb.dma("sp", gT1, din["gT_ffn1"], writes=[b_gT1])
    cx.tt("dve", rwT, rwT, gT1.unsqueeze(2).to_broadcast([128, 8, NE]), ALU.mult, [b_rwT, b_gT1], [b_rwT])
    for T in range(4):
        t0 = T * 1024
        kb.dma("sp", xt, xs[t0:t0 + 1024, :].rearrange("(b p) d -> p b d", p=128), reads=[b_xs], writes=[b_xt])
        kb.dma("sp", ot, OT[:, :, t0:t0 + 1024].rearrange("k p t -> p k t"), reads=[b_OT], writes=[b_ot])
        for b in range(8):
            for half in range(2):
                ps, bps = rot.next()
                for k in range(8):
                    cx.mm(ps, ot[:, k, b * 128:(b + 1) * 128], wo[:, k, half * 512:(half + 1) * 512], k == 0, k == 7,
                          [b_ot, b_wo], [bps])
                xv = xt[:, b, half * 512:(half + 1) * 512]
                cx.tt("dve", xv, xv, ps, ALU.add, [b_xt, bps], [b_xt])
        kb.dma("sp", xs[t0:t0 + 1024, :].rearrange("(b p) d -> p b d", p=128), xt, reads=[b_xt], writes=[b_xs])
        for b in range(8):
            cx.act(junk, xt[:, b, :], AF.Square, [b_xt], [b_junk, b_ss], accum=ss[:, b:b + 1])
        cx.ts("dve", ss, ss, 1.0 / D, EPS, ALU.mult, ALU.add, [b_ss], [b_ss])
        cx.act(ss, ss, AF.Sqrt, [b_ss], [b_ss])
        kb.op("dve", lambda g_: g_.reciprocal(out=ss, in_=ss), [b_ss], [b_ss])
        for b in range(8):
            gb = T * 8 + b
            cx.stt("dve", hall[:, gb, :], xt[:, b, :], ss[:, b:b + 1], gff, ALU.mult, ALU.mult,
                   [b_xt, b_ss, b_gff], [b_hall[gb]])
            pA, bpA = rot.next()
            pB, bpB = rot.next()
            for kc in range(8):
                pp, bpp = (pA, bpA) if kc < 4 else (pB, bpB)
                cx.tr(pp[:, (kc % 4) * 128:(kc % 4 + 1) * 128], xt[:, b, kc * 128:(kc + 1) * 128], identf, [b_xt, b_identf], [bpp])
            cx.cp("act", xT32[:, 0:4, :], pA.rearrange("p (k c) -> p k c", c=128), [bpA], [b_xT32])
            cx.cp("act", xT32[:, 4:8, :], pB.rearrange("p (k c) -> p k c", c=128), [bpB], [b_xT32])
            pL, bpL = rot.next()
            for kc in range(8):
                cx.mm(pL[:, 0:NE], xT32[:, kc, :], rwT[:, kc, :], kc == 0, kc == 7, [b_xT32, b_rwT], [bpL])
            cx.ts("dve", LG[:, gb, :], pL[:, 0:NE], ss[:, b:b + 1], None, ALU.mult, None, [bpL, b_ss], [b_LG])
            kb.op("dve", lambda g_, gb=gb: g_.max(out=m8, in_=LG[:, gb, :]), [b_LG], [b_m8])
            cx.cp("dve", TOP[:, gb, :], m8[:, 0:2], [b_m8], [b_TOP])
            cx.ts("dve", gsum[:, 0:1], m8[:, 0:1], -1.0, None, ALU.mult, None, [b_m8], [b_gsum])
            cx.act(GTS[:, gb, :], LG[:, gb, :], AF.Exp, [b_LG, b_gsum], [b_GTS], bias=gsum[:, 0:1])
            cx.stt("dve", GTS[:, gb, :], LG[:, gb, :], m8[:, 1:2], GTS[:, gb, :], ALU.is_ge, ALU.mult,
                   [b_LG, b_m8, b_GTS], [b_GTS])
            kb.op("dve", lambda g_, gb=gb: g_.reduce_sum(out=gsum[:, 1:2], in_=GTS[:, gb, :], axis=AX.X), [b_GTS], [b_gsum])
            kb.op("dve", lambda g_: g_.reciprocal(out=gsum[:, 1:2], in_=gsum[:, 1:2]), [b_gsum], [b_gsum])
            cx.ts("dve", GTS[:, gb, :], GTS[:, gb, :], gsum[:, 1:2], None, ALU.mult, None, [b_GTS, b_gsum], [b_GTS])
    kb.dma("sp", tmpf, din["ltri"], writes=[b_tmpf])
    cx.cp("dve", ltri, tmpf, [b_tmpf], [b_ltri])
    kb.dma("sp", wtab, din["wtab"].rearrange("p (w e) -> p w e", e=8), writes=[b_wtab])
    kb.dma("sp", ctab, din["ctab"], writes=[b_ctab])
    cx.memset("dve", onesb, 1.0, [b_onesb])
    cx.memset("dve", ones32, 1.0, [b_ones32])
    cx.memset("dve", one8, 1.0, [b_one8])
    bce = lambda t: t.unsqueeze(2).to_broadcast([128, NB, 8])
    cx.tt("dve", M1, LG, bce(TOP[:, :, 0]), ALU.is_equal, [b_LG, b_TOP], [b_M1])
    cx.tt("dve", M2, LG, bce(TOP[:, :, 1]), ALU.is_equal, [b_LG, b_TOP], [b_M2])
    cx.tt("dve", MSK, M1, M2, ALU.add, [b_M1, b_M2], [b_MSK])
    mskf = MSK.rearrange("p b e -> p (b e)")
    ps, bps = rot.next()
    cx.mm(ps[:, 0:256], ltri, mskf, True, True, [b_ltri, b_MSK], [bps])
    cx.cp("dve", WIN.rearrange("p b e -> p (b e)"), ps[:, 0:256], [bps], [b_WIN])
    ps, bps = rot.next()
    cx.mm(ps[:, 0:256], onesb, mskf, True, True, [b_onesb, b_MSK], [bps])
    cx.cp("dve", TOT.rearrange("p b e -> p (b e)"), ps[:, 0:256], [bps], [b_TOT])
    for e in range(NE):
        kb.op("dve", lambda g_, e=e: g_.tensor_tensor_scan(out=INC[:, :, e], data0=ones32, data1=TOT[:, :, e], initial=0.0,
                                                           op0=ALU.mult, op1=ALU.add), [b_ones32, b_TOT], [b_INC])
    cx.ts("dve", NSL, INC[:, NB - 1, :], float(SLAB - 1), 1.0 / SLAB, ALU.add, ALU.mult, [b_INC], [b_NSL])
    cx.ts("dve", NSL, NSL, -0.4995, None, ALU.add, None, [b_NSL], [b_NSL])
    cx.cp("dve", NSLi, NSL, [b_NSL], [b_NSLi])
    cx.cp("dve", NSL, NSLi, [b_NSLi], [b_NSL])
    kb.op("dve", lambda g_: g_.tensor_tensor_scan(out=SEND, data0=one8, data1=NSL, initial=0.0, op0=ALU.mult, op1=ALU.add),
          [b_one8, b_NSL], [b_SEND])
    cx.tt("dve", OFF, SEND, NSL, ALU.subtract, [b_SEND, b_NSL], [b_OFF])
    cx.ts("dve", OFF, OFF, float(SLAB), None, ALU.mult, None, [b_OFF], [b_OFF])
    cx.tt("dve", SL, INC, TOT, ALU.subtract, [b_INC, b_TOT], [b_SL])
    cx.tt("dve", SL, SL, WIN, ALU.add, [b_SL, b_WIN], [b_SL])
    cx.tt("dve", SL, SL, OFF.unsqueeze(1).to_broadcast([128, NB, 8]), ALU.add, [b_SL, b_OFF], [b_SL])
    for k, (Mk, b_Mk) in enumerate(((M1, b_M1), (M2, b_M2))):
        cx.tt("dve", TOT, Mk, SL, ALU.mult, [b_Mk, b_SL], [b_TOT])
        kb.op("dve", lambda g_, k=k: g_.reduce_sum(out=SF[:, k, :], in_=TOT, axis=AX.X), [b_TOT], [b_SF])
        cx.tt("dve", TOT, Mk, GTS, ALU.mult, [b_Mk, b_GTS], [b_TOT])
        kb.op("dve", lambda g_, k=k: g_.reduce_sum(out=WGT[:, k, :], in_=TOT, axis=AX.X), [b_TOT], [b_WGT])
    cx.cp("dve", SLOT, SF, [b_SF], [b_SLOT])
    cx.tt("dve", CMP, SEND.unsqueeze(1).to_broadcast([128, NSLAB, 8]), wtab, ALU.is_le, [b_SEND, b_wtab], [b_CMP])
    kb.op("dve", lambda g_: g_.reduce_sum(out=EW, in_=CMP, axis=AX.X), [b_CMP], [b_EW])
    cx.ts("dve", EW, EW, float(NE - 1), float(NGRP * 128), ALU.min, ALU.mult, [b_EW], [b_EW])
    cx.tt("dve", IDXf, EW.unsqueeze(2).to_broadcast([128, NSLAB, 7]), ctab.unsqueeze(1).to_broadcast([128, NSLAB, 7]),
          ALU.add, [b_EW, b_ctab], [b_IDXf])
    cx.cp("dve", IDX, IDXf, [b_IDXf], [b_IDX])
    for gb in range(NB):
        for k in range(2):
            kb.idma(HS, hall[:, gb, :], out_idx=SLOT[:, k, gb:gb + 1], bound=NSLAB * SLAB - 1,
                    reads=[b_hall[gb], b_SLOT], writes=[b_HS], disjoint=True)
    cx.release(m)
    m2 = cx.mark()
    NBK = SLAB // 128
    hsls = [cx.sb(f"hsl{i}", [128, NBK, 1024], BF16) for i in range(2)]
    hTs = [cx.sb(f"hTs{i}", [128, 8, SLAB], BF16) for i in range(2)]
    yacc, b_yacc = cx.sb("yacc", [128, NBK, 1024], F32)
    wsl = [cx.sb(f"wsl{i}", [128, 3 * 4096], BF16) for i in range(2)]
    actT = [cx.sb(f"actT{i}", [128, 4, SLAB], BF16) for i in range(2)]
    sg = [cx.sb(f"sg{i}", [128, 512], F32) for i in range(2)]
    NH2 = SLAB // 512
    wi = 0
    gi = 0

    def issue_w(w, g, slot):
        t, b = slot
        for j, kind in enumerate(("g", "u", "d")):
            kb.idma(t[:, j * 4096:(j + 1) * 4096], W16[kind], in_idx=IDX[:, w, g:g + 1], bound=NTOT - 1,
                    reads=[b_W16[kind], b_IDX], writes=[b], lane=b.name)

    def load_slab_dma(w):
        hsl, b_hsl = hsls[w % 2]
        kb.dma("sp", hsl, HS[w * SLAB:(w + 1) * SLAB, :].rearrange("(b p) d -> p b d", p=128), reads=[b_HS], writes=[b_hsl])

    def load_slab(w):
        hsl, b_hsl = hsls[w % 2]
        hT_, b_hT_ = hTs[w % 2]
        for b in range(NBK):
            pi = b % 2
            p16 = cx.ps[pi].bitcast(BF16).rearrange("p (k c) -> p k c", c=128)
            for kc in range(8):
                cx.tr(p16[:, kc, :], hsl[:, b, kc * 128:(kc + 1) * 128], identb, [b_hsl, b_identb], [cx.psb[pi]])
            cx.cp("act" if b % 2 else "dve", hT_[:, :, b * 128:(b + 1) * 128], p16, [cx.psb[pi]], [b_hT_])

    seq = [(w, g) for w in range(nslab) for g in range(NGRP)]
    issue_w(seq[0][0], seq[0][1], wsl[0])
    load_slab_dma(0)
    load_slab(0)
    for si, (w, g) in enumerate(seq):
        if si + 1 < len(seq):
            issue_w(seq[si + 1][0], seq[si + 1][1], wsl[(si + 1) % 2])
        wt, b_w = wsl[si % 2]
        gv = wt[:, 0:4096].rearrange("p (k n) -> p k n", n=512)
        uv = wt[:, 4096:8192].rearrange("p (k n) -> p k n", n=512)
        dv = wt[:, 8192:12288].rearrange("p (a d) -> p a d", d=1024)
        hT, b_hT = hTs[w % 2]
        if g == NGRP - 3 and w + 1 < nslab:
            load_slab_dma(w + 1)
        if g == NGRP - 1 and w + 1 < nslab:
            load_slab(w + 1)
        at, b_at = actT[gi % 2]
        gi += 1
        for f4 in range(4):
            for half in range(NH2):
                hs = slice(half * 512, (half + 1) * 512)
                pg, bpg = rot.next()
                pu, bpu = rot.next()
                for kc in range(8):
                    cx.mm(pg, gv[:, kc, f4 * 128:(f4 + 1) * 128], hT[:, kc, hs], kc == 0, kc == 7, [b_w, b_hT], [bpg])
                for kc in range(8):
                    cx.mm(pu, uv[:, kc, f4 * 128:(f4 + 1) * 128], hT[:, kc, hs], kc == 0, kc == 7, [b_w, b_hT], [bpu])
                s1, b_s1 = sg[(f4 * NH2 + half) % 2]
                cx.act(s1, pg, AF.Silu, [bpg], [b_s1])
                cx.tt("dve", at[:, f4, hs], s1, pu, ALU.mult, [b_s1, bpu], [b_at])
        for b in range(NBK):
            for half in range(2):
                ps, bps = rot.next()
                for f4 in range(4):
                    cx.mm(ps, at[:, f4, b * 128:(b + 1) * 128], dv[:, f4, half * 512:(half + 1) * 512],
                          f4 == 0, f4 == 3, [b_at, b_w], [bps])
                yv = yacc[:, b, half * 512:(half + 1) * 512]
                if g == 0:
                    cx.cp("act", yv, ps, [bps], [b_yacc])
                else:
                    cx.tt("dve", yv, yv, ps, ALU.add, [b_yacc, bps], [b_yacc])
        if g == NGRP - 1:
            kb.dma("sp", YS[w * SLAB:(w + 1) * SLAB, :].rearrange("(b p) d -> p b d", p=128), yacc, reads=[b_yacc], writes=[b_YS],
                   disjoint=True)
    cx.release(m2)
    NBUF3 = 4
    y1, b_y1 = cx.sb("y1", [128, NBUF3, 1024], F32)
    y2, b_y2 = cx.sb("y2", [128, NBUF3, 1024], F32)
    xb, b_xb = cx.sb("xb", [128, NBUF3, 1024], F32)
    gfin, b_gfin = cx.sb("gfin", [128, 1024], F32)
    s2, _ = cx.sb("s2", [128, NBUF3], F32)
    b_s2s = kb.bufs_n("s2_", NBUF3)
    kb.dma("sp", gfin, din["g_final"].partition_broadcast(128), writes=[b_gfin])
    b_y1s = kb.bufs_n("y1s", NBUF3); b_y2s = kb.bufs_n("y2s", NBUF3); b_xbs = kb.bufs_n("xbs", NBUF3)
    def fetch3(g2):
        i2 = g2 % NBUF3
        kb.dma("sp", xb[:, i2, :], xs[g2 * 128:(g2 + 1) * 128, :], reads=[b_xs], writes=[b_xbs[i2]])
        kb.idma(y1[:, i2, :], YS, in_idx=SLOT[:, 0, g2:g2 + 1], bound=NSLAB * SLAB - 1, reads=[b_YS, b_SLOT], writes=[b_y1s[i2]])
        kb.idma(y2[:, i2, :], YS, in_idx=SLOT[:, 1, g2:g2 + 1], bound=NSLAB * SLAB - 1, reads=[b_YS, b_SLOT], writes=[b_y2s[i2]])

    for g2 in range(NBUF3 - 1):
        fetch3(g2)
    for gb in range(NB):
        i = gb % NBUF3
        b_s2 = b_s2s[i]
        if gb + NBUF3 - 1 < NB:
            fetch3(gb + NBUF3 - 1)
        cx.stt("dve", xb[:, i, :], y1[:, i, :], WGT[:, 0, gb:gb + 1], xb[:, i, :], ALU.mult, ALU.add,
               [b_y1s[i], b_WGT, b_xbs[i]], [b_xbs[i]])
        cx.stt("dve", xb[:, i, :], y2[:, i, :], WGT[:, 1, gb:gb + 1], xb[:, i, :], ALU.mult, ALU.add,
               [b_y2s[i], b_WGT, b_xbs[i]], [b_xbs[i]])
        cx.act(junk, xb[:, i, :], AF.Square, [b_xbs[i]], [b_junk, b_s2], accum=s2[:, i:i + 1])
        cx.ts("dve", s2[:, i:i + 1], s2[:, i:i + 1], 1.0 / D, EPS, ALU.mult, ALU.add, [b_s2], [b_s2])
        cx.act(s2[:, i:i + 1], s2[:, i:i + 1], AF.Sqrt, [b_s2], [b_s2])
        kb.op("dve", lambda g_, i=i: g_.reciprocal(out=s2[:, i:i + 1], in_=s2[:, i:i + 1]), [b_s2], [b_s2])
        cx.stt("dve", xb[:, i, :], xb[:, i, :], s2[:, i:i + 1], gfin, ALU.mult, ALU.mult, [b_xbs[i], b_s2, b_gfin], [b_xbs[i]])
        kb.dma("sp", out[gb * 128:(gb + 1) * 128, :], xb[:, i, :], reads=[b_xbs[i]], writes=[b_out], disjoint=True)
    cx.release(mp)
```

```python
import math
import numpy as np
import ml_dtypes
import concourse.bass as bass
import concourse.mybir as mybir
from concourse.bass_utils import run_bass_kernel_spmd

F32 = mybir.dt.float32
BF16 = mybir.dt.bfloat16
I32 = mybir.dt.int32
AF = mybir.ActivationFunctionType
ALU = mybir.AluOpType
AX = mybir.AxisListType

L = 4096
D = 1024
NB = L // 128
G = 64
GS = 16
PS = 64
TCH = 8
D_FF = 2688
NE = 8
MOE_FF = 3584
NH = 16
QK_NOPE = 64
QK_ROPE = 32
V_HEAD = 64
Q_LORA = 512
KV_LORA = 256
EPS = 1e-6
DT_MIN = 1e-3
DT_MAX = 1e-1
SLAB = 1024
NSLAB = (2 * L) // SLAB + NE - 1
NGRP = MOE_FF // 512


class Buf:
    __slots__ = ("name", "w", "r")

    def __init__(self, name):
        self.name = name
        self.w = {}
        self.r = {}


class KB:
    def __init__(self, nc):
        self.nc = nc
        self.eng = {"pe": nc.tensor, "act": nc.scalar, "dve": nc.vector,
                    "pool": nc.gpsimd, "sp": nc.sync}
        self.sem = {k: nc.alloc_semaphore(name="sem_" + k) for k in self.eng}
        self.cnt = {k: 0 for k in self.eng}
        self.known = {k: {} for k in self.eng}
        self.lanes = {}
        self.bufs = []
        self.n_ins = 0

    def _lane(self, lane):
        if lane not in self.lanes:
            pool = self.__dict__.setdefault("_lane_pool", [])
            if pool:
                self.lanes[lane] = pool.pop()
            else:
                self._nl = getattr(self, "_nl", 0) + 1
                self.lanes[lane] = [self.nc.alloc_semaphore(name=f"ln{self._nl}"), 0]

    def buf(self, name):
        b = Buf(name)
        self.bufs.append(b)
        return b

    def bufs_n(self, name, n):
        return [self.buf(f"{name}{i}") for i in range(n)]

    def _semof(self, key):
        if key[0] == "e":
            return self.sem[key[1]]
        return self.lanes[key[1]][0]

    def _need(self, reads, writes):
        need = {}
        for b in reads:
            for k, v in b.w.items():
                if need.get(k, 0) < v:
                    need[k] = v
        for b in writes:
            for k, v in b.w.items():
                if need.get(k, 0) < v:
                    need[k] = v
            for k, v in b.r.items():
                if need.get(k, 0) < v:
                    need[k] = v
        return need

    def _wait(self, e, need):
        kn = self.known[e]
        for k, v in need.items():
            if e == "pe" and k == ("e", "pe"):
                continue
            if kn.get(k, 0) >= v:
                continue
            self.eng[e].wait_ge(self._semof(k), v)
            kn[k] = v

    def op(self, e, fn, reads=(), writes=()):
        self._wait(e, self._need(reads, writes))
        ins = fn(self.eng[e])
        self.cnt[e] += 1
        c = self.cnt[e]
        ins.then_inc(self.sem[e], 1)
        key = ("e", e)
        for b in reads:
            b.r[key] = c
        for b in writes:
            b.w = {key: c}
            b.r = {}
        self.n_ins += 1
        return ins

    def dma(self, q, out, in_, reads=(), writes=(), lane=None, disjoint=False, **kw):
        if lane is None:
            lane = writes[0].name
        self._lane(lane)
        need = self._need(reads, writes)
        if disjoint:
            need.pop(("l", lane), None)
        self._wait(q, need)
        ins = self.eng[q].dma_start(out=out, in_=in_, **kw)
        ln = self.lanes[lane]
        ln[1] += 16
        ins.then_inc(ln[0], 16)
        key = ("l", lane)
        for b in reads:
            b.r[key] = ln[1]
        for b in writes:
            neww = {k: v for k, v in b.w.items() if k[0] == "l" and k != key}
            neww[key] = ln[1]
            b.w = neww
            b.r = {}
        self.n_ins += 1
        return ins

    def idma(self, out, in_, out_idx=None, in_idx=None, bound=None, reads=(), writes=(), lane=None, disjoint=False):
        if lane is None:
            lane = writes[0].name
        self._lane(lane)
        need = self._need(reads, writes)
        if disjoint:
            need.pop(("l", lane), None)
        self._wait("pool", need)
        oo = bass.IndirectOffsetOnAxis(ap=out_idx, axis=0) if out_idx is not None else None
        io = bass.IndirectOffsetOnAxis(ap=in_idx, axis=0) if in_idx is not None else None
        ins = self.nc.gpsimd.indirect_dma_start(out=out, out_offset=oo, in_=in_, in_offset=io)
        ln = self.lanes[lane]
        ln[1] += 16
        ins.then_inc(ln[0], 16)
        key = ("l", lane)
        for b in reads:
            b.r[key] = ln[1]
        for b in writes:
            neww = {k: v for k, v in b.w.items() if k[0] == "l" and k != key}
            neww[key] = ln[1]
            b.w = neww
            b.r = {}
        self.n_ins += 1
        return ins

    def finish(self, bufs):
        need = {}
        for b in bufs:
            for k, v in b.w.items():
                need[k] = max(need.get(k, 0), v)
        self._wait("sp", need)
        allneed = {("l", ln): v[1] for ln, v in self.lanes.items() if v[1] > 0}
        for e in self.eng:
            if self.cnt[e] > 0:
                allneed[("e", e)] = self.cnt[e]
        allneed.pop(("e", "sp"), None)
        self._wait("sp", allneed)

    def barrier(self):
        need = {("l", ln): v[1] for ln, v in self.lanes.items() if v[1] > 0}
        for e in self.eng:
            if self.cnt[e] > 0:
                need[("e", e)] = self.cnt[e]
        for e in self.eng:
            n2 = dict(need)
            n2.pop(("e", e), None)
            self._wait(e, n2)
        for b in self.bufs:
            b.w = {}
            b.r = {}
        pool = self.__dict__.setdefault("_lane_pool", [])
        for ln, v in self.lanes.items():
            pool.append(v)
        self.lanes = {}
        for e in self.eng:
            self.known[e] = {k: v for k, v in self.known[e].items() if k[0] == "e"}


class Ctx:
    def __init__(self, nc):
        self.nc = nc
        self.kb = KB(nc)
        self.ps = []
        self.psb = []
        for i in range(8):
            self.ps.append(nc.alloc_psum_tensor(f"ps{i}", [128, 512], F32).ap())
            self.psb.append(self.kb.buf(f"ps{i}"))
        self._mark = None

    def sb(self, name, shape, dtype=F32):
        self._n = getattr(self, "_n", 0) + 1
        t = self.nc.alloc_sbuf_tensor(f"sb{self._n}_{name}", list(shape), dtype).ap()
        return t, self.kb.buf(f"sb{self._n}_{name}")

    def mark(self):
        return (self.nc.sbuf_base, self.nc.sbuf_top)

    def release(self, m):
        self.kb.barrier()
        self.nc.sbuf_base, self.nc.sbuf_top = m

    def tt(self, e, out, in0, in1, op, r, w):
        return self.kb.op(e, lambda g: g.tensor_tensor(out=out, in0=in0, in1=in1, op=op), r, w)

    def ts(self, e, out, in0, s1, s2, op0, op1, r, w):
        if s2 is None:
            return self.kb.op(e, lambda g: g.tensor_scalar(out=out, in0=in0, scalar1=s1, scalar2=None, op0=op0), r, w)
        return self.kb.op(e, lambda g: g.tensor_scalar(out=out, in0=in0, scalar1=s1, scalar2=s2, op0=op0, op1=op1), r, w)

    def stt(self, e, out, in0, scalar, in1, op0, op1, r, w):
        return self.kb.op(e, lambda g: g.scalar_tensor_tensor(out=out, in0=in0, scalar=scalar, in1=in1, op0=op0, op1=op1), r, w)

    def cp(self, e, out, in_, r, w):
        if e == "act":
            return self.kb.op(e, lambda g: g.activation(out=out, in_=in_, func=AF.Copy), r, w)
        return self.kb.op(e, lambda g: g.tensor_copy(out=out, in_=in_), r, w)

    def act(self, out, in_, func, r, w, scale=1.0, bias=None, accum=None):
        kw = {}
        if bias is not None:
            kw["bias"] = bias
        if accum is not None:
            kw["accum_out"] = accum
        return self.kb.op("act", lambda g: g.activation(out=out, in_=in_, func=func, scale=scale, **kw), r, w)

    def mm(self, out, lhsT, rhs, start, stop, r, w, **kw):
        return self.kb.op("pe", lambda g: g.matmul(out, lhsT=lhsT, rhs=rhs, start=start, stop=stop, **kw), r, w)

    def tr(self, out, in_, ident, r, w):
        return self.kb.op("pe", lambda g: g.transpose(out=out, in_=in_, identity=ident), r, w)

    def memset(self, e, out, val, w):
        return self.kb.op(e, lambda g: g.memset(out, val), (), w)


PI = math.pi


def setup_ident(cx, din):
    P = {"bufs": {}}
    P["identf"], bf = cx.sb("identf", [128, 128], F32)
    P["identb"], bb = cx.sb("identb", [128, 128], BF16)
    cx.kb.dma("sp", P["identf"], din["ident"], writes=[bf])
    cx.cp("dve", P["identb"], P["identf"], [bf], [bb])
    P["bufs"]["identf"], P["bufs"]["identb"] = bf, bb
    return P


def phase0(cx, P, din, pre_hook=None):
    nc, kb = cx.nc, cx.kb
    b_identf, b_identb = P["bufs"]["identf"], P["bufs"]["identb"]
    P["WBre"], b_WBre = cx.sb("WBre", [128, 8, 8, 128], BF16)
    P["WBim"], b_WBim = cx.sb("WBim", [128, 8, 8, 128], BF16)
    P["WCre"], b_WCre = cx.sb("WCre", [128, 32, 8, 2, 16], BF16)
    P["WCim"], b_WCim = cx.sb("WCim", [128, 32, 8, 2, 16], BF16)
    P["FIRW"], b_FIRW = cx.sb("FIRW", [128, 8, 8, 128], BF16)
    P["A0c"], b_A0c = cx.sb("A0c", [128, 32, 8], F32)
    P["A0s"], b_A0s = cx.sb("A0s", [128, 32, 8], F32)
    P["A1c"], b_A1c = cx.sb("A1c", [128, 32, 8], F32)
    P["A1s"], b_A1s = cx.sb("A1s", [128, 32, 8], F32)
    P["RM8"], b_RM8 = cx.sb("RM8", [128, 32], F32)
    P["dT"], b_dT = cx.sb("dT", [128, 8], F32)
    P["bufs"].update(dict(WBre=b_WBre, WBim=b_WBim, WCre=b_WCre,
                          WCim=b_WCim, FIRW=b_FIRW, A0c=b_A0c, A0s=b_A0s, A1c=b_A1c, A1s=b_A1s, RM8=b_RM8, dT=b_dT))
    m = cx.mark()
    if pre_hook is not None:
        pre_hook()
    LR, b_LR = cx.sb("LR", [128, 32]); LI, b_LI = cx.sb("LI", [128, 32]); LDT, b_LDT = cx.sb("LDT", [128, 32])
    TH, b_TH = cx.sb("TH", [128, 32]); LM, b_LM = cx.sb("LM", [128, 32])
    EV, b_EV = cx.sb("EV", [128, 9, 32]); ANG, b_ANG = cx.sb("ANG", [128, 9, 32]); MAG, b_MAG = cx.sb("MAG", [128, 9, 32])
    SN, b_SN = cx.sb("SN", [128, 9, 32]); CS, b_CS = cx.sb("CS", [128, 9, 32]); IT, b_IT = cx.sb("IT", [128, 9, 32], I32)
    ARE, b_ARE = cx.sb("ARE", [128, 9, 32]); AIM, b_AIM = cx.sb("AIM", [128, 9, 32])
    NR, b_NR = cx.sb("NR", [128, 32]); DEN, b_DEN = cx.sb("DEN", [128, 32]); TMPa, b_TMPa = cx.sb("TMPa", [128, 32])
    TMPb, b_TMPb = cx.sb("TMPb", [128, 32])
    CRE, b_CRE = cx.sb("CRE", [128, 32]); CIM, b_CIM = cx.sb("CIM", [128, 32])
    WRE, b_WRE = cx.sb("WRE", [128, 8, 32]); WIM, b_WIM = cx.sb("WIM", [128, 8, 32])
    W8a, b_W8a = cx.sb("W8a", [128, 8, 32]); W8b, b_W8b = cx.sb("W8b", [128, 8, 32])
    BTre, b_BTre = cx.sb("BTre", [128, 32, 16]); BTim, b_BTim = cx.sb("BTim", [128, 32, 16])
    CTre, b_CTre = cx.sb("CTre", [128, 32, 16]); CTim, b_CTim = cx.sb("CTim", [128, 32, 16])
    T1, b_T1 = cx.sb("T1", [128, 32, 16]); T2, b_T2 = cx.sb("T2", [128, 32, 16])
    T3, b_T3 = cx.sb("T3", [128, 32, 16]); T4, b_T4 = cx.sb("T4", [128, 32, 16])
    XPre, b_XPre = cx.sb("XPre", [128, 8, 32, 2, 16], BF16); XPim, b_XPim = cx.sb("XPim", [128, 8, 32, 2, 16], BF16)
    CPre, b_CPre = cx.sb("CPre", [128, 32, 2, 16], BF16); CPnim, b_CPnim = cx.sb("CPnim", [128, 32, 2, 16], BF16)
    BM, b_BM = cx.sb("BM", [128, 128], F32)
    TK, b_TK = cx.sb("TK", [128, 128], F32)

    kb.dma("sp", BM, din["bmask"], writes=[b_BM])
    kb.dma("sp", EV, din["ev"].rearrange("p (e q) -> p e q", e=9), writes=[b_EV])
    kb.dma("sp", LR, din["lamT_re"], writes=[b_LR])
    kb.dma("sp", LI, din["lamT_im"], writes=[b_LI])
    kb.dma("sp", LDT, din["ldtT"], writes=[b_LDT])
    kb.dma("sp", P["dT"], din["dT"], writes=[b_dT])
    kb.dma("sp", BTre, din["bT_re"].rearrange("p (q n) -> p q n", n=16), writes=[b_BTre])
    kb.dma("sp", BTim, din["bT_im"].rearrange("p (q n) -> p q n", n=16), writes=[b_BTim])
    kb.dma("sp", CTre, din["cT_re"].rearrange("p (q n) -> p q n", n=16), writes=[b_CTre])
    kb.dma("sp", CTim, din["cT_im"].rearrange("p (q n) -> p q n", n=16), writes=[b_CTim])

    cx.act(LDT, LDT, AF.Exp, [b_LDT], [b_LDT])
    cx.tt("dve", TH, LI, LDT, ALU.mult, [b_LI, b_LDT], [b_TH])
    cx.tt("dve", LM, LR, LDT, ALU.mult, [b_LR, b_LDT], [b_LM])
    bc9 = lambda t: t.unsqueeze(1).to_broadcast([128, 9, 32])
    bc8 = lambda t: t.unsqueeze(1).to_broadcast([128, 8, 32])
    cx.tt("dve", ANG, EV, bc9(TH), ALU.mult, [b_EV, b_TH], [b_ANG])
    cx.tt("dve", MAG, EV, bc9(LM), ALU.mult, [b_EV, b_LM], [b_MAG])
    cx.act(MAG, MAG, AF.Exp, [b_MAG], [b_MAG])
    cx.ts("dve", SN, ANG, 1.0 / (2.0 * PI), None, ALU.mult, None, [b_ANG], [b_SN])
    cx.cp("dve", IT, SN, [b_SN], [b_IT])
    cx.cp("dve", SN, IT, [b_IT], [b_SN])
    cx.stt("dve", SN, SN, -2.0 * PI, ANG, ALU.mult, ALU.add, [b_SN, b_ANG], [b_SN])
    cx.ts("dve", CS, ANG, 1.0 / (2.0 * PI), 0.25, ALU.mult, ALU.add, [b_ANG], [b_CS])
    cx.cp("dve", IT, CS, [b_CS], [b_IT])
    cx.cp("dve", CS, IT, [b_IT], [b_CS])
    cx.stt("dve", CS, CS, -2.0 * PI, ANG, ALU.mult, ALU.add, [b_CS, b_ANG], [b_CS])
    cx.ts("dve", CS, CS, 0.5 * PI, None, ALU.add, None, [b_CS], [b_CS])
    cx.ts("dve", SN, SN, -PI, PI, ALU.max, ALU.min, [b_SN], [b_SN])
    cx.ts("dve", CS, CS, -PI, PI, ALU.max, ALU.min, [b_CS], [b_CS])
    cx.act(SN, SN, AF.Sin, [b_SN], [b_SN])
    cx.act(CS, CS, AF.Sin, [b_CS], [b_CS])
    cx.tt("dve", ARE, MAG, CS, ALU.mult, [b_MAG, b_CS], [b_ARE])
    cx.tt("dve", AIM, MAG, SN, ALU.mult, [b_MAG, b_SN], [b_AIM])
    cx.ts("dve", NR, ARE[:, 1, :], -1.0, None, ALU.add, None, [b_ARE], [b_NR])
    NI = AIM[:, 1, :]
    cx.tt("dve", DEN, LR, LR, ALU.mult, [b_LR], [b_DEN])
    cx.tt("dve", TMPa, LI, LI, ALU.mult, [b_LI], [b_TMPa])
    cx.tt("dve", DEN, DEN, TMPa, ALU.add, [b_DEN, b_TMPa], [b_DEN])
    kb.op("dve", lambda g: g.reciprocal(out=DEN, in_=DEN), [b_DEN], [b_DEN])
    cx.tt("dve", TMPa, NR, LR, ALU.mult, [b_NR, b_LR], [b_TMPa])
    cx.tt("dve", TMPb, NI, LI, ALU.mult, [b_AIM, b_LI], [b_TMPb])
    cx.tt("dve", TMPa, TMPa, TMPb, ALU.add, [b_TMPa, b_TMPb], [b_TMPa])
    cx.tt("dve", CRE, TMPa, DEN, ALU.mult, [b_TMPa, b_DEN], [b_CRE])
    cx.tt("dve", TMPa, NI, LR, ALU.mult, [b_AIM, b_LR], [b_TMPa])
    cx.tt("dve", TMPb, NR, LI, ALU.mult, [b_NR, b_LI], [b_TMPb])
    cx.tt("dve", TMPa, TMPa, TMPb, ALU.subtract, [b_TMPa, b_TMPb], [b_TMPa])
    cx.tt("dve", CIM, TMPa, DEN, ALU.mult, [b_TMPa, b_DEN], [b_CIM])
    cx.tt("dve", W8a, ARE[:, 0:8, :], bc8(CRE), ALU.mult, [b_ARE, b_CRE], [b_W8a])
    cx.tt("dve", W8b, AIM[:, 0:8, :], bc8(CIM), ALU.mult, [b_AIM, b_CIM], [b_W8b])
    cx.tt("dve", WRE, W8a, W8b, ALU.subtract, [b_W8a, b_W8b], [b_WRE])
    cx.tt("dve", W8a, ARE[:, 0:8, :], bc8(CIM), ALU.mult, [b_ARE, b_CIM], [b_W8a])
    cx.tt("dve", W8b, AIM[:, 0:8, :], bc8(CRE), ALU.mult, [b_AIM, b_CRE], [b_W8b])
    cx.tt("dve", WIM, W8a, W8b, ALU.add, [b_W8a, b_W8b], [b_WIM])
    cx.cp("dve", P["RM8"], MAG[:, 8, :], [b_MAG], [b_RM8])
    A0c, A0s, A1c, A1s = P["A0c"], P["A0s"], P["A1c"], P["A1s"]
    cx.cp("dve", A0c[:, :, 0], CS[:, 8, :], [b_CS], [b_A0c])
    cx.cp("dve", A0s[:, :, 0], SN[:, 8, :], [b_SN], [b_A0s])
    for i in range(1, 8):
        cx.tt("dve", TMPa, A0c[:, :, i - 1], A0c[:, :, 0], ALU.mult, [b_A0c], [b_TMPa])
        cx.tt("dve", TMPb, A0s[:, :, i - 1], A0s[:, :, 0], ALU.mult, [b_A0s], [b_TMPb])
        cx.tt("dve", A0c[:, :, i], TMPa, TMPb, ALU.subtract, [b_TMPa, b_TMPb], [b_A0c])
        cx.tt("dve", TMPa, A0c[:, :, i - 1], A0s[:, :, 0], ALU.mult, [b_A0c, b_A0s], [b_TMPa])
        cx.tt("dve", TMPb, A0s[:, :, i - 1], A0c[:, :, 0], ALU.mult, [b_A0s, b_A0c], [b_TMPb])
        cx.tt("dve", A0s[:, :, i], TMPa, TMPb, ALU.add, [b_TMPa, b_TMPb], [b_A0s])
    cx.memset("dve", A1c[:, :, 0], 1.0, [b_A1c])
    cx.memset("dve", A1s[:, :, 0], 0.0, [b_A1s])
    for i in range(1, 8):
        cx.tt("dve", TMPa, A1c[:, :, i - 1], A0c[:, :, 7], ALU.mult, [b_A1c, b_A0c], [b_TMPa])
        cx.tt("dve", TMPb, A1s[:, :, i - 1], A0s[:, :, 7], ALU.mult, [b_A1s, b_A0s], [b_TMPb])
        cx.tt("dve", A1c[:, :, i], TMPa, TMPb, ALU.subtract, [b_TMPa, b_TMPb], [b_A1c])
        cx.tt("dve", TMPa, A1c[:, :, i - 1], A0s[:, :, 7], ALU.mult, [b_A1c, b_A0s], [b_TMPa])
        cx.tt("dve", TMPb, A1s[:, :, i - 1], A0c[:, :, 7], ALU.mult, [b_A1s, b_A0c], [b_TMPb])
        cx.tt("dve", A1s[:, :, i], TMPa, TMPb, ALU.add, [b_TMPa, b_TMPb], [b_A1s])

    cx.memset("pool", XPre, 0.0, [b_XPre])
    cx.memset("pool", XPim, 0.0, [b_XPim])
    cx.memset("pool", CPre, 0.0, [b_CPre])
    cx.memset("pool", CPnim, 0.0, [b_CPnim])
    cx.memset("pool", P["WCre"], 0.0, [b_WCre])
    cx.memset("pool", P["WCim"], 0.0, [b_WCim])
    bcn = lambda t: t.unsqueeze(2).to_broadcast([128, 32, 16])
    for s in range(8):
        e = 7 - s
        cx.tt("dve", T1, BTre, bcn(WRE[:, e, :]), ALU.mult, [b_BTre, b_WRE], [b_T1])
        cx.tt("dve", T2, BTim, bcn(WIM[:, e, :]), ALU.mult, [b_BTim, b_WIM], [b_T2])
        cx.tt("dve", T3, BTim, bcn(WRE[:, e, :]), ALU.mult, [b_BTim, b_WRE], [b_T3])
        cx.tt("dve", T4, BTre, bcn(WIM[:, e, :]), ALU.mult, [b_BTre, b_WIM], [b_T4])
        for par in range(2):
            sl = slice(64 * par, 64 * par + 64)
            cx.tt("dve", XPre[sl, s, :, par, :], T1[sl], T2[sl], ALU.subtract, [b_T1, b_T2], [b_XPre])
            cx.tt("dve", XPim[sl, s, :, par, :], T3[sl], T4[sl], ALU.add, [b_T3, b_T4], [b_XPim])
    for par in range(2):
        sl = slice(64 * par, 64 * par + 64)
        cx.cp("dve", CPre[sl, :, par, :], CTre[sl], [b_CTre], [b_CPre])
        cx.ts("dve", CPnim[sl, :, par, :], CTim[sl], -1.0, None, ALU.mult, None, [b_CTim], [b_CPnim])
    for j in range(8):
        e = j + 1
        cx.tt("dve", T1, CTre, bcn(ARE[:, e, :]), ALU.mult, [b_CTre, b_ARE], [b_T1])
        cx.tt("dve", T2, CTim, bcn(AIM[:, e, :]), ALU.mult, [b_CTim, b_AIM], [b_T2])
        cx.tt("dve", T3, CTre, bcn(AIM[:, e, :]), ALU.mult, [b_CTre, b_AIM], [b_T3])
        cx.tt("dve", T4, CTim, bcn(ARE[:, e, :]), ALU.mult, [b_CTim, b_ARE], [b_T4])
        for par in range(2):
            sl = slice(64 * par, 64 * par + 64)
            cx.tt("dve", P["WCre"][sl, :, j, par, :], T1[sl], T2[sl], ALU.subtract, [b_T1, b_T2], [b_WCre])
            cx.stt("dve", P["WCim"][sl, :, j, par, :], T3[sl], -1.0, T4[sl], ALU.mult, ALU.subtract,
                   [b_T3, b_T4], [b_WCim])
    for fc in range(8):
        pq = slice(4 * fc, 4 * fc + 4)
        for k in range(8):
            pi = (fc * 8 + k) % 4
            ps, bps = cx.ps[pi], cx.psb[pi]
            cx.mm(ps[:, 0:128], XPre[:, 7 - k, pq, :, :], CPre[:, pq, :, :], True, False, [b_XPre, b_CPre], [bps])
            cx.mm(ps[:, 0:128], XPim[:, 7 - k, pq, :, :], CPnim[:, pq, :, :], False, True, [b_XPim, b_CPnim], [bps])
            if k == 0:
                cx.tt("dve", TK, ps[:, 0:128], BM, ALU.mult, [bps, b_BM], [b_TK])
                cx.stt("dve", P["FIRW"][:, fc, 0, :], P["identf"], P["dT"][:, fc:fc + 1], TK, ALU.mult, ALU.add,
                       [b_identf, b_dT, b_TK], [b_FIRW])
            else:
                cx.tt("dve", P["FIRW"][:, fc, k, :], ps[:, 0:128], BM, ALU.mult, [bps, b_BM], [b_FIRW])
    for (XP, b_XP, WB, b_WB) in ((XPre, b_XPre, P["WBre"], b_WBre), (XPim, b_XPim, P["WBim"], b_WBim)):
        for fc in range(8):
            pq = slice(4 * fc, 4 * fc + 4)
            pi = 4 + (fc % 2)
            psb16 = cx.ps[pi].bitcast(BF16).rearrange("p (s c) -> p s c", s=8)
            for s in range(8):
                cx.tr(psb16[:, s, :], XP[:, s, pq, :, :], P["identb"], [b_XP, b_identb], [cx.psb[pi]])
            cx.cp("act" if fc % 2 else "dve", WB[:, fc, :, :], psb16, [cx.psb[pi]], [b_WB])
    cx.release(m)
    return P


class PsRot:
    def __init__(self, cx, banks):
        self.cx = cx
        self.banks = list(banks)
        self.i = 0

    def next(self):
        b = self.banks[self.i % len(self.banks)]
        self.i += 1
        return self.cx.ps[b], self.cx.psb[b]


class WStream:
    def __init__(self, cx, nslots, slot_elems, name="ws", direct=None, ahead=None):
        self.cx = cx
        self.slots = []
        for i in range(nslots):
            t, b = cx.sb(f"{name}{i}", [128, slot_elems], BF16)
            self.slots.append((t, b))
        self.jobs = []
        self.issued = 0
        self.used = 0
        self.res = {}
        self.direct = direct
        self.ahead = ahead

    def plan(self, jobs):
        self.jobs.extend(jobs)

    def _issue(self, i):
        t, b = self.slots[i % len(self.slots)]
        off = 0
        views = []
        if self.direct is not None:
            src, n = self.jobs[i]
            self.cx.kb.dma("sp", t[:, 0:n], src, reads=[self.direct], writes=[b])
            self.res[i] = (t, b)
            return
        for (src, a, n) in self.jobs[i]:
            v = t[:, off:off + a * n].rearrange("p (a n) -> p a n", n=n)
            self.cx.kb.dma("pool", v, src, writes=[b])
            views.append(v)
            off += a * n
        self.res[i] = (views, b)

    def get(self):
        i = self.used
        ahead = self.ahead if self.ahead is not None else max(1, len(self.slots) - 2)
        while self.issued < min(len(self.jobs), i + 1 + ahead):
            self._issue(self.issued)
            self.issued += 1
        self.used += 1
        return self.res.pop(i)


def rms_to_hT(cx, P, xt, b_xt, nblk, gT, b_gT, htok, b_htok, hT, b_hT, ss, b_ss, trbanks, d=D, extra=None):
    nkc = d // 128
    bx = b_xt if isinstance(b_xt, list) else [b_xt]
    for b in range(nblk):
        cx.act(htok[:, b, :], xt[:, b, :], AF.Square, bx, [b_htok, b_ss], accum=ss[:, b:b + 1])
    cx.ts("dve", ss[:, 0:nblk], ss[:, 0:nblk], 1.0 / d, EPS, ALU.mult, ALU.add, [b_ss], [b_ss])
    cx.act(ss[:, 0:nblk], ss[:, 0:nblk], AF.Sqrt, [b_ss], [b_ss])
    cx.kb.op("dve", lambda g: g.reciprocal(out=ss[:, 0:nblk], in_=ss[:, 0:nblk]), [b_ss], [b_ss])
    for b in range(nblk):
        cx.ts("dve", htok[:, b, :], xt[:, b, :], ss[:, b:b + 1], None, ALU.mult, None, bx + [b_ss], [b_htok])
    for b in range(nblk):
        pi = trbanks[b % len(trbanks)]
        p16 = cx.ps[pi].bitcast(BF16).rearrange("p (k c) -> p k c", c=128)
        for kc in range(nkc):
            cx.tr(p16[:, kc, :], htok[:, b, kc * 128:(kc + 1) * 128], P["identb"], [b_htok, P["bufs"]["identb"]], [cx.psb[pi]])
        cx.tt("dve", hT[:, 0:nkc, b * 128:(b + 1) * 128], p16[:, 0:nkc, :],
              gT[:, 0:nkc].unsqueeze(2).to_broadcast([128, nkc, 128]), ALU.mult, [cx.psb[pi], b_gT], [b_hT])
        if extra is not None:
            gT2, hT2, b_hT2 = extra
            cx.tt("dve", hT2[:, 0:nkc, b * 128:(b + 1) * 128], p16[:, 0:nkc, :],
                  gT2[:, 0:nkc].unsqueeze(2).to_broadcast([128, nkc, 128]), ALU.mult, [cx.psb[pi], b_gT], [b_hT2])


N_L0_JOBS = 6 + D_FF // 128


def layer0_convert(cx, din, W0, b_W0, nstage=4):
    kb = cx.kb
    stage = [cx.sb(f"l0st{i}", [128, 4096], BF16) for i in range(nstage)]
    j = 0
    for w in (din["s5_w_in"], din["s5_w_glu"], din["s5_w_out"]):
        for half in range(2):
            st, b_st = stage[j % nstage]
            kb.dma("pool", st.rearrange("p (k n) -> p k n", n=512),
                   w[:, half * 512:(half + 1) * 512].rearrange("(k p) n -> p k n", p=128), writes=[b_st])
            kb.dma("sp", W0[j * 128:(j + 1) * 128, :], st, reads=[b_st], writes=[b_W0], disjoint=True)
            j += 1
    for f in range(D_FF // 128):
        st, b_st = stage[j % nstage]
        cs = slice(f * 128, (f + 1) * 128)
        kb.dma("pool", st[:, 0:1024].rearrange("p (k n) -> p k n", n=128),
               din["ffn_w_gate"][:, cs].rearrange("(k p) n -> p k n", p=128), writes=[b_st])
        kb.dma("pool", st[:, 1024:2048].rearrange("p (k n) -> p k n", n=128),
               din["ffn_w_up"][:, cs].rearrange("(k p) n -> p k n", p=128), writes=[b_st])
        kb.dma("pool", st[:, 2048:3072], din["ffn_w_down"][cs, :], writes=[b_st])
        kb.dma("sp", W0[j * 128:(j + 1) * 128, 0:3072], st[:, 0:3072], reads=[b_st], writes=[b_W0], disjoint=True)
        j += 1


GELU_C = 0.044715
GELU_S = 2.0 * math.sqrt(2.0 / math.pi)


def phase1(cx, P, din, xs, b_xs, W0, b_W0, ntiles=8, hook=None):
    nc, kb = cx.nc, cx.kb
    B = P["bufs"]
    m = cx.mark()
    xt, _ = cx.sb("xt", [128, 4, 1024], F32)
    b_xt = kb.bufs_n("xt8_", 8)
    regA, b_A = cx.sb("regA", [128, 4096], F32)
    regB, b_B = cx.sb("regB", [128, 2048], F32)
    regC, b_C = cx.sb("regC", [128, 2048], F32)
    uT, _ = cx.sb("uT", [128, 8, 512], BF16)
    b_u = kb.bufs_n("uT", 8)
    yT, _ = cx.sb("yT", [128, 8, 512], BF16)
    b_y = kb.bufs_n("yT", 8)
    SR, b_SR = cx.sb("SR", [128, 32, 65], F32)
    SI, b_SI = cx.sb("SI", [128, 32, 65], F32)
    SB, b_SB = cx.sb("SB", [128, 32, 2, 64], BF16)
    ss, b_ss = cx.sb("ss", [128, 8], F32)
    gT, b_gT = cx.sb("gT", [128, 2, 8], F32)
    CAR, b_CAR = cx.sb("CAR", [128, 2, 32], F32)
    ws = WStream(cx, 3, 4096, direct=b_W0, ahead=2)
    y32 = regA.rearrange("p (f t) -> p f t", t=512)
    t3 = regA[:, 0:2048].rearrange("p (q c) -> p q c", c=64)
    t4 = regA[:, 2048:4096].rearrange("p (q c) -> p q c", c=64)
    htok = regB.bitcast(BF16).rearrange("p (b d) -> p b d", d=1024)
    t1 = regB.rearrange("p (q c) -> p q c", c=64)
    hT = regC.bitcast(BF16).rearrange("p (k t) -> p k t", t=512)
    t2 = regC.rearrange("p (q c) -> p q c", c=64)
    zT, b_z = uT, b_u
    sgs = [regB[:, i * 512:(i + 1) * 512] for i in range(2)]
    acts = [yT.rearrange("p f t -> p (f t)")[:, i * 512:(i + 1) * 512] for i in range(4)]
    rot = PsRot(cx, [2, 3, 4, 5, 6, 7])
    ytmp = [(regA[:, i * 512:(i + 1) * 512], kb.buf(f"ytmp{i}")) for i in range(4)]
    nt = 0

    kb.dma("sp", gT[:, 0, :], din["gT_mix0"], writes=[b_gT])
    kb.dma("sp", gT[:, 1, :], din["gT_ffn0"], writes=[b_gT])
    cx.memset("dve", CAR, 0.0, [b_CAR])
    w_in, w_glu, w_out = din["s5_w_in"], din["s5_w_glu"], din["s5_w_out"]
    wg, wu, wd = din["ffn_w_gate"], din["ffn_w_up"], din["ffn_w_down"]

    def wcols(w, c0, n):
        return (w[:, c0:c0 + n].rearrange("(k p) n -> p k n", p=128), 8, n)

    jobs = []
    for T in range(ntiles):
        for j in range(N_L0_JOBS):
            jobs.append((W0[j * 128:(j + 1) * 128, 0:(4096 if j < 6 else 3072)], 4096 if j < 6 else 3072))
    ws.plan(jobs)

    for T in range(ntiles):
        t0 = T * 512
        kb.dma("sp", xt, din["x"][t0:t0 + 512, :].rearrange("(b p) d -> p b d", p=128), writes=b_xt, lane="xt")
        rms_to_hT(cx, P, xt, b_xt, 4, gT[:, 0, :], b_gT, htok, b_B, hT, b_C, ss, b_ss, [0, 1])
        for half in range(2):
            wt_, b_w = ws.get()
            wv = wt_.rearrange("p (k n) -> p k n", n=512)
            for f4 in range(4):
                fc = half * 4 + f4
                ps, bps = rot.next()
                for kc in range(8):
                    cx.mm(ps, wv[:, kc, f4 * 128:(f4 + 1) * 128], hT[:, kc, :], kc == 0, kc == 7, [b_w, b_C], [bps])
                cx.cp("act", uT[:, fc, :], ps, [bps], [b_u[fc]])
        a0c = lambda: P["A0c"].unsqueeze(2).to_broadcast([128, 32, 8, 8])
        a0s = lambda: P["A0s"].unsqueeze(2).to_broadcast([128, 32, 8, 8])
        a1c = lambda: P["A1c"].unsqueeze(3).to_broadcast([128, 32, 8, 8])
        a1s = lambda: P["A1s"].unsqueeze(3).to_broadcast([128, 32, 8, 8])
        v4 = lambda t: t.rearrange("p q (a b) -> p q a b", b=8)
        SRv, SIv = SR[:, :, 0:64], SI[:, :, 0:64]
        for qb in range(4):
            psr, bpsr = rot.next()
            psi, bpsi = rot.next()
            for q8 in range(8):
                q = qb * 8 + q8
                fc, q4 = q // 4, q % 4
                rows = slice(32 * q4, 32 * q4 + 32)
                uv = uT[rows, fc, :].rearrange("p (c s) -> p c s", s=8)
                for (pp, bpp, WB, bWB) in ((psr, bpsr, P["WBre"], B["WBre"]), (psi, bpsi, P["WBim"], B["WBim"])):
                    for s in range(8):
                        cx.mm(pp[:, q8 * 64:(q8 + 1) * 64], WB[rows, fc, s, :], uv[:, :, s], s == 0, s == 7,
                              [bWB, b_u[fc]], [bpp], tile_position=(32 * q4, 0))
            qs = slice(qb * 8, (qb + 1) * 8)
            pr4 = psr.rearrange("p (q a b) -> p q a b", a=8, b=8)
            pi4 = psi.rearrange("p (q a b) -> p q a b", a=8, b=8)
            c4 = P["A0c"][:, qs, :].unsqueeze(2).to_broadcast([128, 8, 8, 8])
            s4 = P["A0s"][:, qs, :].unsqueeze(2).to_broadcast([128, 8, 8, 8])
            cx.tt("dve", v4(t3)[:, qs], pr4, c4, ALU.mult, [bpsr, B["A0c"]], [b_A])
            cx.tt("dve", v4(t4)[:, qs], pi4, s4, ALU.mult, [bpsi, B["A0s"]], [b_A])
            cx.tt("dve", v4(SRv)[:, qs], v4(t3)[:, qs], v4(t4)[:, qs], ALU.add, [b_A], [b_SR])
            cx.tt("dve", v4(t3)[:, qs], pi4, c4, ALU.mult, [bpsi, B["A0c"]], [b_A])
            cx.tt("dve", v4(t4)[:, qs], pr4, s4, ALU.mult, [bpsr, B["A0s"]], [b_A])
            cx.tt("dve", v4(SIv)[:, qs], v4(t3)[:, qs], v4(t4)[:, qs], ALU.subtract, [b_A], [b_SI])
        if hook is not None:
            hook()
        cx.tt("dve", v4(t3), v4(SRv), a1c(), ALU.mult, [b_SR, B["A1c"]], [b_A])
        cx.tt("dve", v4(t4), v4(SIv), a1s(), ALU.mult, [b_SI, B["A1s"]], [b_A])
        cx.tt("dve", v4(t1), v4(t3), v4(t4), ALU.add, [b_A], [b_B])
        cx.tt("dve", v4(t3), v4(SIv), a1c(), ALU.mult, [b_SI, B["A1c"]], [b_A])
        cx.tt("dve", v4(t4), v4(SRv), a1s(), ALU.mult, [b_SR, B["A1s"]], [b_A])
        cx.tt("dve", v4(t2), v4(t3), v4(t4), ALU.subtract, [b_A], [b_C])
        for q in range(32):
            rm = P["RM8"][:, q:q + 1].to_broadcast([128, 64])
            kb.op("dve", lambda g_, q=q, rm=rm: g_.tensor_tensor_scan(out=SRv[:, q, :], data0=rm, data1=t1[:, q, :],
                  initial=CAR[:, 0, q:q + 1], op0=ALU.mult, op1=ALU.add), [b_B, B["RM8"], b_CAR], [b_SR])
            kb.op("dve", lambda g_, q=q, rm=rm: g_.tensor_tensor_scan(out=SIv[:, q, :], data0=rm, data1=t2[:, q, :],
                  initial=CAR[:, 1, q:q + 1], op0=ALU.mult, op1=ALU.add), [b_C, B["RM8"], b_CAR], [b_SI])
        cx.tt("dve", v4(t3), v4(SRv), a1c(), ALU.mult, [b_SR, B["A1c"]], [b_A])
        cx.tt("dve", v4(t4), v4(SIv), a1s(), ALU.mult, [b_SI, B["A1s"]], [b_A])
        cx.tt("dve", v4(t1), v4(t3), v4(t4), ALU.subtract, [b_A], [b_B])
        cx.tt("dve", v4(t3), v4(SIv), a1c(), ALU.mult, [b_SI, B["A1c"]], [b_A])
        cx.tt("dve", v4(t4), v4(SRv), a1s(), ALU.mult, [b_SR, B["A1s"]], [b_A])
        cx.tt("dve", v4(t2), v4(t3), v4(t4), ALU.add, [b_A], [b_C])
        cx.cp("dve", SB[:, :, 0, 0:1], CAR[:, 0, :].unsqueeze(2), [b_CAR], [b_SB])
        cx.cp("dve", SB[:, :, 1, 0:1], CAR[:, 1, :].unsqueeze(2), [b_CAR], [b_SB])
        cx.tt("dve", v4(t3), v4(t1), a0c(), ALU.mult, [b_B, B["A0c"]], [b_A])
        cx.tt("dve", v4(t4), v4(t2), a0s(), ALU.mult, [b_C, B["A0s"]], [b_A])
        cx.tt("dve", SB[:, :, 0, 1:64], t3[:, :, 0:63], t4[:, :, 0:63], ALU.subtract, [b_A], [b_SB])
        cx.tt("dve", CAR[:, 0, :].unsqueeze(2), t3[:, :, 63:64], t4[:, :, 63:64], ALU.subtract, [b_A], [b_CAR])
        cx.tt("dve", v4(t3), v4(t2), a0c(), ALU.mult, [b_C, B["A0c"]], [b_A])
        cx.tt("dve", v4(t4), v4(t1), a0s(), ALU.mult, [b_B, B["A0s"]], [b_A])
        cx.tt("dve", SB[:, :, 1, 1:64], t3[:, :, 0:63], t4[:, :, 0:63], ALU.add, [b_A], [b_SB])
        cx.tt("dve", CAR[:, 1, :].unsqueeze(2), t3[:, :, 63:64], t4[:, :, 63:64], ALU.add, [b_A], [b_CAR])
        for fc in range(8):
            ps, bps = rot.next()
            uv = uT[:, fc, :].rearrange("p (c s) -> p c s", s=8)
            pv = ps.rearrange("p (c s) -> p c s", s=8)
            for k in range(8):
                cx.mm(pv[:, :, k:8], P["FIRW"][:, fc, k, :], uv[:, :, 0:8 - k], k == 0, False,
                      [B["FIRW"], b_u[fc]], [bps])
            for q4 in range(4):
                q = fc * 4 + q4
                rows = slice(32 * q4, 32 * q4 + 32)
                for j in range(8):
                    last = (q4 == 3 and j == 7)
                    cx.mm(pv[rows, :, j], P["WCre"][:, q, j, :, :], SB[:, q, 0, :], False, False,
                          [B["WCre"], b_SB], [bps], tile_position=(0, 32 * q4))
                    cx.mm(pv[rows, :, j], P["WCim"][:, q, j, :, :], SB[:, q, 1, :], False, last,
                          [B["WCim"], b_SB], [bps], tile_position=(0, 32 * q4))
            yv = y32[:, fc, :]
            g1 = sgs[fc % 2]
            cx.act(g1, ps, AF.Square, [bps], [b_B])
            cx.ts("dve", g1, g1, GELU_C, 1.0, ALU.mult, ALU.add, [b_B], [b_B])
            cx.tt("dve", g1, g1, ps, ALU.mult, [b_B, bps], [b_B])
            cx.act(g1, g1, AF.Sigmoid, [b_B], [b_B], scale=GELU_S)
            cx.tt("dve", yv, g1, ps, ALU.mult, [b_B, bps], [b_A])
            cx.cp("act", yT[:, fc, :], yv, [b_A], [b_y[fc]])
        for half in range(2):
            wt_, b_w = ws.get()
            wv = wt_.rearrange("p (k n) -> p k n", n=512)
            for f4 in range(4):
                fc = half * 4 + f4
                ps, bps = rot.next()
                for kc in range(8):
                    cx.mm(ps, wv[:, kc, f4 * 128:(f4 + 1) * 128], yT[:, kc, :], kc == 0, kc == 7, [b_w, b_y[kc]], [bps])
                g1 = sgs[fc % 2]
                cx.act(g1, ps, AF.Sigmoid, [bps], [b_B])
                cx.tt("dve", zT[:, fc, :], y32[:, fc, :], g1, ALU.mult, [b_A, b_B], [b_z[fc]])
        for half in range(2):
            wt_, b_w = ws.get()
            wv = wt_.rearrange("p (k n) -> p k n", n=512)
            for b in range(4):
                ps, bps = rot.next()
                for kc in range(8):
                    cx.mm(ps, zT[:, kc, b * 128:(b + 1) * 128], wv[:, kc, :], kc == 0, kc == 7, [b_z[kc], b_w], [bps])
                xv = xt[:, b, half * 512:(half + 1) * 512]
                cx.tt("dve", xv, xv, ps, ALU.add, [b_xt[b * 2 + half], bps], [b_xt[b * 2 + half]])
        rms_to_hT(cx, P, xt, b_xt, 4, gT[:, 1, :], b_gT, htok, b_B, hT, b_C, ss, b_ss, [0, 1])
        for f in range(D_FF // 128):
            wt_, b_w = ws.get()
            gv = wt_[:, 0:1024].rearrange("p (k n) -> p k n", n=128)
            uvw = wt_[:, 1024:2048].rearrange("p (k n) -> p k n", n=128)
            dv = wt_[:, 2048:3072].rearrange("p (a d) -> p a d", d=1024)
            pg, bpg = rot.next()
            pu, bpu = rot.next()
            for kc in range(8):
                cx.mm(pg, gv[:, kc, :], hT[:, kc, :], kc == 0, kc == 7, [b_w, b_C], [bpg])
            for kc in range(8):
                cx.mm(pu, uvw[:, kc, :], hT[:, kc, :], kc == 0, kc == 7, [b_w, b_C], [bpu])
            g1 = sgs[f % 2]
            a1 = acts[f % 4]
            b_a = b_y[(f % 4)]
            cx.act(g1, pg, AF.Silu, [bpg], [b_B])
            cx.tt("dve", a1, g1, pu, ALU.mult, [b_B, bpu], [b_a])
            for b in range(4):
                for half in range(2):
                    ps, bps = rot.next()
                    cx.mm(ps, a1[:, b * 128:(b + 1) * 128], dv[:, 0, half * 512:(half + 1) * 512], True, True,
                          [b_a, b_w], [bps])
                    xv = xt[:, b, half * 512:(half + 1) * 512]
                    bx = b_xt[b * 2 + half]
                    if (b * 2 + half) % 2 == 0:
                        cx.tt("dve", xv, xv, ps, ALU.add, [bx, bps], [bx])
                    else:
                        tb, b_tb = ytmp[nt % 4]
                        nt += 1
                        cx.cp("act", tb, ps, [bps], [b_tb])
                        cx.tt("pool", xv, xv, tb, ALU.add, [bx, b_tb], [bx])
        kb.dma("sp", xs[t0:t0 + 512, :].rearrange("(b p) d -> p b d", p=128), xt, reads=b_xt, writes=[b_xs], disjoint=True)
    cx.release(m)


IN_SPECS = {
    "x": ([L, D], F32), "pos": ([1, L], I32),
    "ident": ([128, 128], F32), "bmask": ([128, 128], F32), "ev": ([128, 9 * 32], F32),
    "ltri": ([128, 128], F32), "wtab": ([128, NSLAB * NE], F32), "ctab": ([128, 7], F32),
    "invf_t": ([128, 16], F32), "invf_f": ([128, 1], F32), "esel": ([128, 31], F32), "dmask": ([128, 128], F32),
    "lamT_re": ([128, 32], F32), "lamT_im": ([128, 32], F32), "ldtT": ([128, 32], F32),
    "bT_re": ([128, 512], F32), "bT_im": ([128, 512], F32),
    "cT_re": ([128, 512], F32), "cT_im": ([128, 512], F32), "dT": ([128, 8], F32),
    "gT_mix0": ([128, 8], F32), "gT_ffn0": ([128, 8], F32), "gT_kv": ([128, 8], F32),
    "gT_mix1": ([128, 8], F32), "gT_ffn1": ([128, 8], F32), "g_final": ([D], F32),
    "g_kvlat": ([KV_LORA], F32), "g_qlat": ([Q_LORA], F32),
    "s5_w_in": ([D, D], F32), "s5_w_glu": ([D, D], F32), "s5_w_out": ([D, D], F32),
    "ffn_w_gate": ([D, D_FF], F32), "ffn_w_up": ([D, D_FF], F32), "ffn_w_down": ([D_FF, D], F32),
    "w_dkv": ([D, KV_LORA + QK_ROPE], F32), "w_ukv": ([KV_LORA, NH * 128], F32),
    "w_dq": ([D, Q_LORA], F32), "w_uq": ([Q_LORA, NH * 96], F32), "w_o": ([NH * V_HEAD, D], F32),
    "router_w": ([D, NE], F32), "router_wT": ([NE, D], F32), "g_ffn1": ([D], F32), "sel": ([8, NE * 128], F32),
    "moe_w_gate": ([NE, D, MOE_FF], F32), "moe_w_up": ([NE, D, MOE_FF], F32),
    "moe_w_down": ([NE, MOE_FF, D], F32),
}


def host_consts():
    c = {}
    c["ident"] = np.eye(128, dtype=np.float32)
    blk = np.arange(128) // 16
    c["bmask"] = (blk[:, None] == blk[None, :]).astype(np.float32)
    c["ev"] = np.broadcast_to(np.arange(9, dtype=np.float32)[None, :, None], (128, 9, 32)).reshape(128, 288).copy()
    invf = (np.float32(10000.0) ** (-np.arange(16, dtype=np.float32) * np.float32(2.0 / QK_ROPE))).astype(np.float32)
    c["invf_t"] = np.broadcast_to(invf[None, :], (128, 16)).copy()
    ff = np.zeros((128, 1), np.float32)
    ff[64:80, 0] = invf
    ff[80:96, 0] = invf
    c["invf_f"] = ff
    es = np.zeros((128, 31), np.float32)
    es[:, 15] = 1.0
    c["esel"] = es
    kk = np.arange(128)[:, None]
    qq = np.arange(128)[None, :]
    c["dmask"] = ((kk < 64) | (qq >= 64)).astype(np.float32)
    se = np.zeros((8, NE, 128), np.float32)
    for e in range(NE):
        se[e, e, :] = 1.0
    c["sel"] = se.reshape(8, NE * 128)
    c["ltri"] = (np.arange(128)[:, None] < np.arange(128)[None, :]).astype(np.float32)
    c["wtab"] = np.broadcast_to(np.arange(NSLAB, dtype=np.float32)[None, :, None], (128, NSLAB, NE)).reshape(128, NSLAB * NE).copy()
    c["ctab"] = (np.arange(7, dtype=np.float32)[None, :] * 128.0 + np.arange(128, dtype=np.float32)[:, None]).copy()
    return c


def _gT(g):
    return np.ascontiguousarray(g.reshape(8, 128).T)


def host_shared(inp):
    f = lambda a: np.ascontiguousarray(a, dtype=np.float32)
    pair = lambda a: f(a.reshape(32, 2, 64).transpose(1, 2, 0).reshape(128, 32))
    s = dict(host_consts())
    s["lamT_re"] = pair(inp["s5_lambda_re"][0])
    s["lamT_im"] = pair(inp["s5_lambda_im"][0])
    s["ldtT"] = pair(np.broadcast_to(inp["s5_log_dt"][0][:, None], (64, 64)))
    bt = lambda b: f(b.reshape(32, 2, 64, 16).transpose(1, 2, 0, 3).reshape(128, 512))
    ct = lambda c: f(c.reshape(32, 2, 16, 64).transpose(1, 3, 0, 2).reshape(128, 512))
    s["bT_re"], s["bT_im"] = bt(inp["s5_b_re"][0]), bt(inp["s5_b_im"][0])
    s["cT_re"], s["cT_im"] = ct(inp["s5_c_re"][0]), ct(inp["s5_c_im"][0])
    s["dT"] = _gT(f(inp["s5_d"][0]))
    s["gT_mix0"], s["gT_mix1"] = _gT(f(inp["norm_mix"][0])), _gT(f(inp["norm_mix"][1]))
    s["gT_ffn0"], s["gT_ffn1"] = _gT(f(inp["norm_ffn"][0])), _gT(f(inp["norm_ffn"][1]))
    s["gT_kv"] = _gT(f(inp["kv_norm"]))
    s["g_final"] = f(inp["final_norm"])
    s["g_kvlat"] = f(inp["kv_latent_norm"])
    s["g_qlat"] = f(inp["q_latent_norm"][0])
    for k in ("s5_w_in", "s5_w_glu", "s5_w_out", "ffn_w_gate", "ffn_w_up", "ffn_w_down",
              "w_dq", "w_uq", "w_o", "moe_w_gate", "moe_w_up", "moe_w_down"):
        s[k] = f(inp[k][0])
    s["router_wT"] = f(inp["router_w"][0].T)
    s["router_w"] = f(inp["router_w"][0])
    s["g_ffn1"] = f(inp["norm_ffn"][1])
    s["w_dkv"] = f(inp["w_dkv"])
    s["w_ukv"] = f(inp["w_ukv"])
    return s


def declare_inputs(nc, names):
    din = {}
    for k in names:
        shape, dt = IN_SPECS[k]
        din[k] = nc.dram_tensor(k, list(shape), dt, kind="ExternalInput").ap()
    return din


ATT_SCALE = (QK_NOPE + QK_ROPE) ** -0.5
KMAX_MARGIN = 1.02


def range_reduce_sin(cx, out, ang, it, b_out, b_ang, b_it, shift):
    cx.ts("dve", out, ang, 1.0 / (2.0 * PI), shift / (2.0 * PI), ALU.mult, ALU.add, [b_ang], [b_out])
    cx.cp("dve", it, out, [b_out], [b_it])
    cx.cp("dve", out, it, [b_it], [b_out])
    cx.stt("dve", out, out, -2.0 * PI, ang, ALU.mult, ALU.add, [b_out, b_ang], [b_out])
    cx.ts("dve", out, out, shift, -PI, ALU.add, ALU.max, [b_out], [b_out])
    cx.ts("dve", out, out, PI, None, ALU.min, None, [b_out], [b_out])
    cx.act(out, out, AF.Sin, [b_out], [b_out])


def phase15(cx, P, din, xs, b_xs, KT, b_KT, QT, b_QT, VS, b_VS, ntiles=8, hook=None):
    nc, kb = cx.nc, cx.kb
    B = P["bufs"]
    identb, b_identb = P["identb"], B["identb"]
    m = cx.mark()
    xts = [cx.sb(f"xt{i}", [128, 4, 1024], F32) for i in range(2)]
    htok, b_htok = cx.sb("htok", [128, 4, 1024], BF16)
    hT, b_hT = cx.sb("hT", [128, 8, 512], BF16)
    ss, b_ss = cx.sb("ss", [128, 8], F32)
    gT, b_gT = cx.sb("gT", [128, 2, 8], F32)
    wdkv, b_wdkv = cx.sb("wdkv", [128, 8, 288], BF16)
    wukv, b_wukv = cx.sb("wukv", [128, 2, 2048], BF16)
    wdq, b_wdq = cx.sb("wdq", [128, 8, 512], BF16)
    wuq, b_wuq = cx.sb("wuq", [128, 4, 1536], BF16)
    wrot, b_wrot = cx.sb("wrot", [128, 4, 16, 32], BF16)
    gkv, b_gkv = cx.sb("gkv", [128, 256], F32)
    gq, b_gq = cx.sb("gq", [128, 512], F32)
    invf_t, b_invf_t = cx.sb("invf_t", [128, 16], F32)
    invf_f, b_invf_f = cx.sb("invf_f", [128, 1], F32)
    esel, b_esel = cx.sb("esel", [128, 31], BF16)
    eself, b_eself = cx.sb("eself", [128, 31], F32)
    posis = [cx.sb(f"posi{i}", [128, 512], I32) for i in range(2)]
    posf, b_posf = cx.sb("posf", [128, 512], F32)
    ptok_is = [cx.sb(f"ptok_i{i}", [128, 4], I32) for i in range(2)]
    ptok, b_ptok = cx.sb("ptok", [128, 4], F32)
    angf, b_angf = cx.sb("angf", [128, 512], F32)
    cosf, b_cosf = cx.sb("cosf", [128, 512], F32)
    sinf, b_sinf = cx.sb("sinf", [128, 512], F32)
    itf, b_itf = cx.sb("itf", [128, 512], I32)
    angt, b_angt = cx.sb("angt", [128, 16], F32)
    cost, b_cost = cx.sb("cost", [128, 16], F32)
    sint, b_sint = cx.sb("sint", [128, 16], F32)
    itt, b_itt = cx.sb("itt", [128, 16], I32)
    ckvn, b_ckvn = cx.sb("ckvn", [128, 256], BF16)
    kro, b_kro = cx.sb("kro", [128, 32], BF16)
    krt, b_krt = cx.sb("krt", [128, 4, 16], F32)
    ckvT, b_ckvT = cx.sb("ckvT", [128, 2, 512], BF16)
    krT, b_krT = cx.sb("krT", [128, 512], BF16)
    cqn, b_cqn = cx.sb("cqn", [128, 512], BF16)
    cqnT, b_cqnT = cx.sb("cqnT", [128, 4, 512], BF16)
    KTt, b_KTt = cx.sb("KTt", [128, 16, 512], BF16)
    QTt, b_QTt = cx.sb("QTt", [128, 16, 512], BF16)
    SQ, b_SQ = cx.sb("SQ", [128, 16, 512], BF16)
    VA, b_VA = cx.sb("VA", [128, 16, 4, 65], BF16)
    nrm, b_nrm = cx.sb("nrm", [16, 512], F32)
    nrmb, b_nrmb = cx.sb("nrmb", [16, 512], BF16)
    rmax, b_rmax = cx.sb("rmax", [16, 2], F32)
    KM, b_KM = cx.sb("KM", [16, 4096], BF16)
    t96a, b_t96a = cx.sb("t96a", [128, 512], F32)
    t96b, b_t96b = cx.sb("t96b", [128, 512], F32)
    rot = PsRot(cx, [2, 3, 4, 5, 6, 7])

    kb.dma("sp", gT[:, 0, :], din["gT_kv"], writes=[b_gT])
    kb.dma("sp", gT[:, 1, :], din["gT_mix1"], writes=[b_gT])
    kb.dma("sp", gkv, din["g_kvlat"].partition_broadcast(128), writes=[b_gkv])
    kb.dma("sp", gq, din["g_qlat"].partition_broadcast(128), writes=[b_gq])
    kb.dma("sp", invf_t, din["invf_t"], writes=[b_invf_t])
    kb.dma("sp", invf_f, din["invf_f"], writes=[b_invf_f])
    kb.dma("sp", eself, din["esel"], writes=[b_eself])
    cx.cp("dve", esel, eself, [b_eself], [b_esel])
    kb.dma("pool", wdkv, din["w_dkv"].rearrange("(k p) n -> p k n", p=128), writes=[b_wdkv])
    kb.dma("pool", wukv, din["w_ukv"].rearrange("(k p) n -> p k n", p=128), writes=[b_wukv])
    kb.dma("pool", wdq, din["w_dq"].rearrange("(k p) n -> p k n", p=128), writes=[b_wdq])
    kb.dma("pool", wuq, din["w_uq"].rearrange("(k p) n -> p k n", p=128), writes=[b_wuq])
    wuq4 = wuq.rearrange("p k (h c) -> p k h c", c=96)
    for kc in range(4):
        cx.ts("dve", wrot[:, kc, :, 0:16], wuq4[:, kc, :, 80:96], -1.0, None, ALU.mult, None, [b_wuq], [b_wrot])
        cx.cp("dve", wrot[:, kc, :, 16:32], wuq4[:, kc, :, 64:80], [b_wuq], [b_wrot])
    cx.memset("dve", rmax, 0.0, [b_rmax])
    cx.memset("dve", VA, 1.0, [b_VA])
    cx.memset("pool", kro, 0.0, [b_kro])

    def head_norms(Tt, b_Tt, dst_row96, b_dst, t0, is_k):
        cx.act(SQ[0:96], Tt[0:96], AF.Square, [b_Tt], [b_SQ])
        ps, bps = rot.next()
        for h in range(16):
            cx.mm(ps[0:16, :], esel[0:96, 15 - h:31 - h], SQ[0:96, h, :], h == 0, h == 15, [b_esel, b_SQ], [bps])
        if is_k:
            cx.kb.op("dve", lambda g: g.reduce_max(out=rmax[:, 1:2], in_=ps[0:16, :], axis=AX.X), [bps], [b_rmax])
            cx.tt("dve", rmax[:, 0:1], rmax[:, 0:1], rmax[:, 1:2], ALU.max, [b_rmax], [b_rmax])
        else:
            cx.act(nrm, ps[0:16, :], AF.Sqrt, [bps], [b_nrm])
            cx.ts("dve", nrmb, nrm, -1.0, None, ALU.mult, None, [b_nrm], [b_nrmb])
            kb.dma("sp", dst_row96[:, t0:t0 + 512], nrmb, reads=[b_nrmb], writes=[b_dst])

    def load_tile(TT):
        tt0 = TT * 512
        xt_, b_xt_ = xts[TT % 2]
        posi_, b_posi_ = posis[TT % 2]
        ptok_i_, b_ptok_i_ = ptok_is[TT % 2]
        kb.dma("sp", xt_, xs[tt0:tt0 + 512, :].rearrange("(b p) d -> p b d", p=128), reads=[b_xs], writes=[b_xt_])
        kb.dma("sp", posi_, din["pos"][0, tt0:tt0 + 512].partition_broadcast(128), writes=[b_posi_])
        kb.dma("sp", ptok_i_, din["pos"][0, tt0:tt0 + 512].rearrange("(b p) -> p b", p=128), writes=[b_ptok_i_],
               allow_slow_non_contiguous=True)

    load_tile(0)
    for T in range(ntiles):
        t0 = T * 512
        xt, b_xt = xts[T % 2]
        posi, b_posi = posis[T % 2]
        ptok_i, b_ptok_i = ptok_is[T % 2]
        if T + 1 < ntiles:
            load_tile(T + 1)
        cx.cp("dve", posf, posi, [b_posi], [b_posf])
        cx.cp("dve", ptok, ptok_i, [b_ptok_i], [b_ptok])
        cx.ts("dve", angf, posf, invf_f[:, 0:1], None, ALU.mult, None, [b_posf, b_invf_f], [b_angf])
        range_reduce_sin(cx, sinf, angf, itf, b_sinf, b_angf, b_itf, 0.0)
        range_reduce_sin(cx, cosf, angf, itf, b_cosf, b_angf, b_itf, 0.5 * PI)
        cx.ts("dve", sinf, sinf, ATT_SCALE, None, ALU.mult, None, [b_sinf], [b_sinf])
        cx.ts("dve", cosf, cosf, ATT_SCALE, None, ALU.mult, None, [b_cosf], [b_cosf])

        rms_to_hT(cx, P, xt, b_xt, 4, gT[:, 0, :], b_gT, htok, b_htok, hT, b_hT, ss, b_ss, [0, 1])
        for b in range(4):
            ps, bps = rot.next()
            for kc in range(8):
                cx.mm(ps[:, 0:288], hT[:, kc, b * 128:(b + 1) * 128], wdkv[:, kc, :], kc == 0, kc == 7, [b_hT, b_wdkv], [bps])
            cx.act(ckvn, ps[:, 0:256], AF.Square, [bps], [b_ckvn, b_ss], accum=ss[:, 4:5])
            cx.ts("dve", ss[:, 4:5], ss[:, 4:5], 1.0 / KV_LORA, EPS, ALU.mult, ALU.add, [b_ss], [b_ss])
            cx.act(ss[:, 4:5], ss[:, 4:5], AF.Sqrt, [b_ss], [b_ss])
            cx.kb.op("dve", lambda g: g.reciprocal(out=ss[:, 4:5], in_=ss[:, 4:5]), [b_ss], [b_ss])
            cx.stt("dve", ckvn, ps[:, 0:256], ss[:, 4:5], gkv, ALU.mult, ALU.mult, [bps, b_ss, b_gkv], [b_ckvn])
            cx.ts("dve", angt, invf_t, ptok[:, b:b + 1], None, ALU.mult, None, [b_invf_t, b_ptok], [b_angt])
            range_reduce_sin(cx, sint, angt, itt, b_sint, b_angt, b_itt, 0.0)
            range_reduce_sin(cx, cost, angt, itt, b_cost, b_angt, b_itt, 0.5 * PI)
            x1, x2 = ps[:, 256:272], ps[:, 272:288]
            cx.tt("dve", krt[:, 0, :], x1, cost, ALU.mult, [bps, b_cost], [b_krt])
            cx.tt("dve", krt[:, 1, :], x2, sint, ALU.mult, [bps, b_sint], [b_krt])
            cx.tt("dve", krt[:, 2, :], x2, cost, ALU.mult, [bps, b_cost], [b_krt])
            cx.tt("dve", krt[:, 3, :], x1, sint, ALU.mult, [bps, b_sint], [b_krt])
            cx.tt("dve", kro[:, 0:16], krt[:, 0, :], krt[:, 1, :], ALU.subtract, [b_krt], [b_kro])
            cx.tt("dve", kro[:, 16:32], krt[:, 2, :], krt[:, 3, :], ALU.add, [b_krt], [b_kro])
            p16 = cx.ps[b % 2].bitcast(BF16).rearrange("p (k c) -> p k c", c=128)
            bp16 = cx.psb[b % 2]
            for k2 in range(2):
                cx.tr(p16[:, k2, :], ckvn[:, k2 * 128:(k2 + 1) * 128], identb, [b_ckvn, b_identb], [bp16])
            cx.tr(p16[0:32, 2, :], kro, identb, [b_kro, b_identb], [bp16])
            cx.cp("act", ckvT[:, :, b * 128:(b + 1) * 128], p16[:, 0:2, :], [bp16], [b_ckvT])
            cx.cp("dve", krT[64:96, b * 128:(b + 1) * 128], p16[0:32, 2, :], [bp16], [b_krT])
            w4 = wukv.rearrange("p k (h c) -> p k h c", c=128)
            for hh in range(2):
                pv_, bpv = rot.next()
                for k2 in range(2):
                    cx.mm(pv_, ckvT[:, k2, b * 128:(b + 1) * 128], w4[:, k2, hh * 8:(hh + 1) * 8, 64:128],
                          k2 == 0, k2 == 1, [b_ckvT, b_wukv], [bpv])
                cx.cp("act" if hh else "dve", VA[:, hh * 8:(hh + 1) * 8, b, 0:64],
                      pv_.rearrange("p (h c) -> p h c", c=64), [bpv], [b_VA])
        if hook is not None:
            hook()
        for h in range(16):
            ps, bps = rot.next()
            for k2 in range(2):
                cx.mm(ps[0:64, :], wukv[:, k2, h * 128:h * 128 + 64], ckvT[:, k2, :], k2 == 0, k2 == 1, [b_wukv, b_ckvT], [bps])
            cx.cp("act" if h % 2 else "dve", KTt[0:64, h, :], ps[0:64, :], [bps], [b_KTt])
        cx.cp("dve", KTt[64:96, :, :], krT[64:96, :].unsqueeze(1).to_broadcast([32, 16, 512]), [b_krT], [b_KTt])
        kb.dma("sp", KT[:, 0:96, t0:t0 + 512].rearrange("h r t -> r h t"), KTt[0:96], reads=[b_KTt], writes=[b_KT])
        head_norms(KTt, b_KTt, None, None, t0, True)
        kb.dma("sp", VS[:, :, 4 * T:4 * T + 4, :].rearrange("h p b e -> p h b e"), VA, reads=[b_VA], writes=[b_VS])

        if hook is not None:
            hook()
        rms_to_hT(cx, P, xt, b_xt, 4, gT[:, 1, :], b_gT, htok, b_htok, hT, b_hT, ss, b_ss, [0, 1])
        for b in range(4):
            ps, bps = rot.next()
            for kc in range(8):
                cx.mm(ps, hT[:, kc, b * 128:(b + 1) * 128], wdq[:, kc, :], kc == 0, kc == 7, [b_hT, b_wdq], [bps])
            cx.act(cqn, ps, AF.Square, [bps], [b_cqn, b_ss], accum=ss[:, 5:6])
            cx.ts("dve", ss[:, 5:6], ss[:, 5:6], 1.0 / Q_LORA, EPS, ALU.mult, ALU.add, [b_ss], [b_ss])
            cx.act(ss[:, 5:6], ss[:, 5:6], AF.Sqrt, [b_ss], [b_ss])
            cx.kb.op("dve", lambda g: g.reciprocal(out=ss[:, 5:6], in_=ss[:, 5:6]), [b_ss], [b_ss])
            cx.stt("dve", cqn, ps, ss[:, 5:6], gq, ALU.mult, ALU.mult, [bps, b_ss, b_gq], [b_cqn])
            p16 = cx.ps[b % 2].bitcast(BF16).rearrange("p (k c) -> p k c", c=128)
            bp16 = cx.psb[b % 2]
            for k4 in range(4):
                cx.tr(p16[:, k4, :], cqn[:, k4 * 128:(k4 + 1) * 128], identb, [b_cqn, b_identb], [bp16])
            cx.cp("act", cqnT[:, :, b * 128:(b + 1) * 128], p16[:, 0:4, :], [bp16], [b_cqnT])
        if hook is not None:
            hook()
        for h in range(16):
            pa, bpa = rot.next()
            pb_, bpb = rot.next()
            for k4 in range(4):
                cx.mm(pa[0:96, :], wuq[:, k4, h * 96:(h + 1) * 96], cqnT[:, k4, :], k4 == 0, k4 == 3, [b_wuq, b_cqnT], [bpa])
            for k4 in range(4):
                cx.mm(pb_[64:96, :], wrot[:, k4, h, :], cqnT[:, k4, :], k4 == 0, k4 == 3, [b_wrot, b_cqnT], [bpb],
                      tile_position=(0, 64))
            cx.act(QTt[0:64, h, :], pa[0:64, :], AF.Copy, [bpa], [b_QTt], scale=ATT_SCALE)
            cx.tt("dve", t96a[64:96], pa[64:96, :], cosf[64:96], ALU.mult, [bpa, b_cosf], [b_t96a])
            cx.tt("dve", t96b[64:96], pb_[64:96, :], sinf[64:96], ALU.mult, [bpb, b_sinf], [b_t96b])
            cx.tt("pool", QTt[64:96, h, :], t96a[64:96], t96b[64:96], ALU.add, [b_t96a, b_t96b], [b_QTt])
        kb.dma("sp", QT[:, 0:96, t0:t0 + 512].rearrange("h r t -> r h t"), QTt[0:96], reads=[b_QTt], writes=[b_QT])
        if hook is not None:
            hook()
        head_norms(QTt, b_QTt, QT[:, 96, :], b_QT, t0, False)
    cx.act(rmax[:, 1:2], rmax[:, 0:1], AF.Sqrt, [b_rmax], [b_rmax])
    cx.memset("pool", KM, 0.0, [b_KM])
    cx.ts("dve", KM, KM, rmax[:, 1:2], KMAX_MARGIN, ALU.add, ALU.mult, [b_KM, b_rmax], [b_KM])
    kb.dma("sp", KT[:, 96, :], KM, reads=[b_KM], writes=[b_KT])
    cx.release(m)


def phase2(cx, P, din, KT, b_KT, QT, b_QT, VS, b_VS, OT, b_OT, nheads=16, ngroups=8, hook=None):
    nc, kb = cx.nc, cx.kb
    m = cx.mark()
    KTh, QTh, VAh, b_KTh, b_QTh, b_VAh = [], [], [], [], [], []
    for i in range(2):
        t, b = cx.sb(f"KTh{i}", [128, 4096], BF16); KTh.append(t); b_KTh.append(b)
        t, b = cx.sb(f"QTh{i}", [128, 4096], BF16); QTh.append(t); b_QTh.append(b)
        t, b = cx.sb(f"VAh{i}", [128, 32, 65], BF16); VAh.append(t); b_VAh.append(b)
    PT, b_PT = [], []
    for i in range(2):
        t, _ = cx.sb(f"PT{i}", [128, 32, 512], BF16)
        PT.append(t)
        b_PT.append(kb.bufs_n(f"PT{i}_", 32))
    dmask, b_dmask = cx.sb("dmask", [128, 128], F32)
    onesr, b_onesr = cx.sb("onesr", [128, 64], F32)
    R, b_R = cx.sb("R", [128, 512], F32)
    RB, b_RB = cx.sb("RB", [128, 512], F32)
    OTs, b_OTs = [], []
    for i in range(2):
        t, b = cx.sb(f"OTs{i}", [128, 4096], BF16); OTs.append(t); b_OTs.append(b)
    kb.dma("sp", dmask, din["dmask"], writes=[b_dmask])
    cx.memset("dve", onesr, 1.0, [b_onesr])
    rot = PsRot(cx, [0, 1, 2, 3, 4])
    rot_o = PsRot(cx, [5, 6])

    def emit_pv(st, j):
        nkt = st["nkt"]
        c0 = max(0, j - 4 * st["G"]) * 128
        s_ = st["s"]
        cx.mm(st["po"][0:65, c0:512], VAh[s_][:, j, :], st["pt"][:, j, c0:512], j == 0, j == nkt - 1,
              [b_VAh[s_], st["b_pt"][j]], [st["bpo"]])

    def emit_fin(st):
        po, bpo, s_, G = st["po"], st["bpo"], st["s"], st["G"]
        ot, b_ot = st["ot"], st["b_ot"]
        cx.kb.op("dve", lambda g: g.reciprocal(out=R[64:65, :], in_=po[64:65, :]), [bpo], [b_R])
        pb_, bpb = cx.ps[7], cx.psb[7]
        cx.mm(pb_[0:64, :], onesr[64:65, :], R[64:65, :], True, True, [b_onesr, b_R], [bpb])
        cx.cp("dve", RB[0:64, :], pb_[0:64, :], [bpb], [b_RB])
        dst = ot[64 * s_:64 * s_ + 64, G * 512:(G + 1) * 512]
        cx.tt("dve", dst, po[0:64, :], RB[0:64, :], ALU.mult, [bpo, b_RB], [b_ot])
        if st["store"] is not None:
            kb.dma("sp", OT[st["store"]], ot, reads=[b_ot], writes=[b_OT])

    prev = None
    fin_q = None
    gi = 0
    def load_head(hh):
        ss_ = hh % 2
        kb.dma("sp", KTh[ss_][0:97, :], KT[hh], reads=[b_KT], writes=[b_KTh[ss_]])
        kb.dma("sp", QTh[ss_][0:97, :], QT[hh], reads=[b_QT], writes=[b_QTh[ss_]])
        kb.dma("sp", VAh[ss_], VS[hh], reads=[b_VS], writes=[b_VAh[ss_]])

    load_head(0)
    for h in range(nheads):
        s = h % 2
        pr = h // 2
        ot, b_ot = OTs[pr % 2], b_OTs[pr % 2]
        for G in range(ngroups):
            if G == 1 and h + 1 < nheads:
                load_head(h + 1)
            if hook is not None:
                hook()
            pt, b_pt = PT[gi % 2], b_PT[gi % 2]
            gi += 1
            nkt = 4 * G + 4
            if prev is not None:
                prev["po"], prev["bpo"] = rot_o.next()
            npv = prev["nkt"] if prev is not None else 0
            for j in range(max(nkt, npv)):
                if j < nkt:
                    c0 = max(0, j - 4 * G) * 128
                    ps, bps = rot.next()
                    cx.mm(ps[:, c0:512], KTh[s][0:97, j * 128:(j + 1) * 128], QTh[s][0:97, G * 512 + c0:(G + 1) * 512],
                          True, True, [b_KTh[s], b_QTh[s]], [bps])
                    cx.act(pt[:, j, c0:512], ps[:, c0:512], AF.Exp, [bps], [b_pt[j]])
                    if j >= 4 * G:
                        cx.tt("pool", pt[:, j, c0:c0 + 128], pt[:, j, c0:c0 + 128], dmask, ALU.mult, [b_pt[j], b_dmask], [b_pt[j]])
                if j < npv:
                    emit_pv(prev, j)
                if j == 1 and fin_q is not None:
                    emit_fin(fin_q)
                    fin_q = None
            if fin_q is not None:
                emit_fin(fin_q)
                fin_q = None
            fin_q = prev
            last_of_pair = (G == ngroups - 1) and (s == 1 or h == nheads - 1)
            prev = dict(pt=pt, b_pt=b_pt, nkt=nkt, G=G, s=s, ot=ot, b_ot=b_ot, store=(pr if last_of_pair else None))
    if prev is not None:
        prev["po"], prev["bpo"] = rot_o.next()
        for j in range(prev["nkt"]):
            emit_pv(prev, j)
    if fin_q is not None:
        emit_fin(fin_q)
    if prev is not None:
        emit_fin(prev)
    cx.release(m)


def phase3a(cx, P, din, xs, b_xs, OT, b_OT, out, b_out, ntiles=8):
    nc, kb = cx.nc, cx.kb
    m = cx.mark()
    xt, b_xt = cx.sb("xt", [128, 4, 1024], F32)
    ot, b_ot = cx.sb("ot", [128, 8, 512], BF16)
    wo, b_wo = cx.sb("wo", [128, 8, 1024], BF16)
    kb.dma("pool", wo, din["w_o"].rearrange("(k p) n -> p k n", p=128), writes=[b_wo])
    rot = PsRot(cx, [0, 1, 2, 3])
    for T in range(ntiles):
        t0 = T * 512
        kb.dma("sp", xt, xs[t0:t0 + 512, :].rearrange("(b p) d -> p b d", p=128), reads=[b_xs], writes=[b_xt])
        kb.dma("sp", ot, OT[:, :, t0:t0 + 512].rearrange("k p t -> p k t"), reads=[b_OT], writes=[b_ot])
        for b in range(4):
            for half in range(2):
                ps, bps = rot.next()
                for k in range(8):
                    cx.mm(ps, ot[:, k, b * 128:(b + 1) * 128], wo[:, k, half * 512:(half + 1) * 512], k == 0, k == 7, [b_ot, b_wo], [bps])
                xv = xt[:, b, half * 512:(half + 1) * 512]
                cx.tt("dve", xv, xv, ps, ALU.add, [b_xt, bps], [b_xt])
        kb.dma("sp", out[t0:t0 + 512, :].rearrange("(b p) d -> p b d", p=128), xt, reads=[b_xt], writes=[b_out])
    cx.release(m)


def phase3(cx, P, din, xs, b_xs, OT, b_OT, out, b_out, ntiles=4, nexp=NE):
    nc, kb = cx.nc, cx.kb
    B = P["bufs"]
    identf, b_identf = P["identf"], B["identf"]
    m = cx.mark()
    NBK = 8
    xt, b_xt = cx.sb("xt", [128, NBK, 1024], F32)
    htok, b_htok = cx.sb("htok", [128, NBK, 1024], BF16)
    hT, b_hT = cx.sb("hT", [128, 8, 1024], BF16)
    wo, b_wo = cx.sb("wo", [128, 8, 1024], BF16)
    rwg, b_rwg = cx.sb("rwg", [128, NE, 1024], F32)
    gfin, b_gfin = cx.sb("gfin", [128, 1024], F32)
    gT, b_gT = cx.sb("gT", [128, 8], F32)
    ss, b_ss = cx.sb("ss", [128, 16], F32)
    lg, b_lg = cx.sb("lg", [128, NBK, 8], F32)
    m8, b_m8 = cx.sb("m8", [128, 8], F32)
    gts, b_gts = cx.sb("gts", [128, NBK, 8], F32)
    gsum, b_gsum = cx.sb("gsum", [128, 2], F32)
    g8T, b_g8T = cx.sb("g8T", [8, 1024], F32)
    sel, b_sel = cx.sb("sel", [8, NE, 128], F32)
    gbc, b_gbc = cx.sb("gbc", [128, 1024], F32)
    sg = []
    b_sg = []
    tg = []
    b_tg = []
    for i in range(2):
        t, b = cx.sb(f"sg{i}", [128, 512], F32); sg.append(t); b_sg.append(b)
        t, b = cx.sb(f"tg{i}", [128, 512], F32); tg.append(t); b_tg.append(b)
    actT = []
    b_actT = []
    for i in range(2):
        t, b = cx.sb(f"actT{i}", [128, 4, 1024], BF16); actT.append(t); b_actT.append(b)
    junk, b_junk = cx.sb("junk", [128, 1024], BF16)
    ws = WStream(cx, 2, 3 * 4096, name="wm")
    rot = PsRot(cx, [2, 3, 4, 5, 6, 7])
    ot = htok

    kb.dma("pool", wo, din["w_o"].rearrange("(k p) n -> p k n", p=128), writes=[b_wo])
    kb.dma("sp", gfin, din["g_final"].partition_broadcast(128), writes=[b_gfin])
    kb.dma("sp", gT, din["gT_ffn1"], writes=[b_gT])
    kb.dma("sp", gbc, din["g_ffn1"].partition_broadcast(128), writes=[b_gbc])
    kb.dma("sp", sel, din["sel"].rearrange("k (e m) -> k e m", m=128), writes=[b_sel])
    for e in range(NE):
        kb.dma("sp", rwg[:, e, :], din["router_wT"][e].partition_broadcast(128), writes=[b_rwg])
    for e in range(NE):
        cx.tt("pool", rwg[:, e, :], rwg[:, e, :], gbc, ALU.mult, [b_rwg, b_gbc], [b_rwg])
    wg, wu, wd = din["moe_w_gate"], din["moe_w_up"], din["moe_w_down"]
    NG = MOE_FF // 512
    jobs = []
    for T in range(ntiles):
        for e in range(nexp):
            for g in range(NG):
                jobs.append([
                    (wg[e][:, g * 512:(g + 1) * 512].rearrange("(k p) n -> p k n", p=128), 8, 512),
                    (wu[e][:, g * 512:(g + 1) * 512].rearrange("(k p) n -> p k n", p=128), 8, 512),
                    (wd[e][g * 512:(g + 1) * 512, :].rearrange("(a p) d -> p a d", p=128), 4, 1024)])
    ws.plan(jobs)

    for T in range(ntiles):
        t0 = T * 1024
        kb.dma("sp", xt, xs[t0:t0 + 1024, :].rearrange("(b p) d -> p b d", p=128), reads=[b_xs], writes=[b_xt])
        kb.dma("sp", ot, OT[:, :, t0:t0 + 1024].rearrange("k p t -> p k t"), reads=[b_OT], writes=[b_htok])
        for b in range(NBK):
            for half in range(2):
                ps, bps = rot.next()
                for k in range(8):
                    cx.mm(ps, ot[:, k, b * 128:(b + 1) * 128], wo[:, k, half * 512:(half + 1) * 512], k == 0, k == 7,
                          [b_htok, b_wo], [bps])
                xv = xt[:, b, half * 512:(half + 1) * 512]
                cx.tt("dve", xv, xv, ps, ALU.add, [b_xt, bps], [b_xt])
        rms_to_hT(cx, P, xt, b_xt, NBK, gT, b_gT, htok, b_htok, hT, b_hT, ss, b_ss, [0, 1])
        for b in range(NBK):
            for e in range(NE):
                kb.op("dve", lambda g_, b=b, e=e: g_.scalar_tensor_tensor(
                    out=junk, in0=xt[:, b, :], scalar=ss[:, b:b + 1], in1=rwg[:, e, :], op0=ALU.mult, op1=ALU.mult,
                    accum_out=lg[:, b, e:e + 1]), [b_xt, b_ss, b_rwg], [b_junk, b_lg])
            kb.op("dve", lambda g_, b=b: g_.max(out=m8, in_=lg[:, b, :]), [b_lg], [b_m8])
            cx.ts("dve", gsum[:, 0:1], m8[:, 0:1], -1.0, None, ALU.mult, None, [b_m8], [b_gsum])
            cx.act(gts[:, b, :], lg[:, b, :], AF.Exp, [b_lg, b_gsum], [b_gts], bias=gsum[:, 0:1])
            cx.stt("dve", gts[:, b, :], lg[:, b, :], m8[:, 1:2], gts[:, b, :], ALU.is_ge, ALU.mult,
                   [b_lg, b_m8, b_gts], [b_gts])
            kb.op("dve", lambda g_, b=b: g_.reduce_sum(out=gsum[:, 1:2], in_=gts[:, b, :], axis=AX.X), [b_gts], [b_gsum])
            kb.op("dve", lambda g_: g_.reciprocal(out=gsum[:, 1:2], in_=gsum[:, 1:2]), [b_gsum], [b_gsum])
            cx.ts("dve", gts[:, b, :], gts[:, b, :], gsum[:, 1:2], None, ALU.mult, None, [b_gts, b_gsum], [b_gts])
            pt_, bpt = rot.next()
            cx.tr(pt_[0:8, 0:128], gts[:, b, :], identf, [b_gts, b_identf], [bpt])
            cx.cp("dve", g8T[:, b * 128:(b + 1) * 128], pt_[0:8, 0:128], [bpt], [b_g8T])
        gi = 0
        for e in range(nexp):
            for half in range(2):
                ps, bps = rot.next()
                cx.mm(ps, sel[:, e, :], g8T[:, half * 512:(half + 1) * 512], True, True, [b_sel, b_g8T], [bps])
                cx.cp("act", gbc[:, half * 512:(half + 1) * 512], ps, [bps], [b_gbc])
            for g in range(NG):
                (gv, uv, dv), b_w = ws.get()
                at, b_at = actT[gi % 2], b_actT[gi % 2]
                gi += 1
                for f4 in range(4):
                    for half in range(2):
                        hs = slice(half * 512, (half + 1) * 512)
                        pg, bpg = rot.next()
                        pu, bpu = rot.next()
                        for kc in range(8):
                            cx.mm(pg, gv[:, kc, f4 * 128:(f4 + 1) * 128], hT[:, kc, hs], kc == 0, kc == 7, [b_w, b_hT], [bpg])
                        for kc in range(8):
                            cx.mm(pu, uv[:, kc, f4 * 128:(f4 + 1) * 128], hT[:, kc, hs], kc == 0, kc == 7, [b_w, b_hT], [bpu])
                        i2 = (f4 * 2 + half) % 2
                        cx.act(sg[i2], pg, AF.Silu, [bpg], [b_sg[i2]])
                        cx.tt("dve", tg[i2], pu, gbc[:, hs], ALU.mult, [bpu, b_gbc], [b_tg[i2]])
                        cx.tt("pool", at[:, f4, hs], sg[i2], tg[i2], ALU.mult, [b_sg[i2], b_tg[i2]], [b_at])
                for b in range(NBK):
                    for half in range(2):
                        ps, bps = rot.next()
                        for f4 in range(4):
                            cx.mm(ps, at[:, f4, b * 128:(b + 1) * 128], dv[:, f4, half * 512:(half + 1) * 512],
                                  f4 == 0, f4 == 3, [b_at, b_w], [bps])
                        xv = xt[:, b, half * 512:(half + 1) * 512]
                        cx.tt("dve", xv, xv, ps, ALU.add, [b_xt, bps], [b_xt])
        for b in range(NBK):
            cx.act(junk, xt[:, b, :], AF.Square, [b_xt], [b_junk, b_ss], accum=ss[:, 8 + (b % 8):9 + (b % 8)])
        cx.ts("dve", ss[:, 8:16], ss[:, 8:16], 1.0 / D, EPS, ALU.mult, ALU.add, [b_ss], [b_ss])
        cx.act(ss[:, 8:16], ss[:, 8:16], AF.Sqrt, [b_ss], [b_ss])
        kb.op("dve", lambda g_: g_.reciprocal(out=ss[:, 8:16], in_=ss[:, 8:16]), [b_ss], [b_ss])
        for b in range(NBK):
            cx.stt("dve", xt[:, b, :], xt[:, b, :], ss[:, 8 + b:9 + b], gfin, ALU.mult, ALU.mult, [b_xt, b_ss, b_gfin], [b_xt])
        kb.dma("sp", out[t0:t0 + 1024, :].rearrange("(b p) d -> p b d", p=128), xt, reads=[b_xt], writes=[b_out])
    cx.release(m)


def build_program():
    nc = bass.Bass("TRN2", target_bir_lowering=False)
    cx = Ctx(nc)
    kb = cx.kb
    din = declare_inputs(nc, list(IN_SPECS.keys()))
    out = nc.dram_tensor("out", [L, D], F32, kind="ExternalOutput").ap()
    xs = nc.dram_tensor("xs", [L, D], F32, kind="Internal").ap()
    KT = nc.dram_tensor("KT", [NH, 97, L], BF16, kind="Internal").ap()
    QT = nc.dram_tensor("QT", [NH, 97, L], BF16, kind="Internal").ap()
    VS = nc.dram_tensor("VS", [NH, 128, NB, 65], BF16, kind="Internal").ap()
    OT = nc.dram_tensor("OT", [NH // 2, 128, L], BF16, kind="Internal").ap()
    b_out, b_xs, b_KT, b_QT, b_VS, b_OT = (kb.buf(n) for n in ("out", "xs", "KT", "QT", "VS", "OT"))
    W0 = nc.dram_tensor("W0", [N_L0_JOBS * 128, 4096], BF16, kind="Internal").ap()
    b_W0 = kb.buf("W0")
    W16 = {k: nc.dram_tensor("W16" + k, [NE * NGRP * 128, 4096], BF16, kind="Internal").ap() for k in "gud"}
    b_W16 = {k: kb.buf("W16" + k) for k in "gud"}
    HS = nc.dram_tensor("HS", [NSLAB * SLAB, D], BF16, kind="Internal").ap()
    YS = nc.dram_tensor("YS", [NSLAB * SLAB, D], F32, kind="Internal").ap()
    b_HS, b_YS = kb.buf("HS"), kb.buf("YS")
    conv = MoeConv(cx, din, W16, b_W16, nstage=0)
    P = setup_ident(cx, din)
    m0 = cx.mark()
    phase0(cx, P, din, pre_hook=lambda: layer0_convert(cx, din, W0, b_W0))
    phase1(cx, P, din, xs, b_xs, W0, b_W0)
    cx.release(m0)
    m1 = cx.mark()
    conv.stage = [cx.sb(f"cvB{i}", [128, 4096], BF16) for i in range(2)]
    phase15(cx, P, din, xs, b_xs, KT, b_KT, QT, b_QT, VS, b_VS, hook=lambda: conv.step(3))
    phase2(cx, P, din, KT, b_KT, QT, b_QT, VS, b_VS, OT, b_OT, hook=lambda: conv.step(1))
    conv.finish()
    cx.release(m1)
    phase3r(cx, P, din, xs, b_xs, OT, b_OT, out, b_out, W16, b_W16, HS, b_HS, YS, b_YS)
    kb.finish([b_out])
    return nc


_NC_CACHE = {}


def kernel(**inputs):
    inp = {k: np.asarray(v) for k, v in inputs.items()}
    shared = host_shared(inp)
    if "nc" not in _NC_CACHE:
        _NC_CACHE["nc"] = build_program()
    nc = _NC_CACHE["nc"]
    ncores = 8
    in_maps = []
    for c in range(ncores):
        mp = dict(shared)
        mp["x"] = np.ascontiguousarray(inp["x"][c], dtype=np.float32)
        mp["pos"] = np.ascontiguousarray(inp["positions"][c:c + 1], dtype=np.int32)
        in_maps.append(mp)
    res = run_bass_kernel_spmd(nc, in_maps, core_ids=list(range(ncores)))
    return np.stack([np.asarray(r["out"], dtype=np.float32) for r in res.results], axis=0)


class MoeConv:
    def __init__(self, cx, din, W16, b_W16, nstage=2, nexp=NE):
        self.cx = cx
        self.W16, self.b_W16 = W16, b_W16
        self.stage = [cx.sb(f"cvst{i}", [128, 4096], BF16) for i in range(nstage)] if nstage else []
        self.chunks = []
        for e in range(nexp):
            for g in range(NGRP):
                cs = slice(g * 512, (g + 1) * 512)
                self.chunks.append(("g", e, g, din["moe_w_gate"][e][:, cs].rearrange("(k p) n -> p k n", p=128), 512))
                self.chunks.append(("u", e, g, din["moe_w_up"][e][:, cs].rearrange("(k p) n -> p k n", p=128), 512))
                self.chunks.append(("d", e, g, din["moe_w_down"][e][cs, :].rearrange("(a p) d -> p a d", p=128), 1024))
        self.i = 0

    def step(self, n=1):
        kb = self.cx.kb
        for _ in range(n):
            if self.i >= len(self.chunks):
                return
            kind, e, g, src, n_in = self.chunks[self.i]
            st, b_st = self.stage[self.i % len(self.stage)]
            self.i += 1
            kb.dma("pool", st.rearrange("p (a n) -> p a n", n=n_in), src, writes=[b_st])
            r0 = (e * NGRP + g) * 128
            kb.dma("sp", self.W16[kind][r0:r0 + 128, :], st, reads=[b_st], writes=[self.b_W16[kind]], disjoint=True)

    def finish(self):
        self.step(len(self.chunks))


def phase3r(cx, P, din, xs, b_xs, OT, b_OT, out, b_out, W16, b_W16, HS, b_HS, YS, b_YS, nslab=NSLAB):
    nc, kb = cx.nc, cx.kb
    B = P["bufs"]
    identb, b_identb = P["identb"], B["identb"]
    identf, b_identf = P["identf"], B["identf"]
    NTOT = NE * NGRP * 128
    mp = cx.mark()
    ss, b_ss = cx.sb("ss", [128, 8], F32)
    LG, b_LG = cx.sb("LG", [128, NB, 8], F32)
    GTS, b_GTS = cx.sb("GTS", [128, NB, 8], F32)
    TOP, b_TOP = cx.sb("TOP", [128, NB, 2], F32)
    m8, b_m8 = cx.sb("m8", [128, 8], F32)
    gsum, b_gsum = cx.sb("gsum", [128, 2], F32)
    junk, b_junk = cx.sb("junk", [128, 1024], BF16)
    M1, b_M1 = cx.sb("M1", [128, NB, 8], F32)
    M2, b_M2 = cx.sb("M2", [128, NB, 8], F32)
    MSK, b_MSK = cx.sb("MSK", [128, NB, 8], BF16)
    ltri, b_ltri = cx.sb("ltri", [128, 128], BF16)
    onesb, b_onesb = cx.sb("onesb", [128, 128], BF16)
    ones32, b_ones32 = cx.sb("ones32", [128, NB], F32)
    tmpf, b_tmpf = cx.sb("tmpf", [128, 128], F32)
    TOT, b_TOT = cx.sb("TOT", [128, NB, 8], F32)
    INC, b_INC = cx.sb("INC", [128, NB, 8], F32)
    WIN, b_WIN = cx.sb("WIN", [128, NB, 8], F32)
    SL, b_SL = cx.sb("SL", [128, NB, 8], F32)
    NSL, b_NSL = cx.sb("NSL", [128, 8], F32)
    NSLi, b_NSLi = cx.sb("NSLi", [128, 8], I32)
    SEND, b_SEND = cx.sb("SEND", [128, 8], F32)
    OFF, b_OFF = cx.sb("OFF", [128, 8], F32)
    one8, b_one8 = cx.sb("one8", [128, 8], F32)
    SF, b_SF = cx.sb("SF", [128, 2, NB], F32)
    SLOT, b_SLOT = cx.sb("SLOT", [128, 2, NB], I32)
    WGT, b_WGT = cx.sb("WGT", [128, 2, NB], F32)
    wtab, b_wtab = cx.sb("wtab", [128, NSLAB, 8], F32)
    CMP, b_CMP = cx.sb("CMP", [128, NSLAB, 8], F32)
    EW, b_EW = cx.sb("EW", [128, NSLAB], F32)
    ctab, b_ctab = cx.sb("ctab", [128, 7], F32)
    IDXf, b_IDXf = cx.sb("IDXf", [128, NSLAB, 7], F32)
    IDX, b_IDX = cx.sb("IDX", [128, NSLAB, 7], I32)
    m = cx.mark()
    xt, b_xt = cx.sb("xt", [128, 8, 1024], F32)
    ot, b_ot = cx.sb("ot", [128, 8, 1024], BF16)
    wo, b_wo = cx.sb("wo", [128, 8, 1024], BF16)
    rwT, b_rwT = cx.sb("rwT", [128, 8, NE], F32)
    gT1, b_gT1 = cx.sb("gT1", [128, 8], F32)
    xT32, b_xT32 = cx.sb("xT32", [128, 8, 128], F32)
    gff, b_gff = cx.sb("gff", [128, 1024], F32)
    hall, _ = cx.sb("hall", [128, NB, 1024], BF16)
    b_hall = kb.bufs_n("hall", NB)
    rot = PsRot(cx, [2, 3, 4, 5, 6, 7])
    kb.dma("pool", wo, din["w_o"].rearrange("(k p) n -> p k n", p=128), writes=[b_wo])
    kb.dma("sp", gff, din["g_ffn1"].partition_broadcast(128), writes=[b_gff])
    kb.dma("sp", rwT, din["router_w"].rearrange("(k p) e -> p k e", p=128), writes=[b_rwT])
    kb.dma("sp", gT1, din["gT_ffn1"], writes=[b_gT1])
    cx.tt("dve", rwT, rwT, gT1.unsqueeze(2).to_broadcast([128, 8, NE]), ALU.mult, [b_rwT, b_gT1], [b_rwT])
    for T in range(4):
        t0 = T * 1024
        kb.dma("sp", xt, xs[t0:t0 + 1024, :].rearrange("(b p) d -> p b d", p=128), reads=[b_xs], writes=[b_xt])
        kb.dma("sp", ot, OT[:, :, t0:t0 + 1024].rearrange("k p t -> p k t"), reads=[b_OT], writes=[b_ot])
        for b in range(8):
            for half in range(2):
                ps, bps = rot.next()
                for k in range(8):
                    cx.mm(ps, ot[:, k, b * 128:(b + 1) * 128], wo[:, k, half * 512:(half + 1) * 512], k == 0, k == 7,
                          [b_ot, b_wo], [bps])
                xv = xt[:, b, half * 512:(half + 1) * 512]
                cx.tt("dve", xv, xv, ps, ALU.add, [b_xt, bps], [b_xt])
        kb.dma("sp", xs[t0:t0 + 1024, :].rearrange("(b p) d -> p b d", p=128), xt, reads=[b_xt], writes=[b_xs])
        for b in range(8):
            cx.act(junk, xt[:, b, :], AF.Square, [b_xt], [b_junk, b_ss], accum=ss[:, b:b + 1])
        cx.ts("dve", ss, ss, 1.0 / D, EPS, ALU.mult, ALU.add, [b_ss], [b_ss])
        cx.act(ss, ss, AF.Sqrt, [b_ss], [b_ss])
        kb.op("dve", lambda g_: g_.reciprocal(out=ss, in_=ss), [b_ss], [b_ss])
        for b in range(8):
            gb = T * 8 + b
            cx.stt("dve", hall[:, gb, :], xt[:, b, :], ss[:, b:b + 1], gff, ALU.mult, ALU.mult,
                   [b_xt, b_ss, b_gff], [b_hall[gb]])
            pA, bpA = rot.next()
            pB, bpB = rot.next()
            for kc in range(8):
                pp, bpp = (pA, bpA) if kc < 4 else (pB, bpB)
                cx.tr(pp[:, (kc % 4) * 128:(kc % 4 + 1) * 128], xt[:, b, kc * 128:(kc + 1) * 128], identf, [b_xt, b_identf], [bpp])
            cx.cp("act", xT32[:, 0:4, :], pA.rearrange("p (k c) -> p k c", c=128), [bpA], [b_xT32])
            cx.cp("act", xT32[:, 4:8, :], pB.rearrange("p (k c) -> p k c", c=128), [bpB], [b_xT32])
            pL, bpL = rot.next()
            for kc in range(8):
                cx.mm(pL[:, 0:NE], xT32[:, kc, :], rwT[:, kc, :], kc == 0, kc == 7, [b_xT32, b_rwT], [bpL])
            cx.ts("dve", LG[:, gb, :], pL[:, 0:NE], ss[:, b:b + 1], None, ALU.mult, None, [bpL, b_ss], [b_LG])
            kb.op("dve", lambda g_, gb=gb: g_.max(out=m8, in_=LG[:, gb, :]), [b_LG], [b_m8])
            cx.cp("dve", TOP[:, gb, :], m8[:, 0:2], [b_m8], [b_TOP])
            cx.ts("dve", gsum[:, 0:1], m8[:, 0:1], -1.0, None, ALU.mult, None, [b_m8], [b_gsum])
            cx.act(GTS[:, gb, :], LG[:, gb, :], AF.Exp, [b_LG, b_gsum], [b_GTS], bias=gsum[:, 0:1])
            cx.stt("dve", GTS[:, gb, :], LG[:, gb, :], m8[:, 1:2], GTS[:, gb, :], ALU.is_ge, ALU.mult,
                   [b_LG, b_m8, b_GTS], [b_GTS])
            kb.op("dve", lambda g_, gb=gb: g_.reduce_sum(out=gsum[:, 1:2], in_=GTS[:, gb, :], axis=AX.X), [b_GTS], [b_gsum])
            kb.op("dve", lambda g_: g_.reciprocal(out=gsum[:, 1:2], in_=gsum[:, 1:2]), [b_gsum], [b_gsum])
            cx.ts("dve", GTS[:, gb, :], GTS[:, gb, :], gsum[:, 1:2], None, ALU.mult, None, [b_GTS, b_gsum], [b_GTS])
    kb.dma("sp", tmpf, din["ltri"], writes=[b_tmpf])
    cx.cp("dve", ltri, tmpf, [b_tmpf], [b_ltri])
    kb.dma("sp", wtab, din["wtab"].rearrange("p (w e) -> p w e", e=8), writes=[b_wtab])
    kb.dma("sp", ctab, din["ctab"], writes=[b_ctab])
    cx.memset("dve", onesb, 1.0, [b_onesb])
    cx.memset("dve", ones32, 1.0, [b_ones32])
    cx.memset("dve", one8, 1.0, [b_one8])
    bce = lambda t: t.unsqueeze(2).to_broadcast([128, NB, 8])
    cx.tt("dve", M1, LG, bce(TOP[:, :, 0]), ALU.is_equal, [b_LG, b_TOP], [b_M1])
    cx.tt("dve", M2, LG, bce(TOP[:, :, 1]), ALU.is_equal, [b_LG, b_TOP], [b_M2])
    cx.tt("dve", MSK, M1, M2, ALU.add, [b_M1, b_M2], [b_MSK])
    mskf = MSK.rearrange("p b e -> p (b e)")
    ps, bps = rot.next()
    cx.mm(ps[:, 0:256], ltri, mskf, True, True, [b_ltri, b_MSK], [bps])
    cx.cp("dve", WIN.rearrange("p b e -> p (b e)"), ps[:, 0:256], [bps], [b_WIN])
    ps, bps = rot.next()
    cx.mm(ps[:, 0:256], onesb, mskf, True, True, [b_onesb, b_MSK], [bps])
    cx.cp("dve", TOT.rearrange("p b e -> p (b e)"), ps[:, 0:256], [bps], [b_TOT])
    for e in range(NE):
        kb.op("dve", lambda g_, e=e: g_.tensor_tensor_scan(out=INC[:, :, e], data0=ones32, data1=TOT[:, :, e], initial=0.0,
                                                           op0=ALU.mult, op1=ALU.add), [b_ones32, b_TOT], [b_INC])
    cx.ts("dve", NSL, INC[:, NB - 1, :], float(SLAB - 1), 1.0 / SLAB, ALU.add, ALU.mult, [b_INC], [b_NSL])
    cx.ts("dve", NSL, NSL, -0.4995, None, ALU.add, None, [b_NSL], [b_NSL])
    cx.cp("dve", NSLi, NSL, [b_NSL], [b_NSLi])
    cx.cp("dve", NSL, NSLi, [b_NSLi], [b_NSL])
    kb.op("dve", lambda g_: g_.tensor_tensor_scan(out=SEND, data0=one8, data1=NSL, initial=0.0, op0=ALU.mult, op1=ALU.add),
          [b_one8, b_NSL], [b_SEND])
    cx.tt("dve", OFF, SEND, NSL, ALU.subtract, [b_SEND, b_NSL], [b_OFF])
    cx.ts("dve", OFF, OFF, float(SLAB), None, ALU.mult, None, [b_OFF], [b_OFF])
    cx.tt("dve", SL, INC, TOT, ALU.subtract, [b_INC, b_TOT], [b_SL])
    cx.tt("dve", SL, SL, WIN, ALU.add, [b_SL, b_WIN], [b_SL])
    cx.tt("dve", SL, SL, OFF.unsqueeze(1).to_broadcast([128, NB, 8]), ALU.add, [b_SL, b_OFF], [b_SL])
    for k, (Mk, b_Mk) in enumerate(((M1, b_M1), (M2, b_M2))):
        cx.tt("dve", TOT, Mk, SL, ALU.mult, [b_Mk, b_SL], [b_TOT])
        kb.op("dve", lambda g_, k=k: g_.reduce_sum(out=SF[:, k, :], in_=TOT, axis=AX.X), [b_TOT], [b_SF])
        cx.tt("dve", TOT, Mk, GTS, ALU.mult, [b_Mk, b_GTS], [b_TOT])
        kb.op("dve", lambda g_, k=k: g_.reduce_sum(out=WGT[:, k, :], in_=TOT, axis=AX.X), [b_TOT], [b_WGT])
    cx.cp("dve", SLOT, SF, [b_SF], [b_SLOT])
    cx.tt("dve", CMP, SEND.unsqueeze(1).to_broadcast([128, NSLAB, 8]), wtab, ALU.is_le, [b_SEND, b_wtab], [b_CMP])
    kb.op("dve", lambda g_: g_.reduce_sum(out=EW, in_=CMP, axis=AX.X), [b_CMP], [b_EW])
    cx.ts("dve", EW, EW, float(NE - 1), float(NGRP * 128), ALU.min, ALU.mult, [b_EW], [b_EW])
    cx.tt("dve", IDXf, EW.unsqueeze(2).to_broadcast([128, NSLAB, 7]), ctab.unsqueeze(1).to_broadcast([128, NSLAB, 7]),
          ALU.add, [b_EW, b_ctab], [b_IDXf])
    cx.cp("dve", IDX, IDXf, [b_IDXf], [b_IDX])
    for gb in range(NB):
        for k in range(2):
            kb.idma(HS, hall[:, gb, :], out_idx=SLOT[:, k, gb:gb + 1], bound=NSLAB * SLAB - 1,
                    reads=[b_hall[gb], b_SLOT], writes=[b_HS], disjoint=True)
    cx.release(m)
    m2 = cx.mark()
    NBK = SLAB // 128
    hsls = [cx.sb(f"hsl{i}", [128, NBK, 1024], BF16) for i in range(2)]
    hTs = [cx.sb(f"hTs{i}", [128, 8, SLAB], BF16) for i in range(2)]
    yacc, b_yacc = cx.sb("yacc", [128, NBK, 1024], F32)
    wsl = [cx.sb(f"wsl{i}", [128, 3 * 4096], BF16) for i in range(2)]
    actT = [cx.sb(f"actT{i}", [128, 4, SLAB], BF16) for i in range(2)]
    sg = [cx.sb(f"sg{i}", [128, 512], F32) for i in range(2)]
    NH2 = SLAB // 512
    wi = 0
    gi = 0

    def issue_w(w, g, slot):
        t, b = slot
        for j, kind in enumerate(("g", "u", "d")):
            kb.idma(t[:, j * 4096:(j + 1) * 4096], W16[kind], in_idx=IDX[:, w, g:g + 1], bound=NTOT - 1,
                    reads=[b_W16[kind], b_IDX], writes=[b], lane=b.name)

    def load_slab_dma(w):
        hsl, b_hsl = hsls[w % 2]
        kb.dma("sp", hsl, HS[w * SLAB:(w + 1) * SLAB, :].rearrange("(b p) d -> p b d", p=128), reads=[b_HS], writes=[b_hsl])

    def load_slab(w):
        hsl, b_hsl = hsls[w % 2]
        hT_, b_hT_ = hTs[w % 2]
        for b in range(NBK):
            pi = b % 2
            p16 = cx.ps[pi].bitcast(BF16).rearrange("p (k c) -> p k c", c=128)
            for kc in range(8):
                cx.tr(p16[:, kc, :], hsl[:, b, kc * 128:(kc + 1) * 128], identb, [b_hsl, b_identb], [cx.psb[pi]])
            cx.cp("act" if b % 2 else "dve", hT_[:, :, b * 128:(b + 1) * 128], p16, [cx.psb[pi]], [b_hT_])

    seq = [(w, g) for w in range(nslab) for g in range(NGRP)]
    issue_w(seq[0][0], seq[0][1], wsl[0])
    load_slab_dma(0)
    load_slab(0)
    for si, (w, g) in enumerate(seq):
        if si + 1 < len(seq):
            issue_w(seq[si + 1][0], seq[si + 1][1], wsl[(si + 1) % 2])
        wt, b_w = wsl[si % 2]
        gv = wt[:, 0:4096].rearrange("p (k n) -> p k n", n=512)
        uv = wt[:, 4096:8192].rearrange("p (k n) -> p k n", n=512)
        dv = wt[:, 8192:12288].rearrange("p (a d) -> p a d", d=1024)
        hT, b_hT = hTs[w % 2]
        if g == NGRP - 3 and w + 1 < nslab:
            load_slab_dma(w + 1)
        if g == NGRP - 1 and w + 1 < nslab:
            load_slab(w + 1)
        at, b_at = actT[gi % 2]
        gi += 1
        for f4 in range(4):
            for half in range(NH2):
                hs = slice(half * 512, (half + 1) * 512)
                pg, bpg = rot.next()
                pu, bpu = rot.next()
                for kc in range(8):
                    cx.mm(pg, gv[:, kc, f4 * 128:(f4 + 1) * 128], hT[:, kc, hs], kc == 0, kc == 7, [b_w, b_hT], [bpg])
                for kc in range(8):
                    cx.mm(pu, uv[:, kc, f4 * 128:(f4 + 1) * 128], hT[:, kc, hs], kc == 0, kc == 7, [b_w, b_hT], [bpu])
                s1, b_s1 = sg[(f4 * NH2 + half) % 2]
                cx.act(s1, pg, AF.Silu, [bpg], [b_s1])
                cx.tt("dve", at[:, f4, hs], s1, pu, ALU.mult, [b_s1, bpu], [b_at])
        for b in range(NBK):
            for half in range(2):
                ps, bps = rot.next()
                for f4 in range(4):
                    cx.mm(ps, at[:, f4, b * 128:(b + 1) * 128], dv[:, f4, half * 512:(half + 1) * 512],
                          f4 == 0, f4 == 3, [b_at, b_w], [bps])
                yv = yacc[:, b, half * 512:(half + 1) * 512]
                if g == 0:
                    cx.cp("act", yv, ps, [bps], [b_yacc])
                else:
                    cx.tt("dve", yv, yv, ps, ALU.add, [b_yacc, bps], [b_yacc])
        if g == NGRP - 1:
            kb.dma("sp", YS[w * SLAB:(w + 1) * SLAB, :].rearrange("(b p) d -> p b d", p=128), yacc, reads=[b_yacc], writes=[b_YS],
                   disjoint=True)
    cx.release(m2)
    NBUF3 = 4
    y1, b_y1 = cx.sb("y1", [128, NBUF3, 1024], F32)
    y2, b_y2 = cx.sb("y2", [128, NBUF3, 1024], F32)
    xb, b_xb = cx.sb("xb", [128, NBUF3, 1024], F32)
    gfin, b_gfin = cx.sb("gfin", [128, 1024], F32)
    s2, _ = cx.sb("s2", [128, NBUF3], F32)
    b_s2s = kb.bufs_n("s2_", NBUF3)
    kb.dma("sp", gfin, din["g_final"].partition_broadcast(128), writes=[b_gfin])
    b_y1s = kb.bufs_n("y1s", NBUF3); b_y2s = kb.bufs_n("y2s", NBUF3); b_xbs = kb.bufs_n("xbs", NBUF3)
    def fetch3(g2):
        i2 = g2 % NBUF3
        kb.dma("sp", xb[:, i2, :], xs[g2 * 128:(g2 + 1) * 128, :], reads=[b_xs], writes=[b_xbs[i2]])
        kb.idma(y1[:, i2, :], YS, in_idx=SLOT[:, 0, g2:g2 + 1], bound=NSLAB * SLAB - 1, reads=[b_YS, b_SLOT], writes=[b_y1s[i2]])
        kb.idma(y2[:, i2, :], YS, in_idx=SLOT[:, 1, g2:g2 + 1], bound=NSLAB * SLAB - 1, reads=[b_YS, b_SLOT], writes=[b_y2s[i2]])

    for g2 in range(NBUF3 - 1):
        fetch3(g2)
    for gb in range(NB):
        i = gb % NBUF3
        b_s2 = b_s2s[i]
        if gb + NBUF3 - 1 < NB:
            fetch3(gb + NBUF3 - 1)
        cx.stt("dve", xb[:, i, :], y1[:, i, :], WGT[:, 0, gb:gb + 1], xb[:, i, :], ALU.mult, ALU.add,
               [b_y1s[i], b_WGT, b_xbs[i]], [b_xbs[i]])
        cx.stt("dve", xb[:, i, :], y2[:, i, :], WGT[:, 1, gb:gb + 1], xb[:, i, :], ALU.mult, ALU.add,
               [b_y2s[i], b_WGT, b_xbs[i]], [b_xbs[i]])
        cx.act(junk, xb[:, i, :], AF.Square, [b_xbs[i]], [b_junk, b_s2], accum=s2[:, i:i + 1])
        cx.ts("dve", s2[:, i:i + 1], s2[:, i:i + 1], 1.0 / D, EPS, ALU.mult, ALU.add, [b_s2], [b_s2])
        cx.act(s2[:, i:i + 1], s2[:, i:i + 1], AF.Sqrt, [b_s2], [b_s2])
        kb.op("dve", lambda g_, i=i: g_.reciprocal(out=s2[:, i:i + 1], in_=s2[:, i:i + 1]), [b_s2], [b_s2])
        cx.stt("dve", xb[:, i, :], xb[:, i, :], s2[:, i:i + 1], gfin, ALU.mult, ALU.mult, [b_xbs[i], b_s2, b_gfin], [b_xbs[i]])
        kb.dma("sp", out[gb * 128:(gb + 1) * 128, :], xb[:, i, :], reads=[b_xbs[i]], writes=[b_out], disjoint=True)
    cx.release(mp)
```
